# Optimizing a Trainium2 kernel written in Bass

```python
import math
import jax, jax.numpy as jnp
from jax import lax
import numpy as np

D_MODEL = 1024
BATCH = 4
SEQ = 8192
DEPTH = 2

RWKV_WIDTH = D_MODEL // 2
RWKV_HEAD_DIM = 64
RWKV_HEADS = RWKV_WIDTH // RWKV_HEAD_DIM
W_LORA = 64
A_LORA = 64
G_LORA = 128
RWKV_PROJ = 3 * RWKV_WIDTH + W_LORA + A_LORA + G_LORA
RWKV_NORM_EPS = 64e-5
POOL_WINDOWS = (2, 4, 8, 16)
POOL_GROUPS = len(POOL_WINDOWS)
POOL_WIDTH = D_MODEL // 2
POOL_GROUP_DIM = POOL_WIDTH // POOL_GROUPS
EVEN_PROJ = RWKV_PROJ + POOL_WIDTH
CONV_CHANNELS = D_MODEL // 2
CONV_WIDTH = 31
RET_WIDTH = D_MODEL // 2
RET_HEAD_DIM = 64
RET_HEADS = RET_WIDTH // RET_HEAD_DIM
RET_CHUNK = 128
ROPE_BASE = 10000.0
ODD_PROJ = 2 * CONV_CHANNELS + 4 * RET_WIDTH
MIX_WIDTH = D_MODEL
MOE_GROUPS = 4
EXPERTS_PER_GROUP = 8
N_EXPERTS = MOE_GROUPS * EXPERTS_PER_GROUP
TOP_K_IN_GROUP = 2
D_EXPERT = 128
ALPHA = (2.0 * DEPTH) ** 0.25
BETA = (8.0 * DEPTH) ** -0.25
LN_EPS = 1e-5

kernel_name = "hybrid_rwkv7_pool_conformer_retention_hmoe"


def layer_norm(x, g, b, eps=LN_EPS):
    xf = x.astype(jnp.float32)
    mu = jnp.mean(xf, axis=-1, keepdims=True)
    var = jnp.mean(jnp.square(xf - mu), axis=-1, keepdims=True)
    return ((xf - mu) * lax.rsqrt(var + eps) * g + b).astype(x.dtype)


def head_norm(y, eps):
    yf = y.astype(jnp.float32)
    mu = jnp.mean(yf, axis=-1, keepdims=True)
    var = jnp.mean(jnp.square(yf - mu), axis=-1, keepdims=True)
    out = (yf - mu) * lax.rsqrt(var + eps)
    return out.reshape(y.shape[:-2] + (-1,))


def rwkv7_scan(r, w, k, v, kk, a):
    B, S, H, d = r.shape

    def step(state, inp):
        r_t, w_t, k_t, v_t, kk_t, a_t = inp
        sa = jnp.einsum('bhvk,bhk->bhv', state, -kk_t)
        state = (state * w_t[:, :, None, :]
                 + sa[..., None] * (kk_t * a_t)[:, :, None, :]
                 + v_t[..., None] * k_t[:, :, None, :])
        y_t = jnp.einsum('bhvk,bhk->bhv', state, r_t)
        return state, y_t

    xs = (jnp.moveaxis(r, 1, 0), jnp.moveaxis(w, 1, 0), jnp.moveaxis(k, 1, 0),
          jnp.moveaxis(v, 1, 0), jnp.moveaxis(kk, 1, 0), jnp.moveaxis(a, 1, 0))
    init = jnp.zeros((B, H, d, d), jnp.float32)
    _, y = lax.scan(step, init, xs)
    return jnp.moveaxis(y, 0, 1)


def multiscale_pool(u):
    B, S, _ = u.shape
    uf = u.astype(jnp.float32).reshape(B, S, POOL_GROUPS, POOL_GROUP_DIM)
    c = jnp.cumsum(uf, axis=1)
    t = jnp.arange(S)
    outs = []
    for gi, win in enumerate(POOL_WINDOWS):
        cg = c[:, :, gi]
        c_prev = jnp.pad(cg, ((0, 0), (win, 0), (0, 0)))[:, :S]
        count = jnp.minimum(t + 1, win).astype(jnp.float32)[None, :, None]
        outs.append((cg - c_prev) / count - uf[:, :, gi])
    return jnp.stack(outs, axis=2)


def rwkv_pool_mixer(x, w_in, mu, w0, w2, a0, a2, g2, k_k, k_a, r_k,
                    lnx_g, lnx_b, pool_w, pool_scale, w_out):
    B, S, _ = x.shape
    p = x @ w_in
    pr, u = p[..., :RWKV_PROJ], p[..., RWKV_PROJ:]
    prev = jnp.pad(pr, ((0, 0), (1, 0), (0, 0)))[:, :-1]
    z = pr + mu * (prev - pr)
    c1 = RWKV_WIDTH
    r, k, v, wlo, alo, glo = jnp.split(
        z, [c1, 2 * c1, 3 * c1, 3 * c1 + W_LORA, 3 * c1 + W_LORA + A_LORA], axis=-1)
    wl = -jax.nn.softplus(-(w0 + jnp.tanh(wlo) @ w2)) - 0.5
    decay = jnp.exp(-jnp.exp(wl.astype(jnp.float32)))
    a = jax.nn.sigmoid(a0 + alo @ a2)
    g = jax.nn.sigmoid(glo) @ g2

    def hs(t):
        return t.astype(jnp.float32).reshape(B, S, RWKV_HEADS, RWKV_HEAD_DIM)

    kk = hs(k * k_k)
    kk = kk * lax.rsqrt(jnp.maximum(jnp.sum(jnp.square(kk), -1, keepdims=True), 1e-24))
    k = k * (1.0 + (a - 1.0) * k_a)
    rh, kh, vh, ah, dh = hs(r), hs(k), hs(v), hs(a), hs(decay)
    y = rwkv7_scan(rh, dh, kh, vh, kk, ah)
    y = head_norm(y, RWKV_NORM_EPS) * lnx_g + lnx_b
    bonus = (jnp.sum(rh * kh * r_k, axis=-1, keepdims=True) * vh).reshape(B, S, RWKV_WIDTH)
    y_rwkv = ((y + bonus) * g).astype(x.dtype)

    pooled = multiscale_pool(u).astype(x.dtype)
    y_pool = jnp.einsum('bsgc,gcd->bsgd', pooled, pool_w).reshape(B, S, POOL_WIDTH) * pool_scale
    return jnp.concatenate([y_rwkv, y_pool.astype(x.dtype)], axis=-1) @ w_out


def rotary(t, pos):
    half = t.shape[-1] // 2
    inv = ROPE_BASE ** (-jnp.arange(half, dtype=jnp.float32) / half)
    ang = pos[:, None] * inv[None, :]
    cos = jnp.cos(ang)[None, :, None, :]
    sin = jnp.sin(ang)[None, :, None, :]
    t1, t2 = t[..., :half], t[..., half:]
    return jnp.concatenate([t1 * cos - t2 * sin, t1 * sin + t2 * cos], axis=-1)


def retention(q, k, v):
    B, S, _ = q.shape
    H, d, C = RET_HEADS, RET_HEAD_DIM, RET_CHUNK
    N = S // C
    pos = jnp.arange(S, dtype=jnp.float32)
    qh = rotary(q.astype(jnp.float32).reshape(B, S, H, d), pos)
    kh = rotary(k.astype(jnp.float32).reshape(B, S, H, d), pos) * (d ** -0.5)
    vh = v.astype(jnp.float32).reshape(B, S, H, d)

    def to_chunks(t):
        return t.reshape(B, N, C, H, d).transpose(0, 3, 1, 2, 4)

    qc, kc, vc = to_chunks(qh), to_chunks(kh), to_chunks(vh)
    log_gamma = jnp.log1p(-jnp.power(2.0, -5.0 - jnp.arange(H, dtype=jnp.float32)))
    idx = jnp.arange(C, dtype=jnp.float32)
    diff = idx[:, None] - idx[None, :]
    decay_mask = jnp.where(diff >= 0,
                           jnp.exp(jnp.maximum(diff, 0.0)[None] * log_gamma[:, None, None]),
                           0.0)
    scores = jnp.einsum('bhnid,bhnjd->bhnij', qc, kc) * decay_mask[None, :, None]
    intra = jnp.einsum('bhnij,bhnjd->bhnid', scores, vc)
    xi = jnp.exp((idx + 1.0)[None, :] * log_gamma[:, None])
    zeta = jnp.exp((C - 1.0 - idx)[None, :] * log_gamma[:, None])
    gamma_chunk = jnp.exp(C * log_gamma)
    kv = jnp.einsum('bhncd,bhnce->bhnde', kc * zeta[None, :, None, :, None], vc)

    def step(R, kv_n):
        return gamma_chunk[None, :, None, None] * R + kv_n, R

    _, R_prev = lax.scan(step, jnp.zeros((B, H, d, d), jnp.float32), jnp.moveaxis(kv, 2, 0))
    R_prev = jnp.moveaxis(R_prev, 0, 2)
    cross = jnp.einsum('bhncd,bhnde->bhnce', qc * xi[None, :, None, :, None], R_prev)
    return (intra + cross).transpose(0, 2, 3, 1, 4).reshape(B, S, H, d)


def conv_retention_mixer(x, w_in, conv_w, conv_b, cln_g, cln_b, gn_g, gn_b, w_out):
    p = x @ w_in
    cc, rw = CONV_CHANNELS, RET_WIDTH
    ca, cb, q, k, v, gr = jnp.split(
        p, [cc, 2 * cc, 2 * cc + rw, 2 * cc + 2 * rw, 2 * cc + 3 * rw], axis=-1)
    u = ca * jax.nn.sigmoid(cb)
    u = lax.conv_general_dilated(
        u, conv_w[:, None, :], window_strides=(1,), padding=[(CONV_WIDTH - 1, 0)],
        dimension_numbers=('NWC', 'WIO', 'NWC'), feature_group_count=CONV_CHANNELS) + conv_b
    y_conv = jax.nn.silu(layer_norm(u, cln_g, cln_b))
    ret = head_norm(retention(q, k, v), LN_EPS) * gn_g + gn_b
    y_ret = (jax.nn.silu(gr) * ret).astype(x.dtype)
    return jnp.concatenate([y_conv.astype(x.dtype), y_ret], axis=-1) @ w_out


def hier_moe(x, rg_w, rg_b, re_w, re_b, w1, w3, w2):
    B, S, D = x.shape
    xf = x.reshape(B * S, D)
    g_logits = (xf @ rg_w + rg_b).astype(jnp.float32)
    g_prob = jax.nn.softmax(g_logits, axis=-1)
    g_idx = jnp.argmax(g_logits, axis=-1)
    g_w = jnp.max(g_prob, axis=-1, keepdims=True)
    e_logits = (xf @ re_w + re_b).astype(jnp.float32).reshape(-1, MOE_GROUPS, EXPERTS_PER_GROUP)
    e_sel = jnp.einsum('nge,ng->ne', e_logits, jax.nn.one_hot(g_idx, MOE_GROUPS, dtype=jnp.float32))
    e_prob = jax.nn.softmax(e_sel, axis=-1)
    top_p, top_i = lax.top_k(e_prob, TOP_K_IN_GROUP)
    top_p = top_p / jnp.sum(top_p, axis=-1, keepdims=True)
    global_idx = g_idx[:, None] * EXPERTS_PER_GROUP + top_i
    gates = jnp.sum(jax.nn.one_hot(global_idx, N_EXPERTS, dtype=jnp.float32)
                    * (g_w * top_p)[..., None], axis=1)

    def expert(acc, params):
        w1e, w3e, w2e, ge = params
        h = jax.nn.silu(xf @ w1e) * (xf @ w3e)
        return acc + ge[:, None] * (h @ w2e), None

    out, _ = lax.scan(expert, jnp.zeros_like(xf), (w1, w3, w2, gates.T.astype(x.dtype)))
    return out.reshape(B, S, D)


def setup_inputs(seed: int = 0) -> dict:
    key = jax.random.key(seed)
    ks = iter(jax.random.split(key, 48))
    ne, no = (DEPTH + 1) // 2, DEPTH // 2
    D = D_MODEL

    def nrm(shape, scale):
        return jax.random.normal(next(ks), shape, jnp.float32) * scale

    def gain(shape):
        return 1.0 + nrm(shape, 0.02)

    lin = jnp.linspace(0.0, 1.0, RWKV_WIDTH, dtype=jnp.float32)
    return {
        "x": nrm((BATCH, SEQ, D), 1.0),
        "ev_w_in": nrm((ne, D, EVEN_PROJ), D ** -0.5),
        "ev_mu": jax.random.uniform(next(ks), (ne, RWKV_PROJ), jnp.float32, 0.2, 0.8),
        "ev_w0": -6.5 + 5.0 * lin ** 0.9 + nrm((ne, RWKV_WIDTH), 0.1),
        "ev_w2": nrm((ne, W_LORA, RWKV_WIDTH), 0.1),
        "ev_a0": nrm((ne, RWKV_WIDTH), 0.1),
        "ev_a2": nrm((ne, A_LORA, RWKV_WIDTH), A_LORA ** -0.5),
        "ev_g2": nrm((ne, G_LORA, RWKV_WIDTH), G_LORA ** -0.5),
        "ev_k_k": 0.85 + nrm((ne, RWKV_WIDTH), 0.02),
        "ev_k_a": gain((ne, RWKV_WIDTH)),
        "ev_r_k": nrm((ne, RWKV_HEADS, RWKV_HEAD_DIM), 0.1),
        "ev_lnx_g": gain((ne, RWKV_WIDTH)),
        "ev_lnx_b": nrm((ne, RWKV_WIDTH), 0.02),
        "ev_pool_w": nrm((ne, POOL_GROUPS, POOL_GROUP_DIM, POOL_GROUP_DIM), POOL_GROUP_DIM ** -0.5),
        "ev_pool_scale": gain((ne, POOL_WIDTH)),
        "ev_w_out": nrm((ne, MIX_WIDTH, D), MIX_WIDTH ** -0.5 * BETA),
        "od_w_in": nrm((no, D, ODD_PROJ), D ** -0.5),
        "od_conv_w": nrm((no, CONV_WIDTH, CONV_CHANNELS), CONV_WIDTH ** -0.5),
        "od_conv_b": nrm((no, CONV_CHANNELS), 0.02),
        "od_cln_g": gain((no, CONV_CHANNELS)),
        "od_cln_b": nrm((no, CONV_CHANNELS), 0.02),
        "od_gn_g": gain((no, RET_WIDTH)),
        "od_gn_b": nrm((no, RET_WIDTH), 0.02),
        "od_w_out": nrm((no, MIX_WIDTH, D), MIX_WIDTH ** -0.5 * BETA),
        "ln_mix_g": gain((DEPTH, D)),
        "ln_mix_b": nrm((DEPTH, D), 0.02),
        "rg_w": nrm((DEPTH, D, MOE_GROUPS), D ** -0.5),
        "rg_b": nrm((DEPTH, MOE_GROUPS), 0.01),
        "re_w": nrm((DEPTH, D, N_EXPERTS), D ** -0.5),
        "re_b": nrm((DEPTH, N_EXPERTS), 0.01),
        "e_w1": nrm((DEPTH, N_EXPERTS, D, D_EXPERT), D ** -0.5),
        "e_w3": nrm((DEPTH, N_EXPERTS, D, D_EXPERT), D ** -0.5),
        "e_w2": nrm((DEPTH, N_EXPERTS, D_EXPERT, D), D_EXPERT ** -0.5 * BETA),
        "ln_ffn_g": gain((DEPTH, D)),
        "ln_ffn_b": nrm((DEPTH, D), 0.02),
    }


def reference(x, ev_w_in, ev_mu, ev_w0, ev_w2, ev_a0, ev_a2, ev_g2, ev_k_k, ev_k_a,
              ev_r_k, ev_lnx_g, ev_lnx_b, ev_pool_w, ev_pool_scale, ev_w_out,
              od_w_in, od_conv_w, od_conv_b, od_cln_g, od_cln_b, od_gn_g, od_gn_b,
              od_w_out, ln_mix_g, ln_mix_b, rg_w, rg_b, re_w, re_b, e_w1, e_w3, e_w2,
              ln_ffn_g, ln_ffn_b):
    for i in range(DEPTH):
        j = i // 2
        if i % 2 == 0:
            h = rwkv_pool_mixer(x, ev_w_in[j], ev_mu[j], ev_w0[j], ev_w2[j], ev_a0[j],
                                ev_a2[j], ev_g2[j], ev_k_k[j], ev_k_a[j], ev_r_k[j],
                                ev_lnx_g[j], ev_lnx_b[j], ev_pool_w[j], ev_pool_scale[j],
                                ev_w_out[j])
        else:
            h = conv_retention_mixer(x, od_w_in[j], od_conv_w[j], od_conv_b[j], od_cln_g[j],
                                     od_cln_b[j], od_gn_g[j], od_gn_b[j], od_w_out[j])
        x = layer_norm(ALPHA * x + h, ln_mix_g[i], ln_mix_b[i])
        f = hier_moe(x, rg_w[i], rg_b[i], re_w[i], re_b[i], e_w1[i], e_w3[i], e_w2[i])
        x = layer_norm(ALPHA * x + f, ln_ffn_g[i], ln_ffn_b[i])
    return x
```

```python
import numpy as np


import concourse.bass as bass
import concourse.mybir as mybir

F32 = mybir.dt.float32
BF16 = mybir.dt.bfloat16
I32 = mybir.dt.int32
U32 = mybir.dt.uint32
AF = mybir.ActivationFunctionType
ALU = mybir.AluOpType
AX = mybir.AxisListType


class _Buf:
    def __init__(self, name, t):
        self.name = name
        self.t = t

    def __getitem__(self, idx):
        return self.t[idx]


class Sched:
    ENGS = ('pe', 'act', 'dve', 'pool', 'sp')

    def __init__(self, nc, self_wait=True):
        self.nc = nc
        self.self_wait = self_wait
        self.ops = {e: [] for e in self.ENGS}
        self.sems = {}
        self.cnt = {}
        self.waited = {e: {} for e in self.ENGS}
        self.last_w = {}
        self.readers = {}
        self.ctx = []
        self.pending_noinc = {e: False for e in self.ENGS}
        for e in ('pe', 'act', 'dve', 'pool'):
            self.sems[e] = nc.alloc_semaphore(name='sem_' + e)
            self.cnt[e] = 0
        self.ntile = 0

    def sbuf(self, name, shape, dtype):
        g = self.nc.sbuf_tensor('sb_' + name, list(shape), dtype)
        t = g.__enter__()
        self.ctx.append(g)
        return _Buf(name, t)

    def psum(self, name, shape, dtype=F32):
        g = self.nc.psum_tensor('ps_' + name, list(shape), dtype)
        t = g.__enter__()
        self.ctx.append(g)
        return _Buf(name, t)

    def dma_sem(self, key):
        if key not in self.sems:
            self.sems[key] = self.nc.alloc_semaphore(name='dsem_%d' % len(self.sems))
            self.cnt[key] = 0
        return key

    def _deps(self, reads, writes):
        deps = []
        for r in reads:
            if r in self.last_w:
                deps.append(self.last_w[r])
        for w in writes:
            if w in self.last_w:
                deps.append(self.last_w[w])
            deps.extend(self.readers.get(w, []))
        return deps

    def _emit_waits(self, eng, deps):
        waits = []
        best = {}
        for (sk, v) in deps:
            if sk == eng and (eng == 'pe' or not self.self_wait):
                continue
            if self.waited[eng].get(sk, 0) >= v:
                continue
            if best.get(sk, 0) < v:
                best[sk] = v
        for sk, v in best.items():
            self.waited[eng][sk] = v
            waits.append((self.sems[sk], v))
        return waits

    def _record(self, ev, reads, writes):
        for r in reads:
            self.readers.setdefault(r, []).append(ev)
        for w in writes:
            self.last_w[w] = ev
            self.readers[w] = []

    def op(self, eng, fn, reads=(), writes=(), inc=True):
        reads = [k for k in reads]
        writes = [k for k in writes]
        waits = self._emit_waits(eng, self._deps(reads, writes))
        sem = self.sems[eng]
        if inc:
            self.cnt[eng] += 1
            ev = (eng, self.cnt[eng])
            self.pending_noinc[eng] = False
        else:
            ev = (eng, self.cnt[eng] + 1)
            self.pending_noinc[eng] = True

        def emit(e, fn=fn, waits=waits, inc=inc, sem=sem):
            for (s, v) in waits:
                e.wait_ge(s, v)
            ins = fn(e)
            if inc:
                ins.then_inc(sem, 1)
        self.ops[eng].append(emit)
        self._record(ev, reads, writes)
        return ev

    def dma(self, eng, out, in_, semkey, reads=(), writes=(), **kw):
        self.dma_sem(semkey)
        reads = list(reads)
        writes = list(writes)
        waits = self._emit_waits(eng, self._deps(reads, writes))
        self.cnt[semkey] += 16
        ev = (semkey, self.cnt[semkey])
        sem = self.sems[semkey]

        def emit(e, waits=waits, sem=sem, out=out, in_=in_, kw=kw):
            for (s, v) in waits:
                e.wait_ge(s, v)
            e.dma_start(out=out, in_=in_, **kw).then_inc(sem, 16)
        self.ops[eng].append(emit)
        self._record(ev, reads, writes)
        return ev

    def wait_all(self, eng, keys):
        deps = []
        for k in keys:
            if k in self.last_w:
                deps.append(self.last_w[k])
        waits = self._emit_waits(eng, deps)

        def emit(e, waits=waits):
            for (s, v) in waits:
                e.wait_ge(s, v)
        self.ops[eng].append(emit)

    def build(self):
        nc = self.nc
        for e in self.ENGS:
            assert not self.pending_noinc[e], 'dangling noinc on ' + e
        with nc.Block() as block:
            @block.tensor
            def _(e):
                for f in self.ops['pe']:
                    f(e)

            @block.scalar
            def _(e):
                for f in self.ops['act']:
                    f(e)

            @block.vector
            def _(e):
                for f in self.ops['dve']:
                    f(e)

            @block.gpsimd
            def _(e):
                for f in self.ops['pool']:
                    f(e)

            @block.sync
            def _(e):
                for f in self.ops['sp']:
                    f(e)
        for g in reversed(self.ctx):
            g.__exit__(None, None, None)


D = 1024
HALO = 16
NCH = 18


def build_0a(ntok=4096, tile=512, dbg=None):
    nc = bass.Bass('TRN2', target_bir_lowering=False)
    dt = nc.dram_tensor
    xT = dt("xT", [D, HALO + ntok], F32, kind="ExternalInput").ap()
    w_in = dt("w_in", [D, 2304], F32, kind="ExternalInput").ap()
    cols = dt("cols", [128, 40], F32, kind="ExternalInput").ap()
    w2 = dt("w2", [64, 512], F32, kind="ExternalInput").ap()
    a2 = dt("a2", [64, 512], F32, kind="ExternalInput").ap()
    g2 = dt("g2", [128, 512], F32, kind="ExternalInput").ap()
    pool_w = dt("pool_w", [4, 128, 128], F32, kind="ExternalInput").ap()
    bones_d = dt("bones", [128, 128], F32, kind="ExternalInput").ap()
    invc_d = dt("invc", [128, 2, 4, tile], F32, kind="ExternalInput").ap()
    outs = {n: dt(n, [512, ntok], F32, kind="ExternalOutput").ap()
            for n in ('oR', 'oK', 'oV', 'oA', 'oB', 'oW', 'oG', 'oBon', 'oYP')}
    s = Sched(nc)
    emit_0a(s, xT, w_in, cols, w2, a2, g2, pool_w, bones_d, invc_d, outs, ntok, tile)
    s.build()
    return nc


OQ = 'sp'


def emit_0a(s, xT, w_in, cols_d, w2_d, a2_d, g2_d, pool_w_d, bones_d, invc_d, outs, ntok, tile):
    T = tile
    W = s.sbuf('W', [128, 8, 2304], BF16)
    for k in range(8):
        s.dma('pool', W[:, k, :], w_in[k * 128:(k + 1) * 128, :], 'w0', writes=[('W', k)])
    WK = [('W', k) for k in range(8)]
    COLS = s.sbuf('COLS', [128, 40], F32)
    s.dma('sp', COLS[:], cols_d, 'c0', writes=['COLS'])
    W2 = s.sbuf('W2', [64, 512], BF16)
    A2 = s.sbuf('A2', [128, 512], BF16)
    G2 = s.sbuf('G2', [128, 512], BF16)
    PW = s.sbuf('PW', [128, 4, 128], BF16)
    BONES = s.sbuf('BONES', [128, 128], BF16)
    INVC = s.sbuf('INVC', [128, 2, 4, T], F32)
    s.dma('pool', W2[:], w2_d, 'c1', writes=['W2'])
    s.dma('pool', A2[64:128, :], a2_d, 'c2', writes=['A2'])
    s.dma('pool', G2[:], g2_d, 'c3', writes=['G2'])
    for g in range(4):
        s.dma('pool', PW[:, g, :], pool_w_d[g], ('c4', g), writes=[('PW', g)])
    s.dma('pool', BONES[:], bones_d, 'c5', writes=['BONES'])
    for a in range(2):
        s.dma('sp', INVC[:, a, :, :], invc_d[:, a, :, :], ('c6', a), writes=[('INVC', a)])
    c24 = s.sbuf('c24', [128, 1], F32)
    s.op('dve', lambda e: e.memset(c24[:], 0.0), writes=['c24'])

    PR = s.sbuf('PR', [128, NCH, HALO + T], F32)
    Z = s.sbuf('Z', [128, 14, T], F32)
    xb = [s.sbuf('xb%d' % i, [128, 8, T], BF16) for i in range(2)]
    xh = s.sbuf('xh', [128, 8, HALO], BF16)
    P = [s.psum('P%d' % i, [128, 512]) for i in range(8)]
    tw = s.sbuf('tw', [64, T], BF16)
    al = s.sbuf('al', [128, T], BF16)
    sg = s.sbuf('sg', [128, T], BF16)
    tmp = [s.sbuf('tmp%d' % i, [128, T], F32) for i in range(4)]
    tb = [s.sbuf('tb%d' % i, [128, T], BF16) for i in range(2)]
    O = {n: [s.sbuf('O_%s%d' % (n, i), [128, T], F32) for i in range(2)] for n in ('oA', 'oB', 'oK', 'oW', 'oG', 'oBon', 'oYP')}
    SS = [s.sbuf('SS%d' % i, [128, HALO + T], F32) for i in range(2)]

    s.dma('pool', xh[:], xT[:, 0:HALO].rearrange("(k p) t -> p k t", p=128), 'xh', writes=['xh'])
    for c in range(NCH):
        pb = P[c % 2]
        pk = ('P', c % 2)
        for k in range(8):
            s.op('pe', lambda e, pb=pb, k=k, c=c: e.matmul(pb[:, 0:HALO], lhsT=W[:, k, c * 128:(c + 1) * 128],
                                                           rhs=xh[:, k, :], start=(k == 0), stop=(k == 7)),
                 reads=['xh'] + WK, writes=[pk], inc=(k == 7))
        s.op('act', lambda e, pb=pb, c=c: e.copy(out=PR[:, c, 0:HALO], in_=pb[:, 0:HALO]), reads=[pk], writes=[('PR', c)])

    ntile = ntok // T
    def tile_body(ti):
        t0 = ti * T
        x_ = xb[ti % 2]
        xk = ('xb', ti % 2)
        for k in range(8):
            s.dma('pool', x_[:, k, :], xT[k * 128:(k + 1) * 128, HALO + t0:HALO + t0 + T], ('x', ti % 2), writes=[(xk, k)])
        XK = [(xk, k) for k in range(8)]
        for c in range(NCH):
            pb = P[c % 2]
            pk = ('P', c % 2)
            for k in range(8):
                s.op('pe', lambda e, pb=pb, k=k, c=c, x_=x_: e.matmul(pb[:], lhsT=W[:, k, c * 128:(c + 1) * 128],
                                                                      rhs=x_[:, k, :], start=(k == 0), stop=(k == 7)),
                     reads=XK + WK, writes=[pk], inc=(k == 7))
            s.op('act', lambda e, pb=pb, c=c: e.copy(out=PR[:, c, HALO:HALO + T], in_=pb[:]), reads=[pk], writes=[('PR', c)])
        for c in range(14):
            d_ = tmp[c % 2]
            dk = ('tmp', c % 2)
            s.op('dve', lambda e, c=c, d_=d_: e.tensor_tensor(out=d_[:], in0=PR[:, c, HALO - 1:HALO - 1 + T],
                                                              in1=PR[:, c, HALO:HALO + T], op=ALU.subtract),
                 reads=[('PR', c)], writes=[dk])
            s.op('dve', lambda e, c=c, d_=d_: e.scalar_tensor_tensor(out=Z[:, c, :], in0=d_[:], scalar=COLS[:, c:c + 1],
                                                                     in1=PR[:, c, HALO:HALO + T], op0=ALU.mult, op1=ALU.add),
                 reads=[dk, ('PR', c), 'COLS'], writes=[('Z', c)])
        s.op('act', lambda e: e.activation(out=tw[:], in_=Z[0:64, 12, :], func=AF.Tanh), reads=[('Z', 12)], writes=['tw'])
        s.op('act', lambda e: e.copy(out=al[64:128, :], in_=Z[64:128, 12, :]), reads=[('Z', 12)], writes=['al'])
        s.op('act', lambda e: e.activation(out=sg[:], in_=Z[:, 13, :], func=AF.Sigmoid), reads=[('Z', 13)], writes=['sg'])
        ob = ti % 2
        for cc in range(4):
            cs = slice(cc * 128, (cc + 1) * 128)
            rows = slice(cc * 128, (cc + 1) * 128)
            tsl = slice(t0, t0 + T)
            s.op('pe', lambda e, cs=cs: e.matmul(P[2][:], lhsT=W2[:, cs], rhs=tw[:], start=True, stop=True),
                 reads=['W2', 'tw'], writes=[('P', 2)])
            s.op('act', lambda e, cc=cc: e.activation(out=O['oW'][ob][:], in_=P[2][:], func=AF.Sigmoid,
                                                     bias=COLS[:, 14 + cc:15 + cc], scale=1.0),
                 reads=[('P', 2), 'COLS'], writes=[('oW', ob)])
            s.dma(OQ, outs['oW'][rows, tsl], O['oW'][ob][:], ('so', 'oW', ob), reads=[('oW', ob)], writes=[('d_oW', ob)])
            s.op('pe', lambda e, cs=cs: e.matmul(P[3][:], lhsT=A2[64:128, cs], rhs=al[64:128, :], start=True, stop=True),
                 reads=['A2', 'al'], writes=[('P', 3)])
            asig = tmp[2]
            s.op('act', lambda e, cc=cc: e.activation(out=asig[:], in_=P[3][:], func=AF.Sigmoid,
                                                     bias=COLS[:, 18 + cc:19 + cc], scale=1.0),
                 reads=[('P', 3), 'COLS'], writes=[('tmp', 2)])
            s.op('pe', lambda e, cs=cs: e.matmul(P[4][:], lhsT=G2[:, cs], rhs=sg[:], start=True, stop=True),
                 reads=['G2', 'sg'], writes=[('P', 4)])
            s.op('act', lambda e: e.copy(out=O['oG'][ob][:], in_=P[4][:]), reads=[('P', 4)], writes=[('oG', ob)])
            s.dma(OQ, outs['oG'][rows, tsl], O['oG'][ob][:], ('so', 'oG', ob), reads=[('oG', ob)], writes=[('d_oG', ob)])
            kk0 = tmp[3]
            s.op('dve', lambda e, cc=cc: e.tensor_scalar(out=kk0[:], in0=Z[:, 4 + cc, :], scalar1=COLS[:, 22 + cc:23 + cc],
                                                         scalar2=None, op0=ALU.mult),
                 reads=[('Z', 4 + cc), 'COLS'], writes=[('tmp', 3)])
            s.op('dve', lambda e: e.tensor_tensor(out=tb[0][:], in0=kk0[:], in1=kk0[:], op=ALU.mult),
                 reads=[('tmp', 3)], writes=[('tb', 0)])
            s.op('pe', lambda e: e.matmul(P[5][:], lhsT=BONES[:], rhs=tb[0][:], start=True, stop=True),
                 reads=['BONES', ('tb', 0)], writes=[('P', 5)])
            rn = tmp[0]
            s.op('dve', lambda e: e.tensor_scalar(out=rn[:], in0=P[5][:], scalar1=1e-24, scalar2=None, op0=ALU.max),
                 reads=[('P', 5)], writes=[('tmp', 0)])
            s.op('act', lambda e: e.activation(out=rn[:], in_=rn[:], func=AF.Ln), reads=[('tmp', 0)], writes=[('tmp', 0)])
            s.op('act', lambda e: e.activation(out=rn[:], in_=rn[:], func=AF.Exp, scale=-0.5), reads=[('tmp', 0)], writes=[('tmp', 0)])
            s.op('dve', lambda e: e.tensor_tensor(out=kk0[:], in0=kk0[:], in1=rn[:], op=ALU.mult),
                 reads=[('tmp', 3), ('tmp', 0)], writes=[('tmp', 3)])
            s.op('act', lambda e: e.mul(out=O['oA'][ob][:], in_=kk0[:], mul=-1.0), reads=[('tmp', 3)], writes=[('oA', ob)])
            s.dma(OQ, outs['oA'][rows, tsl], O['oA'][ob][:], ('so', 'oA', ob), reads=[('oA', ob)], writes=[('d_oA', ob)])
            s.op('dve', lambda e: e.tensor_tensor(out=O['oB'][ob][:], in0=kk0[:], in1=asig[:], op=ALU.mult),
                 reads=[('tmp', 3), ('tmp', 2)], writes=[('oB', ob)])
            s.dma(OQ, outs['oB'][rows, tsl], O['oB'][ob][:], ('so', 'oB', ob), reads=[('oB', ob)], writes=[('d_oB', ob)])
            t1 = tmp[1]
            s.op('dve', lambda e, cc=cc: e.tensor_scalar(out=t1[:], in0=asig[:], scalar1=-1.0, scalar2=COLS[:, 26 + cc:27 + cc],
                                                         op0=ALU.add, op1=ALU.mult),
                 reads=[('tmp', 2), 'COLS'], writes=[('tmp', 1)])
            s.op('dve', lambda e, cc=cc: e.scalar_tensor_tensor(out=O['oK'][ob][:], in0=t1[:], scalar=1.0, in1=Z[:, 4 + cc, :],
                                                                op0=ALU.add, op1=ALU.mult),
                 reads=[('tmp', 1), ('Z', 4 + cc)], writes=[('oK', ob)])
            s.dma(OQ, outs['oK'][rows, tsl], O['oK'][ob][:], ('so', 'oK', ob), reads=[('oK', ob)], writes=[('d_oK', ob)])
            s.dma(OQ, outs['oR'][rows, tsl], Z[:, cc, :], ('so', 'oR', cc), reads=[('Z', cc)], writes=[('d_oR', cc)])
            s.dma(OQ, outs['oV'][rows, tsl], Z[:, 8 + cc, :], ('so', 'oV', cc), reads=[('Z', 8 + cc)], writes=[('d_oV', cc)])
            s.op('dve', lambda e, cc=cc: e.scalar_tensor_tensor(out=tb[1][:], in0=Z[:, cc, :], scalar=COLS[:, 30 + cc:31 + cc],
                                                                in1=O['oK'][ob][:], op0=ALU.mult, op1=ALU.mult),
                 reads=[('Z', cc), 'COLS', ('oK', ob)], writes=[('tb', 1)])
            s.op('pe', lambda e: e.matmul(P[6][:], lhsT=BONES[:], rhs=tb[1][:], start=True, stop=True),
                 reads=['BONES', ('tb', 1)], writes=[('P', 6)])
            s.op('dve', lambda e, cc=cc: e.tensor_tensor(out=O['oBon'][ob][:], in0=Z[:, 8 + cc, :], in1=P[6][:], op=ALU.mult),
                 reads=[('Z', 8 + cc), ('P', 6)], writes=[('oBon', ob)])
            s.dma(OQ, outs['oBon'][rows, tsl], O['oBon'][ob][:], ('so', 'oBon', ob), reads=[('oBon', ob)], writes=[('d_oBon', ob)])
        for g in range(4):
            c = 14 + g
            src = PR
            cur = None
            n = HALO + T
            prev_ap = lambda lo, hi, c=c: PR[:, c, lo:hi]
            a_, b_ = SS[0], SS[1]
            sh = 1
            first = True
            for lv in range(g + 1):
                lo = 2 * sh - 1
                if first:
                    s.op('pool', lambda e, c=c, sh=sh, n=n, lo=lo: e.tensor_tensor(out=SS[0][:, lo:n], in0=PR[:, c, lo:n], in1=PR[:, c, sh - 1:n - sh], op=ALU.add),
                         reads=[('PR', c)], writes=[('SS', 0)])
                    first = False
                    cur = 0
                else:
                    src_, dst_ = SS[cur], SS[1 - cur]
                    s.op('pool', lambda e, sh=sh, n=n, lo=lo, src_=src_, dst_=dst_: e.tensor_tensor(out=dst_[:, lo:n], in0=src_[:, lo:n],
                                                                                                in1=src_[:, sh - 1:n - sh], op=ALU.add),
                         reads=[('SS', cur)], writes=[('SS', 1 - cur)])
                    cur = 1 - cur
                sh *= 2
            ia = 0 if ti == 0 else 1
            s.op('dve', lambda e, g=g, cur=cur, ia=ia: e.tensor_tensor(out=tmp[0][:], in0=SS[cur][:, HALO:HALO + T], in1=INVC[:, ia, g, :], op=ALU.mult),
                 reads=[('SS', cur), ('INVC', ia)], writes=[('tmp', 0)])
            s.op('dve', lambda e, c=c: e.tensor_tensor(out=tb[0][:], in0=tmp[0][:], in1=PR[:, c, HALO:HALO + T], op=ALU.subtract),
                 reads=[('tmp', 0), ('PR', c)], writes=[('tb', 0)])
            s.op('pe', lambda e, g=g: e.matmul(P[7][:], lhsT=PW[:, g, :], rhs=tb[0][:], start=True, stop=True),
                 reads=[('PW', g), ('tb', 0)], writes=[('P', 7)])
            s.op('act', lambda e, g=g: e.activation(out=O['oYP'][ob][:], in_=P[7][:], func=AF.Identity, scale=COLS[:, 34 + g:35 + g]),
                 reads=[('P', 7), 'COLS'], writes=[('oYP', ob)])
            s.dma(OQ, outs['oYP'][g * 128:(g + 1) * 128, t0:t0 + T], O['oYP'][ob][:], ('so', 'oYP', ob), reads=[('oYP', ob)], writes=[('d_oYP', ob)])
        for c in range(NCH):
            s.op('pool', lambda e, c=c: e.tensor_copy(out=PR[:, c, 0:HALO], in_=PR[:, c, T:T + HALO]),
                 reads=[('PR', c)], writes=[('PR', c)])
    for ti in range(ntile):
        tile_body(ti)
    s.wait_all(OQ, [k for k in s.last_w if isinstance(k, tuple) and isinstance(k[0], str) and k[0].startswith('d_')])


def host_inputs_0a(xT_halo, w_in, mu, w0, a0, k_k, k_a, r_k, pool_scale, w2, a2, g2, pool_w, first, tile=512):
    cols = np.zeros((128, 40), np.float32)
    cols[:, 0:14] = mu.reshape(14, 128).T
    cols[:, 14:18] = w0.reshape(4, 128).T
    cols[:, 18:22] = a0.reshape(4, 128).T
    cols[:, 22:26] = k_k.reshape(4, 128).T
    cols[:, 26:30] = k_a.reshape(4, 128).T
    cols[:, 30:34] = r_k.reshape(4, 128).T
    cols[:, 34:38] = pool_scale.reshape(4, 128).T
    bones = np.zeros((128, 128), np.float32)
    bones[:64, :64] = 1.0
    bones[64:, 64:] = 1.0
    invc = np.zeros((128, 2, 4, tile), np.float32)
    t = np.arange(tile)
    for g, win in enumerate((2, 4, 8, 16)):
        invc[:, 1, g, :] = 1.0 / win
        invc[:, 0, g, :] = (1.0 / np.minimum(t + 1, win)) if first else 1.0 / win
    return dict(xT=np.ascontiguousarray(xT_halo), w_in=np.ascontiguousarray(w_in), cols=cols,
                w2=np.ascontiguousarray(w2), a2=np.ascontiguousarray(a2), g2=np.ascontiguousarray(g2),
                pool_w=np.ascontiguousarray(pool_w), bones=bones, invc=invc)


C = 64
NH = 4
C0 = 0.6065306597126334
RWKV_EPS = 64e-5


def MM(s, out, lhsT, rhs, start=True, stop=True, reads=(), writes=(), inc=True):
    s.op('pe', lambda e: e.matmul(out, lhsT=lhsT, rhs=rhs, start=start, stop=stop), reads=reads, writes=writes, inc=inc)


def TT(s, eng, out, in0, in1, op, reads, writes):
    s.op(eng, lambda e: e.tensor_tensor(out=out, in0=in0, in1=in1, op=op), reads=reads, writes=writes)


def ACTF(s, out, in_, func, reads, writes, bias=None, scale=1.0):
    if bias is None:
        s.op('act', lambda e: e.activation(out=out, in_=in_, func=func, scale=scale), reads=reads, writes=writes)
    else:
        s.op('act', lambda e: e.activation(out=out, in_=in_, func=func, bias=bias, scale=scale), reads=reads, writes=writes)


def TS(s, eng, out, in0, s1, s2, op0, op1, reads, writes):
    if op1 is None:
        s.op(eng, lambda e: e.tensor_scalar(out=out, in0=in0, scalar1=s1, scalar2=None, op0=op0), reads=reads, writes=writes)
    else:
        s.op(eng, lambda e: e.tensor_scalar(out=out, in0=in0, scalar1=s1, scalar2=s2, op0=op0, op1=op1), reads=reads, writes=writes)


def STT(s, out, in0, scalar, in1, op0, op1, reads, writes):
    s.op('dve', lambda e: e.scalar_tensor_tensor(out=out, in0=in0, scalar=scalar, in1=in1, op0=op0, op1=op1),
         reads=reads, writes=writes)


def CP(s, eng, out, in_, reads, writes):
    if eng == 'act':
        s.op('act', lambda e: e.copy(out=out, in_=in_), reads=reads, writes=writes)
    else:
        s.op(eng, lambda e: e.tensor_copy(out=out, in_=in_), reads=reads, writes=writes)


def RED(s, out, in_, reads, writes):
    s.op('dve', lambda e: e.reduce_sum(out=out, in_=in_, axis=AX.X), reads=reads, writes=writes)


def build_0b(T=8192, GC=4):
    nc = bass.Bass('TRN2', target_bir_lowering=False)
    dt = nc.dram_tensor
    tin = {n: dt(n, [64, NH, T], F32, kind="ExternalInput").ap() for n in ('RT', 'KT', 'AT', 'BT')}
    nin = {n: dt(n, [T, NH * 64], F32, kind="ExternalInput").ap() for n in ('Wn', 'Bn', 'Kn', 'Vn', 'BONn', 'Gn')}
    cst = {n: dt(n, shp, F32, kind="ExternalInput").ap() for n, shp in
           (('MU1', [64, NH, 128]), ('MLS', [64, NH, 64]), ('I4', [64, NH, 64]), ('TRI', [64, 128]), ('UPS', [64, 64]),
            ('LGB', [64, 2, NH * 64]))}
    y = dt("y", [T, NH * 64], F32, kind="ExternalOutput").ap()
    s = Sched(nc)
    emit_0b(s, tin, nin, cst, y, T, GC)
    s.build()
    return nc


def emit_0b(s, tin, nin, cst, y, T, GC):
    K = {}
    for n, shp in (('MU1', [64, NH, 128]), ('MLS', [64, NH, 64]), ('I4', [64, NH, 64]), ('TRI', [64, 128]), ('UPS', [64, 64]),
                   ('LGB', [64, 2, NH * 64])):
        K[n] = s.sbuf('K_' + n, shp, F32)
        s.dma('sp', K[n][:], cst[n], ('k', n), writes=[n])
    epst = s.sbuf('epst', [64, 1], F32)
    s.op('dve', lambda e: e.memset(epst[:], RWKV_EPS), writes=['epst'])
    S0 = s.sbuf('S0', [64, NH, 64], F32)
    s.op('dve', lambda e: e.memset(S0[:], 0.0), writes=['S0'])
    PS = [s.psum('PS%d' % i, [128, 512]) for i in range(8)]

    def bank(i, w):
        return PS[i][0:64, 0:NH * w].rearrange("p (h x) -> p h x", h=NH)
    GT = GC * C
    tbuf = {n: [s.sbuf('t_%s%d' % (n, i), [64, NH, GT], F32) for i in range(2)] for n in tin}
    nbuf = {n: [s.sbuf('n_%s%d' % (n, i), [64, GC, NH * 64], F32) for i in range(2)] for n in nin}
    f4 = lambda name, w=64: s.sbuf(name, [64, NH, w], F32)
    PT, PINV, PPREV = f4('PT'), f4('PINV'), f4('PPREV')
    E1 = s.sbuf('E1', [64, NH * 64], F32)
    AR = s.sbuf('AR', [64, NH, 2, 64], F32)
    BK = s.sbuf('BK', [64, NH, 2, 64], F32)
    BKn = s.sbuf('BKn', [64, 2, NH * 64], F32)
    X1, X2 = f4('X1', 128), f4('X2', 128)
    Nb = [f4('N0'), f4('N1')]
    NTb = [f4('NT0'), f4('NT1')]
    Rb = [f4('R0'), f4('R1')]
    Wsb, Usb, Ysb, Ysq, Yn = f4('Wsb'), f4('Usb'), f4('Ysb'), f4('Ysq'), f4('Yn')
    st = s.sbuf('st', [64, 6, NH], F32)
    Yo = [s.sbuf('Yo%d' % i, [64, NH * 64], F32) for i in range(2)]
    nchunk = T // C
    ngrp = nchunk // GC

    def load_group(gi):
        b = gi % 2
        t0 = gi * GT
        for n in tin:
            s.dma('sp', tbuf[n][b][:], tin[n][:, :, t0:t0 + GT], ('lt', n, b), writes=[('t', n, b)])
        for n in nin:
            s.dma('pool', nbuf[n][b][:], nin[n][t0:t0 + GT, :].rearrange("(c p) n -> p c n", p=64), ('ln', n, b),
                  writes=[('n', n, b)])

    def chunk(ci):
        gi, cj = divmod(ci, GC)
        b = gi % 2
        tv = lambda n: tbuf[n][b][:, :, cj * C:(cj + 1) * C]
        tk = lambda n: ('t', n, b)
        nv = lambda n: nbuf[n][b][:, cj, :]
        nk = lambda n: ('n', n, b)
        for h in range(NH):
            MM(s, PS[0][0:64, h * 128:(h + 1) * 128], nbuf['Wn'][b][:, cj, h * 64:(h + 1) * 64], K['TRI'][:],
               reads=[nk('Wn'), 'TRI'], writes=['B0'], inc=(h == NH - 1))
        MM(s, PS[1][0:64, 0:NH * 64], K['UPS'][:], nv('Wn'), reads=[nk('Wn'), 'UPS'], writes=['B1'])
        cumI = bank(0, 128)[:, :, 0:64]
        cumS = bank(0, 128)[:, :, 64:128]
        ACTF(s, PT[:], cumI, AF.Exp, ['B0'], ['PT'], scale=-C0)
        ACTF(s, PINV[:], cumI, AF.Exp, ['B0'], ['PINV'], scale=C0)
        ACTF(s, PPREV[:], cumS, AF.Exp, ['B0'], ['PPREV'], scale=-C0)
        ACTF(s, E1[:], PS[1][0:64, 0:NH * 64], AF.Exp, ['B1'], ['E1'], scale=-C0)
        TT(s, 'dve', AR[:, :, 0, :], tv('AT'), PPREV[:], ALU.mult, [tk('AT'), 'PPREV'], ['AR0'])
        TT(s, 'pool', AR[:, :, 1, :], tv('RT'), PT[:], ALU.mult, [tk('RT'), 'PT'], ['AR1'])
        TT(s, 'dve', BK[:, :, 0, :], tv('BT'), PINV[:], ALU.mult, [tk('BT'), 'PINV'], ['BK0'])
        TT(s, 'pool', BK[:, :, 1, :], tv('KT'), PINV[:], ALU.mult, [tk('KT'), 'PINV'], ['BK1'])
        TT(s, 'pool', BKn[:, 0, :], nv('Bn'), E1[:], ALU.mult, [nk('Bn'), 'E1'], ['BKn0'])
        TT(s, 'pool', BKn[:, 1, :], nv('Kn'), E1[:], ALU.mult, [nk('Kn'), 'E1'], ['BKn1'])
        for h in range(NH):
            arh = AR[:, h, :, :].rearrange("p a x -> p (a x)")
            MM(s, PS[2][0:64, h * 128:(h + 1) * 128], BK[:, h, 0, :], arh, reads=['BK0', 'AR0', 'AR1'], writes=['B2'],
               inc=(h == NH - 1))
        for h in range(NH):
            arh = AR[:, h, :, :].rearrange("p a x -> p (a x)")
            MM(s, PS[3][0:64, h * 128:(h + 1) * 128], BK[:, h, 1, :], arh, reads=['BK1', 'AR0', 'AR1'], writes=['B3'],
               inc=(h == NH - 1))
        for h in range(NH):
            MM(s, PS[4][0:64, h * 64:(h + 1) * 64], AR[:, h, 0, :], BK[:, h, 0, :], reads=['BK0', 'AR0'], writes=['B4'],
               inc=(h == NH - 1))
        TT(s, 'dve', X1[:], bank(2, 128), K['MU1'][:], ALU.mult, ['B2', 'MU1'], ['X1'])
        TT(s, 'dve', X2[:], bank(3, 128), K['MU1'][:], ALU.mult, ['B3', 'MU1'], ['X2'])
        TT(s, 'dve', NTb[0][:], bank(4, 64), K['MLS'][:], ALU.mult, ['B4', 'MLS'], [('NT', 0)])
        CP(s, 'pool', Nb[0][:], X1[:, :, 0:64], ['X1'], [('N', 0)])
        TT(s, 'pool', Rb[0][:], X1[:, :, 0:64], K['I4'][:], ALU.add, ['X1', 'I4'], [('R', 0)])
        cur = 0
        for lv in range(5):
            nx = 1 - cur
            if lv < 4:
                for h in range(NH):
                    MM(s, PS[5][0:64, h * 64:(h + 1) * 64], NTb[cur][:, h, :], Nb[cur][:, h, :],
                       reads=[('NT', cur), ('N', cur)], writes=['B5'], inc=(h == NH - 1))
            for h in range(NH):
                MM(s, PS[6][0:64, h * 64:(h + 1) * 64], Nb[cur][:, h, :], NTb[cur][:, h, :],
                   reads=[('NT', cur), ('N', cur)], writes=['B6'], inc=(h == NH - 1))
            if lv < 4:
                CP(s, 'act', Nb[nx][:], bank(5, 64), ['B5'], [('N', nx)])
            CP(s, 'dve', NTb[nx][:], bank(6, 64), ['B6'], [('NT', nx)])
            for h in range(NH):
                MM(s, PS[7][0:64, h * 64:(h + 1) * 64], NTb[nx][:, h, :], Rb[cur][:, h, :],
                   reads=[('NT', nx), ('R', cur)], writes=['B7'], inc=(h == NH - 1))
            TT(s, 'dve', Rb[nx][:], Rb[cur][:], bank(7, 64), ALU.add, [('R', cur), 'B7'], [('R', nx)])
            cur = nx
        R = Rb[cur]
        Rk = ('R', cur)
        for h in range(NH):
            vh = nbuf['Vn'][b][:, cj, h * 64:(h + 1) * 64]
            MM(s, PS[2][0:64, h * 64:(h + 1) * 64], X2[:, h, 0:64], vh, start=True, stop=False,
               reads=['X2', nk('Vn')], writes=['B2'], inc=False)
            MM(s, PS[2][0:64, h * 64:(h + 1) * 64], AR[:, h, 0, :], S0[:, h, :], start=False, stop=True,
               reads=['AR0', 'S0'], writes=['B2'], inc=(h == NH - 1))
        CP(s, 'dve', Wsb[:], bank(2, 64), ['B2'], ['Wsb'])
        for h in range(NH):
            MM(s, PS[3][0:64, h * 64:(h + 1) * 64], R[:, h, :], Wsb[:, h, :], reads=[Rk, 'Wsb'], writes=['B3'],
               inc=(h == NH - 1))
        CP(s, 'dve', Usb[:], bank(3, 64), ['B3'], ['Usb'])
        for h in range(NH):
            vh = nbuf['Vn'][b][:, cj, h * 64:(h + 1) * 64]
            o = PS[4][0:64, h * 64:(h + 1) * 64]
            MM(s, o, AR[:, h, 1, :], S0[:, h, :], start=True, stop=False, reads=['AR1', 'S0'], writes=['B4'], inc=False)
            MM(s, o, X1[:, h, 64:128], Usb[:, h, :], start=False, stop=False, reads=['X1', 'Usb'], writes=['B4'], inc=False)
            MM(s, o, X2[:, h, 64:128], vh, start=False, stop=True, reads=['X2', nk('Vn')], writes=['B4'], inc=(h == NH - 1))
        for h in range(NH):
            vh = nbuf['Vn'][b][:, cj, h * 64:(h + 1) * 64]
            o = PS[7][0:64, h * 64:(h + 1) * 64]
            MM(s, o, BKn[:, 0, h * 64:(h + 1) * 64], Usb[:, h, :], start=True, stop=False, reads=['BKn0', 'Usb'], writes=['B7'], inc=False)
            MM(s, o, BKn[:, 1, h * 64:(h + 1) * 64], vh, start=False, stop=True, reads=['BKn1', nk('Vn')], writes=['B7'], inc=(h == NH - 1))
        CP(s, 'dve', Ysb[:], bank(4, 64), ['B4'], ['Ysb'])
        for h in range(NH):
            STT(s, S0[:, h, :], S0[:, h, :], PT[:, h, 63:64], PS[7][0:64, h * 64:(h + 1) * 64], ALU.mult, ALU.add,
                ['S0', 'PT', 'B7'], ['S0'])
        RED(s, st[:, 0, :], Ysb[:], ['Ysb'], ['st0'])
        TT(s, 'pool', Ysq[:], Ysb[:], Ysb[:], ALU.mult, ['Ysb'], ['Ysq'])
        RED(s, st[:, 1, :], Ysq[:], ['Ysq'], ['st1'])
        TS(s, 'dve', st[:, 2, :], st[:, 0, :], 1.0 / 64, None, ALU.mult, None, ['st0'], ['st2'])
        TT(s, 'dve', st[:, 3, :], st[:, 2, :], st[:, 2, :], ALU.mult, ['st2'], ['st3'])
        STT(s, st[:, 4, :], st[:, 1, :], 1.0 / 64, st[:, 3, :], ALU.mult, ALU.subtract, ['st1', 'st3'], ['st4'])
        ACTF(s, st[:, 5, :], st[:, 4, :], AF.Ln, ['st4', 'epst'], ['st5'], bias=epst[:], scale=1.0)
        ACTF(s, st[:, 5, :], st[:, 5, :], AF.Exp, ['st5'], ['st5'], scale=-0.5)
        for h in range(NH):
            TS(s, 'dve', Yn[:, h, :], Ysb[:, h, :], st[:, 2, h:h + 1], st[:, 5, h:h + 1], ALU.subtract, ALU.mult,
               ['Ysb', 'st2', 'st5'], ['Yn'])
        ynf = Yn[:].rearrange("p h x -> p (h x)")
        yo = Yo[ci % 2]
        yok = ('Yo', ci % 2)
        TT(s, 'pool', ynf, ynf, K['LGB'][:, 0, :], ALU.mult, ['Yn', 'LGB'], ['Yn'])
        TT(s, 'pool', ynf, ynf, K['LGB'][:, 1, :], ALU.add, ['Yn', 'LGB'], ['Yn'])
        TT(s, 'pool', ynf, ynf, nv('BONn'), ALU.add, ['Yn', nk('BONn')], ['Yn'])
        TT(s, 'pool', yo[:], ynf, nv('Gn'), ALU.mult, ['Yn', nk('Gn')], [yok])
        s.dma('sp', y[ci * C:(ci + 1) * C, :], yo[:], ('so', ci % 2), reads=[yok], writes=[('yd', ci % 2)])

    load_group(0)
    for gi in range(ngrp):
        if gi + 1 < ngrp:
            load_group(gi + 1)
        for cj in range(GC):
            chunk(gi * GC + cj)
    s.wait_all('sp', [('yd', 0), ('yd', 1)])


def host_consts_0b(lnx_g, lnx_b):
    j = np.arange(64)[:, None]
    x = np.arange(64)[None, :]
    su = (x > j).astype(np.float32)
    iu = (x >= j).astype(np.float32)
    sl = (x < j).astype(np.float32)
    MU1 = np.broadcast_to(np.concatenate([su, iu], 1)[:, None, :], (64, NH, 128))
    MLS = np.broadcast_to(sl[:, None, :], (64, NH, 64))
    I4 = np.broadcast_to(np.eye(64, dtype=np.float32)[:, None, :], (64, NH, 64))
    TRI = np.concatenate([iu, su], 1)
    UPS = sl
    LGB = np.broadcast_to(np.stack([lnx_g, lnx_b])[None], (64, 2, NH * 64))
    return {k: np.ascontiguousarray(v, dtype=np.float32) for k, v in
            dict(MU1=MU1, MLS=MLS, I4=I4, TRI=TRI, UPS=UPS, LGB=LGB).items()}


def host_inputs_0b(arrT, lnx_g, lnx_b):
    T = arrT['oR'].shape[1]
    tr = lambda a: np.ascontiguousarray(a.reshape(NH, 64, T).transpose(1, 0, 2))
    na = lambda a: np.ascontiguousarray(a.T)
    d = dict(RT=tr(arrT['oR']), KT=tr(arrT['oK']), AT=tr(arrT['oA']), BT=tr(arrT['oB']),
             Wn=na(arrT['oW']), Bn=na(arrT['oB']), Kn=na(arrT['oK']), Vn=na(arrT['oV']),
             BONn=na(arrT['oBon']), Gn=na(arrT['oG']))
    d.update(host_consts_0b(lnx_g, lnx_b))
    return d


MMDT = BF16


def build_0b(T=8192, GP=2):
    nc = bass.Bass('TRN2', target_bir_lowering=False)
    dt = nc.dram_tensor
    tin = {n: dt(n, [64, NH, T], F32, kind="ExternalInput").ap() for n in ('RT', 'KT', 'AT', 'BT')}
    nin = {n: dt(n, [T, NH * 64], F32, kind="ExternalInput").ap() for n in ('Wn', 'Bn', 'Kn', 'Vn', 'BONn', 'Gn')}
    cst = {n: dt(n, shp, F32, kind="ExternalInput").ap() for n, shp in
           (('MU1', [128, NH, 128]), ('MLS', [128, NH, 64]), ('I4', [128, NH, 64]), ('TRI', [128, 128]), ('UPS', [128, 64]),
            ('LGB', [128, 2, NH * 64]))}
    y = dt("y", [T, NH * 64], F32, kind="ExternalOutput").ap()
    s = Sched(nc)
    emit_0b(s, tin, nin, cst, y, T, GP)
    s.build()
    return nc


def emit_0b(s, tin, nin, cst, y, T, GP):
    K = {}
    for n, shp in (('MU1', [128, NH, 128]), ('MLS', [128, NH, 64]), ('I4', [128, NH, 64]), ('TRI', [128, 128]), ('UPS', [128, 64]),
                   ('LGB', [128, 2, NH * 64])):
        K[n] = s.sbuf('K_' + n, shp, F32)
        s.dma('sp', K[n][:], cst[n], ('k', n), writes=[n])
    epst = s.sbuf('epst', [128, 1], F32)
    s.op('dve', lambda e: e.memset(epst[:], RWKV_EPS), writes=['epst'])
    S0 = s.sbuf('S0', [128, NH, 64], MMDT)
    s.op('dve', lambda e: e.memset(S0[:], 0.0), writes=[('S0', 0), ('S0', 1)])
    PS = [s.psum('PS%d' % i, [128, 512]) for i in range(8)]
    HS = [slice(0, 64), slice(64, 128)]

    def bank(i, w):
        return PS[i][:, 0:NH * w].rearrange("p (h x) -> p h x", h=NH)
    B2 = lambda k: [(k, 0), (k, 1)]
    GT = GP * 2 * C
    tbuf = {n: [s.sbuf('t_%s%d' % (n, i), [128, NH, GP, C], F32) for i in range(2)] for n in tin}
    nbuf = {n: [s.sbuf('n_%s%d' % (n, i), [128, GP, NH * 64], MMDT if n == 'Vn' else F32) for i in range(2)] for n in nin}
    f4 = lambda name, w=64, dt_=F32: s.sbuf(name, [128, NH, w], dt_)
    PT, PINV, PPREV = f4('PT'), f4('PINV'), f4('PPREV')
    E1 = s.sbuf('E1', [128, NH * 64], F32)
    AR = s.sbuf('AR', [128, NH, 2, 64], MMDT)
    BK = s.sbuf('BK', [128, NH, 2, 64], MMDT)
    BKn = s.sbuf('BKn', [128, 2, NH * 64], MMDT)
    DG = f4('DG', 64, MMDT)
    X1, X2 = f4('X1', 128, MMDT), f4('X2', 128, MMDT)
    Nb = [f4('N0', 64, MMDT), f4('N1', 64, MMDT)]
    NTb = [f4('NT0', 64, MMDT), f4('NT1', 64, MMDT)]
    Rb = [f4('R0', 64, MMDT), f4('R1', 64, MMDT)]
    Wsb, Usb = f4('Wsb', 64, MMDT), f4('Usb', 64, MMDT)
    Ysb, Ysq, Yn = f4('Ysb'), f4('Ysq'), f4('Yn')
    st = s.sbuf('st', [128, 6, NH], F32)
    Yo = [s.sbuf('Yo%d' % i, [128, NH * 64], F32) for i in range(2)]
    npair = T // (2 * C)
    ngrp = npair // GP

    def load_group(gi):
        b = gi % 2
        t0 = gi * GT
        for n in tin:
            src = tin[n][:, :, t0:t0 + GT].rearrange("k h (g two t) -> k h g two t", two=2, t=C)
            for hf in range(2):
                s.dma('sp', tbuf[n][b][HS[hf], :, :, :], src[:, :, :, hf, :], ('lt', n, b, hf), writes=[('t', n, b, hf)])
        for n in nin:
            s.dma('pool', nbuf[n][b][:], nin[n][t0:t0 + GT, :].rearrange("(c p) n -> p c n", p=128), ('ln', n, b),
                  writes=[('n', n, b)])

    def pair(pi):
        gi, pj = divmod(pi, GP)
        b = gi % 2
        tv = lambda n: tbuf[n][b][:, :, pj, :]
        tk = lambda n: [('t', n, b, 0), ('t', n, b, 1)]
        nv = lambda n: nbuf[n][b][:, pj, :]
        nk = lambda n: ('n', n, b)
        for hf in range(2):
            P_ = HS[hf]
            for h in range(NH):
                MM(s, PS[0][P_, h * 128:(h + 1) * 128], nbuf['Wn'][b][P_, pj, h * 64:(h + 1) * 64], K['TRI'][P_, :],
                   reads=[nk('Wn'), 'TRI'], writes=['B0'], inc=(h == NH - 1 and hf == 1))
        for hf in range(2):
            P_ = HS[hf]
            MM(s, PS[1][P_, 0:NH * 64], K['UPS'][P_, :], nbuf['Wn'][b][P_, pj, :], reads=[nk('Wn'), 'UPS'], writes=['B1'],
               inc=(hf == 1))
        cumI = bank(0, 128)[:, :, 0:64]
        cumS = bank(0, 128)[:, :, 64:128]
        ACTF(s, PT[:], cumI, AF.Exp, ['B0'], ['PT'], scale=-C0)
        ACTF(s, PINV[:], cumI, AF.Exp, ['B0'], ['PINV'], scale=C0)
        ACTF(s, PPREV[:], cumS, AF.Exp, ['B0'], ['PPREV'], scale=-C0)
        ACTF(s, E1[:], PS[1][:, 0:NH * 64], AF.Exp, ['B1'], ['E1'], scale=-C0)
        TT(s, 'dve', AR[:, :, 0, :], tv('AT'), PPREV[:], ALU.mult, tk('AT') + ['PPREV'], ['AR0'])
        TT(s, 'pool', AR[:, :, 1, :], tv('RT'), PT[:], ALU.mult, tk('RT') + ['PT'], ['AR1'])
        TT(s, 'dve', BK[:, :, 0, :], tv('BT'), PINV[:], ALU.mult, tk('BT') + ['PINV'], ['BK0'])
        TT(s, 'pool', BK[:, :, 1, :], tv('KT'), PINV[:], ALU.mult, tk('KT') + ['PINV'], ['BK1'])
        TT(s, 'pool', BKn[:, 0, :], nv('Bn'), E1[:], ALU.mult, [nk('Bn'), 'E1'], ['BKn0'])
        TT(s, 'pool', BKn[:, 1, :], nv('Kn'), E1[:], ALU.mult, [nk('Kn'), 'E1'], ['BKn1'])
        for h in range(NH):
            TS(s, 'pool', DG[:, h, :], K['I4'][:, h, :], PT[:, h, 63:64], None, ALU.mult, None, ['I4', 'PT'], ['DG'])
        for hf in range(2):
            P_ = HS[hf]
            last = (hf == 1)
            for h in range(NH):
                arh = AR[P_, h, :, :].rearrange("p a x -> p (a x)")
                MM(s, PS[2][P_, h * 128:(h + 1) * 128], BK[P_, h, 0, :], arh, reads=['BK0', 'AR0', 'AR1'], writes=B2('B2'),
                   inc=(h == NH - 1 and last))
            for h in range(NH):
                arh = AR[P_, h, :, :].rearrange("p a x -> p (a x)")
                MM(s, PS[3][P_, h * 128:(h + 1) * 128], BK[P_, h, 1, :], arh, reads=['BK1', 'AR0', 'AR1'], writes=B2('B3'),
                   inc=(h == NH - 1 and last))
            for h in range(NH):
                MM(s, PS[4][P_, h * 64:(h + 1) * 64], AR[P_, h, 0, :], BK[P_, h, 0, :], reads=['BK0', 'AR0'], writes=B2('B4'),
                   inc=(h == NH - 1 and last))
        TT(s, 'dve', X1[:], bank(2, 128), K['MU1'][:], ALU.mult, B2('B2') + ['MU1'], ['X1'])
        TT(s, 'dve', X2[:], bank(3, 128), K['MU1'][:], ALU.mult, B2('B3') + ['MU1'], ['X2'])
        TT(s, 'dve', NTb[0][:], bank(4, 64), K['MLS'][:], ALU.mult, B2('B4') + ['MLS'], [('NT', 0)])
        CP(s, 'pool', Nb[0][:], X1[:, :, 0:64], ['X1'], [('N', 0)])
        TT(s, 'pool', Rb[0][:], X1[:, :, 0:64], K['I4'][:], ALU.add, ['X1', 'I4'], [('R', 0)])
        cur = 0
        for lv in range(5):
            nx = 1 - cur
            if lv < 4:
                for hf in range(2):
                    P_ = HS[hf]
                    for h in range(NH):
                        MM(s, PS[5][P_, h * 64:(h + 1) * 64], NTb[cur][P_, h, :], Nb[cur][P_, h, :],
                           reads=[('NT', cur), ('N', cur)], writes=['B5'], inc=(h == NH - 1 and hf == 1))
            for hf in range(2):
                P_ = HS[hf]
                for h in range(NH):
                    MM(s, PS[6][P_, h * 64:(h + 1) * 64], Nb[cur][P_, h, :], NTb[cur][P_, h, :],
                       reads=[('NT', cur), ('N', cur)], writes=['B6'], inc=(h == NH - 1 and hf == 1))
            if lv < 4:
                CP(s, 'act', Nb[nx][:], bank(5, 64), ['B5'], [('N', nx)])
            CP(s, 'dve', NTb[nx][:], bank(6, 64), ['B6'], [('NT', nx)])
            for hf in range(2):
                P_ = HS[hf]
                for h in range(NH):
                    MM(s, PS[7][P_, h * 64:(h + 1) * 64], NTb[nx][P_, h, :], Rb[cur][P_, h, :],
                       reads=[('NT', nx), ('R', cur)], writes=B2('B7'), inc=(h == NH - 1 and hf == 1))
            TT(s, 'dve', Rb[nx][:], Rb[cur][:], bank(7, 64), ALU.add, [('R', cur)] + B2('B7'), [('R', nx)])
            cur = nx
        R = Rb[cur]
        Rk = ('R', cur)
        for hf in range(2):
            P_ = HS[hf]
            Q_ = HS[1 - hf]
            vsl = lambda h: nbuf['Vn'][b][P_, pj, h * 64:(h + 1) * 64]
            for h in range(NH):
                o = PS[2][P_, h * 64:(h + 1) * 64]
                MM(s, o, X2[P_, h, 0:64], vsl(h), start=True, stop=False, reads=['X2', nk('Vn')], writes=[('B2', hf)], inc=False)
                MM(s, o, AR[P_, h, 0, :], S0[P_, h, :], start=False, stop=True, reads=['AR0', ('S0', hf)], writes=[('B2', hf)],
                   inc=(h == NH - 1))
            CP(s, 'dve', Wsb[P_, :, :], PS[2][P_, 0:NH * 64].rearrange("p (h x) -> p h x", h=NH), [('B2', hf)], [('Wsb', hf)])
            for h in range(NH):
                MM(s, PS[3][P_, h * 64:(h + 1) * 64], R[P_, h, :], Wsb[P_, h, :], reads=[Rk, ('Wsb', hf)], writes=[('B3', hf)],
                   inc=(h == NH - 1))
            CP(s, 'dve', Usb[P_, :, :], PS[3][P_, 0:NH * 64].rearrange("p (h x) -> p h x", h=NH), [('B3', hf)], [('Usb', hf)])
            for h in range(NH):
                o = PS[7][Q_, h * 64:(h + 1) * 64]
                MM(s, o, DG[P_, h, :], S0[P_, h, :], start=True, stop=False, reads=['DG', ('S0', hf)], writes=[('B7', 1 - hf)], inc=False)
                MM(s, o, BKn[P_, 0, h * 64:(h + 1) * 64], Usb[P_, h, :], start=False, stop=False, reads=['BKn0', ('Usb', hf)],
                   writes=[('B7', 1 - hf)], inc=False)
                MM(s, o, BKn[P_, 1, h * 64:(h + 1) * 64], vsl(h), start=False, stop=True, reads=['BKn1', nk('Vn')],
                   writes=[('B7', 1 - hf)], inc=(h == NH - 1))
            for h in range(NH):
                o = PS[4][P_, h * 64:(h + 1) * 64]
                MM(s, o, AR[P_, h, 1, :], S0[P_, h, :], start=True, stop=False, reads=['AR1', ('S0', hf)], writes=[('B4', hf)], inc=False)
                MM(s, o, X1[P_, h, 64:128], Usb[P_, h, :], start=False, stop=False, reads=['X1', ('Usb', hf)], writes=[('B4', hf)], inc=False)
                MM(s, o, X2[P_, h, 64:128], vsl(h), start=False, stop=True, reads=['X2', nk('Vn')], writes=[('B4', hf)],
                   inc=(h == NH - 1))
            CP(s, 'dve', S0[Q_, :, :], PS[7][Q_, 0:NH * 64].rearrange("p (h x) -> p h x", h=NH), [('B7', 1 - hf)], [('S0', 1 - hf)])
        CP(s, 'dve', Ysb[:], bank(4, 64), B2('B4'), ['Ysb'])
        RED(s, st[:, 0, :], Ysb[:], ['Ysb'], ['st0'])
        TT(s, 'pool', Ysq[:], Ysb[:], Ysb[:], ALU.mult, ['Ysb'], ['Ysq'])
        RED(s, st[:, 1, :], Ysq[:], ['Ysq'], ['st1'])
        TS(s, 'dve', st[:, 2, :], st[:, 0, :], 1.0 / 64, None, ALU.mult, None, ['st0'], ['st2'])
        TT(s, 'dve', st[:, 3, :], st[:, 2, :], st[:, 2, :], ALU.mult, ['st2'], ['st3'])
        STT(s, st[:, 4, :], st[:, 1, :], 1.0 / 64, st[:, 3, :], ALU.mult, ALU.subtract, ['st1', 'st3'], ['st4'])
        ACTF(s, st[:, 5, :], st[:, 4, :], AF.Ln, ['st4', 'epst'], ['st5'], bias=epst[:], scale=1.0)
        ACTF(s, st[:, 5, :], st[:, 5, :], AF.Exp, ['st5'], ['st5'], scale=-0.5)
        for h in range(NH):
            TS(s, 'dve', Yn[:, h, :], Ysb[:, h, :], st[:, 2, h:h + 1], st[:, 5, h:h + 1], ALU.subtract, ALU.mult,
               ['Ysb', 'st2', 'st5'], ['Yn'])
        ynf = Yn[:].rearrange("p h x -> p (h x)")
        yo = Yo[pi % 2]
        yok = ('Yo', pi % 2)
        TT(s, 'pool', ynf, ynf, K['LGB'][:, 0, :], ALU.mult, ['Yn', 'LGB'], ['Yn'])
        TT(s, 'pool', ynf, ynf, K['LGB'][:, 1, :], ALU.add, ['Yn', 'LGB'], ['Yn'])
        TT(s, 'pool', ynf, ynf, nv('BONn'), ALU.add, ['Yn', nk('BONn')], ['Yn'])
        TT(s, 'pool', yo[:], ynf, nv('Gn'), ALU.mult, ['Yn', nk('Gn')], [yok])
        s.dma('sp', y[pi * 2 * C:(pi + 1) * 2 * C, :], yo[:], ('so', pi % 2), reads=[yok], writes=[('yd', pi % 2)])

    load_group(0)
    for gi in range(ngrp):
        if gi + 1 < ngrp:
            load_group(gi + 1)
        for pj in range(GP):
            pair(gi * GP + pj)
    s.wait_all('sp', [('yd', 0), ('yd', 1)])


def host_consts_0b(lnx_g, lnx_b):
    j = np.arange(64)[:, None]
    x = np.arange(64)[None, :]
    su = (x > j).astype(np.float32)
    iu = (x >= j).astype(np.float32)
    sl = (x < j).astype(np.float32)
    two = lambda a: np.concatenate([a, a], axis=0)
    MU1 = two(np.broadcast_to(np.concatenate([su, iu], 1)[:, None, :], (64, NH, 128)))
    MLS = two(np.broadcast_to(sl[:, None, :], (64, NH, 64)))
    I4 = two(np.broadcast_to(np.eye(64, dtype=np.float32)[:, None, :], (64, NH, 64)))
    TRI = two(np.concatenate([iu, su], 1))
    UPS = two(sl)
    LGB = np.broadcast_to(np.stack([lnx_g, lnx_b])[None], (128, 2, NH * 64))
    return {k: np.ascontiguousarray(v, dtype=np.float32) for k, v in
            dict(MU1=MU1, MLS=MLS, I4=I4, TRI=TRI, UPS=UPS, LGB=LGB).items()}


def host_inputs_0b(arrT, lnx_g, lnx_b):
    T = arrT['oR'].shape[1]
    tr = lambda a: np.ascontiguousarray(a.reshape(NH, 64, T).transpose(1, 0, 2))
    na = lambda a: np.ascontiguousarray(a.T)
    d = dict(RT=tr(arrT['oR']), KT=tr(arrT['oK']), AT=tr(arrT['oA']), BT=tr(arrT['oB']),
             Wn=na(arrT['oW']), Bn=na(arrT['oB']), Kn=na(arrT['oK']), Vn=na(arrT['oV']),
             BONn=na(arrT['oBon']), Gn=na(arrT['oG']))
    d.update(host_consts_0b(lnx_g, lnx_b))
    return d


D = 1024
NE = 32
GE = 4
ALPHA = (2.0 * 2) ** 0.25
LN_EPS = 1e-5


def build_C(ntok=4096, st=1024, level=9):
    nc = bass.Bass('TRN2', target_bir_lowering=False)
    dt = nc.dram_tensor
    xin = dt("xin", [ntok, D], F32, kind="ExternalInput").ap()
    ymT = dt("ymT", [D, ntok], F32, kind="ExternalInput").ap()
    w_out = dt("w_out", [D, D], F32, kind="ExternalInput").ap()
    wr = dt("wr", [D, 36], F32, kind="ExternalInput").ap()
    br = dt("br", [1, 36], F32, kind="ExternalInput").ap()
    lnp = dt("lnp", [128, 4, D], F32, kind="ExternalInput").ap()
    ident_d = dt("ident", [128, 128], F32, kind="ExternalInput").ap()
    sel_d = dt("sel", [32, NE * 128], F32, kind="ExternalInput").ap()
    e_w1 = dt("e_w1", [NE, D, 128], F32, kind="ExternalInput").ap()
    e_w3 = dt("e_w3", [NE, D, 128], F32, kind="ExternalInput").ap()
    e_w2 = dt("e_w2", [NE, 128, D], F32, kind="ExternalInput").ap()
    xout = dt("xout", [ntok, D], F32, kind="ExternalOutput").ap()
    s = Sched(nc)
    emit_C(s, xin, ymT, w_out, wr, br, lnp, ident_d, sel_d, e_w1, e_w3, e_w2, xout, ntok, st, level)
    s.build()
    return nc


def emit_C(s, xin, ymT, w_out, wr, br, lnp, ident_d, sel_d, e_w1, e_w3, e_w2, xout, ntok, st, level=9):
    wout_bf = s.sbuf('wout_bf', [128, 8, D], BF16)
    wr_f = s.sbuf('wr_f', [128, 8, 36], F32)
    br_f = s.sbuf('br_f', [1, 36], F32)
    ones1 = s.sbuf('ones1', [1, 128], F32)
    LNP = s.sbuf('LNP', [128, 4, D], F32)
    ident = s.sbuf('ident', [128, 128], F32)
    sel = s.sbuf('sel', [32, NE * 128], BF16)
    for k in range(8):
        s.dma('pool', wout_bf[:, k, :], w_out[k * 128:(k + 1) * 128, :], 'c0', writes=[('wout_bf', k)])
    s.dma('sp', wr_f[:], wr.rearrange("(k p) n -> p k n", p=128), 'c1', writes=['wr_f'])
    s.dma('sp', br_f[:], br, 'c2', writes=['br_f'])
    for a in range(4):
        s.dma('sp', LNP[:, a, :], lnp[:, a, :], ('c3', a), writes=[('LNP', a)])
    s.dma('sp', ident[:], ident_d, 'c4', writes=['ident'])
    s.dma('pool', sel[:], sel_d, 'c5', writes=['sel'])
    s.op('dve', lambda e: e.memset(ones1[:], 1.0), writes=['ones1'])
    epst = s.sbuf('epst', [128, 1], F32)
    s.op('dve', lambda e: e.memset(epst[:], LN_EPS), writes=['epst'])
    WOK = [('wout_bf', k) for k in range(8)]

    nst = ntok // st
    nsub = st // 128
    ntt = st // 256
    F = s.sbuf('F', [128, nsub, D], F32)
    xaT = s.sbuf('xaT', [128, 8, st], BF16)
    gT = s.sbuf('gT', [32, st], BF16)
    P = [s.psum('P%d' % i, [128, 512]) for i in range(8)]
    ym_bf = [s.sbuf('ym_bf%d' % i, [128, 8, 128], BF16) for i in range(2)]
    xin_t = [s.sbuf('xin_t%d' % i, [128, D], F32) for i in range(2)]
    z_t = [s.sbuf('z_t%d' % i, [128, D], F32) for i in range(2)]
    xa_t = [s.sbuf('xa_t%d' % i, [128, D], F32) for i in range(2)]
    xaTf = [s.sbuf('xaTf%d' % i, [128, 8, 128], F32) for i in range(2)]
    st6 = [s.sbuf('st6_%d' % i, [128, 2, 6], F32) for i in range(2)]
    mv = [s.sbuf('mv%d' % i, [128, 2], F32) for i in range(2)]
    rstd = [s.sbuf('rstd%d' % i, [128, 1], F32) for i in range(2)]
    nmr = [s.sbuf('nmr%d' % i, [128, 1], F32) for i in range(2)]
    lg = [s.sbuf('lg%d' % i, [128, 36], F32) for i in range(2)]
    rt = [s.sbuf('rt%d' % i, [128, 16], F32) for i in range(2)]
    r8 = [s.sbuf('r8_%d' % i, [128, 5, 8], F32) for i in range(2)]
    gates = [s.sbuf('gates%d' % i, [128, 32], F32) for i in range(2)]
    W1g = [s.sbuf('W1g%d' % i, [128, GE, 8, 128], BF16) for i in range(2)]
    W3g = [s.sbuf('W3g%d' % i, [128, GE, 8, 128], BF16) for i in range(2)]
    W2g = [s.sbuf('W2g%d' % i, [128, GE, D], BF16) for i in range(2)]
    s1 = [s.sbuf('s1_%d' % i, [128, 256], F32) for i in range(2)]
    uu = [s.sbuf('uu_%d' % i, [128, 256], F32) for i in range(2)]
    hg = [s.sbuf('hg_%d' % i, [128, 256], BF16) for i in range(2)]
    o_t = [s.sbuf('o_t%d' % i, [128, D], F32) for i in range(2)]

    def layer_norm_tile(src, srck, dst, dstk, gi, pi, eng_gb='pool'):
        st_, mv_, rs_, nm_ = st6[pi], mv[pi], rstd[pi], nmr[pi]
        for hh in range(2):
            s.op('dve', lambda e, hh=hh: e.bn_stats(out=st_[:, hh, :], in_=src[:, hh * 512:(hh + 1) * 512]),
                 reads=[srck], writes=[('st6', pi, hh)])
        s.op('dve', lambda e: e.bn_aggr(out=mv_[:], in_=st_[:].rearrange("p a b -> p (a b)")),
             reads=[('st6', pi, 0), ('st6', pi, 1)], writes=[('mv', pi)])
        s.op('act', lambda e: e.activation(out=rs_[:], in_=mv_[:, 1:2], func=AF.Ln, bias=epst[:], scale=1.0),
             reads=[('mv', pi), 'epst'], writes=[('rstd', pi)])
        s.op('act', lambda e: e.activation(out=rs_[:], in_=rs_[:], func=AF.Exp, scale=-0.5),
             reads=[('rstd', pi)], writes=[('rstd', pi)])
        s.op('dve', lambda e: e.scalar_tensor_tensor(out=nm_[:], in0=mv_[:, 0:1], scalar=-1.0, in1=rs_[:],
                                                     op0=ALU.mult, op1=ALU.mult),
             reads=[('mv', pi), ('rstd', pi)], writes=[('nmr', pi)])
        s.op('act', lambda e: e.activation(out=dst[:], in_=src[:], func=AF.Identity, bias=nm_[:], scale=rs_[:]),
             reads=[srck, ('rstd', pi), ('nmr', pi)], writes=[dstk])
        s.op(eng_gb, lambda e: e.tensor_tensor(out=dst[:], in0=dst[:], in1=LNP[:, gi, :], op=ALU.mult),
             reads=[dstk, ('LNP', gi)], writes=[dstk])
        s.op(eng_gb, lambda e: e.tensor_tensor(out=dst[:], in0=dst[:], in1=LNP[:, gi + 1, :], op=ALU.add),
             reads=[dstk, ('LNP', gi + 1)], writes=[dstk])

    wl_cnt = [0]

    def load_group(gi):
        b = wl_cnt[0] % 2
        wl_cnt[0] += 1
        for el in range(GE):
            ex = gi * GE + el
            s.dma('pool', W1g[b][:, el, :, :], e_w1[ex].rearrange("(k p) j -> p k j", p=128), ('w1', b, el),
                  writes=[('W1g', b, el)])
            s.dma('pool', W3g[b][:, el, :, :], e_w3[ex].rearrange("(k p) j -> p k j", p=128), ('w3', b, el),
                  writes=[('W3g', b, el)])
            s.dma('pool', W2g[b][:, el, :], e_w2[ex], ('w2', b, el), writes=[('W2g', b, el)])
        return b

    ngrp = NE // GE
    for sti in range(nst):
        t0 = sti * st
        for sub in range(nsub):
            pi = sub % 2
            tok0 = t0 + sub * 128
            yb = ym_bf[pi]
            ybk = ('ym_bf', pi)
            xt, zt, xat = xin_t[pi], z_t[pi], xa_t[pi]
            s.dma('pool', yb[:], ymT[:, tok0:tok0 + 128].rearrange("(k p) t -> p k t", p=128), ('ym', pi), writes=[ybk])
            s.dma('sp', xt[:], xin[tok0:tok0 + 128, :], ('xin', pi), writes=[('xin_t', pi)])
            for hh in range(2):
                pb = P[2 * pi + hh]
                pk = ('P', 2 * pi + hh)
                for k in range(8):
                    s.op('pe', lambda e, pb=pb, k=k, hh=hh, yb=yb: e.matmul(
                        pb[:], lhsT=yb[:, k, :], rhs=wout_bf[:, k, hh * 512:(hh + 1) * 512],
                        start=(k == 0), stop=(k == 7)), reads=[ybk] + WOK, writes=[pk], inc=(k == 7))
                s.op('dve', lambda e, pb=pb, hh=hh, xt=xt, zt=zt: e.scalar_tensor_tensor(
                    out=zt[:, hh * 512:(hh + 1) * 512], in0=xt[:, hh * 512:(hh + 1) * 512], scalar=ALPHA,
                    in1=pb[:], op0=ALU.mult, op1=ALU.add), reads=[pk, ('xin_t', pi)], writes=[('z_t', pi)])
            if level < 0.5:
                s.op('act', lambda e, sub=sub, zt=zt: e.copy(out=F[:, sub, :], in_=zt[:]),
                     reads=[('z_t', pi)], writes=[('F', sub)])
                continue
            layer_norm_tile(zt, ('z_t', pi), xat, ('xa_t', pi), 0, pi)
            s.op('act', lambda e, sub=sub, xat=xat: e.mul(out=F[:, sub, :], in_=xat[:], mul=ALPHA),
                 reads=[('xa_t', pi)], writes=[('F', sub)])
            if level < 2:
                continue
            xf = xaTf[pi]
            for k in range(8):
                pb = P[4 + 2 * pi + k // 4]
                pk = ('P', 4 + 2 * pi + k // 4)
                s.op('pe', lambda e, pb=pb, k=k, xat=xat: e.transpose(
                    out=pb[:, (k % 4) * 128:(k % 4 + 1) * 128], in_=xat[:, k * 128:(k + 1) * 128], identity=ident[:]),
                    reads=[('xa_t', pi), 'ident'], writes=[pk])
            for hb in range(2):
                pb = P[4 + 2 * pi + hb]
                pk = ('P', 4 + 2 * pi + hb)
                s.op('act', lambda e, pb=pb, hb=hb, xf=xf: e.copy(
                    out=xf[:, hb * 4:(hb + 1) * 4, :], in_=pb[:].rearrange("p (k t) -> p k t", k=4)),
                    reads=[pk], writes=[('xaTf', pi, hb)])
                s.op('pool', lambda e, hb=hb, sub=sub, xf=xf: e.tensor_copy(
                    out=xaT[:, hb * 4:(hb + 1) * 4, sub * 128:(sub + 1) * 128], in_=xf[:, hb * 4:(hb + 1) * 4, :]),
                    reads=[('xaTf', pi, hb)], writes=[('xaT', sub)])
            if level < 3:
                continue
            pb = P[4 + 2 * pi]
            pk = ('P', 4 + 2 * pi)
            for k in range(8):
                s.op('pe', lambda e, pb=pb, k=k, xf=xf: e.matmul(
                    pb[:, 0:36], lhsT=xf[:, k, :], rhs=wr_f[:, k, :], start=(k == 0), stop=False),
                    reads=[('xaTf', pi, k // 4), 'wr_f'], writes=[pk], inc=False)
            s.op('pe', lambda e, pb=pb: e.matmul(pb[:, 0:36], lhsT=ones1[:], rhs=br_f[:], start=False, stop=True),
                 reads=['ones1', 'br_f'], writes=[pk])
            L = lg[pi]
            Lk = ('lg', pi)
            R = rt[pi]
            Rk = ('rt', pi)
            R8 = r8[pi]
            R8k = ('r8', pi)
            G = gates[pi]
            s.op('act', lambda e, pb=pb, L=L: e.copy(out=L[:], in_=pb[:, 0:36]), reads=[pk], writes=[Lk])
            if level < 4:
                continue
            V = lambda fn, rd, wr_: s.op('dve', fn, reads=rd, writes=wr_)
            V(lambda e, L=L, R=R: e.reduce_max(out=R[:, 0:1], in_=L[:, 0:4], axis=AX.X), [Lk], [Rk])
            V(lambda e, L=L, R=R: e.tensor_scalar(out=R[:, 4:8], in0=L[:, 0:4], scalar1=R[:, 0:1], scalar2=None,
                                                  op0=ALU.is_ge), [Lk, Rk], [Rk])
            V(lambda e, R=R: e.tensor_scalar(out=R[:, 1:2], in0=R[:, 0:1], scalar1=-1.0, scalar2=None, op0=ALU.mult),
              [Rk], [Rk])
            s.op('act', lambda e, L=L, R=R: e.activation(out=R[:, 8:12], in_=L[:, 0:4], func=AF.Exp,
                                                         bias=R[:, 1:2], scale=1.0), reads=[Lk, Rk], writes=[Rk])
            V(lambda e, R=R: e.reduce_sum(out=R[:, 2:3], in_=R[:, 8:12], axis=AX.X), [Rk], [Rk])
            V(lambda e, L=L, R=R, R8=R8: e.tensor_scalar(out=R8[:, 0, :], in0=L[:, 4:12], scalar1=R[:, 4:5],
                                                         scalar2=None, op0=ALU.mult), [Lk, Rk], [R8k])
            for g in range(1, 4):
                V(lambda e, L=L, R=R, R8=R8, g=g: e.scalar_tensor_tensor(
                    out=R8[:, 0, :], in0=L[:, 4 + 8 * g:12 + 8 * g], scalar=R[:, 4 + g:5 + g], in1=R8[:, 0, :],
                    op0=ALU.mult, op1=ALU.add), [Lk, Rk, R8k], [R8k])
            V(lambda e, R8=R8: e.max(out=R8[:, 1, :], in_=R8[:, 0, :]), [R8k], [R8k])
            V(lambda e, R8=R8: e.tensor_scalar(out=R8[:, 2, :], in0=R8[:, 0, :], scalar1=R8[:, 1, 1:2], scalar2=None,
                                               op0=ALU.is_ge), [R8k], [R8k])
            V(lambda e, R=R, R8=R8: e.tensor_scalar(out=R[:, 12:13], in0=R8[:, 1, 0:1], scalar1=-1.0, scalar2=None,
                                                    op0=ALU.mult), [R8k, Rk], [Rk])
            s.op('act', lambda e, R=R, R8=R8: e.activation(out=R8[:, 3, :], in_=R8[:, 0, :], func=AF.Exp,
                                                           bias=R[:, 12:13], scale=1.0), reads=[R8k, Rk], writes=[R8k])
            V(lambda e, R8=R8: e.tensor_tensor(out=R8[:, 4, :], in0=R8[:, 3, :], in1=R8[:, 2, :], op=ALU.mult),
              [R8k], [R8k])
            V(lambda e, R=R, R8=R8: e.reduce_sum(out=R[:, 13:14], in_=R8[:, 4, :], axis=AX.X), [R8k, Rk], [Rk])
            V(lambda e, R=R: e.tensor_tensor(out=R[:, 14:15], in0=R[:, 13:14], in1=R[:, 2:3], op=ALU.mult), [Rk], [Rk])
            V(lambda e, R=R: e.reciprocal(out=R[:, 15:16], in_=R[:, 14:15]), [Rk], [Rk])
            V(lambda e, R=R, R8=R8: e.tensor_scalar(out=R8[:, 3, :], in0=R8[:, 4, :], scalar1=R[:, 15:16],
                                                    scalar2=None, op0=ALU.mult), [R8k, Rk], [R8k])
            for g in range(4):
                V(lambda e, R=R, R8=R8, G=G, g=g: e.tensor_scalar(
                    out=G[:, 8 * g:8 * g + 8], in0=R8[:, 3, :], scalar1=R[:, 4 + g:5 + g], scalar2=None,
                    op0=ALU.mult), [R8k, Rk], [('gates', pi)])
            if level < 6:
                if level >= 5:
                    s.op('act', lambda e, sub=sub, G=G: e.copy(out=F[:, sub, 0:32], in_=G[:]),
                         reads=[('gates', pi), ('F', sub)], writes=[('F', sub)])
                continue
            pb2 = P[5 + 2 * pi]
            pk2 = ('P', 5 + 2 * pi)
            s.op('pe', lambda e, pb2=pb2, G=G: e.transpose(out=pb2[0:32, 0:128], in_=G[:], identity=ident[:]),
                 reads=[('gates', pi), 'ident'], writes=[pk2])
            s.op('act', lambda e, pb2=pb2, sub=sub: e.copy(out=gT[:, sub * 128:(sub + 1) * 128],
                                                          in_=pb2[0:32, 0:128]),
                 reads=[pk2], writes=[('gT', sub)])

        for gi in range(ngrp if level >= 7 else 0):
            b = load_group(gi)
            for tt in range(ntt):
                c0 = tt * 256
                for el in range(GE):
                    ex = gi * GE + el
                    hb = (tt * GE + el) % 2
                    p1 = P[4 + 3 * hb]
                    p1k = ('P', 4 + 3 * hb)
                    p3 = P[5 + hb]
                    p3k = ('P', 5 + hb)
                    for k in range(8):
                        s.op('pe', lambda e, p1=p1, k=k, el=el, b=b, c0=c0: e.matmul(
                            p1[:, 0:256], lhsT=W1g[b][:, el, k, :], rhs=xaT[:, k, c0:c0 + 256],
                            start=(k == 0), stop=(k == 7)),
                            reads=[('W1g', b, el), ('xaT', 2 * tt), ('xaT', 2 * tt + 1)], writes=[p1k], inc=(k == 7))
                    for k in range(8):
                        s.op('pe', lambda e, p3=p3, k=k, el=el, b=b, c0=c0: e.matmul(
                            p3[:, 0:256], lhsT=W3g[b][:, el, k, :], rhs=xaT[:, k, c0:c0 + 256],
                            start=(k == 0), stop=(k == 7)),
                            reads=[('W3g', b, el), ('xaT', 2 * tt), ('xaT', 2 * tt + 1)], writes=[p3k], inc=False)
                    s.op('pe', lambda e, p3=p3, ex=ex, c0=c0: e.matmul(
                        p3[:, 256:512], lhsT=sel[:, ex * 128:(ex + 1) * 128], rhs=gT[:, c0:c0 + 256],
                        start=True, stop=True), reads=['sel', ('gT', 2 * tt), ('gT', 2 * tt + 1)], writes=[p3k])
                    s.op('act', lambda e, p1=p1, hb=hb: e.activation(out=s1[hb][:], in_=p1[:, 0:256], func=AF.Silu),
                         reads=[p1k], writes=[('s1', hb)])
                    s.op('dve', lambda e, p3=p3, hb=hb: e.tensor_tensor(out=uu[hb][:], in0=s1[hb][:], in1=p3[:, 256:512],
                                                                        op=ALU.mult),
                         reads=[('s1', hb), p3k], writes=[('uu', hb)])
                    s.op('dve', lambda e, p3=p3, hb=hb: e.tensor_tensor(out=hg[hb][:], in0=uu[hb][:],
                                                                        in1=p3[:, 0:256], op=ALU.mult),
                         reads=[('uu', hb), p3k], writes=[('hg', hb)])
                    for sb in range(2):
                        for hh in range(2):
                            pa = P[2 * sb + hh]
                            pak = ('P', 2 * sb + hh)
                            s.op('pe', lambda e, pa=pa, sb=sb, hh=hh, hb=hb, el=el, b=b: e.matmul(
                                pa[:], lhsT=hg[hb][:, sb * 128:(sb + 1) * 128], rhs=W2g[b][:, el, hh * 512:(hh + 1) * 512],
                                start=(el == 0), stop=(el == GE - 1)),
                                reads=[('hg', hb), ('W2g', b, el)], writes=[pak], inc=(sb == 1 and hh == 1))
                for sb in range(2):
                    sub = 2 * tt + sb
                    for hh in range(2):
                        pa = P[2 * sb + hh]
                        pak = ('P', 2 * sb + hh)
                        s.op('dve', lambda e, pa=pa, sub=sub, hh=hh: e.tensor_tensor(
                            out=F[:, sub, hh * 512:(hh + 1) * 512], in0=F[:, sub, hh * 512:(hh + 1) * 512], in1=pa[:],
                            op=ALU.add), reads=[pak, ('F', sub)], writes=[('F', sub)])
        for sub in range(nsub):
            pi = sub % 2
            ot = o_t[pi]
            if level < 0.3 or (5 <= level < 6):
                s.op('act', lambda e, sub=sub, ot=ot: e.copy(out=ot[:], in_=F[:, sub, :]),
                     reads=[('F', sub)], writes=[('o_t', pi)])
            else:
                layer_norm_tile(F[:, sub, :], ('F', sub), ot, ('o_t', pi), 2, pi)
            s.dma('sp', xout[t0 + sub * 128: t0 + (sub + 1) * 128, :], ot[:], ('st', pi), reads=[('o_t', pi)],
                  writes=[('xout', pi)])
    s.wait_all('sp', [('xout', 0), ('xout', 1)])


def host_inputs_C(xin, ymT, w_out, g1, b1, rg_w, rg_b, re_w, re_b, w1, w3, w2, g2, b2):
    wr = np.ascontiguousarray(np.concatenate([rg_w, re_w], axis=1))
    br = np.ascontiguousarray(np.concatenate([rg_b, re_b])[None, :])
    lnp = np.ascontiguousarray(np.broadcast_to(np.stack([g1, b1, g2, b2])[None], (128, 4, D)))
    ident = np.eye(128, dtype=np.float32)
    sel = np.zeros((32, NE, 128), np.float32)
    for e in range(NE):
        sel[e, e, :] = 1.0
    return dict(xin=np.ascontiguousarray(xin), ymT=np.ascontiguousarray(ymT), w_out=np.ascontiguousarray(w_out),
                wr=wr, br=br, lnp=lnp, ident=ident, sel=sel.reshape(32, NE * 128),
                e_w1=np.ascontiguousarray(w1), e_w3=np.ascontiguousarray(w3), e_w2=np.ascontiguousarray(w2))


HALO1 = 32
CONV_W = 31
LN_EPS1 = 1e-5


def build_1a(ntok=4096, tile=512):
    nc = bass.Bass('TRN2', target_bir_lowering=False)
    dt = nc.dram_tensor
    xT = dt("xT", [1024, HALO1 + ntok], F32, kind="ExternalInput").ap()
    w_ext = dt("w_ext", [1024, 4096], F32, kind="ExternalInput").ap()
    ccols = dt("ccols", [128, 4, 34], F32, kind="ExternalInput").ap()
    rot = dt("rot", [128, 2, ntok], F32, kind="ExternalInput").ap()
    ones_d = dt("ones", [128, 128], F32, kind="ExternalInput").ap()
    outs = {n: dt(n, [512, ntok], F32, kind="ExternalOutput").ap() for n in ('oYC', 'oQ', 'oKr', 'oV', 'oGr')}
    s = Sched(nc)
    emit_1a(s, xT, w_ext, ccols, rot, ones_d, outs, ntok, tile)
    s.build()
    return nc


def emit_1a(s, xT, w_ext, ccols_d, rot_d, ones_d, outs, ntok, T):
    W = s.sbuf('W', [128, 8, 4096], BF16)
    for k in range(8):
        for hf in range(2):
            s.dma('pool', W[:, k, hf * 2048:(hf + 1) * 2048], w_ext[k * 128:(k + 1) * 128, hf * 2048:(hf + 1) * 2048], 'w0',
                  writes=[('W', k, hf)])
    WK = [('W', k, hf) for k in range(8) for hf in range(2)]
    CC = s.sbuf('CC', [128, 4, 34], F32)
    s.dma('sp', CC[:], ccols_d, 'c0', writes=['CC'])
    ONES = s.sbuf('ONES', [128, 128], F32)
    s.dma('sp', ONES[:], ones_d, 'c1', writes=['ONES'])
    epst = s.sbuf('epst', [128, 1], F32)
    s.op('dve', lambda e: e.memset(epst[:], LN_EPS1), writes=['epst'])
    P = [s.psum('P%d' % i, [128, 512]) for i in range(8)]
    xb = [s.sbuf('xb%d' % i, [128, 8, T], BF16) for i in range(2)]
    xh = s.sbuf('xh', [128, 8, HALO1], BF16)
    CA = s.sbuf('CA', [128, T], F32)
    SG = s.sbuf('SG', [128, T], F32)
    CAH = s.sbuf('CAH', [128, 4, HALO1], F32)
    U = s.sbuf('U', [128, 4, HALO1 + T], F32)
    ACC = s.sbuf('ACC', [128, 4, T], F32)
    SQ = s.sbuf('SQ', [128, T], F32)
    XP = s.sbuf('XP', [128, 16, T], F32)
    ROT = [s.sbuf('ROT%d' % i, [128, 2, T], F32) for i in range(2)]
    MEAN = s.sbuf('MEAN', [128, T], F32)
    MSQ = s.sbuf('MSQ', [128, T], F32)
    RSTD = s.sbuf('RSTD', [128, T], F32)
    t1 = [s.sbuf('t1_%d' % i, [128, T], F32) for i in range(2)]
    t2 = [s.sbuf('t2_%d' % i, [128, T], F32) for i in range(2)]
    OB = {n: [s.sbuf('O_%s%d' % (n, i), [128, T], F32) for i in range(2)] for n in ('oYC', 'oQ', 'oKr', 'oV', 'oGr')}
    ocnt = {n: 0 for n in OB}

    def out_buf(n):
        i = ocnt[n] % 2
        ocnt[n] += 1
        return OB[n][i], (n, i)

    def proj(c, x_, XK, ncol, pb, pk):
        for k in range(8):
            MM(s, pb[:, 0:ncol], W[:, k, c * 128:(c + 1) * 128], x_[:, k, :], start=(k == 0), stop=(k == 7),
               reads=XK + WK, writes=[pk], inc=(k == 7))

    s.dma('pool', xh[:], xT[:, 0:HALO1].rearrange("(k p) t -> p k t", p=128), 'xh', writes=['xh'])
    for c in range(8):
        pb, pk = P[c % 2], ('P', c % 2)
        proj(c, xh, ['xh'], HALO1, pb, pk)
        if c < 4:
            CP(s, 'act', CAH[:, c, :], pb[:, 0:HALO1], [pk], [('CAH', c)])
        else:
            ACTF(s, SG[:, 0:HALO1], pb[:, 0:HALO1], AF.Sigmoid, [pk], ['SG'])
            TT(s, 'dve', U[:, c - 4, 0:HALO1], CAH[:, c - 4, :], SG[:, 0:HALO1], ALU.mult, [('CAH', c - 4), 'SG'], [('U', c - 4)])

    def tile_body(ti):
        t0 = ti * T
        x_ = xb[ti % 2]
        XK = [('xb', ti % 2, k) for k in range(8)]
        for k in range(8):
            s.dma('pool', x_[:, k, :], xT[k * 128:(k + 1) * 128, HALO1 + t0:HALO1 + t0 + T], ('x', ti % 2), writes=[XK[k]])
        rt = ROT[ti % 2]
        rk = ('ROT', ti % 2)
        s.dma('sp', rt[:], rot_d[:, :, t0:t0 + T], ('rot', ti % 2), writes=[rk])
        tsl = slice(t0, t0 + T)
        for cc in range(4):
            pb, pk = P[0], ('P', 0)
            proj(cc, x_, XK, T, pb, pk)
            CP(s, 'act', CA[:], pb[:], [pk], ['CA'])
            pb, pk = P[1], ('P', 1)
            proj(4 + cc, x_, XK, T, pb, pk)
            ACTF(s, SG[:], pb[:], AF.Sigmoid, [pk], ['SG'])
            TT(s, 'pool', U[:, cc, HALO1:HALO1 + T], CA[:], SG[:], ALU.mult, ['CA', 'SG'], [('U', cc)])
        order = [(8 + i, i, 1.0) for i in range(4)] + [(24 + i, 8 + i, 1.0) for i in range(4)] + \
                [(12 + i, 4 + i, 0.125) for i in range(4)] + [(28 + i, 12 + i, 0.125) for i in range(4)]
        for n_, (c, slot, sc) in enumerate(order):
            pb, pk = P[n_ % 2], ('P', n_ % 2)
            proj(c, x_, XK, T, pb, pk)
            s.op('act', lambda e, pb=pb, slot=slot, sc=sc: e.mul(out=XP[:, slot, :], in_=pb[:], mul=sc), reads=[pk], writes=[('XP', slot)])
        for i in range(4):
            rows = slice(i * 128, (i + 1) * 128)
            pb, pk = P[0], ('P', 0)
            proj(16 + i, x_, XK, T, pb, pk)
            ob, obk = out_buf('oV')
            CP(s, 'act', ob[:], pb[:], [pk], [obk])
            s.dma('sp', outs['oV'][rows, tsl], ob[:], ('so',) + obk, reads=[obk], writes=[('d',) + obk])
            pb, pk = P[1], ('P', 1)
            proj(20 + i, x_, XK, T, pb, pk)
            ob, obk = out_buf('oGr')
            ACTF(s, ob[:], pb[:], AF.Silu, [pk], [obk])
            s.dma('sp', outs['oGr'][rows, tsl], ob[:], ('so',) + obk, reads=[obk], writes=[('d',) + obk])
        for i in range(8):
            name = 'oQ' if i < 4 else 'oKr'
            rows = slice((i % 4) * 128, (i % 4 + 1) * 128)
            a, b = t1[i % 2], t2[i % 2]
            TT(s, 'dve', a[:], XP[:, i, :], rt[:, 0, :], ALU.mult, [('XP', i), rk], [('t1', i % 2)])
            TT(s, 'pool', b[:], XP[:, 8 + i, :], rt[:, 1, :], ALU.mult, [('XP', 8 + i), rk], [('t2', i % 2)])
            ob, obk = out_buf(name)
            TT(s, 'dve', ob[:], a[:], b[:], ALU.add, [('t1', i % 2), ('t2', i % 2)], [obk])
            s.dma('sp', outs[name][rows, tsl], ob[:], ('so',) + obk, reads=[obk], writes=[('d',) + obk])
        for cc in range(4):
            TS(s, 'dve', ACC[:, cc, :], U[:, cc, 2:2 + T], CC[:, cc, 0:1], CC[:, cc, 31:32], ALU.mult, ALU.add,
               [('U', cc), 'CC'], [('ACC', cc)])
            for j in range(1, CONV_W):
                STT(s, ACC[:, cc, :], U[:, cc, 2 + j:2 + j + T], CC[:, cc, j:j + 1], ACC[:, cc, :], ALU.mult, ALU.add,
                    [('U', cc), 'CC', ('ACC', cc)], [('ACC', cc)])
        for cc in range(4):
            MM(s, P[2][:], ONES[:], ACC[:, cc, :], start=(cc == 0), stop=(cc == 3), reads=['ONES', ('ACC', cc)], writes=[('P', 2)],
               inc=(cc == 3))
        for cc in range(4):
            s.op('act', lambda e, cc=cc: e.activation(out=SQ[:], in_=ACC[:, cc, :], func=AF.Square), reads=[('ACC', cc)], writes=['SQ'])
            MM(s, P[3][:], ONES[:], SQ[:], start=(cc == 0), stop=(cc == 3), reads=['ONES', 'SQ'], writes=[('P', 3)])
        s.op('act', lambda e: e.mul(out=MEAN[:], in_=P[2][:], mul=1.0 / 512), reads=[('P', 2)], writes=['MEAN'])
        TT(s, 'pool', MSQ[:], MEAN[:], MEAN[:], ALU.mult, ['MEAN'], ['MSQ'])
        STT(s, RSTD[:], P[3][:], 1.0 / 512, MSQ[:], ALU.mult, ALU.subtract, [('P', 3), 'MSQ'], ['RSTD'])
        ACTF(s, RSTD[:], RSTD[:], AF.Ln, ['RSTD', 'epst'], ['RSTD'], bias=epst[:], scale=1.0)
        ACTF(s, RSTD[:], RSTD[:], AF.Exp, ['RSTD'], ['RSTD'], scale=-0.5)
        for cc in range(4):
            rows = slice(cc * 128, (cc + 1) * 128)
            a = t1[cc % 2]
            TT(s, 'dve', a[:], ACC[:, cc, :], MEAN[:], ALU.subtract, [('ACC', cc), 'MEAN'], [('t1', cc % 2)])
            TT(s, 'pool', a[:], a[:], RSTD[:], ALU.mult, [('t1', cc % 2), 'RSTD'], [('t1', cc % 2)])
            ob, obk = out_buf('oYC')
            ACTF(s, ob[:], a[:], AF.Silu, [('t1', cc % 2), 'CC'], [obk], bias=CC[:, cc, 33:34], scale=CC[:, cc, 32:33])
            s.dma('sp', outs['oYC'][rows, tsl], ob[:], ('so',) + obk, reads=[obk], writes=[('d',) + obk])
        for cc in range(4):
            CP(s, 'pool', U[:, cc, 0:HALO1], U[:, cc, T:T + HALO1], [('U', cc)], [('U', cc)])

    for ti in range(ntok // T):
        tile_body(ti)
    s.wait_all('sp', [k for k in s.last_w if isinstance(k, tuple) and k[0] == 'd'])


def rot_perm():
    idx = np.arange(512).reshape(8, 2, 32)[:, ::-1, :].reshape(-1)
    return idx


def host_inputs_1a(xT_halo, w_in, conv_w, conv_b, cln_g, cln_b, pos0, ntok=4096):
    perm = rot_perm()
    w_ext = np.concatenate([w_in, w_in[:, 1024:1536][:, perm], w_in[:, 1536:2048][:, perm]], axis=1)
    ccols = np.zeros((128, 4, 34), np.float32)
    ccols[:, :, 0:31] = conv_w.T.reshape(4, 128, 31).transpose(1, 0, 2)
    ccols[:, :, 31] = conv_b.reshape(4, 128).T
    ccols[:, :, 32] = cln_g.reshape(4, 128).T
    ccols[:, :, 33] = cln_b.reshape(4, 128).T
    half = 32
    inv = (np.float32(10000.0) ** (-np.arange(half, dtype=np.float32) / np.float32(half))).astype(np.float32)
    pos = np.arange(pos0, pos0 + ntok, dtype=np.float32)
    ang = (pos[:, None] * inv[None, :]).astype(np.float32)
    cos, sin = np.cos(ang).astype(np.float32), np.sin(ang).astype(np.float32)
    p = np.arange(128)
    i = p % 32
    second = (p % 64) >= 32
    rot = np.empty((128, 2, ntok), np.float32)
    rot[:, 0, :] = cos.T[i]
    rot[:, 1, :] = np.where(second[:, None], sin.T[i], -sin.T[i])
    return dict(xT=np.ascontiguousarray(xT_halo), w_ext=np.ascontiguousarray(w_ext), ccols=ccols, rot=rot,
                ones=np.ones((128, 128), np.float32))


RC = 128
RH = 4
RET_EPS = 1e-5


def build_1b(T=8192, GCH=2):
    nc = bass.Bass('TRN2', target_bir_lowering=False)
    dt = nc.dram_tensor
    tin = {n: dt(n, [64, RH, T], F32, kind="ExternalInput").ap() for n in ('QT', 'KT')}
    nin = {n: dt(n, [T, RH * 64], F32, kind="ExternalInput").ap() for n in ('Kn', 'Vn', 'Grn')}
    cst = {n: dt(n, shp, F32, kind="ExternalInput").ap() for n, shp in
           (('DM', [128, RH, 128]), ('XI', [64, RH, 128]), ('ZE', [128, RH * 64]), ('GCC', [64, RH]), ('GNB', [128, 2, RH * 64]))}
    y = dt("y", [T, RH * 64], F32, kind="ExternalOutput").ap()
    s = Sched(nc)
    emit_1b(s, tin, nin, cst, y, T, GCH)
    s.build()
    return nc


def emit_1b(s, tin, nin, cst, y, T, GCH):
    K = {}
    for n, shp in (('DM', [128, RH, 128]), ('XI', [64, RH, 128]), ('ZE', [128, RH * 64]), ('GCC', [64, RH]), ('GNB', [128, 2, RH * 64])):
        K[n] = s.sbuf('K_' + n, shp, F32)
        s.dma('sp', K[n][:], cst[n], ('k', n), writes=[n])
    epst = s.sbuf('epst', [128, 1], F32)
    s.op('dve', lambda e: e.memset(epst[:], RET_EPS), writes=['epst'])
    Rst = s.sbuf('Rst', [64, RH, 64], F32)
    s.op('dve', lambda e: e.memset(Rst[:], 0.0), writes=['Rst'])
    PS = [s.psum('PS%d' % i, [128, 512]) for i in range(4)]
    GT = GCH * RC
    tbuf = {n: [s.sbuf('t_%s%d' % (n, i), [64, RH, GT], F32) for i in range(2)] for n in tin}
    nbuf = {n: [s.sbuf('n_%s%d' % (n, i), [128, GCH, RH * 64], F32) for i in range(2)] for n in nin}
    QX = s.sbuf('QX', [64, RH, RC], F32)
    KZ = s.sbuf('KZ', [128, RH * 64], F32)
    SM = s.sbuf('SM', [128, RH, RC], F32)
    Ysb = s.sbuf('Ysb', [128, RH, 64], F32)
    Ysq = s.sbuf('Ysq', [128, RH, 64], F32)
    Yn = s.sbuf('Yn', [128, RH, 64], F32)
    st = s.sbuf('st', [128, 6, RH], F32)
    Yo = [s.sbuf('Yo%d' % i, [128, RH * 64], F32) for i in range(2)]
    nchunk = T // RC
    ngrp = nchunk // GCH

    def load_group(gi):
        b = gi % 2
        t0 = gi * GT
        for n in tin:
            s.dma('sp', tbuf[n][b][:], tin[n][:, :, t0:t0 + GT], ('lt', n, b), writes=[('t', n, b)])
        for n in nin:
            s.dma('pool', nbuf[n][b][:], nin[n][t0:t0 + GT, :].rearrange("(c p) n -> p c n", p=128), ('ln', n, b),
                  writes=[('n', n, b)])

    def chunk(ci):
        gi, cj = divmod(ci, GCH)
        b = gi % 2
        tv = lambda n: tbuf[n][b][:, :, cj * RC:(cj + 1) * RC]
        tk = lambda n: ('t', n, b)
        nv = lambda n: nbuf[n][b][:, cj, :]
        nk = lambda n: ('n', n, b)
        TT(s, 'pool', QX[:], tv('QT'), K['XI'][:], ALU.mult, [tk('QT'), 'XI'], ['QX'])
        TT(s, 'pool', KZ[:], nv('Kn'), K['ZE'][:], ALU.mult, [nk('Kn'), 'ZE'], ['KZ'])
        for h in range(RH):
            MM(s, PS[0][:, h * RC:(h + 1) * RC], tbuf['KT'][b][:, h, cj * RC:(cj + 1) * RC], tbuf['QT'][b][:, h, cj * RC:(cj + 1) * RC],
               reads=[tk('KT'), tk('QT')], writes=['B0'], inc=(h == RH - 1))
        TT(s, 'dve', SM[:], PS[0][:, :].rearrange("p (h x) -> p h x", h=RH), K['DM'][:], ALU.mult, ['B0', 'DM'], ['SM'])
        for h in range(RH):
            vh = nbuf['Vn'][b][:, cj, h * 64:(h + 1) * 64]
            o = PS[1][:, h * 64:(h + 1) * 64]
            MM(s, o, SM[:, h, :], vh, start=True, stop=False, reads=['SM', nk('Vn')], writes=['B1'], inc=False)
            MM(s, o, QX[:, h, :], Rst[:, h, :], start=False, stop=True, reads=['QX', 'Rst'], writes=['B1'], inc=(h == RH - 1))
        for h in range(RH):
            vh = nbuf['Vn'][b][:, cj, h * 64:(h + 1) * 64]
            MM(s, PS[2][0:64, h * 64:(h + 1) * 64], KZ[:, h * 64:(h + 1) * 64], vh, reads=['KZ', nk('Vn')], writes=['B2'],
               inc=(h == RH - 1))
        CP(s, 'dve', Ysb[:], PS[1][:, 0:RH * 64].rearrange("p (h x) -> p h x", h=RH), ['B1'], ['Ysb'])
        for h in range(RH):
            STT(s, Rst[:, h, :], Rst[:, h, :], K['GCC'][:, h:h + 1], PS[2][0:64, h * 64:(h + 1) * 64], ALU.mult, ALU.add,
                ['Rst', 'GCC', 'B2'], ['Rst'])
        RED(s, st[:, 0, :], Ysb[:], ['Ysb'], ['st0'])
        TT(s, 'pool', Ysq[:], Ysb[:], Ysb[:], ALU.mult, ['Ysb'], ['Ysq'])
        RED(s, st[:, 1, :], Ysq[:], ['Ysq'], ['st1'])
        TS(s, 'dve', st[:, 2, :], st[:, 0, :], 1.0 / 64, None, ALU.mult, None, ['st0'], ['st2'])
        TT(s, 'dve', st[:, 3, :], st[:, 2, :], st[:, 2, :], ALU.mult, ['st2'], ['st3'])
        STT(s, st[:, 4, :], st[:, 1, :], 1.0 / 64, st[:, 3, :], ALU.mult, ALU.subtract, ['st1', 'st3'], ['st4'])
        ACTF(s, st[:, 5, :], st[:, 4, :], AF.Ln, ['st4', 'epst'], ['st5'], bias=epst[:], scale=1.0)
        ACTF(s, st[:, 5, :], st[:, 5, :], AF.Exp, ['st5'], ['st5'], scale=-0.5)
        for h in range(RH):
            TS(s, 'dve', Yn[:, h, :], Ysb[:, h, :], st[:, 2, h:h + 1], st[:, 5, h:h + 1], ALU.subtract, ALU.mult,
               ['Ysb', 'st2', 'st5'], ['Yn'])
        ynf = Yn[:].rearrange("p h x -> p (h x)")
        yo = Yo[ci % 2]
        yok = ('Yo', ci % 2)
        TT(s, 'pool', ynf, ynf, K['GNB'][:, 0, :], ALU.mult, ['Yn', 'GNB'], ['Yn'])
        TT(s, 'pool', ynf, ynf, K['GNB'][:, 1, :], ALU.add, ['Yn', 'GNB'], ['Yn'])
        TT(s, 'pool', yo[:], ynf, nv('Grn'), ALU.mult, ['Yn', nk('Grn')], [yok])
        s.dma('sp', y[ci * RC:(ci + 1) * RC, :], yo[:], ('so', ci % 2), reads=[yok], writes=[('yd', ci % 2)])

    load_group(0)
    for gi in range(ngrp):
        if gi + 1 < ngrp:
            load_group(gi + 1)
        for cj in range(GCH):
            chunk(gi * GCH + cj)
    s.wait_all('sp', [('yd', 0), ('yd', 1)])


def host_inputs_1b(arrT, gn_g, gn_b, head0):
    T = arrT['oQ'].shape[1]
    tr = lambda a: np.ascontiguousarray(a.reshape(RH, 64, T).transpose(1, 0, 2))
    na = lambda a: np.ascontiguousarray(a.T)
    hidx = np.arange(head0, head0 + RH, dtype=np.float32)
    log_gamma = np.log1p(-np.power(np.float32(2.0), -5.0 - hidx)).astype(np.float32)
    idx = np.arange(RC, dtype=np.float32)
    diff = idx[None, :] - idx[:, None]
    DM = np.where(diff[:, None, :] >= 0, np.exp(np.maximum(diff, 0.0)[:, None, :] * log_gamma[None, :, None]), 0.0)
    xi = np.exp((idx + 1.0)[None, :] * log_gamma[:, None])
    zeta = np.exp((RC - 1.0 - idx)[None, :] * log_gamma[:, None])
    gch = np.exp(RC * log_gamma)
    XI = np.broadcast_to(xi[None], (64, RH, RC))
    ZE = np.repeat(zeta.T, 64, axis=1)
    GCC = np.broadcast_to(gch[None], (64, RH))
    GNB = np.broadcast_to(np.stack([gn_g, gn_b])[None], (128, 2, RH * 64))
    f = lambda a: np.ascontiguousarray(a, dtype=np.float32)
    return dict(QT=tr(arrT['oQ']), KT=tr(arrT['oKr']), Kn=na(arrT['oKr']), Vn=na(arrT['oV']), Grn=na(arrT['oGr']),
                DM=f(DM), XI=f(XI), ZE=f(ZE), GCC=f(GCC), GNB=f(GNB))


NTOK = 4096
SHARDS = [(c // 2, (c % 2) * NTOK) for c in range(8)]


def _run(nc, in_maps):
    from concourse.bass_utils import run_bass_kernel_spmd
    return run_bass_kernel_spmd(nc, in_maps, core_ids=list(range(len(in_maps)))).results


def _post_mixer(inp, cur, ymTs, layer, w_out):
    ncC = build_C(ntok=NTOK, st=1024)
    maps = []
    for ci, (b, t0) in enumerate(SHARDS):
        maps.append(host_inputs_C(cur[b, t0:t0 + NTOK], ymTs[ci], w_out, inp['ln_mix_g'][layer], inp['ln_mix_b'][layer],
                                  inp['rg_w'][layer], inp['rg_b'][layer], inp['re_w'][layer], inp['re_b'][layer],
                                  inp['e_w1'][layer], inp['e_w3'][layer], inp['e_w2'][layer],
                                  inp['ln_ffn_g'][layer], inp['ln_ffn_b'][layer]))
    rc = _run(ncC, maps)
    nxt = np.empty_like(cur)
    for ci, (b, t0) in enumerate(SHARDS):
        nxt[b, t0:t0 + NTOK] = rc[ci]['xout']
    return nxt


def layer0(inp, x):
    nc0 = build_0a(ntok=NTOK)
    maps = []
    for (b, t0) in SHARDS:
        xT = np.zeros((D, HALO + NTOK), np.float32)
        xT[:, HALO:] = x[b, t0:t0 + NTOK].T
        if t0 > 0:
            xT[:, :HALO] = x[b, t0 - HALO:t0].T
        maps.append(host_inputs_0a(xT, inp['ev_w_in'][0], inp['ev_mu'][0], inp['ev_w0'][0], inp['ev_a0'][0],
                                   inp['ev_k_k'][0], inp['ev_k_a'][0], inp['ev_r_k'][0].reshape(-1),
                                   inp['ev_pool_scale'][0], inp['ev_w2'][0], inp['ev_a2'][0], inp['ev_g2'][0],
                                   inp['ev_pool_w'][0], t0 == 0))
    r0 = _run(nc0, maps)
    ncb = build_0b(T=2 * NTOK)
    maps = []
    for c in range(8):
        b, j = c // 2, c % 2
        rows = slice(256 * j, 256 * j + 256)
        arrT = {n: np.concatenate([r0[2 * b][n][rows], r0[2 * b + 1][n][rows]], axis=1)
                for n in ('oR', 'oK', 'oV', 'oA', 'oB', 'oW', 'oG', 'oBon')}
        maps.append(host_inputs_0b(arrT, inp['ev_lnx_g'][0][rows], inp['ev_lnx_b'][0][rows]))
    rb = _run(ncb, maps)
    ymTs = []
    for ci, (b, t0) in enumerate(SHARDS):
        ymT = np.empty((D, NTOK), np.float32)
        for j in range(2):
            ymT[256 * j:256 * j + 256] = rb[2 * b + j]['y'][t0:t0 + NTOK].T
        ymT[512:] = r0[ci]['oYP']
        ymTs.append(ymT)
    return _post_mixer(inp, x, ymTs, 0, inp['ev_w_out'][0])


def layer1(inp, x1):
    nc1 = build_1a(ntok=NTOK)
    maps = []
    for (b, t0) in SHARDS:
        xT = np.zeros((D, HALO1 + NTOK), np.float32)
        xT[:, HALO1:] = x1[b, t0:t0 + NTOK].T
        if t0 > 0:
            xT[:, :HALO1] = x1[b, t0 - HALO1:t0].T
        maps.append(host_inputs_1a(xT, inp['od_w_in'][0], inp['od_conv_w'][0], inp['od_conv_b'][0], inp['od_cln_g'][0],
                                   inp['od_cln_b'][0], t0, NTOK))
    r1 = _run(nc1, maps)
    ncb = build_1b(T=2 * NTOK)
    maps = []
    for c in range(8):
        b, j = c // 2, c % 2
        rows = slice(256 * j, 256 * j + 256)
        arrT = {n: np.concatenate([r1[2 * b][n][rows], r1[2 * b + 1][n][rows]], axis=1) for n in ('oQ', 'oKr', 'oV', 'oGr')}
        maps.append(host_inputs_1b(arrT, inp['od_gn_g'][0][rows], inp['od_gn_b'][0][rows], 4 * j))
    rb = _run(ncb, maps)
    ymTs = []
    for ci, (b, t0) in enumerate(SHARDS):
        ymT = np.empty((D, NTOK), np.float32)
        ymT[:512] = r1[ci]['oYC']
        for j in range(2):
            ymT[512 + 256 * j:512 + 256 * j + 256] = rb[2 * b + j]['y'][t0:t0 + NTOK].T
        ymTs.append(ymT)
    return _post_mixer(inp, x1, ymTs, 1, inp['od_w_out'][0])


def kernel(**inp):
    inp = {k: np.asarray(v) for k, v in inp.items()}
    x1 = layer0(inp, inp['x'])
    return layer1(inp, x1)
```

```python
import numpy as np


import concourse.bass as bass
import concourse.mybir as mybir

F32 = mybir.dt.float32
BF16 = mybir.dt.bfloat16
I32 = mybir.dt.int32
U32 = mybir.dt.uint32
AF = mybir.ActivationFunctionType
ALU = mybir.AluOpType
AX = mybir.AxisListType


class _Buf:
    def __init__(self, name, t):
        self.name = name
        self.t = t

    def __getitem__(self, idx):
        return self.t[idx]


class Sched:
    ENGS = ('pe', 'act', 'dve', 'pool', 'sp')

    def __init__(self, nc, self_wait=True):
        self.nc = nc
        self.self_wait = self_wait
        self.ops = {e: [] for e in self.ENGS}
        self.sems = {}
        self.cnt = {}
        self.waited = {e: {} for e in self.ENGS}
        self.last_w = {}
        self.readers = {}
        self.ctx = []
        self.pending_noinc = {e: False for e in self.ENGS}
        for e in ('pe', 'act', 'dve', 'pool'):
            self.sems[e] = nc.alloc_semaphore(name='sem_' + e)
            self.cnt[e] = 0
        self.ntile = 0

    def sbuf(self, name, shape, dtype):
        g = self.nc.sbuf_tensor('sb_' + name, list(shape), dtype)
        t = g.__enter__()
        self.ctx.append(g)
        return _Buf(name, t)

    def psum(self, name, shape, dtype=F32):
        g = self.nc.psum_tensor('ps_' + name, list(shape), dtype)
        t = g.__enter__()
        self.ctx.append(g)
        return _Buf(name, t)

    def dma_sem(self, key):
        if key not in self.sems:
            self.sems[key] = self.nc.alloc_semaphore(name='dsem_%d' % len(self.sems))
            self.cnt[key] = 0
        return key

    def _deps(self, reads, writes):
        deps = []
        for r in reads:
            if r in self.last_w:
                deps.append(self.last_w[r])
        for w in writes:
            if w in self.last_w:
                deps.append(self.last_w[w])
            deps.extend(self.readers.get(w, []))
        return deps

    def _emit_waits(self, eng, deps):
        waits = []
        best = {}
        for (sk, v) in deps:
            if sk == eng and (eng == 'pe' or not self.self_wait):
                continue
            if self.waited[eng].get(sk, 0) >= v:
                continue
            if best.get(sk, 0) < v:
                best[sk] = v
        for sk, v in best.items():
            self.waited[eng][sk] = v
            waits.append((self.sems[sk], v))
        return waits

    def _record(self, ev, reads, writes):
        for r in reads:
            self.readers.setdefault(r, []).append(ev)
        for w in writes:
            self.last_w[w] = ev
            self.readers[w] = []

    def op(self, eng, fn, reads=(), writes=(), inc=True):
        reads = [k for k in reads]
        writes = [k for k in writes]
        waits = self._emit_waits(eng, self._deps(reads, writes))
        sem = self.sems[eng]
        if inc:
            self.cnt[eng] += 1
            ev = (eng, self.cnt[eng])
            self.pending_noinc[eng] = False
        else:
            ev = (eng, self.cnt[eng] + 1)
            self.pending_noinc[eng] = True

        def emit(e, fn=fn, waits=waits, inc=inc, sem=sem):
            for (s, v) in waits:
                e.wait_ge(s, v)
            ins = fn(e)
            if inc:
                ins.then_inc(sem, 1)
        self.ops[eng].append(emit)
        self._record(ev, reads, writes)
        return ev

    def dma(self, eng, out, in_, semkey, reads=(), writes=(), **kw):
        self.dma_sem(semkey)
        reads = list(reads)
        writes = list(writes)
        waits = self._emit_waits(eng, self._deps(reads, writes))
        self.cnt[semkey] += 16
        ev = (semkey, self.cnt[semkey])
        sem = self.sems[semkey]

        def emit(e, waits=waits, sem=sem, out=out, in_=in_, kw=kw):
            for (s, v) in waits:
                e.wait_ge(s, v)
            e.dma_start(out=out, in_=in_, **kw).then_inc(sem, 16)
        self.ops[eng].append(emit)
        self._record(ev, reads, writes)
        return ev

    def wait_all(self, eng, keys):
        deps = []
        for k in keys:
            if k in self.last_w:
                deps.append(self.last_w[k])
        waits = self._emit_waits(eng, deps)

        def emit(e, waits=waits):
            for (s, v) in waits:
                e.wait_ge(s, v)
        self.ops[eng].append(emit)

    def build(self):
        nc = self.nc
        for e in self.ENGS:
            assert not self.pending_noinc[e], 'dangling noinc on ' + e
        with nc.Block() as block:
            @block.tensor
            def _(e):
                for f in self.ops['pe']:
                    f(e)

            @block.scalar
            def _(e):
                for f in self.ops['act']:
                    f(e)

            @block.vector
            def _(e):
                for f in self.ops['dve']:
                    f(e)

            @block.gpsimd
            def _(e):
                for f in self.ops['pool']:
                    f(e)

            @block.sync
            def _(e):
                for f in self.ops['sp']:
                    f(e)
        for g in reversed(self.ctx):
            g.__exit__(None, None, None)


D = 1024
HALO = 16
NCH = 18


def build_0a(ntok=4096, tile=512, dbg=None):
    nc = bass.Bass('TRN2', target_bir_lowering=False)
    dt = nc.dram_tensor
    xT = dt("xT", [D, HALO + ntok], F32, kind="ExternalInput").ap()
    w_in = dt("w_in", [D, 2304], F32, kind="ExternalInput").ap()
    cols = dt("cols", [128, 40], F32, kind="ExternalInput").ap()
    w2 = dt("w2", [64, 512], F32, kind="ExternalInput").ap()
    a2 = dt("a2", [64, 512], F32, kind="ExternalInput").ap()
    g2 = dt("g2", [128, 512], F32, kind="ExternalInput").ap()
    pool_w = dt("pool_w", [4, 128, 128], F32, kind="ExternalInput").ap()
    bones_d = dt("bones", [128, 128], F32, kind="ExternalInput").ap()
    invc_d = dt("invc", [128, 2, 4, tile], F32, kind="ExternalInput").ap()
    outs = {n: dt(n, [512, ntok], F32, kind="ExternalOutput").ap()
            for n in ('oR', 'oK', 'oV', 'oA', 'oB', 'oW', 'oG', 'oBon', 'oYP')}
    s = Sched(nc)
    emit_0a(s, xT, w_in, cols, w2, a2, g2, pool_w, bones_d, invc_d, outs, ntok, tile)
    s.build()
    return nc


OQ = 'sp'


def emit_0a(s, xT, w_in, cols_d, w2_d, a2_d, g2_d, pool_w_d, bones_d, invc_d, outs, ntok, tile):
    T = tile
    W = s.sbuf('W', [128, 8, 2304], BF16)
    for k in range(8):
        s.dma('pool', W[:, k, :], w_in[k * 128:(k + 1) * 128, :], 'w0', writes=[('W', k)])
    WK = [('W', k) for k in range(8)]
    COLS = s.sbuf('COLS', [128, 40], F32)
    s.dma('sp', COLS[:], cols_d, 'c0', writes=['COLS'])
    W2 = s.sbuf('W2', [64, 512], BF16)
    A2 = s.sbuf('A2', [128, 512], BF16)
    G2 = s.sbuf('G2', [128, 512], BF16)
    PW = s.sbuf('PW', [128, 4, 128], BF16)
    BONES = s.sbuf('BONES', [128, 128], BF16)
    INVC = s.sbuf('INVC', [128, 2, 4, T], F32)
    s.dma('pool', W2[:], w2_d, 'c1', writes=['W2'])
    s.dma('pool', A2[64:128, :], a2_d, 'c2', writes=['A2'])
    s.dma('pool', G2[:], g2_d, 'c3', writes=['G2'])
    for g in range(4):
        s.dma('pool', PW[:, g, :], pool_w_d[g], ('c4', g), writes=[('PW', g)])
    s.dma('pool', BONES[:], bones_d, 'c5', writes=['BONES'])
    for a in range(2):
        s.dma('sp', INVC[:, a, :, :], invc_d[:, a, :, :], ('c6', a), writes=[('INVC', a)])
    c24 = s.sbuf('c24', [128, 1], F32)
    s.op('dve', lambda e: e.memset(c24[:], 0.0), writes=['c24'])

    PR = s.sbuf('PR', [128, NCH, HALO + T], F32)
    Z = s.sbuf('Z', [128, 14, T], F32)
    xb = [s.sbuf('xb%d' % i, [128, 8, T], BF16) for i in range(2)]
    xh = s.sbuf('xh', [128, 8, HALO], BF16)
    P = [s.psum('P%d' % i, [128, 512]) for i in range(8)]
    tw = s.sbuf('tw', [64, T], BF16)
    al = s.sbuf('al', [128, T], BF16)
    sg = s.sbuf('sg', [128, T], BF16)
    tmp = [s.sbuf('tmp%d' % i, [128, T], F32) for i in range(4)]
    tb = [s.sbuf('tb%d' % i, [128, T], BF16) for i in range(2)]
    O = {n: [s.sbuf('O_%s%d' % (n, i), [128, T], F32) for i in range(2)] for n in ('oA', 'oB', 'oK', 'oW', 'oG', 'oBon', 'oYP')}
    SS = [s.sbuf('SS%d' % i, [128, HALO + T], F32) for i in range(2)]

    s.dma('pool', xh[:], xT[:, 0:HALO].rearrange("(k p) t -> p k t", p=128), 'xh', writes=['xh'])
    for c in range(NCH):
        pb = P[c % 2]
        pk = ('P', c % 2)
        for k in range(8):
            s.op('pe', lambda e, pb=pb, k=k, c=c: e.matmul(pb[:, 0:HALO], lhsT=W[:, k, c * 128:(c + 1) * 128],
                                                           rhs=xh[:, k, :], start=(k == 0), stop=(k == 7)),
                 reads=['xh'] + WK, writes=[pk], inc=(k == 7))
        s.op('act', lambda e, pb=pb, c=c: e.copy(out=PR[:, c, 0:HALO], in_=pb[:, 0:HALO]), reads=[pk], writes=[('PR', c)])

    ntile = ntok // T
    def tile_body(ti):
        t0 = ti * T
        x_ = xb[ti % 2]
        xk = ('xb', ti % 2)
        for k in range(8):
            s.dma('pool', x_[:, k, :], xT[k * 128:(k + 1) * 128, HALO + t0:HALO + t0 + T], ('x', ti % 2), writes=[(xk, k)])
        XK = [(xk, k) for k in range(8)]
        for c in range(NCH):
            pb = P[c % 2]
            pk = ('P', c % 2)
            for k in range(8):
                s.op('pe', lambda e, pb=pb, k=k, c=c, x_=x_: e.matmul(pb[:], lhsT=W[:, k, c * 128:(c + 1) * 128],
                                                                      rhs=x_[:, k, :], start=(k == 0), stop=(k == 7)),
                     reads=XK + WK, writes=[pk], inc=(k == 7))
            s.op('act', lambda e, pb=pb, c=c: e.copy(out=PR[:, c, HALO:HALO + T], in_=pb[:]), reads=[pk], writes=[('PR', c)])
        for c in range(14):
            d_ = tmp[c % 2]
            dk = ('tmp', c % 2)
            s.op('dve', lambda e, c=c, d_=d_: e.tensor_tensor(out=d_[:], in0=PR[:, c, HALO - 1:HALO - 1 + T],
                                                              in1=PR[:, c, HALO:HALO + T], op=ALU.subtract),
                 reads=[('PR', c)], writes=[dk])
            s.op('dve', lambda e, c=c, d_=d_: e.scalar_tensor_tensor(out=Z[:, c, :], in0=d_[:], scalar=COLS[:, c:c + 1],
                                                                     in1=PR[:, c, HALO:HALO + T], op0=ALU.mult, op1=ALU.add),
                 reads=[dk, ('PR', c), 'COLS'], writes=[('Z', c)])
        s.op('act', lambda e: e.activation(out=tw[:], in_=Z[0:64, 12, :], func=AF.Tanh), reads=[('Z', 12)], writes=['tw'])
        s.op('act', lambda e: e.copy(out=al[64:128, :], in_=Z[64:128, 12, :]), reads=[('Z', 12)], writes=['al'])
        s.op('act', lambda e: e.activation(out=sg[:], in_=Z[:, 13, :], func=AF.Sigmoid), reads=[('Z', 13)], writes=['sg'])
        ob = ti % 2
        for cc in range(4):
            cs = slice(cc * 128, (cc + 1) * 128)
            rows = slice(cc * 128, (cc + 1) * 128)
            tsl = slice(t0, t0 + T)
            s.op('pe', lambda e, cs=cs: e.matmul(P[2][:], lhsT=W2[:, cs], rhs=tw[:], start=True, stop=True),
                 reads=['W2', 'tw'], writes=[('P', 2)])
            s.op('act', lambda e, cc=cc: e.activation(out=O['oW'][ob][:], in_=P[2][:], func=AF.Sigmoid,
                                                     bias=COLS[:, 14 + cc:15 + cc], scale=1.0),
                 reads=[('P', 2), 'COLS'], writes=[('oW', ob)])
            s.dma(OQ, outs['oW'][rows, tsl], O['oW'][ob][:], ('so', 'oW', ob), reads=[('oW', ob)], writes=[('d_oW', ob)])
            s.op('pe', lambda e, cs=cs: e.matmul(P[3][:], lhsT=A2[64:128, cs], rhs=al[64:128, :], start=True, stop=True),
                 reads=['A2', 'al'], writes=[('P', 3)])
            asig = tmp[2]
            s.op('act', lambda e, cc=cc: e.activation(out=asig[:], in_=P[3][:], func=AF.Sigmoid,
                                                     bias=COLS[:, 18 + cc:19 + cc], scale=1.0),
                 reads=[('P', 3), 'COLS'], writes=[('tmp', 2)])
            s.op('pe', lambda e, cs=cs: e.matmul(P[4][:], lhsT=G2[:, cs], rhs=sg[:], start=True, stop=True),
                 reads=['G2', 'sg'], writes=[('P', 4)])
            s.op('act', lambda e: e.copy(out=O['oG'][ob][:], in_=P[4][:]), reads=[('P', 4)], writes=[('oG', ob)])
            s.dma(OQ, outs['oG'][rows, tsl], O['oG'][ob][:], ('so', 'oG', ob), reads=[('oG', ob)], writes=[('d_oG', ob)])
            kk0 = tmp[3]
            s.op('dve', lambda e, cc=cc: e.tensor_scalar(out=kk0[:], in0=Z[:, 4 + cc, :], scalar1=COLS[:, 22 + cc:23 + cc],
                                                         scalar2=None, op0=ALU.mult),
                 reads=[('Z', 4 + cc), 'COLS'], writes=[('tmp', 3)])
            s.op('dve', lambda e: e.tensor_tensor(out=tb[0][:], in0=kk0[:], in1=kk0[:], op=ALU.mult),
                 reads=[('tmp', 3)], writes=[('tb', 0)])
            s.op('pe', lambda e: e.matmul(P[5][:], lhsT=BONES[:], rhs=tb[0][:], start=True, stop=True),
                 reads=['BONES', ('tb', 0)], writes=[('P', 5)])
            rn = tmp[0]
            s.op('dve', lambda e: e.tensor_scalar(out=rn[:], in0=P[5][:], scalar1=1e-24, scalar2=None, op0=ALU.max),
                 reads=[('P', 5)], writes=[('tmp', 0)])
            s.op('act', lambda e: e.activation(out=rn[:], in_=rn[:], func=AF.Ln), reads=[('tmp', 0)], writes=[('tmp', 0)])
            s.op('act', lambda e: e.activation(out=rn[:], in_=rn[:], func=AF.Exp, scale=-0.5), reads=[('tmp', 0)], writes=[('tmp', 0)])
            s.op('dve', lambda e: e.tensor_tensor(out=kk0[:], in0=kk0[:], in1=rn[:], op=ALU.mult),
                 reads=[('tmp', 3), ('tmp', 0)], writes=[('tmp', 3)])
            s.op('act', lambda e: e.mul(out=O['oA'][ob][:], in_=kk0[:], mul=-1.0), reads=[('tmp', 3)], writes=[('oA', ob)])
            s.dma(OQ, outs['oA'][rows, tsl], O['oA'][ob][:], ('so', 'oA', ob), reads=[('oA', ob)], writes=[('d_oA', ob)])
            s.op('dve', lambda e: e.tensor_tensor(out=O['oB'][ob][:], in0=kk0[:], in1=asig[:], op=ALU.mult),
                 reads=[('tmp', 3), ('tmp', 2)], writes=[('oB', ob)])
            s.dma(OQ, outs['oB'][rows, tsl], O['oB'][ob][:], ('so', 'oB', ob), reads=[('oB', ob)], writes=[('d_oB', ob)])
            t1 = tmp[1]
            s.op('dve', lambda e, cc=cc: e.tensor_scalar(out=t1[:], in0=asig[:], scalar1=-1.0, scalar2=COLS[:, 26 + cc:27 + cc],
                                                         op0=ALU.add, op1=ALU.mult),
                 reads=[('tmp', 2), 'COLS'], writes=[('tmp', 1)])
            s.op('dve', lambda e, cc=cc: e.scalar_tensor_tensor(out=O['oK'][ob][:], in0=t1[:], scalar=1.0, in1=Z[:, 4 + cc, :],
                                                                op0=ALU.add, op1=ALU.mult),
                 reads=[('tmp', 1), ('Z', 4 + cc)], writes=[('oK', ob)])
            s.dma(OQ, outs['oK'][rows, tsl], O['oK'][ob][:], ('so', 'oK', ob), reads=[('oK', ob)], writes=[('d_oK', ob)])
            s.dma(OQ, outs['oR'][rows, tsl], Z[:, cc, :], ('so', 'oR', cc), reads=[('Z', cc)], writes=[('d_oR', cc)])
            s.dma(OQ, outs['oV'][rows, tsl], Z[:, 8 + cc, :], ('so', 'oV', cc), reads=[('Z', 8 + cc)], writes=[('d_oV', cc)])
            s.op('dve', lambda e, cc=cc: e.scalar_tensor_tensor(out=tb[1][:], in0=Z[:, cc, :], scalar=COLS[:, 30 + cc:31 + cc],
                                                                in1=O['oK'][ob][:], op0=ALU.mult, op1=ALU.mult),
                 reads=[('Z', cc), 'COLS', ('oK', ob)], writes=[('tb', 1)])
            s.op('pe', lambda e: e.matmul(P[6][:], lhsT=BONES[:], rhs=tb[1][:], start=True, stop=True),
                 reads=['BONES', ('tb', 1)], writes=[('P', 6)])
            s.op('dve', lambda e, cc=cc: e.tensor_tensor(out=O['oBon'][ob][:], in0=Z[:, 8 + cc, :], in1=P[6][:], op=ALU.mult),
                 reads=[('Z', 8 + cc), ('P', 6)], writes=[('oBon', ob)])
            s.dma(OQ, outs['oBon'][rows, tsl], O['oBon'][ob][:], ('so', 'oBon', ob), reads=[('oBon', ob)], writes=[('d_oBon', ob)])
        for g in range(4):
            c = 14 + g
            src = PR
            cur = None
            n = HALO + T
            prev_ap = lambda lo, hi, c=c: PR[:, c, lo:hi]
            a_, b_ = SS[0], SS[1]
            sh = 1
            first = True
            for lv in range(g + 1):
                lo = 2 * sh - 1
                if first:
                    s.op('pool', lambda e, c=c, sh=sh, n=n, lo=lo: e.tensor_tensor(out=SS[0][:, lo:n], in0=PR[:, c, lo:n], in1=PR[:, c, sh - 1:n - sh], op=ALU.add),
                         reads=[('PR', c)], writes=[('SS', 0)])
                    first = False
                    cur = 0
                else:
                    src_, dst_ = SS[cur], SS[1 - cur]
                    s.op('pool', lambda e, sh=sh, n=n, lo=lo, src_=src_, dst_=dst_: e.tensor_tensor(out=dst_[:, lo:n], in0=src_[:, lo:n],
                                                                                                in1=src_[:, sh - 1:n - sh], op=ALU.add),
                         reads=[('SS', cur)], writes=[('SS', 1 - cur)])
                    cur = 1 - cur
                sh *= 2
            ia = 0 if ti == 0 else 1
            s.op('dve', lambda e, g=g, cur=cur, ia=ia: e.tensor_tensor(out=tmp[0][:], in0=SS[cur][:, HALO:HALO + T], in1=INVC[:, ia, g, :], op=ALU.mult),
                 reads=[('SS', cur), ('INVC', ia)], writes=[('tmp', 0)])
            s.op('dve', lambda e, c=c: e.tensor_tensor(out=tb[0][:], in0=tmp[0][:], in1=PR[:, c, HALO:HALO + T], op=ALU.subtract),
                 reads=[('tmp', 0), ('PR', c)], writes=[('tb', 0)])
            s.op('pe', lambda e, g=g: e.matmul(P[7][:], lhsT=PW[:, g, :], rhs=tb[0][:], start=True, stop=True),
                 reads=[('PW', g), ('tb', 0)], writes=[('P', 7)])
            s.op('act', lambda e, g=g: e.activation(out=O['oYP'][ob][:], in_=P[7][:], func=AF.Identity, scale=COLS[:, 34 + g:35 + g]),
                 reads=[('P', 7), 'COLS'], writes=[('oYP', ob)])
            s.dma(OQ, outs['oYP'][g * 128:(g + 1) * 128, t0:t0 + T], O['oYP'][ob][:], ('so', 'oYP', ob), reads=[('oYP', ob)], writes=[('d_oYP', ob)])
        for c in range(NCH):
            s.op('pool', lambda e, c=c: e.tensor_copy(out=PR[:, c, 0:HALO], in_=PR[:, c, T:T + HALO]),
                 reads=[('PR', c)], writes=[('PR', c)])
    for ti in range(ntile):
        tile_body(ti)
    s.wait_all(OQ, [k for k in s.last_w if isinstance(k, tuple) and isinstance(k[0], str) and k[0].startswith('d_')])


def host_inputs_0a(xT_halo, w_in, mu, w0, a0, k_k, k_a, r_k, pool_scale, w2, a2, g2, pool_w, first, tile=512):
    cols = np.zeros((128, 40), np.float32)
    cols[:, 0:14] = mu.reshape(14, 128).T
    cols[:, 14:18] = w0.reshape(4, 128).T
    cols[:, 18:22] = a0.reshape(4, 128).T
    cols[:, 22:26] = k_k.reshape(4, 128).T
    cols[:, 26:30] = k_a.reshape(4, 128).T
    cols[:, 30:34] = r_k.reshape(4, 128).T
    cols[:, 34:38] = pool_scale.reshape(4, 128).T
    bones = np.zeros((128, 128), np.float32)
    bones[:64, :64] = 1.0
    bones[64:, 64:] = 1.0
    invc = np.zeros((128, 2, 4, tile), np.float32)
    t = np.arange(tile)
    for g, win in enumerate((2, 4, 8, 16)):
        invc[:, 1, g, :] = 1.0 / win
        invc[:, 0, g, :] = (1.0 / np.minimum(t + 1, win)) if first else 1.0 / win
    return dict(xT=np.ascontiguousarray(xT_halo), w_in=np.ascontiguousarray(w_in), cols=cols,
                w2=np.ascontiguousarray(w2), a2=np.ascontiguousarray(a2), g2=np.ascontiguousarray(g2),
                pool_w=np.ascontiguousarray(pool_w), bones=bones, invc=invc)


C = 64
NH = 4
C0 = 0.6065306597126334
RWKV_EPS = 64e-5


def MM(s, out, lhsT, rhs, start=True, stop=True, reads=(), writes=(), inc=True):
    s.op('pe', lambda e: e.matmul(out, lhsT=lhsT, rhs=rhs, start=start, stop=stop), reads=reads, writes=writes, inc=inc)


def TT(s, eng, out, in0, in1, op, reads, writes):
    s.op(eng, lambda e: e.tensor_tensor(out=out, in0=in0, in1=in1, op=op), reads=reads, writes=writes)


def ACTF(s, out, in_, func, reads, writes, bias=None, scale=1.0):
    if bias is None:
        s.op('act', lambda e: e.activation(out=out, in_=in_, func=func, scale=scale), reads=reads, writes=writes)
    else:
        s.op('act', lambda e: e.activation(out=out, in_=in_, func=func, bias=bias, scale=scale), reads=reads, writes=writes)


def TS(s, eng, out, in0, s1, s2, op0, op1, reads, writes):
    if op1 is None:
        s.op(eng, lambda e: e.tensor_scalar(out=out, in0=in0, scalar1=s1, scalar2=None, op0=op0), reads=reads, writes=writes)
    else:
        s.op(eng, lambda e: e.tensor_scalar(out=out, in0=in0, scalar1=s1, scalar2=s2, op0=op0, op1=op1), reads=reads, writes=writes)


def STT(s, out, in0, scalar, in1, op0, op1, reads, writes):
    s.op('dve', lambda e: e.scalar_tensor_tensor(out=out, in0=in0, scalar=scalar, in1=in1, op0=op0, op1=op1),
         reads=reads, writes=writes)


def CP(s, eng, out, in_, reads, writes):
    if eng == 'act':
        s.op('act', lambda e: e.copy(out=out, in_=in_), reads=reads, writes=writes)
    else:
        s.op(eng, lambda e: e.tensor_copy(out=out, in_=in_), reads=reads, writes=writes)


def RED(s, out, in_, reads, writes):
    s.op('dve', lambda e: e.reduce_sum(out=out, in_=in_, axis=AX.X), reads=reads, writes=writes)


def build_0b(T=8192, GC=4):
    nc = bass.Bass('TRN2', target_bir_lowering=False)
    dt = nc.dram_tensor
    tin = {n: dt(n, [64, NH, T], F32, kind="ExternalInput").ap() for n in ('RT', 'KT', 'AT', 'BT')}
    nin = {n: dt(n, [T, NH * 64], F32, kind="ExternalInput").ap() for n in ('Wn', 'Bn', 'Kn', 'Vn', 'BONn', 'Gn')}
    cst = {n: dt(n, shp, F32, kind="ExternalInput").ap() for n, shp in
           (('MU1', [64, NH, 128]), ('MLS', [64, NH, 64]), ('I4', [64, NH, 64]), ('TRI', [64, 128]), ('UPS', [64, 64]),
            ('LGB', [64, 2, NH * 64]))}
    y = dt("y", [T, NH * 64], F32, kind="ExternalOutput").ap()
    s = Sched(nc)
    emit_0b(s, tin, nin, cst, y, T, GC)
    s.build()
    return nc


def emit_0b(s, tin, nin, cst, y, T, GC):
    K = {}
    for n, shp in (('MU1', [64, NH, 128]), ('MLS', [64, NH, 64]), ('I4', [64, NH, 64]), ('TRI', [64, 128]), ('UPS', [64, 64]),
                   ('LGB', [64, 2, NH * 64])):
        K[n] = s.sbuf('K_' + n, shp, F32)
        s.dma('sp', K[n][:], cst[n], ('k', n), writes=[n])
    epst = s.sbuf('epst', [64, 1], F32)
    s.op('dve', lambda e: e.memset(epst[:], RWKV_EPS), writes=['epst'])
    S0 = s.sbuf('S0', [64, NH, 64], F32)
    s.op('dve', lambda e: e.memset(S0[:], 0.0), writes=['S0'])
    PS = [s.psum('PS%d' % i, [128, 512]) for i in range(8)]

    def bank(i, w):
        return PS[i][0:64, 0:NH * w].rearrange("p (h x) -> p h x", h=NH)
    GT = GC * C
    tbuf = {n: [s.sbuf('t_%s%d' % (n, i), [64, NH, GT], F32) for i in range(2)] for n in tin}
    nbuf = {n: [s.sbuf('n_%s%d' % (n, i), [64, GC, NH * 64], F32) for i in range(2)] for n in nin}
    f4 = lambda name, w=64: s.sbuf(name, [64, NH, w], F32)
    PT, PINV, PPREV = f4('PT'), f4('PINV'), f4('PPREV')
    E1 = s.sbuf('E1', [64, NH * 64], F32)
    AR = s.sbuf('AR', [64, NH, 2, 64], F32)
    BK = s.sbuf('BK', [64, NH, 2, 64], F32)
    BKn = s.sbuf('BKn', [64, 2, NH * 64], F32)
    X1, X2 = f4('X1', 128), f4('X2', 128)
    Nb = [f4('N0'), f4('N1')]
    NTb = [f4('NT0'), f4('NT1')]
    Rb = [f4('R0'), f4('R1')]
    Wsb, Usb, Ysb, Ysq, Yn = f4('Wsb'), f4('Usb'), f4('Ysb'), f4('Ysq'), f4('Yn')
    st = s.sbuf('st', [64, 6, NH], F32)
    Yo = [s.sbuf('Yo%d' % i, [64, NH * 64], F32) for i in range(2)]
    nchunk = T // C
    ngrp = nchunk // GC

    def load_group(gi):
        b = gi % 2
        t0 = gi * GT
        for n in tin:
            s.dma('sp', tbuf[n][b][:], tin[n][:, :, t0:t0 + GT], ('lt', n, b), writes=[('t', n, b)])
        for n in nin:
            s.dma('pool', nbuf[n][b][:], nin[n][t0:t0 + GT, :].rearrange("(c p) n -> p c n", p=64), ('ln', n, b),
                  writes=[('n', n, b)])

    def chunk(ci):
        gi, cj = divmod(ci, GC)
        b = gi % 2
        tv = lambda n: tbuf[n][b][:, :, cj * C:(cj + 1) * C]
        tk = lambda n: ('t', n, b)
        nv = lambda n: nbuf[n][b][:, cj, :]
        nk = lambda n: ('n', n, b)
        for h in range(NH):
            MM(s, PS[0][0:64, h * 128:(h + 1) * 128], nbuf['Wn'][b][:, cj, h * 64:(h + 1) * 64], K['TRI'][:],
               reads=[nk('Wn'), 'TRI'], writes=['B0'], inc=(h == NH - 1))
        MM(s, PS[1][0:64, 0:NH * 64], K['UPS'][:], nv('Wn'), reads=[nk('Wn'), 'UPS'], writes=['B1'])
        cumI = bank(0, 128)[:, :, 0:64]
        cumS = bank(0, 128)[:, :, 64:128]
        ACTF(s, PT[:], cumI, AF.Exp, ['B0'], ['PT'], scale=-C0)
        ACTF(s, PINV[:], cumI, AF.Exp, ['B0'], ['PINV'], scale=C0)
        ACTF(s, PPREV[:], cumS, AF.Exp, ['B0'], ['PPREV'], scale=-C0)
        ACTF(s, E1[:], PS[1][0:64, 0:NH * 64], AF.Exp, ['B1'], ['E1'], scale=-C0)
        TT(s, 'dve', AR[:, :, 0, :], tv('AT'), PPREV[:], ALU.mult, [tk('AT'), 'PPREV'], ['AR0'])
        TT(s, 'pool', AR[:, :, 1, :], tv('RT'), PT[:], ALU.mult, [tk('RT'), 'PT'], ['AR1'])
        TT(s, 'dve', BK[:, :, 0, :], tv('BT'), PINV[:], ALU.mult, [tk('BT'), 'PINV'], ['BK0'])
        TT(s, 'pool', BK[:, :, 1, :], tv('KT'), PINV[:], ALU.mult, [tk('KT'), 'PINV'], ['BK1'])
        TT(s, 'pool', BKn[:, 0, :], nv('Bn'), E1[:], ALU.mult, [nk('Bn'), 'E1'], ['BKn0'])
        TT(s, 'pool', BKn[:, 1, :], nv('Kn'), E1[:], ALU.mult, [nk('Kn'), 'E1'], ['BKn1'])
        for h in range(NH):
            arh = AR[:, h, :, :].rearrange("p a x -> p (a x)")
            MM(s, PS[2][0:64, h * 128:(h + 1) * 128], BK[:, h, 0, :], arh, reads=['BK0', 'AR0', 'AR1'], writes=['B2'],
               inc=(h == NH - 1))
        for h in range(NH):
            arh = AR[:, h, :, :].rearrange("p a x -> p (a x)")
            MM(s, PS[3][0:64, h * 128:(h + 1) * 128], BK[:, h, 1, :], arh, reads=['BK1', 'AR0', 'AR1'], writes=['B3'],
               inc=(h == NH - 1))
        for h in range(NH):
            MM(s, PS[4][0:64, h * 64:(h + 1) * 64], AR[:, h, 0, :], BK[:, h, 0, :], reads=['BK0', 'AR0'], writes=['B4'],
               inc=(h == NH - 1))
        TT(s, 'dve', X1[:], bank(2, 128), K['MU1'][:], ALU.mult, ['B2', 'MU1'], ['X1'])
        TT(s, 'dve', X2[:], bank(3, 128), K['MU1'][:], ALU.mult, ['B3', 'MU1'], ['X2'])
        TT(s, 'dve', NTb[0][:], bank(4, 64), K['MLS'][:], ALU.mult, ['B4', 'MLS'], [('NT', 0)])
        CP(s, 'pool', Nb[0][:], X1[:, :, 0:64], ['X1'], [('N', 0)])
        TT(s, 'pool', Rb[0][:], X1[:, :, 0:64], K['I4'][:], ALU.add, ['X1', 'I4'], [('R', 0)])
        cur = 0
        for lv in range(5):
            nx = 1 - cur
            if lv < 4:
                for h in range(NH):
                    MM(s, PS[5][0:64, h * 64:(h + 1) * 64], NTb[cur][:, h, :], Nb[cur][:, h, :],
                       reads=[('NT', cur), ('N', cur)], writes=['B5'], inc=(h == NH - 1))
            for h in range(NH):
                MM(s, PS[6][0:64, h * 64:(h + 1) * 64], Nb[cur][:, h, :], NTb[cur][:, h, :],
                   reads=[('NT', cur), ('N', cur)], writes=['B6'], inc=(h == NH - 1))
            if lv < 4:
                CP(s, 'act', Nb[nx][:], bank(5, 64), ['B5'], [('N', nx)])
            CP(s, 'dve', NTb[nx][:], bank(6, 64), ['B6'], [('NT', nx)])
            for h in range(NH):
                MM(s, PS[7][0:64, h * 64:(h + 1) * 64], NTb[nx][:, h, :], Rb[cur][:, h, :],
                   reads=[('NT', nx), ('R', cur)], writes=['B7'], inc=(h == NH - 1))
            TT(s, 'dve', Rb[nx][:], Rb[cur][:], bank(7, 64), ALU.add, [('R', cur), 'B7'], [('R', nx)])
            cur = nx
        R = Rb[cur]
        Rk = ('R', cur)
        for h in range(NH):
            vh = nbuf['Vn'][b][:, cj, h * 64:(h + 1) * 64]
            MM(s, PS[2][0:64, h * 64:(h + 1) * 64], X2[:, h, 0:64], vh, start=True, stop=False,
               reads=['X2', nk('Vn')], writes=['B2'], inc=False)
            MM(s, PS[2][0:64, h * 64:(h + 1) * 64], AR[:, h, 0, :], S0[:, h, :], start=False, stop=True,
               reads=['AR0', 'S0'], writes=['B2'], inc=(h == NH - 1))
        CP(s, 'dve', Wsb[:], bank(2, 64), ['B2'], ['Wsb'])
        for h in range(NH):
            MM(s, PS[3][0:64, h * 64:(h + 1) * 64], R[:, h, :], Wsb[:, h, :], reads=[Rk, 'Wsb'], writes=['B3'],
               inc=(h == NH - 1))
        CP(s, 'dve', Usb[:], bank(3, 64), ['B3'], ['Usb'])
        for h in range(NH):
            vh = nbuf['Vn'][b][:, cj, h * 64:(h + 1) * 64]
            o = PS[4][0:64, h * 64:(h + 1) * 64]
            MM(s, o, AR[:, h, 1, :], S0[:, h, :], start=True, stop=False, reads=['AR1', 'S0'], writes=['B4'], inc=False)
            MM(s, o, X1[:, h, 64:128], Usb[:, h, :], start=False, stop=False, reads=['X1', 'Usb'], writes=['B4'], inc=False)
            MM(s, o, X2[:, h, 64:128], vh, start=False, stop=True, reads=['X2', nk('Vn')], writes=['B4'], inc=(h == NH - 1))
        for h in range(NH):
            vh = nbuf['Vn'][b][:, cj, h * 64:(h + 1) * 64]
            o = PS[7][0:64, h * 64:(h + 1) * 64]
            MM(s, o, BKn[:, 0, h * 64:(h + 1) * 64], Usb[:, h, :], start=True, stop=False, reads=['BKn0', 'Usb'], writes=['B7'], inc=False)
            MM(s, o, BKn[:, 1, h * 64:(h + 1) * 64], vh, start=False, stop=True, reads=['BKn1', nk('Vn')], writes=['B7'], inc=(h == NH - 1))
        CP(s, 'dve', Ysb[:], bank(4, 64), ['B4'], ['Ysb'])
        for h in range(NH):
            STT(s, S0[:, h, :], S0[:, h, :], PT[:, h, 63:64], PS[7][0:64, h * 64:(h + 1) * 64], ALU.mult, ALU.add,
                ['S0', 'PT', 'B7'], ['S0'])
        RED(s, st[:, 0, :], Ysb[:], ['Ysb'], ['st0'])
        TT(s, 'pool', Ysq[:], Ysb[:], Ysb[:], ALU.mult, ['Ysb'], ['Ysq'])
        RED(s, st[:, 1, :], Ysq[:], ['Ysq'], ['st1'])
        TS(s, 'dve', st[:, 2, :], st[:, 0, :], 1.0 / 64, None, ALU.mult, None, ['st0'], ['st2'])
        TT(s, 'dve', st[:, 3, :], st[:, 2, :], st[:, 2, :], ALU.mult, ['st2'], ['st3'])
        STT(s, st[:, 4, :], st[:, 1, :], 1.0 / 64, st[:, 3, :], ALU.mult, ALU.subtract, ['st1', 'st3'], ['st4'])
        ACTF(s, st[:, 5, :], st[:, 4, :], AF.Ln, ['st4', 'epst'], ['st5'], bias=epst[:], scale=1.0)
        ACTF(s, st[:, 5, :], st[:, 5, :], AF.Exp, ['st5'], ['st5'], scale=-0.5)
        for h in range(NH):
            TS(s, 'dve', Yn[:, h, :], Ysb[:, h, :], st[:, 2, h:h + 1], st[:, 5, h:h + 1], ALU.subtract, ALU.mult,
               ['Ysb', 'st2', 'st5'], ['Yn'])
        ynf = Yn[:].rearrange("p h x -> p (h x)")
        yo = Yo[ci % 2]
        yok = ('Yo', ci % 2)
        TT(s, 'pool', ynf, ynf, K['LGB'][:, 0, :], ALU.mult, ['Yn', 'LGB'], ['Yn'])
        TT(s, 'pool', ynf, ynf, K['LGB'][:, 1, :], ALU.add, ['Yn', 'LGB'], ['Yn'])
        TT(s, 'pool', ynf, ynf, nv('BONn'), ALU.add, ['Yn', nk('BONn')], ['Yn'])
        TT(s, 'pool', yo[:], ynf, nv('Gn'), ALU.mult, ['Yn', nk('Gn')], [yok])
        s.dma('sp', y[ci * C:(ci + 1) * C, :], yo[:], ('so', ci % 2), reads=[yok], writes=[('yd', ci % 2)])

    load_group(0)
    for gi in range(ngrp):
        if gi + 1 < ngrp:
            load_group(gi + 1)
        for cj in range(GC):
            chunk(gi * GC + cj)
    s.wait_all('sp', [('yd', 0), ('yd', 1)])


def host_consts_0b(lnx_g, lnx_b):
    j = np.arange(64)[:, None]
    x = np.arange(64)[None, :]
    su = (x > j).astype(np.float32)
    iu = (x >= j).astype(np.float32)
    sl = (x < j).astype(np.float32)
    MU1 = np.broadcast_to(np.concatenate([su, iu], 1)[:, None, :], (64, NH, 128))
    MLS = np.broadcast_to(sl[:, None, :], (64, NH, 64))
    I4 = np.broadcast_to(np.eye(64, dtype=np.float32)[:, None, :], (64, NH, 64))
    TRI = np.concatenate([iu, su], 1)
    UPS = sl
    LGB = np.broadcast_to(np.stack([lnx_g, lnx_b])[None], (64, 2, NH * 64))
    return {k: np.ascontiguousarray(v, dtype=np.float32) for k, v in
            dict(MU1=MU1, MLS=MLS, I4=I4, TRI=TRI, UPS=UPS, LGB=LGB).items()}


def host_inputs_0b(arrT, lnx_g, lnx_b):
    T = arrT['oR'].shape[1]
    tr = lambda a: np.ascontiguousarray(a.reshape(NH, 64, T).transpose(1, 0, 2))
    na = lambda a: np.ascontiguousarray(a.T)
    d = dict(RT=tr(arrT['oR']), KT=tr(arrT['oK']), AT=tr(arrT['oA']), BT=tr(arrT['oB']),
             Wn=na(arrT['oW']), Bn=na(arrT['oB']), Kn=na(arrT['oK']), Vn=na(arrT['oV']),
             BONn=na(arrT['oBon']), Gn=na(arrT['oG']))
    d.update(host_consts_0b(lnx_g, lnx_b))
    return d


MMDT = BF16


def build_0b(T=8192, GP=2):
    nc = bass.Bass('TRN2', target_bir_lowering=False)
    dt = nc.dram_tensor
    tin = {n: dt(n, [64, NH, T], F32, kind="ExternalInput").ap() for n in ('RT', 'KT', 'AT', 'BT')}
    nin = {n: dt(n, [T, NH * 64], F32, kind="ExternalInput").ap() for n in ('Wn', 'Bn', 'Kn', 'Vn', 'BONn', 'Gn')}
    cst = {n: dt(n, shp, F32, kind="ExternalInput").ap() for n, shp in
           (('MU1', [128, NH, 128]), ('MLS', [128, NH, 64]), ('I4', [128, NH, 64]), ('TRI', [128, 128]), ('UPS', [128, 64]),
            ('LGB', [128, 2, NH * 64]))}
    y = dt("y", [T, NH * 64], F32, kind="ExternalOutput").ap()
    s = Sched(nc)
    emit_0b(s, tin, nin, cst, y, T, GP)
    s.build()
    return nc


def emit_0b(s, tin, nin, cst, y, T, GP):
    K = {}
    for n, shp in (('MU1', [128, NH, 128]), ('MLS', [128, NH, 64]), ('I4', [128, NH, 64]), ('TRI', [128, 128]), ('UPS', [128, 64]),
                   ('LGB', [128, 2, NH * 64])):
        K[n] = s.sbuf('K_' + n, shp, F32)
        s.dma('sp', K[n][:], cst[n], ('k', n), writes=[n])
    epst = s.sbuf('epst', [128, 1], F32)
    s.op('dve', lambda e: e.memset(epst[:], RWKV_EPS), writes=['epst'])
    S0 = s.sbuf('S0', [128, NH, 64], MMDT)
    s.op('dve', lambda e: e.memset(S0[:], 0.0), writes=[('S0', 0), ('S0', 1)])
    PS = [s.psum('PS%d' % i, [128, 512]) for i in range(8)]
    HS = [slice(0, 64), slice(64, 128)]

    def bank(i, w):
        return PS[i][:, 0:NH * w].rearrange("p (h x) -> p h x", h=NH)
    B2 = lambda k: [(k, 0), (k, 1)]
    GT = GP * 2 * C
    tbuf = {n: [s.sbuf('t_%s%d' % (n, i), [128, NH, GP, C], F32) for i in range(2)] for n in tin}
    nbuf = {n: [s.sbuf('n_%s%d' % (n, i), [128, GP, NH * 64], MMDT if n == 'Vn' else F32) for i in range(2)] for n in nin}
    f4 = lambda name, w=64, dt_=F32: s.sbuf(name, [128, NH, w], dt_)
    PT, PINV, PPREV = f4('PT'), f4('PINV'), f4('PPREV')
    E1 = s.sbuf('E1', [128, NH * 64], F32)
    AR = s.sbuf('AR', [128, NH, 2, 64], MMDT)
    BK = s.sbuf('BK', [128, NH, 2, 64], MMDT)
    BKn = s.sbuf('BKn', [128, 2, NH * 64], MMDT)
    DG = f4('DG', 64, MMDT)
    X1, X2 = f4('X1', 128, MMDT), f4('X2', 128, MMDT)
    Nb = [f4('N0', 64, MMDT), f4('N1', 64, MMDT)]
    NTb = [f4('NT0', 64, MMDT), f4('NT1', 64, MMDT)]
    Rb = [f4('R0', 64, MMDT), f4('R1', 64, MMDT)]
    Wsb, Usb = f4('Wsb', 64, MMDT), f4('Usb', 64, MMDT)
    Ysb, Ysq, Yn = f4('Ysb'), f4('Ysq'), f4('Yn')
    st = s.sbuf('st', [128, 6, NH], F32)
    Yo = [s.sbuf('Yo%d' % i, [128, NH * 64], F32) for i in range(2)]
    npair = T // (2 * C)
    ngrp = npair // GP

    def load_group(gi):
        b = gi % 2
        t0 = gi * GT
        for n in tin:
            src = tin[n][:, :, t0:t0 + GT].rearrange("k h (g two t) -> k h g two t", two=2, t=C)
            for hf in range(2):
                s.dma('sp', tbuf[n][b][HS[hf], :, :, :], src[:, :, :, hf, :], ('lt', n, b, hf), writes=[('t', n, b, hf)])
        for n in nin:
            s.dma('pool', nbuf[n][b][:], nin[n][t0:t0 + GT, :].rearrange("(c p) n -> p c n", p=128), ('ln', n, b),
                  writes=[('n', n, b)])

    def pair(pi):
        gi, pj = divmod(pi, GP)
        b = gi % 2
        tv = lambda n: tbuf[n][b][:, :, pj, :]
        tk = lambda n: [('t', n, b, 0), ('t', n, b, 1)]
        nv = lambda n: nbuf[n][b][:, pj, :]
        nk = lambda n: ('n', n, b)
        for hf in range(2):
            P_ = HS[hf]
            for h in range(NH):
                MM(s, PS[0][P_, h * 128:(h + 1) * 128], nbuf['Wn'][b][P_, pj, h * 64:(h + 1) * 64], K['TRI'][P_, :],
                   reads=[nk('Wn'), 'TRI'], writes=['B0'], inc=(h == NH - 1 and hf == 1))
        for hf in range(2):
            P_ = HS[hf]
            MM(s, PS[1][P_, 0:NH * 64], K['UPS'][P_, :], nbuf['Wn'][b][P_, pj, :], reads=[nk('Wn'), 'UPS'], writes=['B1'],
               inc=(hf == 1))
        cumI = bank(0, 128)[:, :, 0:64]
        cumS = bank(0, 128)[:, :, 64:128]
        ACTF(s, PT[:], cumI, AF.Exp, ['B0'], ['PT'], scale=-C0)
        ACTF(s, PINV[:], cumI, AF.Exp, ['B0'], ['PINV'], scale=C0)
        ACTF(s, PPREV[:], cumS, AF.Exp, ['B0'], ['PPREV'], scale=-C0)
        ACTF(s, E1[:], PS[1][:, 0:NH * 64], AF.Exp, ['B1'], ['E1'], scale=-C0)
        TT(s, 'dve', AR[:, :, 0, :], tv('AT'), PPREV[:], ALU.mult, tk('AT') + ['PPREV'], ['AR0'])
        TT(s, 'pool', AR[:, :, 1, :], tv('RT'), PT[:], ALU.mult, tk('RT') + ['PT'], ['AR1'])
        TT(s, 'dve', BK[:, :, 0, :], tv('BT'), PINV[:], ALU.mult, tk('BT') + ['PINV'], ['BK0'])
        TT(s, 'pool', BK[:, :, 1, :], tv('KT'), PINV[:], ALU.mult, tk('KT') + ['PINV'], ['BK1'])
        TT(s, 'pool', BKn[:, 0, :], nv('Bn'), E1[:], ALU.mult, [nk('Bn'), 'E1'], ['BKn0'])
        TT(s, 'pool', BKn[:, 1, :], nv('Kn'), E1[:], ALU.mult, [nk('Kn'), 'E1'], ['BKn1'])
        for h in range(NH):
            TS(s, 'pool', DG[:, h, :], K['I4'][:, h, :], PT[:, h, 63:64], None, ALU.mult, None, ['I4', 'PT'], ['DG'])
        for hf in range(2):
            P_ = HS[hf]
            last = (hf == 1)
            for h in range(NH):
                arh = AR[P_, h, :, :].rearrange("p a x -> p (a x)")
                MM(s, PS[2][P_, h * 128:(h + 1) * 128], BK[P_, h, 0, :], arh, reads=['BK0', 'AR0', 'AR1'], writes=B2('B2'),
                   inc=(h == NH - 1 and last))
            for h in range(NH):
                arh = AR[P_, h, :, :].rearrange("p a x -> p (a x)")
                MM(s, PS[3][P_, h * 128:(h + 1) * 128], BK[P_, h, 1, :], arh, reads=['BK1', 'AR0', 'AR1'], writes=B2('B3'),
                   inc=(h == NH - 1 and last))
            for h in range(NH):
                MM(s, PS[4][P_, h * 64:(h + 1) * 64], AR[P_, h, 0, :], BK[P_, h, 0, :], reads=['BK0', 'AR0'], writes=B2('B4'),
                   inc=(h == NH - 1 and last))
        TT(s, 'dve', X1[:], bank(2, 128), K['MU1'][:], ALU.mult, B2('B2') + ['MU1'], ['X1'])
        TT(s, 'dve', X2[:], bank(3, 128), K['MU1'][:], ALU.mult, B2('B3') + ['MU1'], ['X2'])
        TT(s, 'dve', NTb[0][:], bank(4, 64), K['MLS'][:], ALU.mult, B2('B4') + ['MLS'], [('NT', 0)])
        CP(s, 'pool', Nb[0][:], X1[:, :, 0:64], ['X1'], [('N', 0)])
        TT(s, 'pool', Rb[0][:], X1[:, :, 0:64], K['I4'][:], ALU.add, ['X1', 'I4'], [('R', 0)])
        cur = 0
        for lv in range(5):
            nx = 1 - cur
            if lv < 4:
                for hf in range(2):
                    P_ = HS[hf]
                    for h in range(NH):
                        MM(s, PS[5][P_, h * 64:(h + 1) * 64], NTb[cur][P_, h, :], Nb[cur][P_, h, :],
                           reads=[('NT', cur), ('N', cur)], writes=['B5'], inc=(h == NH - 1 and hf == 1))
            for hf in range(2):
                P_ = HS[hf]
                for h in range(NH):
                    MM(s, PS[6][P_, h * 64:(h + 1) * 64], Nb[cur][P_, h, :], NTb[cur][P_, h, :],
                       reads=[('NT', cur), ('N', cur)], writes=['B6'], inc=(h == NH - 1 and hf == 1))
            if lv < 4:
                CP(s, 'act', Nb[nx][:], bank(5, 64), ['B5'], [('N', nx)])
            CP(s, 'dve', NTb[nx][:], bank(6, 64), ['B6'], [('NT', nx)])
            for hf in range(2):
                P_ = HS[hf]
                for h in range(NH):
                    MM(s, PS[7][P_, h * 64:(h + 1) * 64], NTb[nx][P_, h, :], Rb[cur][P_, h, :],
                       reads=[('NT', nx), ('R', cur)], writes=B2('B7'), inc=(h == NH - 1 and hf == 1))
            TT(s, 'dve', Rb[nx][:], Rb[cur][:], bank(7, 64), ALU.add, [('R', cur)] + B2('B7'), [('R', nx)])
            cur = nx
        R = Rb[cur]
        Rk = ('R', cur)
        for hf in range(2):
            P_ = HS[hf]
            Q_ = HS[1 - hf]
            vsl = lambda h: nbuf['Vn'][b][P_, pj, h * 64:(h + 1) * 64]
            for h in range(NH):
                o = PS[2][P_, h * 64:(h + 1) * 64]
                MM(s, o, X2[P_, h, 0:64], vsl(h), start=True, stop=False, reads=['X2', nk('Vn')], writes=[('B2', hf)], inc=False)
                MM(s, o, AR[P_, h, 0, :], S0[P_, h, :], start=False, stop=True, reads=['AR0', ('S0', hf)], writes=[('B2', hf)],
                   inc=(h == NH - 1))
            CP(s, 'dve', Wsb[P_, :, :], PS[2][P_, 0:NH * 64].rearrange("p (h x) -> p h x", h=NH), [('B2', hf)], [('Wsb', hf)])
            for h in range(NH):
                MM(s, PS[3][P_, h * 64:(h + 1) * 64], R[P_, h, :], Wsb[P_, h, :], reads=[Rk, ('Wsb', hf)], writes=[('B3', hf)],
                   inc=(h == NH - 1))
            CP(s, 'dve', Usb[P_, :, :], PS[3][P_, 0:NH * 64].rearrange("p (h x) -> p h x", h=NH), [('B3', hf)], [('Usb', hf)])
            for h in range(NH):
                o = PS[7][Q_, h * 64:(h + 1) * 64]
                MM(s, o, DG[P_, h, :], S0[P_, h, :], start=True, stop=False, reads=['DG', ('S0', hf)], writes=[('B7', 1 - hf)], inc=False)
                MM(s, o, BKn[P_, 0, h * 64:(h + 1) * 64], Usb[P_, h, :], start=False, stop=False, reads=['BKn0', ('Usb', hf)],
                   writes=[('B7', 1 - hf)], inc=False)
                MM(s, o, BKn[P_, 1, h * 64:(h + 1) * 64], vsl(h), start=False, stop=True, reads=['BKn1', nk('Vn')],
                   writes=[('B7', 1 - hf)], inc=(h == NH - 1))
            for h in range(NH):
                o = PS[4][P_, h * 64:(h + 1) * 64]
                MM(s, o, AR[P_, h, 1, :], S0[P_, h, :], start=True, stop=False, reads=['AR1', ('S0', hf)], writes=[('B4', hf)], inc=False)
                MM(s, o, X1[P_, h, 64:128], Usb[P_, h, :], start=False, stop=False, reads=['X1', ('Usb', hf)], writes=[('B4', hf)], inc=False)
                MM(s, o, X2[P_, h, 64:128], vsl(h), start=False, stop=True, reads=['X2', nk('Vn')], writes=[('B4', hf)],
                   inc=(h == NH - 1))
            CP(s, 'dve', S0[Q_, :, :], PS[7][Q_, 0:NH * 64].rearrange("p (h x) -> p h x", h=NH), [('B7', 1 - hf)], [('S0', 1 - hf)])
        CP(s, 'dve', Ysb[:], bank(4, 64), B2('B4'), ['Ysb'])
        RED(s, st[:, 0, :], Ysb[:], ['Ysb'], ['st0'])
        TT(s, 'pool', Ysq[:], Ysb[:], Ysb[:], ALU.mult, ['Ysb'], ['Ysq'])
        RED(s, st[:, 1, :], Ysq[:], ['Ysq'], ['st1'])
        TS(s, 'dve', st[:, 2, :], st[:, 0, :], 1.0 / 64, None, ALU.mult, None, ['st0'], ['st2'])
        TT(s, 'dve', st[:, 3, :], st[:, 2, :], st[:, 2, :], ALU.mult, ['st2'], ['st3'])
        STT(s, st[:, 4, :], st[:, 1, :], 1.0 / 64, st[:, 3, :], ALU.mult, ALU.subtract, ['st1', 'st3'], ['st4'])
        ACTF(s, st[:, 5, :], st[:, 4, :], AF.Ln, ['st4', 'epst'], ['st5'], bias=epst[:], scale=1.0)
        ACTF(s, st[:, 5, :], st[:, 5, :], AF.Exp, ['st5'], ['st5'], scale=-0.5)
        for h in range(NH):
            TS(s, 'dve', Yn[:, h, :], Ysb[:, h, :], st[:, 2, h:h + 1], st[:, 5, h:h + 1], ALU.subtract, ALU.mult,
               ['Ysb', 'st2', 'st5'], ['Yn'])
        ynf = Yn[:].rearrange("p h x -> p (h x)")
        yo = Yo[pi % 2]
        yok = ('Yo', pi % 2)
        TT(s, 'pool', ynf, ynf, K['LGB'][:, 0, :], ALU.mult, ['Yn', 'LGB'], ['Yn'])
        TT(s, 'pool', ynf, ynf, K['LGB'][:, 1, :], ALU.add, ['Yn', 'LGB'], ['Yn'])
        TT(s, 'pool', ynf, ynf, nv('BONn'), ALU.add, ['Yn', nk('BONn')], ['Yn'])
        TT(s, 'pool', yo[:], ynf, nv('Gn'), ALU.mult, ['Yn', nk('Gn')], [yok])
        s.dma('sp', y[pi * 2 * C:(pi + 1) * 2 * C, :], yo[:], ('so', pi % 2), reads=[yok], writes=[('yd', pi % 2)])

    load_group(0)
    for gi in range(ngrp):
        if gi + 1 < ngrp:
            load_group(gi + 1)
        for pj in range(GP):
            pair(gi * GP + pj)
    s.wait_all('sp', [('yd', 0), ('yd', 1)])


def host_consts_0b(lnx_g, lnx_b):
    j = np.arange(64)[:, None]
    x = np.arange(64)[None, :]
    su = (x > j).astype(np.float32)
    iu = (x >= j).astype(np.float32)
    sl = (x < j).astype(np.float32)
    two = lambda a: np.concatenate([a, a], axis=0)
    MU1 = two(np.broadcast_to(np.concatenate([su, iu], 1)[:, None, :], (64, NH, 128)))
    MLS = two(np.broadcast_to(sl[:, None, :], (64, NH, 64)))
    I4 = two(np.broadcast_to(np.eye(64, dtype=np.float32)[:, None, :], (64, NH, 64)))
    TRI = two(np.concatenate([iu, su], 1))
    UPS = two(sl)
    LGB = np.broadcast_to(np.stack([lnx_g, lnx_b])[None], (128, 2, NH * 64))
    return {k: np.ascontiguousarray(v, dtype=np.float32) for k, v in
            dict(MU1=MU1, MLS=MLS, I4=I4, TRI=TRI, UPS=UPS, LGB=LGB).items()}


def host_inputs_0b(arrT, lnx_g, lnx_b):
    T = arrT['oR'].shape[1]
    tr = lambda a: np.ascontiguousarray(a.reshape(NH, 64, T).transpose(1, 0, 2))
    na = lambda a: np.ascontiguousarray(a.T)
    d = dict(RT=tr(arrT['oR']), KT=tr(arrT['oK']), AT=tr(arrT['oA']), BT=tr(arrT['oB']),
             Wn=na(arrT['oW']), Bn=na(arrT['oB']), Kn=na(arrT['oK']), Vn=na(arrT['oV']),
             BONn=na(arrT['oBon']), Gn=na(arrT['oG']))
    d.update(host_consts_0b(lnx_g, lnx_b))
    return d


D = 1024
NE = 32
GE = 4
ALPHA = (2.0 * 2) ** 0.25
LN_EPS = 1e-5


def build_C(ntok=4096, st=1024, level=9):
    nc = bass.Bass('TRN2', target_bir_lowering=False)
    dt = nc.dram_tensor
    xin = dt("xin", [ntok, D], F32, kind="ExternalInput").ap()
    ymT = dt("ymT", [D, ntok], F32, kind="ExternalInput").ap()
    w_out = dt("w_out", [D, D], F32, kind="ExternalInput").ap()
    wr = dt("wr", [D, 36], F32, kind="ExternalInput").ap()
    br = dt("br", [1, 36], F32, kind="ExternalInput").ap()
    lnp = dt("lnp", [128, 4, D], F32, kind="ExternalInput").ap()
    ident_d = dt("ident", [128, 128], F32, kind="ExternalInput").ap()
    sel_d = dt("sel", [32, NE * 128], F32, kind="ExternalInput").ap()
    e_w1 = dt("e_w1", [NE, D, 128], F32, kind="ExternalInput").ap()
    e_w3 = dt("e_w3", [NE, D, 128], F32, kind="ExternalInput").ap()
    e_w2 = dt("e_w2", [NE, 128, D], F32, kind="ExternalInput").ap()
    xout = dt("xout", [ntok, D], F32, kind="ExternalOutput").ap()
    s = Sched(nc)
    emit_C(s, xin, ymT, w_out, wr, br, lnp, ident_d, sel_d, e_w1, e_w3, e_w2, xout, ntok, st, level)
    s.build()
    return nc


def emit_C(s, xin, ymT, w_out, wr, br, lnp, ident_d, sel_d, e_w1, e_w3, e_w2, xout, ntok, st, level=9):
    wout_bf = s.sbuf('wout_bf', [128, 8, D], BF16)
    wr_f = s.sbuf('wr_f', [128, 8, 36], F32)
    br_f = s.sbuf('br_f', [1, 36], F32)
    ones1 = s.sbuf('ones1', [1, 128], F32)
    LNP = s.sbuf('LNP', [128, 4, D], F32)
    ident = s.sbuf('ident', [128, 128], F32)
    sel = s.sbuf('sel', [32, NE * 128], BF16)
    for k in range(8):
        s.dma('pool', wout_bf[:, k, :], w_out[k * 128:(k + 1) * 128, :], 'c0', writes=[('wout_bf', k)])
    s.dma('sp', wr_f[:], wr.rearrange("(k p) n -> p k n", p=128), 'c1', writes=['wr_f'])
    s.dma('sp', br_f[:], br, 'c2', writes=['br_f'])
    for a in range(4):
        s.dma('sp', LNP[:, a, :], lnp[:, a, :], ('c3', a), writes=[('LNP', a)])
    s.dma('sp', ident[:], ident_d, 'c4', writes=['ident'])
    s.dma('pool', sel[:], sel_d, 'c5', writes=['sel'])
    s.op('dve', lambda e: e.memset(ones1[:], 1.0), writes=['ones1'])
    epst = s.sbuf('epst', [128, 1], F32)
    s.op('dve', lambda e: e.memset(epst[:], LN_EPS), writes=['epst'])
    WOK = [('wout_bf', k) for k in range(8)]

    nst = ntok // st
    nsub = st // 128
    ntt = st // 256
    F = s.sbuf('F', [128, nsub, D], F32)
    xaT = s.sbuf('xaT', [128, 8, st], BF16)
    gT = s.sbuf('gT', [32, st], BF16)
    P = [s.psum('P%d' % i, [128, 512]) for i in range(8)]
    ym_bf = [s.sbuf('ym_bf%d' % i, [128, 8, 128], BF16) for i in range(2)]
    xin_t = [s.sbuf('xin_t%d' % i, [128, D], F32) for i in range(2)]
    z_t = [s.sbuf('z_t%d' % i, [128, D], F32) for i in range(2)]
    xa_t = [s.sbuf('xa_t%d' % i, [128, D], F32) for i in range(2)]
    xaTf = [s.sbuf('xaTf%d' % i, [128, 8, 128], F32) for i in range(2)]
    st6 = [s.sbuf('st6_%d' % i, [128, 2, 6], F32) for i in range(2)]
    mv = [s.sbuf('mv%d' % i, [128, 2], F32) for i in range(2)]
    rstd = [s.sbuf('rstd%d' % i, [128, 1], F32) for i in range(2)]
    nmr = [s.sbuf('nmr%d' % i, [128, 1], F32) for i in range(2)]
    lg = [s.sbuf('lg%d' % i, [128, 36], F32) for i in range(2)]
    rt = [s.sbuf('rt%d' % i, [128, 16], F32) for i in range(2)]
    r8 = [s.sbuf('r8_%d' % i, [128, 5, 8], F32) for i in range(2)]
    gates = [s.sbuf('gates%d' % i, [128, 32], F32) for i in range(2)]
    W1g = [s.sbuf('W1g%d' % i, [128, GE, 8, 128], BF16) for i in range(2)]
    W3g = [s.sbuf('W3g%d' % i, [128, GE, 8, 128], BF16) for i in range(2)]
    W2g = [s.sbuf('W2g%d' % i, [128, GE, D], BF16) for i in range(2)]
    s1 = [s.sbuf('s1_%d' % i, [128, 256], F32) for i in range(2)]
    uu = [s.sbuf('uu_%d' % i, [128, 256], F32) for i in range(2)]
    hg = [s.sbuf('hg_%d' % i, [128, 256], BF16) for i in range(2)]
    o_t = [s.sbuf('o_t%d' % i, [128, D], F32) for i in range(2)]

    def layer_norm_tile(src, srck, dst, dstk, gi, pi, eng_gb='pool'):
        st_, mv_, rs_, nm_ = st6[pi], mv[pi], rstd[pi], nmr[pi]
        for hh in range(2):
            s.op('dve', lambda e, hh=hh: e.bn_stats(out=st_[:, hh, :], in_=src[:, hh * 512:(hh + 1) * 512]),
                 reads=[srck], writes=[('st6', pi, hh)])
        s.op('dve', lambda e: e.bn_aggr(out=mv_[:], in_=st_[:].rearrange("p a b -> p (a b)")),
             reads=[('st6', pi, 0), ('st6', pi, 1)], writes=[('mv', pi)])
        s.op('act', lambda e: e.activation(out=rs_[:], in_=mv_[:, 1:2], func=AF.Ln, bias=epst[:], scale=1.0),
             reads=[('mv', pi), 'epst'], writes=[('rstd', pi)])
        s.op('act', lambda e: e.activation(out=rs_[:], in_=rs_[:], func=AF.Exp, scale=-0.5),
             reads=[('rstd', pi)], writes=[('rstd', pi)])
        s.op('dve', lambda e: e.scalar_tensor_tensor(out=nm_[:], in0=mv_[:, 0:1], scalar=-1.0, in1=rs_[:],
                                                     op0=ALU.mult, op1=ALU.mult),
             reads=[('mv', pi), ('rstd', pi)], writes=[('nmr', pi)])
        s.op('act', lambda e: e.activation(out=dst[:], in_=src[:], func=AF.Identity, bias=nm_[:], scale=rs_[:]),
             reads=[srck, ('rstd', pi), ('nmr', pi)], writes=[dstk])
        s.op(eng_gb, lambda e: e.tensor_tensor(out=dst[:], in0=dst[:], in1=LNP[:, gi, :], op=ALU.mult),
             reads=[dstk, ('LNP', gi)], writes=[dstk])
        s.op(eng_gb, lambda e: e.tensor_tensor(out=dst[:], in0=dst[:], in1=LNP[:, gi + 1, :], op=ALU.add),
             reads=[dstk, ('LNP', gi + 1)], writes=[dstk])

    wl_cnt = [0]

    def load_group(gi):
        b = wl_cnt[0] % 2
        wl_cnt[0] += 1
        for el in range(GE):
            ex = gi * GE + el
            s.dma('pool', W1g[b][:, el, :, :], e_w1[ex].rearrange("(k p) j -> p k j", p=128), ('w1', b, el),
                  writes=[('W1g', b, el)])
            s.dma('pool', W3g[b][:, el, :, :], e_w3[ex].rearrange("(k p) j -> p k j", p=128), ('w3', b, el),
                  writes=[('W3g', b, el)])
            s.dma('pool', W2g[b][:, el, :], e_w2[ex], ('w2', b, el), writes=[('W2g', b, el)])
        return b

    ngrp = NE // GE
    for sti in range(nst):
        t0 = sti * st
        for sub in range(nsub):
            pi = sub % 2
            tok0 = t0 + sub * 128
            yb = ym_bf[pi]
            ybk = ('ym_bf', pi)
            xt, zt, xat = xin_t[pi], z_t[pi], xa_t[pi]
            s.dma('pool', yb[:], ymT[:, tok0:tok0 + 128].rearrange("(k p) t -> p k t", p=128), ('ym', pi), writes=[ybk])
            s.dma('sp', xt[:], xin[tok0:tok0 + 128, :], ('xin', pi), writes=[('xin_t', pi)])
            for hh in range(2):
                pb = P[2 * pi + hh]
                pk = ('P', 2 * pi + hh)
                for k in range(8):
                    s.op('pe', lambda e, pb=pb, k=k, hh=hh, yb=yb: e.matmul(
                        pb[:], lhsT=yb[:, k, :], rhs=wout_bf[:, k, hh * 512:(hh + 1) * 512],
                        start=(k == 0), stop=(k == 7)), reads=[ybk] + WOK, writes=[pk], inc=(k == 7))
                s.op('dve', lambda e, pb=pb, hh=hh, xt=xt, zt=zt: e.scalar_tensor_tensor(
                    out=zt[:, hh * 512:(hh + 1) * 512], in0=xt[:, hh * 512:(hh + 1) * 512], scalar=ALPHA,
                    in1=pb[:], op0=ALU.mult, op1=ALU.add), reads=[pk, ('xin_t', pi)], writes=[('z_t', pi)])
            if level < 0.5:
                s.op('act', lambda e, sub=sub, zt=zt: e.copy(out=F[:, sub, :], in_=zt[:]),
                     reads=[('z_t', pi)], writes=[('F', sub)])
                continue
            layer_norm_tile(zt, ('z_t', pi), xat, ('xa_t', pi), 0, pi)
            s.op('act', lambda e, sub=sub, xat=xat: e.mul(out=F[:, sub, :], in_=xat[:], mul=ALPHA),
                 reads=[('xa_t', pi)], writes=[('F', sub)])
            if level < 2:
                continue
            xf = xaTf[pi]
            for k in range(8):
                pb = P[4 + 2 * pi + k // 4]
                pk = ('P', 4 + 2 * pi + k // 4)
                s.op('pe', lambda e, pb=pb, k=k, xat=xat: e.transpose(
                    out=pb[:, (k % 4) * 128:(k % 4 + 1) * 128], in_=xat[:, k * 128:(k + 1) * 128], identity=ident[:]),
                    reads=[('xa_t', pi), 'ident'], writes=[pk])
            for hb in range(2):
                pb = P[4 + 2 * pi + hb]
                pk = ('P', 4 + 2 * pi + hb)
                s.op('act', lambda e, pb=pb, hb=hb, xf=xf: e.copy(
                    out=xf[:, hb * 4:(hb + 1) * 4, :], in_=pb[:].rearrange("p (k t) -> p k t", k=4)),
                    reads=[pk], writes=[('xaTf', pi, hb)])
                s.op('pool', lambda e, hb=hb, sub=sub, xf=xf: e.tensor_copy(
                    out=xaT[:, hb * 4:(hb + 1) * 4, sub * 128:(sub + 1) * 128], in_=xf[:, hb * 4:(hb + 1) * 4, :]),
                    reads=[('xaTf', pi, hb)], writes=[('xaT', sub)])
            if level < 3:
                continue
            pb = P[4 + 2 * pi]
            pk = ('P', 4 + 2 * pi)
            for k in range(8):
                s.op('pe', lambda e, pb=pb, k=k, xf=xf: e.matmul(
                    pb[:, 0:36], lhsT=xf[:, k, :], rhs=wr_f[:, k, :], start=(k == 0), stop=False),
                    reads=[('xaTf', pi, k // 4), 'wr_f'], writes=[pk], inc=False)
            s.op('pe', lambda e, pb=pb: e.matmul(pb[:, 0:36], lhsT=ones1[:], rhs=br_f[:], start=False, stop=True),
                 reads=['ones1', 'br_f'], writes=[pk])
            L = lg[pi]
            Lk = ('lg', pi)
            R = rt[pi]
            Rk = ('rt', pi)
            R8 = r8[pi]
            R8k = ('r8', pi)
            G = gates[pi]
            s.op('act', lambda e, pb=pb, L=L: e.copy(out=L[:], in_=pb[:, 0:36]), reads=[pk], writes=[Lk])
            if level < 4:
                continue
            V = lambda fn, rd, wr_: s.op('dve', fn, reads=rd, writes=wr_)
            V(lambda e, L=L, R=R: e.reduce_max(out=R[:, 0:1], in_=L[:, 0:4], axis=AX.X), [Lk], [Rk])
            V(lambda e, L=L, R=R: e.tensor_scalar(out=R[:, 4:8], in0=L[:, 0:4], scalar1=R[:, 0:1], scalar2=None,
                                                  op0=ALU.is_ge), [Lk, Rk], [Rk])
            V(lambda e, R=R: e.tensor_scalar(out=R[:, 1:2], in0=R[:, 0:1], scalar1=-1.0, scalar2=None, op0=ALU.mult),
              [Rk], [Rk])
            s.op('act', lambda e, L=L, R=R: e.activation(out=R[:, 8:12], in_=L[:, 0:4], func=AF.Exp,
                                                         bias=R[:, 1:2], scale=1.0), reads=[Lk, Rk], writes=[Rk])
            V(lambda e, R=R: e.reduce_sum(out=R[:, 2:3], in_=R[:, 8:12], axis=AX.X), [Rk], [Rk])
            V(lambda e, L=L, R=R, R8=R8: e.tensor_scalar(out=R8[:, 0, :], in0=L[:, 4:12], scalar1=R[:, 4:5],
                                                         scalar2=None, op0=ALU.mult), [Lk, Rk], [R8k])
            for g in range(1, 4):
                V(lambda e, L=L, R=R, R8=R8, g=g: e.scalar_tensor_tensor(
                    out=R8[:, 0, :], in0=L[:, 4 + 8 * g:12 + 8 * g], scalar=R[:, 4 + g:5 + g], in1=R8[:, 0, :],
                    op0=ALU.mult, op1=ALU.add), [Lk, Rk, R8k], [R8k])
            V(lambda e, R8=R8: e.max(out=R8[:, 1, :], in_=R8[:, 0, :]), [R8k], [R8k])
            V(lambda e, R8=R8: e.tensor_scalar(out=R8[:, 2, :], in0=R8[:, 0, :], scalar1=R8[:, 1, 1:2], scalar2=None,
                                               op0=ALU.is_ge), [R8k], [R8k])
            V(lambda e, R=R, R8=R8: e.tensor_scalar(out=R[:, 12:13], in0=R8[:, 1, 0:1], scalar1=-1.0, scalar2=None,
                                                    op0=ALU.mult), [R8k, Rk], [Rk])
            s.op('act', lambda e, R=R, R8=R8: e.activation(out=R8[:, 3, :], in_=R8[:, 0, :], func=AF.Exp,
                                                           bias=R[:, 12:13], scale=1.0), reads=[R8k, Rk], writes=[R8k])
            V(lambda e, R8=R8: e.tensor_tensor(out=R8[:, 4, :], in0=R8[:, 3, :], in1=R8[:, 2, :], op=ALU.mult),
              [R8k], [R8k])
            V(lambda e, R=R, R8=R8: e.reduce_sum(out=R[:, 13:14], in_=R8[:, 4, :], axis=AX.X), [R8k, Rk], [Rk])
            V(lambda e, R=R: e.tensor_tensor(out=R[:, 14:15], in0=R[:, 13:14], in1=R[:, 2:3], op=ALU.mult), [Rk], [Rk])
            V(lambda e, R=R: e.reciprocal(out=R[:, 15:16], in_=R[:, 14:15]), [Rk], [Rk])
            V(lambda e, R=R, R8=R8: e.tensor_scalar(out=R8[:, 3, :], in0=R8[:, 4, :], scalar1=R[:, 15:16],
                                                    scalar2=None, op0=ALU.mult), [R8k, Rk], [R8k])
            for g in range(4):
                V(lambda e, R=R, R8=R8, G=G, g=g: e.tensor_scalar(
                    out=G[:, 8 * g:8 * g + 8], in0=R8[:, 3, :], scalar1=R[:, 4 + g:5 + g], scalar2=None,
                    op0=ALU.mult), [R8k, Rk], [('gates', pi)])
            if level < 6:
                if level >= 5:
                    s.op('act', lambda e, sub=sub, G=G: e.copy(out=F[:, sub, 0:32], in_=G[:]),
                         reads=[('gates', pi), ('F', sub)], writes=[('F', sub)])
                continue
            pb2 = P[5 + 2 * pi]
            pk2 = ('P', 5 + 2 * pi)
            s.op('pe', lambda e, pb2=pb2, G=G: e.transpose(out=pb2[0:32, 0:128], in_=G[:], identity=ident[:]),
                 reads=[('gates', pi), 'ident'], writes=[pk2])
            s.op('act', lambda e, pb2=pb2, sub=sub: e.copy(out=gT[:, sub * 128:(sub + 1) * 128],
                                                          in_=pb2[0:32, 0:128]),
                 reads=[pk2], writes=[('gT', sub)])

        for gi in range(ngrp if level >= 7 else 0):
            b = load_group(gi)
            for tt in range(ntt):
                c0 = tt * 256
                for el in range(GE):
                    ex = gi * GE + el
                    hb = (tt * GE + el) % 2
                    p1 = P[4 + 3 * hb]
                    p1k = ('P', 4 + 3 * hb)
                    p3 = P[5 + hb]
                    p3k = ('P', 5 + hb)
                    for k in range(8):
                        s.op('pe', lambda e, p1=p1, k=k, el=el, b=b, c0=c0: e.matmul(
                            p1[:, 0:256], lhsT=W1g[b][:, el, k, :], rhs=xaT[:, k, c0:c0 + 256],
                            start=(k == 0), stop=(k == 7)),
                            reads=[('W1g', b, el), ('xaT', 2 * tt), ('xaT', 2 * tt + 1)], writes=[p1k], inc=(k == 7))
                    for k in range(8):
                        s.op('pe', lambda e, p3=p3, k=k, el=el, b=b, c0=c0: e.matmul(
                            p3[:, 0:256], lhsT=W3g[b][:, el, k, :], rhs=xaT[:, k, c0:c0 + 256],
                            start=(k == 0), stop=(k == 7)),
                            reads=[('W3g', b, el), ('xaT', 2 * tt), ('xaT', 2 * tt + 1)], writes=[p3k], inc=False)
                    s.op('pe', lambda e, p3=p3, ex=ex, c0=c0: e.matmul(
                        p3[:, 256:512], lhsT=sel[:, ex * 128:(ex + 1) * 128], rhs=gT[:, c0:c0 + 256],
                        start=True, stop=True), reads=['sel', ('gT', 2 * tt), ('gT', 2 * tt + 1)], writes=[p3k])
                    s.op('act', lambda e, p1=p1, hb=hb: e.activation(out=s1[hb][:], in_=p1[:, 0:256], func=AF.Silu),
                         reads=[p1k], writes=[('s1', hb)])
                    s.op('dve', lambda e, p3=p3, hb=hb: e.tensor_tensor(out=uu[hb][:], in0=s1[hb][:], in1=p3[:, 256:512],
                                                                        op=ALU.mult),
                         reads=[('s1', hb), p3k], writes=[('uu', hb)])
                    s.op('dve', lambda e, p3=p3, hb=hb: e.tensor_tensor(out=hg[hb][:], in0=uu[hb][:],
                                                                        in1=p3[:, 0:256], op=ALU.mult),
                         reads=[('uu', hb), p3k], writes=[('hg', hb)])
                    for sb in range(2):
                        for hh in range(2):
                            pa = P[2 * sb + hh]
                            pak = ('P', 2 * sb + hh)
                            s.op('pe', lambda e, pa=pa, sb=sb, hh=hh, hb=hb, el=el, b=b: e.matmul(
                                pa[:], lhsT=hg[hb][:, sb * 128:(sb + 1) * 128], rhs=W2g[b][:, el, hh * 512:(hh + 1) * 512],
                                start=(el == 0), stop=(el == GE - 1)),
                                reads=[('hg', hb), ('W2g', b, el)], writes=[pak], inc=(sb == 1 and hh == 1))
                for sb in range(2):
                    sub = 2 * tt + sb
                    for hh in range(2):
                        pa = P[2 * sb + hh]
                        pak = ('P', 2 * sb + hh)
                        s.op('dve', lambda e, pa=pa, sub=sub, hh=hh: e.tensor_tensor(
                            out=F[:, sub, hh * 512:(hh + 1) * 512], in0=F[:, sub, hh * 512:(hh + 1) * 512], in1=pa[:],
                            op=ALU.add), reads=[pak, ('F', sub)], writes=[('F', sub)])
        for sub in range(nsub):
            pi = sub % 2
            ot = o_t[pi]
            if level < 0.3 or (5 <= level < 6):
                s.op('act', lambda e, sub=sub, ot=ot: e.copy(out=ot[:], in_=F[:, sub, :]),
                     reads=[('F', sub)], writes=[('o_t', pi)])
            else:
                layer_norm_tile(F[:, sub, :], ('F', sub), ot, ('o_t', pi), 2, pi)
            s.dma('sp', xout[t0 + sub * 128: t0 + (sub + 1) * 128, :], ot[:], ('st', pi), reads=[('o_t', pi)],
                  writes=[('xout', pi)])
    s.wait_all('sp', [('xout', 0), ('xout', 1)])


def host_inputs_C(xin, ymT, w_out, g1, b1, rg_w, rg_b, re_w, re_b, w1, w3, w2, g2, b2):
    wr = np.ascontiguousarray(np.concatenate([rg_w, re_w], axis=1))
    br = np.ascontiguousarray(np.concatenate([rg_b, re_b])[None, :])
    lnp = np.ascontiguousarray(np.broadcast_to(np.stack([g1, b1, g2, b2])[None], (128, 4, D)))
    ident = np.eye(128, dtype=np.float32)
    sel = np.zeros((32, NE, 128), np.float32)
    for e in range(NE):
        sel[e, e, :] = 1.0
    return dict(xin=np.ascontiguousarray(xin), ymT=np.ascontiguousarray(ymT), w_out=np.ascontiguousarray(w_out),
                wr=wr, br=br, lnp=lnp, ident=ident, sel=sel.reshape(32, NE * 128),
                e_w1=np.ascontiguousarray(w1), e_w3=np.ascontiguousarray(w3), e_w2=np.ascontiguousarray(w2))


HALO1 = 32
CONV_W = 31
LN_EPS1 = 1e-5


def build_1a(ntok=4096, tile=512):
    nc = bass.Bass('TRN2', target_bir_lowering=False)
    dt = nc.dram_tensor
    xT = dt("xT", [1024, HALO1 + ntok], F32, kind="ExternalInput").ap()
    w_ext = dt("w_ext", [1024, 4096], F32, kind="ExternalInput").ap()
    ccols = dt("ccols", [128, 4, 34], F32, kind="ExternalInput").ap()
    rot = dt("rot", [128, 2, ntok], F32, kind="ExternalInput").ap()
    ones_d = dt("ones", [128, 128], F32, kind="ExternalInput").ap()
    outs = {n: dt(n, [512, ntok], F32, kind="ExternalOutput").ap() for n in ('oYC', 'oQ', 'oKr', 'oV', 'oGr')}
    s = Sched(nc)
    emit_1a(s, xT, w_ext, ccols, rot, ones_d, outs, ntok, tile)
    s.build()
    return nc


def emit_1a(s, xT, w_ext, ccols_d, rot_d, ones_d, outs, ntok, T):
    W = s.sbuf('W', [128, 8, 4096], BF16)
    for k in range(8):
        for hf in range(2):
            s.dma('pool', W[:, k, hf * 2048:(hf + 1) * 2048], w_ext[k * 128:(k + 1) * 128, hf * 2048:(hf + 1) * 2048], 'w0',
                  writes=[('W', k, hf)])
    WK = [('W', k, hf) for k in range(8) for hf in range(2)]
    CC = s.sbuf('CC', [128, 4, 34], F32)
    s.dma('sp', CC[:], ccols_d, 'c0', writes=['CC'])
    ONES = s.sbuf('ONES', [128, 128], F32)
    s.dma('sp', ONES[:], ones_d, 'c1', writes=['ONES'])
    epst = s.sbuf('epst', [128, 1], F32)
    s.op('dve', lambda e: e.memset(epst[:], LN_EPS1), writes=['epst'])
    P = [s.psum('P%d' % i, [128, 512]) for i in range(8)]
    xb = [s.sbuf('xb%d' % i, [128, 8, T], BF16) for i in range(2)]
    xh = s.sbuf('xh', [128, 8, HALO1], BF16)
    CA = s.sbuf('CA', [128, T], F32)
    SG = s.sbuf('SG', [128, T], F32)
    CAH = s.sbuf('CAH', [128, 4, HALO1], F32)
    Ub = [s.sbuf('U%d' % i, [128, 4, HALO1 + T], F32) for i in range(2)]
    ACC = s.sbuf('ACC', [128, 4, T], F32)
    SQ = s.sbuf('SQ', [128, T], F32)
    XP = s.sbuf('XP', [128, 16, T], F32)
    ROT = [s.sbuf('ROT%d' % i, [128, 2, T], F32) for i in range(2)]
    MEAN = s.sbuf('MEAN', [128, T], F32)
    MSQ = s.sbuf('MSQ', [128, T], F32)
    RSTD = s.sbuf('RSTD', [128, T], F32)
    t1 = [s.sbuf('t1_%d' % i, [128, T], F32) for i in range(2)]
    t2 = [s.sbuf('t2_%d' % i, [128, T], F32) for i in range(2)]
    OB = {n: [s.sbuf('O_%s%d' % (n, i), [128, T], F32) for i in range(2)] for n in ('oYC', 'oQ', 'oKr', 'oV', 'oGr')}
    ocnt = {n: 0 for n in OB}

    def out_buf(n):
        i = ocnt[n] % 2
        ocnt[n] += 1
        return OB[n][i], (n, i)

    def proj(c, x_, XK, ncol, pb, pk):
        for k in range(8):
            MM(s, pb[:, 0:ncol], W[:, k, c * 128:(c + 1) * 128], x_[:, k, :], start=(k == 0), stop=(k == 7),
               reads=XK + WK, writes=[pk], inc=(k == 7))

    s.dma('pool', xh[:], xT[:, 0:HALO1].rearrange("(k p) t -> p k t", p=128), 'xh', writes=['xh'])
    for c in range(8):
        pb, pk = P[c % 2], ('P', c % 2)
        proj(c, xh, ['xh'], HALO1, pb, pk)
        if c < 4:
            CP(s, 'act', CAH[:, c, :], pb[:, 0:HALO1], [pk], [('CAH', c)])
        else:
            ACTF(s, SG[:, 0:HALO1], pb[:, 0:HALO1], AF.Sigmoid, [pk], ['SG'])
            TT(s, 'dve', Ub[0][:, c - 4, 0:HALO1], CAH[:, c - 4, :], SG[:, 0:HALO1], ALU.mult, [('CAH', c - 4), 'SG'], [('U', 0, c - 4)])

    def tile_body(ti):
        t0 = ti * T
        x_ = xb[ti % 2]
        XK = [('xb', ti % 2, k) for k in range(8)]
        for k in range(8):
            s.dma('pool', x_[:, k, :], xT[k * 128:(k + 1) * 128, HALO1 + t0:HALO1 + t0 + T], ('x', ti % 2), writes=[XK[k]])
        rt = ROT[ti % 2]
        rk = ('ROT', ti % 2)
        s.dma('sp', rt[:], rot_d[:, :, t0:t0 + T], ('rot', ti % 2), writes=[rk])
        tsl = slice(t0, t0 + T)
        ub = ti % 2
        U = Ub[ub]
        for cc in range(4):
            pb, pk = P[0], ('P', 0)
            proj(cc, x_, XK, T, pb, pk)
            CP(s, 'act', CA[:], pb[:], [pk], ['CA'])
            pb, pk = P[1], ('P', 1)
            proj(4 + cc, x_, XK, T, pb, pk)
            ACTF(s, SG[:], pb[:], AF.Sigmoid, [pk], ['SG'])
            TT(s, 'pool', U[:, cc, HALO1:HALO1 + T], CA[:], SG[:], ALU.mult, ['CA', 'SG'], [('U', ub, cc)])
        for cc in range(4):
            CP(s, 'pool', Ub[1 - ub][:, cc, 0:HALO1], U[:, cc, T:T + HALO1], [('U', ub, cc)], [('U', 1 - ub, cc)])
        for cc in range(4):
            TS(s, 'dve', ACC[:, cc, :], U[:, cc, 2:2 + T], CC[:, cc, 0:1], CC[:, cc, 31:32], ALU.mult, ALU.add,
               [('U', ub, cc), 'CC'], [('ACC', cc)])
            for j in range(1, CONV_W):
                STT(s, ACC[:, cc, :], U[:, cc, 2 + j:2 + j + T], CC[:, cc, j:j + 1], ACC[:, cc, :], ALU.mult, ALU.add,
                    [('U', ub, cc), 'CC', ('ACC', cc)], [('ACC', cc)])
        order = [(8 + i, i, 1.0) for i in range(4)] + [(24 + i, 8 + i, 1.0) for i in range(4)] + \
                [(12 + i, 4 + i, 0.125) for i in range(4)] + [(28 + i, 12 + i, 0.125) for i in range(4)]
        for n_, (c, slot, sc) in enumerate(order):
            pb, pk = P[n_ % 2], ('P', n_ % 2)
            proj(c, x_, XK, T, pb, pk)
            s.op('act', lambda e, pb=pb, slot=slot, sc=sc: e.mul(out=XP[:, slot, :], in_=pb[:], mul=sc), reads=[pk], writes=[('XP', slot)])
        for i in range(4):
            rows = slice(i * 128, (i + 1) * 128)
            pb, pk = P[0], ('P', 0)
            proj(16 + i, x_, XK, T, pb, pk)
            ob, obk = out_buf('oV')
            CP(s, 'act', ob[:], pb[:], [pk], [obk])
            s.dma('sp', outs['oV'][rows, tsl], ob[:], ('so',) + obk, reads=[obk], writes=[('d',) + obk])
            pb, pk = P[1], ('P', 1)
            proj(20 + i, x_, XK, T, pb, pk)
            ob, obk = out_buf('oGr')
            ACTF(s, ob[:], pb[:], AF.Silu, [pk], [obk])
            s.dma('sp', outs['oGr'][rows, tsl], ob[:], ('so',) + obk, reads=[obk], writes=[('d',) + obk])
        for i in range(8):
            name = 'oQ' if i < 4 else 'oKr'
            rows = slice((i % 4) * 128, (i % 4 + 1) * 128)
            a, b = t1[i % 2], t2[i % 2]
            TT(s, 'pool', a[:], XP[:, i, :], rt[:, 0, :], ALU.mult, [('XP', i), rk], [('t1', i % 2)])
            TT(s, 'pool', b[:], XP[:, 8 + i, :], rt[:, 1, :], ALU.mult, [('XP', 8 + i), rk], [('t2', i % 2)])
            ob, obk = out_buf(name)
            TT(s, 'pool', ob[:], a[:], b[:], ALU.add, [('t1', i % 2), ('t2', i % 2)], [obk])
            s.dma('sp', outs[name][rows, tsl], ob[:], ('so',) + obk, reads=[obk], writes=[('d',) + obk])
        for cc in range(4):
            MM(s, P[2][:], ONES[:], ACC[:, cc, :], start=(cc == 0), stop=(cc == 3), reads=['ONES', ('ACC', cc)], writes=[('P', 2)],
               inc=(cc == 3))
        for cc in range(4):
            s.op('act', lambda e, cc=cc: e.activation(out=SQ[:], in_=ACC[:, cc, :], func=AF.Square), reads=[('ACC', cc)], writes=['SQ'])
            MM(s, P[3][:], ONES[:], SQ[:], start=(cc == 0), stop=(cc == 3), reads=['ONES', 'SQ'], writes=[('P', 3)])
        s.op('act', lambda e: e.mul(out=MEAN[:], in_=P[2][:], mul=1.0 / 512), reads=[('P', 2)], writes=['MEAN'])
        TT(s, 'pool', MSQ[:], MEAN[:], MEAN[:], ALU.mult, ['MEAN'], ['MSQ'])
        STT(s, RSTD[:], P[3][:], 1.0 / 512, MSQ[:], ALU.mult, ALU.subtract, [('P', 3), 'MSQ'], ['RSTD'])
        ACTF(s, RSTD[:], RSTD[:], AF.Ln, ['RSTD', 'epst'], ['RSTD'], bias=epst[:], scale=1.0)
        ACTF(s, RSTD[:], RSTD[:], AF.Exp, ['RSTD'], ['RSTD'], scale=-0.5)
        for cc in range(4):
            rows = slice(cc * 128, (cc + 1) * 128)
            a = t1[cc % 2]
            TT(s, 'pool', a[:], ACC[:, cc, :], MEAN[:], ALU.subtract, [('ACC', cc), 'MEAN'], [('t1', cc % 2)])
            TT(s, 'pool', a[:], a[:], RSTD[:], ALU.mult, [('t1', cc % 2), 'RSTD'], [('t1', cc % 2)])
            ob, obk = out_buf('oYC')
            ACTF(s, ob[:], a[:], AF.Silu, [('t1', cc % 2), 'CC'], [obk], bias=CC[:, cc, 33:34], scale=CC[:, cc, 32:33])
            s.dma('sp', outs['oYC'][rows, tsl], ob[:], ('so',) + obk, reads=[obk], writes=[('d',) + obk])

    for ti in range(ntok // T):
        tile_body(ti)
    s.wait_all('sp', [k for k in s.last_w if isinstance(k, tuple) and k[0] == 'd'])


def rot_perm():
    idx = np.arange(512).reshape(8, 2, 32)[:, ::-1, :].reshape(-1)
    return idx


def host_inputs_1a(xT_halo, w_in, conv_w, conv_b, cln_g, cln_b, pos0, ntok=4096):
    perm = rot_perm()
    w_ext = np.concatenate([w_in, w_in[:, 1024:1536][:, perm], w_in[:, 1536:2048][:, perm]], axis=1)
    ccols = np.zeros((128, 4, 34), np.float32)
    ccols[:, :, 0:31] = conv_w.T.reshape(4, 128, 31).transpose(1, 0, 2)
    ccols[:, :, 31] = conv_b.reshape(4, 128).T
    ccols[:, :, 32] = cln_g.reshape(4, 128).T
    ccols[:, :, 33] = cln_b.reshape(4, 128).T
    half = 32
    inv = (np.float32(10000.0) ** (-np.arange(half, dtype=np.float32) / np.float32(half))).astype(np.float32)
    pos = np.arange(pos0, pos0 + ntok, dtype=np.float32)
    ang = (pos[:, None] * inv[None, :]).astype(np.float32)
    cos, sin = np.cos(ang).astype(np.float32), np.sin(ang).astype(np.float32)
    p = np.arange(128)
    i = p % 32
    second = (p % 64) >= 32
    rot = np.empty((128, 2, ntok), np.float32)
    rot[:, 0, :] = cos.T[i]
    rot[:, 1, :] = np.where(second[:, None], sin.T[i], -sin.T[i])
    return dict(xT=np.ascontiguousarray(xT_halo), w_ext=np.ascontiguousarray(w_ext), ccols=ccols, rot=rot,
                ones=np.ones((128, 128), np.float32))


RC = 128
RH = 4
RET_EPS = 1e-5


def build_1b(T=8192, GCH=2):
    nc = bass.Bass('TRN2', target_bir_lowering=False)
    dt = nc.dram_tensor
    tin = {n: dt(n, [64, RH, T], F32, kind="ExternalInput").ap() for n in ('QT', 'KT')}
    nin = {n: dt(n, [T, RH * 64], F32, kind="ExternalInput").ap() for n in ('Kn', 'Vn', 'Grn')}
    cst = {n: dt(n, shp, F32, kind="ExternalInput").ap() for n, shp in
           (('DM', [128, RH, 128]), ('XI', [64, RH, 128]), ('ZE', [128, RH * 64]), ('GCC', [64, RH]), ('GNB', [128, 2, RH * 64]))}
    y = dt("y", [T, RH * 64], F32, kind="ExternalOutput").ap()
    s = Sched(nc)
    emit_1b(s, tin, nin, cst, y, T, GCH)
    s.build()
    return nc


def emit_1b(s, tin, nin, cst, y, T, GCH):
    K = {}
    for n, shp in (('DM', [128, RH, 128]), ('XI', [64, RH, 128]), ('ZE', [128, RH * 64]), ('GCC', [64, RH]), ('GNB', [128, 2, RH * 64])):
        K[n] = s.sbuf('K_' + n, shp, F32)
        s.dma('sp', K[n][:], cst[n], ('k', n), writes=[n])
    epst = s.sbuf('epst', [128, 1], F32)
    s.op('dve', lambda e: e.memset(epst[:], RET_EPS), writes=['epst'])
    Rst = s.sbuf('Rst', [64, RH, 64], BF16)
    s.op('dve', lambda e: e.memset(Rst[:], 0.0), writes=['Rst'])
    PS = [s.psum('PS%d' % i, [128, 512]) for i in range(4)]
    GT = GCH * RC
    tbuf = {n: [s.sbuf('t_%s%d' % (n, i), [64, RH, GT], BF16) for i in range(2)] for n in tin}
    nbuf = {n: [s.sbuf('n_%s%d' % (n, i), [128, GCH, RH * 64], BF16 if n == 'Vn' else F32) for i in range(2)] for n in nin}
    QX = s.sbuf('QX', [64, RH, RC], BF16)
    KZ = s.sbuf('KZ', [128, RH * 64], BF16)
    SM = s.sbuf('SM', [128, RH, RC], BF16)
    Ysb = s.sbuf('Ysb', [128, RH, 64], F32)
    Ysq = s.sbuf('Ysq', [128, RH, 64], F32)
    Yn = s.sbuf('Yn', [128, RH, 64], F32)
    st = s.sbuf('st', [128, 6, RH], F32)
    Yo = [s.sbuf('Yo%d' % i, [128, RH * 64], F32) for i in range(2)]
    nchunk = T // RC
    ngrp = nchunk // GCH

    def load_group(gi):
        b = gi % 2
        t0 = gi * GT
        for n in tin:
            s.dma('pool', tbuf[n][b][:], tin[n][:, :, t0:t0 + GT], ('lt', n, b), writes=[('t', n, b)])
        for n in nin:
            s.dma('pool', nbuf[n][b][:], nin[n][t0:t0 + GT, :].rearrange("(c p) n -> p c n", p=128), ('ln', n, b),
                  writes=[('n', n, b)])

    def chunk(ci):
        gi, cj = divmod(ci, GCH)
        b = gi % 2
        tv = lambda n: tbuf[n][b][:, :, cj * RC:(cj + 1) * RC]
        tk = lambda n: ('t', n, b)
        nv = lambda n: nbuf[n][b][:, cj, :]
        nk = lambda n: ('n', n, b)
        TT(s, 'pool', QX[:], tv('QT'), K['XI'][:], ALU.mult, [tk('QT'), 'XI'], ['QX'])
        TT(s, 'pool', KZ[:], nv('Kn'), K['ZE'][:], ALU.mult, [nk('Kn'), 'ZE'], ['KZ'])
        for h in range(RH):
            MM(s, PS[0][:, h * RC:(h + 1) * RC], tbuf['KT'][b][:, h, cj * RC:(cj + 1) * RC], tbuf['QT'][b][:, h, cj * RC:(cj + 1) * RC],
               reads=[tk('KT'), tk('QT')], writes=['B0'], inc=(h == RH - 1))
        TT(s, 'dve', SM[:], PS[0][:, :].rearrange("p (h x) -> p h x", h=RH), K['DM'][:], ALU.mult, ['B0', 'DM'], ['SM'])
        for h in range(RH):
            vh = nbuf['Vn'][b][:, cj, h * 64:(h + 1) * 64]
            o = PS[1][:, h * 64:(h + 1) * 64]
            MM(s, o, SM[:, h, :], vh, start=True, stop=False, reads=['SM', nk('Vn')], writes=['B1'], inc=False)
            MM(s, o, QX[:, h, :], Rst[:, h, :], start=False, stop=True, reads=['QX', 'Rst'], writes=['B1'], inc=(h == RH - 1))
        for h in range(RH):
            vh = nbuf['Vn'][b][:, cj, h * 64:(h + 1) * 64]
            MM(s, PS[2][0:64, h * 64:(h + 1) * 64], KZ[:, h * 64:(h + 1) * 64], vh, reads=['KZ', nk('Vn')], writes=['B2'],
               inc=(h == RH - 1))
        CP(s, 'dve', Ysb[:], PS[1][:, 0:RH * 64].rearrange("p (h x) -> p h x", h=RH), ['B1'], ['Ysb'])
        for h in range(RH):
            STT(s, Rst[:, h, :], Rst[:, h, :], K['GCC'][:, h:h + 1], PS[2][0:64, h * 64:(h + 1) * 64], ALU.mult, ALU.add,
                ['Rst', 'GCC', 'B2'], ['Rst'])
        RED(s, st[:, 0, :], Ysb[:], ['Ysb'], ['st0'])
        TT(s, 'pool', Ysq[:], Ysb[:], Ysb[:], ALU.mult, ['Ysb'], ['Ysq'])
        RED(s, st[:, 1, :], Ysq[:], ['Ysq'], ['st1'])
        TS(s, 'dve', st[:, 2, :], st[:, 0, :], 1.0 / 64, None, ALU.mult, None, ['st0'], ['st2'])
        TT(s, 'dve', st[:, 3, :], st[:, 2, :], st[:, 2, :], ALU.mult, ['st2'], ['st3'])
        STT(s, st[:, 4, :], st[:, 1, :], 1.0 / 64, st[:, 3, :], ALU.mult, ALU.subtract, ['st1', 'st3'], ['st4'])
        ACTF(s, st[:, 5, :], st[:, 4, :], AF.Ln, ['st4', 'epst'], ['st5'], bias=epst[:], scale=1.0)
        ACTF(s, st[:, 5, :], st[:, 5, :], AF.Exp, ['st5'], ['st5'], scale=-0.5)
        for h in range(RH):
            TS(s, 'dve', Yn[:, h, :], Ysb[:, h, :], st[:, 2, h:h + 1], st[:, 5, h:h + 1], ALU.subtract, ALU.mult,
               ['Ysb', 'st2', 'st5'], ['Yn'])
        ynf = Yn[:].rearrange("p h x -> p (h x)")
        yo = Yo[ci % 2]
        yok = ('Yo', ci % 2)
        TT(s, 'pool', ynf, ynf, K['GNB'][:, 0, :], ALU.mult, ['Yn', 'GNB'], ['Yn'])
        TT(s, 'pool', ynf, ynf, K['GNB'][:, 1, :], ALU.add, ['Yn', 'GNB'], ['Yn'])
        TT(s, 'pool', yo[:], ynf, nv('Grn'), ALU.mult, ['Yn', nk('Grn')], [yok])
        s.dma('sp', y[ci * RC:(ci + 1) * RC, :], yo[:], ('so', ci % 2), reads=[yok], writes=[('yd', ci % 2)])

    load_group(0)
    for gi in range(ngrp):
        if gi + 1 < ngrp:
            load_group(gi + 1)
        for cj in range(GCH):
            chunk(gi * GCH + cj)
    s.wait_all('sp', [('yd', 0), ('yd', 1)])


def host_inputs_1b(arrT, gn_g, gn_b, head0):
    T = arrT['oQ'].shape[1]
    tr = lambda a: np.ascontiguousarray(a.reshape(RH, 64, T).transpose(1, 0, 2))
    na = lambda a: np.ascontiguousarray(a.T)
    hidx = np.arange(head0, head0 + RH, dtype=np.float32)
    log_gamma = np.log1p(-np.power(np.float32(2.0), -5.0 - hidx)).astype(np.float32)
    idx = np.arange(RC, dtype=np.float32)
    diff = idx[None, :] - idx[:, None]
    DM = np.where(diff[:, None, :] >= 0, np.exp(np.maximum(diff, 0.0)[:, None, :] * log_gamma[None, :, None]), 0.0)
    xi = np.exp((idx + 1.0)[None, :] * log_gamma[:, None])
    zeta = np.exp((RC - 1.0 - idx)[None, :] * log_gamma[:, None])
    gch = np.exp(RC * log_gamma)
    XI = np.broadcast_to(xi[None], (64, RH, RC))
    ZE = np.repeat(zeta.T, 64, axis=1)
    GCC = np.broadcast_to(gch[None], (64, RH))
    GNB = np.broadcast_to(np.stack([gn_g, gn_b])[None], (128, 2, RH * 64))
    f = lambda a: np.ascontiguousarray(a, dtype=np.float32)
    return dict(QT=tr(arrT['oQ']), KT=tr(arrT['oKr']), Kn=na(arrT['oKr']), Vn=na(arrT['oV']), Grn=na(arrT['oGr']),
                DM=f(DM), XI=f(XI), ZE=f(ZE), GCC=f(GCC), GNB=f(GNB))


NTOK = 4096
SHARDS = [(c // 2, (c % 2) * NTOK) for c in range(8)]


def _run(nc, in_maps):
    from concourse.bass_utils import run_bass_kernel_spmd
    return run_bass_kernel_spmd(nc, in_maps, core_ids=list(range(len(in_maps)))).results


def _post_mixer(inp, cur, ymTs, layer, w_out):
    ncC = build_C(ntok=NTOK, st=1024)
    maps = []
    for ci, (b, t0) in enumerate(SHARDS):
        maps.append(host_inputs_C(cur[b, t0:t0 + NTOK], ymTs[ci], w_out, inp['ln_mix_g'][layer], inp['ln_mix_b'][layer],
                                  inp['rg_w'][layer], inp['rg_b'][layer], inp['re_w'][layer], inp['re_b'][layer],
                                  inp['e_w1'][layer], inp['e_w3'][layer], inp['e_w2'][layer],
                                  inp['ln_ffn_g'][layer], inp['ln_ffn_b'][layer]))
    rc = _run(ncC, maps)
    nxt = np.empty_like(cur)
    for ci, (b, t0) in enumerate(SHARDS):
        nxt[b, t0:t0 + NTOK] = rc[ci]['xout']
    return nxt


def layer0(inp, x):
    nc0 = build_0a(ntok=NTOK)
    maps = []
    for (b, t0) in SHARDS:
        xT = np.zeros((D, HALO + NTOK), np.float32)
        xT[:, HALO:] = x[b, t0:t0 + NTOK].T
        if t0 > 0:
            xT[:, :HALO] = x[b, t0 - HALO:t0].T
        maps.append(host_inputs_0a(xT, inp['ev_w_in'][0], inp['ev_mu'][0], inp['ev_w0'][0], inp['ev_a0'][0],
                                   inp['ev_k_k'][0], inp['ev_k_a'][0], inp['ev_r_k'][0].reshape(-1),
                                   inp['ev_pool_scale'][0], inp['ev_w2'][0], inp['ev_a2'][0], inp['ev_g2'][0],
                                   inp['ev_pool_w'][0], t0 == 0))
    r0 = _run(nc0, maps)
    ncb = build_0b(T=2 * NTOK)
    maps = []
    for c in range(8):
        b, j = c // 2, c % 2
        rows = slice(256 * j, 256 * j + 256)
        arrT = {n: np.concatenate([r0[2 * b][n][rows], r0[2 * b + 1][n][rows]], axis=1)
                for n in ('oR', 'oK', 'oV', 'oA', 'oB', 'oW', 'oG', 'oBon')}
        maps.append(host_inputs_0b(arrT, inp['ev_lnx_g'][0][rows], inp['ev_lnx_b'][0][rows]))
    rb = _run(ncb, maps)
    ymTs = []
    for ci, (b, t0) in enumerate(SHARDS):
        ymT = np.empty((D, NTOK), np.float32)
        for j in range(2):
            ymT[256 * j:256 * j + 256] = rb[2 * b + j]['y'][t0:t0 + NTOK].T
        ymT[512:] = r0[ci]['oYP']
        ymTs.append(ymT)
    return _post_mixer(inp, x, ymTs, 0, inp['ev_w_out'][0])


def layer1(inp, x1):
    nc1 = build_1a(ntok=NTOK)
    maps = []
    for (b, t0) in SHARDS:
        xT = np.zeros((D, HALO1 + NTOK), np.float32)
        xT[:, HALO1:] = x1[b, t0:t0 + NTOK].T
        if t0 > 0:
            xT[:, :HALO1] = x1[b, t0 - HALO1:t0].T
        maps.append(host_inputs_1a(xT, inp['od_w_in'][0], inp['od_conv_w'][0], inp['od_conv_b'][0], inp['od_cln_g'][0],
                                   inp['od_cln_b'][0], t0, NTOK))
    r1 = _run(nc1, maps)
    ncb = build_1b(T=2 * NTOK)
    maps = []
    for c in range(8):
        b, j = c // 2, c % 2
        rows = slice(256 * j, 256 * j + 256)
        arrT = {n: np.concatenate([r1[2 * b][n][rows], r1[2 * b + 1][n][rows]], axis=1) for n in ('oQ', 'oKr', 'oV', 'oGr')}
        maps.append(host_inputs_1b(arrT, inp['od_gn_g'][0][rows], inp['od_gn_b'][0][rows], 4 * j))
    rb = _run(ncb, maps)
    ymTs = []
    for ci, (b, t0) in enumerate(SHARDS):
        ymT = np.empty((D, NTOK), np.float32)
        ymT[:512] = r1[ci]['oYC']
        for j in range(2):
            ymT[512 + 256 * j:512 + 256 * j + 256] = rb[2 * b + j]['y'][t0:t0 + NTOK].T
        ymTs.append(ymT)
    return _post_mixer(inp, x1, ymTs, 1, inp['od_w_out'][0])


def kernel(**inp):
    inp = {k: np.asarray(v) for k, v in inp.items()}
    x1 = layer0(inp, inp['x'])
    return layer1(inp, x1)
```

```python
import numpy as np


import concourse.bass as bass
import concourse.mybir as mybir

F32 = mybir.dt.float32
BF16 = mybir.dt.bfloat16
I32 = mybir.dt.int32
U32 = mybir.dt.uint32
AF = mybir.ActivationFunctionType
ALU = mybir.AluOpType
AX = mybir.AxisListType


class _Buf:
    def __init__(self, name, t):
        self.name = name
        self.t = t

    def __getitem__(self, idx):
        return self.t[idx]


class Sched:
    ENGS = ('pe', 'act', 'dve', 'pool', 'sp')

    def __init__(self, nc, self_wait=True):
        self.nc = nc
        self.self_wait = self_wait
        self.ops = {e: [] for e in self.ENGS}
        self.sems = {}
        self.cnt = {}
        self.waited = {e: {} for e in self.ENGS}
        self.last_w = {}
        self.readers = {}
        self.ctx = []
        self.pending_noinc = {e: False for e in self.ENGS}
        for e in ('pe', 'act', 'dve', 'pool'):
            self.sems[e] = nc.alloc_semaphore(name='sem_' + e)
            self.cnt[e] = 0
        self.ntile = 0

    def sbuf(self, name, shape, dtype):
        g = self.nc.sbuf_tensor('sb_' + name, list(shape), dtype)
        t = g.__enter__()
        self.ctx.append(g)
        return _Buf(name, t)

    def psum(self, name, shape, dtype=F32):
        g = self.nc.psum_tensor('ps_' + name, list(shape), dtype)
        t = g.__enter__()
        self.ctx.append(g)
        return _Buf(name, t)

    def dma_sem(self, key):
        if key not in self.sems:
            self.sems[key] = self.nc.alloc_semaphore(name='dsem_%d' % len(self.sems))
            self.cnt[key] = 0
        return key

    def _deps(self, reads, writes):
        deps = []
        for r in reads:
            if r in self.last_w:
                deps.append(self.last_w[r])
        for w in writes:
            if w in self.last_w:
                deps.append(self.last_w[w])
            deps.extend(self.readers.get(w, []))
        return deps

    def _emit_waits(self, eng, deps):
        waits = []
        best = {}
        for (sk, v) in deps:
            if sk == eng and (eng == 'pe' or not self.self_wait):
                continue
            if self.waited[eng].get(sk, 0) >= v:
                continue
            if best.get(sk, 0) < v:
                best[sk] = v
        for sk, v in best.items():
            self.waited[eng][sk] = v
            waits.append((self.sems[sk], v))
        return waits

    def _record(self, ev, reads, writes):
        for r in reads:
            self.readers.setdefault(r, []).append(ev)
        for w in writes:
            self.last_w[w] = ev
            self.readers[w] = []

    def op(self, eng, fn, reads=(), writes=(), inc=True):
        reads = [k for k in reads]
        writes = [k for k in writes]
        waits = self._emit_waits(eng, self._deps(reads, writes))
        sem = self.sems[eng]
        if inc:
            self.cnt[eng] += 1
            ev = (eng, self.cnt[eng])
            self.pending_noinc[eng] = False
        else:
            ev = (eng, self.cnt[eng] + 1)
            self.pending_noinc[eng] = True

        def emit(e, fn=fn, waits=waits, inc=inc, sem=sem):
            for (s, v) in waits:
                e.wait_ge(s, v)
            ins = fn(e)
            if inc:
                ins.then_inc(sem, 1)
        self.ops[eng].append(emit)
        self._record(ev, reads, writes)
        return ev

    def dma(self, eng, out, in_, semkey, reads=(), writes=(), **kw):
        self.dma_sem(semkey)
        reads = list(reads)
        writes = list(writes)
        waits = self._emit_waits(eng, self._deps(reads, writes))
        self.cnt[semkey] += 16
        ev = (semkey, self.cnt[semkey])
        sem = self.sems[semkey]

        def emit(e, waits=waits, sem=sem, out=out, in_=in_, kw=kw):
            for (s, v) in waits:
                e.wait_ge(s, v)
            e.dma_start(out=out, in_=in_, **kw).then_inc(sem, 16)
        self.ops[eng].append(emit)
        self._record(ev, reads, writes)
        return ev

    def wait_all(self, eng, keys):
        deps = []
        for k in keys:
            if k in self.last_w:
                deps.append(self.last_w[k])
        waits = self._emit_waits(eng, deps)

        def emit(e, waits=waits):
            for (s, v) in waits:
                e.wait_ge(s, v)
        self.ops[eng].append(emit)

    def build(self):
        nc = self.nc
        for e in self.ENGS:
            assert not self.pending_noinc[e], 'dangling noinc on ' + e
        with nc.Block() as block:
            @block.tensor
            def _(e):
                for f in self.ops['pe']:
                    f(e)

            @block.scalar
            def _(e):
                for f in self.ops['act']:
                    f(e)

            @block.vector
            def _(e):
                for f in self.ops['dve']:
                    f(e)

            @block.gpsimd
            def _(e):
                for f in self.ops['pool']:
                    f(e)

            @block.sync
            def _(e):
                for f in self.ops['sp']:
                    f(e)
        for g in reversed(self.ctx):
            g.__exit__(None, None, None)


D = 1024
HALO = 16
NCH = 18


def build_0a(ntok=4096, tile=512, dbg=None):
    nc = bass.Bass('TRN2', target_bir_lowering=False)
    dt = nc.dram_tensor
    xT = dt("xT", [D, HALO + ntok], F32, kind="ExternalInput").ap()
    w_in = dt("w_in", [D, 2304], F32, kind="ExternalInput").ap()
    cols = dt("cols", [128, 40], F32, kind="ExternalInput").ap()
    w2 = dt("w2", [64, 512], F32, kind="ExternalInput").ap()
    a2 = dt("a2", [64, 512], F32, kind="ExternalInput").ap()
    g2 = dt("g2", [128, 512], F32, kind="ExternalInput").ap()
    pool_w = dt("pool_w", [4, 128, 128], F32, kind="ExternalInput").ap()
    bones_d = dt("bones", [128, 128], F32, kind="ExternalInput").ap()
    invc_d = dt("invc", [128, 2, 4, tile], F32, kind="ExternalInput").ap()
    outs = {n: dt(n, [512, ntok], F32, kind="ExternalOutput").ap()
            for n in ('oR', 'oK', 'oV', 'oA', 'oB', 'oW', 'oG', 'oBon', 'oYP')}
    s = Sched(nc)
    emit_0a(s, xT, w_in, cols, w2, a2, g2, pool_w, bones_d, invc_d, outs, ntok, tile)
    s.build()
    return nc


OQ = 'sp'


def emit_0a(s, xT, w_in, cols_d, w2_d, a2_d, g2_d, pool_w_d, bones_d, invc_d, outs, ntok, tile):
    T = tile
    W = s.sbuf('W', [128, 8, 2304], BF16)
    for k in range(8):
        s.dma('pool', W[:, k, :], w_in[k * 128:(k + 1) * 128, :], 'w0', writes=[('W', k)])
    WK = [('W', k) for k in range(8)]
    COLS = s.sbuf('COLS', [128, 40], F32)
    s.dma('sp', COLS[:], cols_d, 'c0', writes=['COLS'])
    W2 = s.sbuf('W2', [64, 512], BF16)
    A2 = s.sbuf('A2', [128, 512], BF16)
    G2 = s.sbuf('G2', [128, 512], BF16)
    PW = s.sbuf('PW', [128, 4, 128], BF16)
    BONES = s.sbuf('BONES', [128, 128], BF16)
    INVC = s.sbuf('INVC', [128, 2, 4, T], F32)
    s.dma('pool', W2[:], w2_d, 'c1', writes=['W2'])
    s.dma('pool', A2[64:128, :], a2_d, 'c2', writes=['A2'])
    s.dma('pool', G2[:], g2_d, 'c3', writes=['G2'])
    for g in range(4):
        s.dma('pool', PW[:, g, :], pool_w_d[g], ('c4', g), writes=[('PW', g)])
    s.dma('pool', BONES[:], bones_d, 'c5', writes=['BONES'])
    for a in range(2):
        s.dma('sp', INVC[:, a, :, :], invc_d[:, a, :, :], ('c6', a), writes=[('INVC', a)])
    c24 = s.sbuf('c24', [128, 1], F32)
    s.op('dve', lambda e: e.memset(c24[:], 0.0), writes=['c24'])

    PR = s.sbuf('PR', [128, NCH, HALO + T], F32)
    Z = s.sbuf('Z', [128, 14, T], F32)
    xb = [s.sbuf('xb%d' % i, [128, 8, T], BF16) for i in range(2)]
    xh = s.sbuf('xh', [128, 8, HALO], BF16)
    P = [s.psum('P%d' % i, [128, 512]) for i in range(8)]
    tw = s.sbuf('tw', [64, T], BF16)
    al = s.sbuf('al', [128, T], BF16)
    sg = s.sbuf('sg', [128, T], BF16)
    tmp = [s.sbuf('tmp%d' % i, [128, T], F32) for i in range(4)]
    tb = [s.sbuf('tb%d' % i, [128, T], BF16) for i in range(2)]
    O = {n: [s.sbuf('O_%s%d' % (n, i), [128, T], F32) for i in range(2)] for n in ('oA', 'oB', 'oK', 'oW', 'oG', 'oBon', 'oYP')}
    SS = [s.sbuf('SS%d' % i, [128, HALO + T], F32) for i in range(2)]

    s.dma('pool', xh[:], xT[:, 0:HALO].rearrange("(k p) t -> p k t", p=128), 'xh', writes=['xh'])
    for c in range(NCH):
        pb = P[c % 2]
        pk = ('P', c % 2)
        for k in range(8):
            s.op('pe', lambda e, pb=pb, k=k, c=c: e.matmul(pb[:, 0:HALO], lhsT=W[:, k, c * 128:(c + 1) * 128],
                                                           rhs=xh[:, k, :], start=(k == 0), stop=(k == 7)),
                 reads=['xh'] + WK, writes=[pk], inc=(k == 7))
        s.op('act', lambda e, pb=pb, c=c: e.copy(out=PR[:, c, 0:HALO], in_=pb[:, 0:HALO]), reads=[pk], writes=[('PR', c)])

    ntile = ntok // T
    def tile_body(ti):
        t0 = ti * T
        x_ = xb[ti % 2]
        xk = ('xb', ti % 2)
        for k in range(8):
            s.dma('pool', x_[:, k, :], xT[k * 128:(k + 1) * 128, HALO + t0:HALO + t0 + T], ('x', ti % 2), writes=[(xk, k)])
        XK = [(xk, k) for k in range(8)]
        for c in range(NCH):
            pb = P[c % 2]
            pk = ('P', c % 2)
            for k in range(8):
                s.op('pe', lambda e, pb=pb, k=k, c=c, x_=x_: e.matmul(pb[:], lhsT=W[:, k, c * 128:(c + 1) * 128],
                                                                      rhs=x_[:, k, :], start=(k == 0), stop=(k == 7)),
                     reads=XK + WK, writes=[pk], inc=(k == 7))
            s.op('act', lambda e, pb=pb, c=c: e.copy(out=PR[:, c, HALO:HALO + T], in_=pb[:]), reads=[pk], writes=[('PR', c)])
        for c in range(14):
            d_ = tmp[c % 2]
            dk = ('tmp', c % 2)
            s.op('dve', lambda e, c=c, d_=d_: e.tensor_tensor(out=d_[:], in0=PR[:, c, HALO - 1:HALO - 1 + T],
                                                              in1=PR[:, c, HALO:HALO + T], op=ALU.subtract),
                 reads=[('PR', c)], writes=[dk])
            s.op('dve', lambda e, c=c, d_=d_: e.scalar_tensor_tensor(out=Z[:, c, :], in0=d_[:], scalar=COLS[:, c:c + 1],
                                                                     in1=PR[:, c, HALO:HALO + T], op0=ALU.mult, op1=ALU.add),
                 reads=[dk, ('PR', c), 'COLS'], writes=[('Z', c)])
        s.op('act', lambda e: e.activation(out=tw[:], in_=Z[0:64, 12, :], func=AF.Tanh), reads=[('Z', 12)], writes=['tw'])
        s.op('act', lambda e: e.copy(out=al[64:128, :], in_=Z[64:128, 12, :]), reads=[('Z', 12)], writes=['al'])
        s.op('act', lambda e: e.activation(out=sg[:], in_=Z[:, 13, :], func=AF.Sigmoid), reads=[('Z', 13)], writes=['sg'])
        ob = ti % 2
        for cc in range(4):
            cs = slice(cc * 128, (cc + 1) * 128)
            rows = slice(cc * 128, (cc + 1) * 128)
            tsl = slice(t0, t0 + T)
            s.op('pe', lambda e, cs=cs: e.matmul(P[2][:], lhsT=W2[:, cs], rhs=tw[:], start=True, stop=True),
                 reads=['W2', 'tw'], writes=[('P', 2)])
            s.op('act', lambda e, cc=cc: e.activation(out=O['oW'][ob][:], in_=P[2][:], func=AF.Sigmoid,
                                                     bias=COLS[:, 14 + cc:15 + cc], scale=1.0),
                 reads=[('P', 2), 'COLS'], writes=[('oW', ob)])
            s.dma(OQ, outs['oW'][rows, tsl], O['oW'][ob][:], ('so', 'oW', ob), reads=[('oW', ob)], writes=[('d_oW', ob)])
            s.op('pe', lambda e, cs=cs: e.matmul(P[3][:], lhsT=A2[64:128, cs], rhs=al[64:128, :], start=True, stop=True),
                 reads=['A2', 'al'], writes=[('P', 3)])
            asig = tmp[2]
            s.op('act', lambda e, cc=cc: e.activation(out=asig[:], in_=P[3][:], func=AF.Sigmoid,
                                                     bias=COLS[:, 18 + cc:19 + cc], scale=1.0),
                 reads=[('P', 3), 'COLS'], writes=[('tmp', 2)])
            s.op('pe', lambda e, cs=cs: e.matmul(P[4][:], lhsT=G2[:, cs], rhs=sg[:], start=True, stop=True),
                 reads=['G2', 'sg'], writes=[('P', 4)])
            s.op('act', lambda e: e.copy(out=O['oG'][ob][:], in_=P[4][:]), reads=[('P', 4)], writes=[('oG', ob)])
            s.dma(OQ, outs['oG'][rows, tsl], O['oG'][ob][:], ('so', 'oG', ob), reads=[('oG', ob)], writes=[('d_oG', ob)])
            kk0 = tmp[3]
            s.op('dve', lambda e, cc=cc: e.tensor_scalar(out=kk0[:], in0=Z[:, 4 + cc, :], scalar1=COLS[:, 22 + cc:23 + cc],
                                                         scalar2=None, op0=ALU.mult),
                 reads=[('Z', 4 + cc), 'COLS'], writes=[('tmp', 3)])
            s.op('dve', lambda e: e.tensor_tensor(out=tb[0][:], in0=kk0[:], in1=kk0[:], op=ALU.mult),
                 reads=[('tmp', 3)], writes=[('tb', 0)])
            s.op('pe', lambda e: e.matmul(P[5][:], lhsT=BONES[:], rhs=tb[0][:], start=True, stop=True),
                 reads=['BONES', ('tb', 0)], writes=[('P', 5)])
            rn = tmp[0]
            s.op('dve', lambda e: e.tensor_scalar(out=rn[:], in0=P[5][:], scalar1=1e-24, scalar2=None, op0=ALU.max),
                 reads=[('P', 5)], writes=[('tmp', 0)])
            s.op('act', lambda e: e.activation(out=rn[:], in_=rn[:], func=AF.Ln), reads=[('tmp', 0)], writes=[('tmp', 0)])
            s.op('act', lambda e: e.activation(out=rn[:], in_=rn[:], func=AF.Exp, scale=-0.5), reads=[('tmp', 0)], writes=[('tmp', 0)])
            s.op('dve', lambda e: e.tensor_tensor(out=kk0[:], in0=kk0[:], in1=rn[:], op=ALU.mult),
                 reads=[('tmp', 3), ('tmp', 0)], writes=[('tmp', 3)])
            s.op('act', lambda e: e.mul(out=O['oA'][ob][:], in_=kk0[:], mul=-1.0), reads=[('tmp', 3)], writes=[('oA', ob)])
            s.dma(OQ, outs['oA'][rows, tsl], O['oA'][ob][:], ('so', 'oA', ob), reads=[('oA', ob)], writes=[('d_oA', ob)])
            s.op('dve', lambda e: e.tensor_tensor(out=O['oB'][ob][:], in0=kk0[:], in1=asig[:], op=ALU.mult),
                 reads=[('tmp', 3), ('tmp', 2)], writes=[('oB', ob)])
            s.dma(OQ, outs['oB'][rows, tsl], O['oB'][ob][:], ('so', 'oB', ob), reads=[('oB', ob)], writes=[('d_oB', ob)])
            t1 = tmp[1]
            s.op('dve', lambda e, cc=cc: e.tensor_scalar(out=t1[:], in0=asig[:], scalar1=-1.0, scalar2=COLS[:, 26 + cc:27 + cc],
                                                         op0=ALU.add, op1=ALU.mult),
                 reads=[('tmp', 2), 'COLS'], writes=[('tmp', 1)])
            s.op('dve', lambda e, cc=cc: e.scalar_tensor_tensor(out=O['oK'][ob][:], in0=t1[:], scalar=1.0, in1=Z[:, 4 + cc, :],
                                                                op0=ALU.add, op1=ALU.mult),
                 reads=[('tmp', 1), ('Z', 4 + cc)], writes=[('oK', ob)])
            s.dma(OQ, outs['oK'][rows, tsl], O['oK'][ob][:], ('so', 'oK', ob), reads=[('oK', ob)], writes=[('d_oK', ob)])
            s.dma(OQ, outs['oR'][rows, tsl], Z[:, cc, :], ('so', 'oR', cc), reads=[('Z', cc)], writes=[('d_oR', cc)])
            s.dma(OQ, outs['oV'][rows, tsl], Z[:, 8 + cc, :], ('so', 'oV', cc), reads=[('Z', 8 + cc)], writes=[('d_oV', cc)])
            s.op('dve', lambda e, cc=cc: e.scalar_tensor_tensor(out=tb[1][:], in0=Z[:, cc, :], scalar=COLS[:, 30 + cc:31 + cc],
                                                                in1=O['oK'][ob][:], op0=ALU.mult, op1=ALU.mult),
                 reads=[('Z', cc), 'COLS', ('oK', ob)], writes=[('tb', 1)])
            s.op('pe', lambda e: e.matmul(P[6][:], lhsT=BONES[:], rhs=tb[1][:], start=True, stop=True),
                 reads=['BONES', ('tb', 1)], writes=[('P', 6)])
            s.op('dve', lambda e, cc=cc: e.tensor_tensor(out=O['oBon'][ob][:], in0=Z[:, 8 + cc, :], in1=P[6][:], op=ALU.mult),
                 reads=[('Z', 8 + cc), ('P', 6)], writes=[('oBon', ob)])
            s.dma(OQ, outs['oBon'][rows, tsl], O['oBon'][ob][:], ('so', 'oBon', ob), reads=[('oBon', ob)], writes=[('d_oBon', ob)])
        for g in range(4):
            c = 14 + g
            src = PR
            cur = None
            n = HALO + T
            prev_ap = lambda lo, hi, c=c: PR[:, c, lo:hi]
            a_, b_ = SS[0], SS[1]
            sh = 1
            first = True
            for lv in range(g + 1):
                lo = 2 * sh - 1
                if first:
                    s.op('pool', lambda e, c=c, sh=sh, n=n, lo=lo: e.tensor_tensor(out=SS[0][:, lo:n], in0=PR[:, c, lo:n], in1=PR[:, c, sh - 1:n - sh], op=ALU.add),
                         reads=[('PR', c)], writes=[('SS', 0)])
                    first = False
                    cur = 0
                else:
                    src_, dst_ = SS[cur], SS[1 - cur]
                    s.op('pool', lambda e, sh=sh, n=n, lo=lo, src_=src_, dst_=dst_: e.tensor_tensor(out=dst_[:, lo:n], in0=src_[:, lo:n],
                                                                                                in1=src_[:, sh - 1:n - sh], op=ALU.add),
                         reads=[('SS', cur)], writes=[('SS', 1 - cur)])
                    cur = 1 - cur
                sh *= 2
            ia = 0 if ti == 0 else 1
            s.op('dve', lambda e, g=g, cur=cur, ia=ia: e.tensor_tensor(out=tmp[0][:], in0=SS[cur][:, HALO:HALO + T], in1=INVC[:, ia, g, :], op=ALU.mult),
                 reads=[('SS', cur), ('INVC', ia)], writes=[('tmp', 0)])
            s.op('dve', lambda e, c=c: e.tensor_tensor(out=tb[0][:], in0=tmp[0][:], in1=PR[:, c, HALO:HALO + T], op=ALU.subtract),
                 reads=[('tmp', 0), ('PR', c)], writes=[('tb', 0)])
            s.op('pe', lambda e, g=g: e.matmul(P[7][:], lhsT=PW[:, g, :], rhs=tb[0][:], start=True, stop=True),
                 reads=[('PW', g), ('tb', 0)], writes=[('P', 7)])
            s.op('act', lambda e, g=g: e.activation(out=O['oYP'][ob][:], in_=P[7][:], func=AF.Identity, scale=COLS[:, 34 + g:35 + g]),
                 reads=[('P', 7), 'COLS'], writes=[('oYP', ob)])
            s.dma(OQ, outs['oYP'][g * 128:(g + 1) * 128, t0:t0 + T], O['oYP'][ob][:], ('so', 'oYP', ob), reads=[('oYP', ob)], writes=[('d_oYP', ob)])
        for c in range(NCH):
            s.op('pool', lambda e, c=c: e.tensor_copy(out=PR[:, c, 0:HALO], in_=PR[:, c, T:T + HALO]),
                 reads=[('PR', c)], writes=[('PR', c)])
    for ti in range(ntile):
        tile_body(ti)
    s.wait_all(OQ, [k for k in s.last_w if isinstance(k, tuple) and isinstance(k[0], str) and k[0].startswith('d_')])


def host_inputs_0a(xT_halo, w_in, mu, w0, a0, k_k, k_a, r_k, pool_scale, w2, a2, g2, pool_w, first, tile=512):
    cols = np.zeros((128, 40), np.float32)
    cols[:, 0:14] = mu.reshape(14, 128).T
    cols[:, 14:18] = w0.reshape(4, 128).T
    cols[:, 18:22] = a0.reshape(4, 128).T
    cols[:, 22:26] = k_k.reshape(4, 128).T
    cols[:, 26:30] = k_a.reshape(4, 128).T
    cols[:, 30:34] = r_k.reshape(4, 128).T
    cols[:, 34:38] = pool_scale.reshape(4, 128).T
    bones = np.zeros((128, 128), np.float32)
    bones[:64, :64] = 1.0
    bones[64:, 64:] = 1.0
    invc = np.zeros((128, 2, 4, tile), np.float32)
    t = np.arange(tile)
    for g, win in enumerate((2, 4, 8, 16)):
        invc[:, 1, g, :] = 1.0 / win
        invc[:, 0, g, :] = (1.0 / np.minimum(t + 1, win)) if first else 1.0 / win
    return dict(xT=np.ascontiguousarray(xT_halo), w_in=np.ascontiguousarray(w_in), cols=cols,
                w2=np.ascontiguousarray(w2), a2=np.ascontiguousarray(a2), g2=np.ascontiguousarray(g2),
                pool_w=np.ascontiguousarray(pool_w), bones=bones, invc=invc)


C = 64
NH = 4
C0 = 0.6065306597126334
RWKV_EPS = 64e-5


def MM(s, out, lhsT, rhs, start=True, stop=True, reads=(), writes=(), inc=True):
    s.op('pe', lambda e: e.matmul(out, lhsT=lhsT, rhs=rhs, start=start, stop=stop), reads=reads, writes=writes, inc=inc)


def TT(s, eng, out, in0, in1, op, reads, writes):
    s.op(eng, lambda e: e.tensor_tensor(out=out, in0=in0, in1=in1, op=op), reads=reads, writes=writes)


def ACTF(s, out, in_, func, reads, writes, bias=None, scale=1.0):
    if bias is None:
        s.op('act', lambda e: e.activation(out=out, in_=in_, func=func, scale=scale), reads=reads, writes=writes)
    else:
        s.op('act', lambda e: e.activation(out=out, in_=in_, func=func, bias=bias, scale=scale), reads=reads, writes=writes)


def TS(s, eng, out, in0, s1, s2, op0, op1, reads, writes):
    if op1 is None:
        s.op(eng, lambda e: e.tensor_scalar(out=out, in0=in0, scalar1=s1, scalar2=None, op0=op0), reads=reads, writes=writes)
    else:
        s.op(eng, lambda e: e.tensor_scalar(out=out, in0=in0, scalar1=s1, scalar2=s2, op0=op0, op1=op1), reads=reads, writes=writes)


def STT(s, out, in0, scalar, in1, op0, op1, reads, writes):
    s.op('dve', lambda e: e.scalar_tensor_tensor(out=out, in0=in0, scalar=scalar, in1=in1, op0=op0, op1=op1),
         reads=reads, writes=writes)


def CP(s, eng, out, in_, reads, writes):
    if eng == 'act':
        s.op('act', lambda e: e.copy(out=out, in_=in_), reads=reads, writes=writes)
    else:
        s.op(eng, lambda e: e.tensor_copy(out=out, in_=in_), reads=reads, writes=writes)


def RED(s, out, in_, reads, writes):
    s.op('dve', lambda e: e.reduce_sum(out=out, in_=in_, axis=AX.X), reads=reads, writes=writes)


def build_0b(T=8192, GC=4):
    nc = bass.Bass('TRN2', target_bir_lowering=False)
    dt = nc.dram_tensor
    tin = {n: dt(n, [64, NH, T], F32, kind="ExternalInput").ap() for n in ('RT', 'KT', 'AT', 'BT')}
    nin = {n: dt(n, [T, NH * 64], F32, kind="ExternalInput").ap() for n in ('Wn', 'Bn', 'Kn', 'Vn', 'BONn', 'Gn')}
    cst = {n: dt(n, shp, F32, kind="ExternalInput").ap() for n, shp in
           (('MU1', [64, NH, 128]), ('MLS', [64, NH, 64]), ('I4', [64, NH, 64]), ('TRI', [64, 128]), ('UPS', [64, 64]),
            ('LGB', [64, 2, NH * 64]))}
    y = dt("y", [T, NH * 64], F32, kind="ExternalOutput").ap()
    s = Sched(nc)
    emit_0b(s, tin, nin, cst, y, T, GC)
    s.build()
    return nc


def emit_0b(s, tin, nin, cst, y, T, GC):
    K = {}
    for n, shp in (('MU1', [64, NH, 128]), ('MLS', [64, NH, 64]), ('I4', [64, NH, 64]), ('TRI', [64, 128]), ('UPS', [64, 64]),
                   ('LGB', [64, 2, NH * 64])):
        K[n] = s.sbuf('K_' + n, shp, F32)
        s.dma('sp', K[n][:], cst[n], ('k', n), writes=[n])
    epst = s.sbuf('epst', [64, 1], F32)
    s.op('dve', lambda e: e.memset(epst[:], RWKV_EPS), writes=['epst'])
    S0 = s.sbuf('S0', [64, NH, 64], F32)
    s.op('dve', lambda e: e.memset(S0[:], 0.0), writes=['S0'])
    PS = [s.psum('PS%d' % i, [128, 512]) for i in range(8)]

    def bank(i, w):
        return PS[i][0:64, 0:NH * w].rearrange("p (h x) -> p h x", h=NH)
    GT = GC * C
    tbuf = {n: [s.sbuf('t_%s%d' % (n, i), [64, NH, GT], F32) for i in range(2)] for n in tin}
    nbuf = {n: [s.sbuf('n_%s%d' % (n, i), [64, GC, NH * 64], F32) for i in range(2)] for n in nin}
    f4 = lambda name, w=64: s.sbuf(name, [64, NH, w], F32)
    PT, PINV, PPREV = f4('PT'), f4('PINV'), f4('PPREV')
    E1 = s.sbuf('E1', [64, NH * 64], F32)
    AR = s.sbuf('AR', [64, NH, 2, 64], F32)
    BK = s.sbuf('BK', [64, NH, 2, 64], F32)
    BKn = s.sbuf('BKn', [64, 2, NH * 64], F32)
    X1, X2 = f4('X1', 128), f4('X2', 128)
    Nb = [f4('N0'), f4('N1')]
    NTb = [f4('NT0'), f4('NT1')]
    Rb = [f4('R0'), f4('R1')]
    Wsb, Usb, Ysb, Ysq, Yn = f4('Wsb'), f4('Usb'), f4('Ysb'), f4('Ysq'), f4('Yn')
    st = s.sbuf('st', [64, 6, NH], F32)
    Yo = [s.sbuf('Yo%d' % i, [64, NH * 64], F32) for i in range(2)]
    nchunk = T // C
    ngrp = nchunk // GC

    def load_group(gi):
        b = gi % 2
        t0 = gi * GT
        for n in tin:
            s.dma('sp', tbuf[n][b][:], tin[n][:, :, t0:t0 + GT], ('lt', n, b), writes=[('t', n, b)])
        for n in nin:
            s.dma('pool', nbuf[n][b][:], nin[n][t0:t0 + GT, :].rearrange("(c p) n -> p c n", p=64), ('ln', n, b),
                  writes=[('n', n, b)])

    def chunk(ci):
        gi, cj = divmod(ci, GC)
        b = gi % 2
        tv = lambda n: tbuf[n][b][:, :, cj * C:(cj + 1) * C]
        tk = lambda n: ('t', n, b)
        nv = lambda n: nbuf[n][b][:, cj, :]
        nk = lambda n: ('n', n, b)
        for h in range(NH):
            MM(s, PS[0][0:64, h * 128:(h + 1) * 128], nbuf['Wn'][b][:, cj, h * 64:(h + 1) * 64], K['TRI'][:],
               reads=[nk('Wn'), 'TRI'], writes=['B0'], inc=(h == NH - 1))
        MM(s, PS[1][0:64, 0:NH * 64], K['UPS'][:], nv('Wn'), reads=[nk('Wn'), 'UPS'], writes=['B1'])
        cumI = bank(0, 128)[:, :, 0:64]
        cumS = bank(0, 128)[:, :, 64:128]
        ACTF(s, PT[:], cumI, AF.Exp, ['B0'], ['PT'], scale=-C0)
        ACTF(s, PINV[:], cumI, AF.Exp, ['B0'], ['PINV'], scale=C0)
        ACTF(s, PPREV[:], cumS, AF.Exp, ['B0'], ['PPREV'], scale=-C0)
        ACTF(s, E1[:], PS[1][0:64, 0:NH * 64], AF.Exp, ['B1'], ['E1'], scale=-C0)
        TT(s, 'dve', AR[:, :, 0, :], tv('AT'), PPREV[:], ALU.mult, [tk('AT'), 'PPREV'], ['AR0'])
        TT(s, 'pool', AR[:, :, 1, :], tv('RT'), PT[:], ALU.mult, [tk('RT'), 'PT'], ['AR1'])
        TT(s, 'dve', BK[:, :, 0, :], tv('BT'), PINV[:], ALU.mult, [tk('BT'), 'PINV'], ['BK0'])
        TT(s, 'pool', BK[:, :, 1, :], tv('KT'), PINV[:], ALU.mult, [tk('KT'), 'PINV'], ['BK1'])
        TT(s, 'pool', BKn[:, 0, :], nv('Bn'), E1[:], ALU.mult, [nk('Bn'), 'E1'], ['BKn0'])
        TT(s, 'pool', BKn[:, 1, :], nv('Kn'), E1[:], ALU.mult, [nk('Kn'), 'E1'], ['BKn1'])
        for h in range(NH):
            arh = AR[:, h, :, :].rearrange("p a x -> p (a x)")
            MM(s, PS[2][0:64, h * 128:(h + 1) * 128], BK[:, h, 0, :], arh, reads=['BK0', 'AR0', 'AR1'], writes=['B2'],
               inc=(h == NH - 1))
        for h in range(NH):
            arh = AR[:, h, :, :].rearrange("p a x -> p (a x)")
            MM(s, PS[3][0:64, h * 128:(h + 1) * 128], BK[:, h, 1, :], arh, reads=['BK1', 'AR0', 'AR1'], writes=['B3'],
               inc=(h == NH - 1))
        for h in range(NH):
            MM(s, PS[4][0:64, h * 64:(h + 1) * 64], AR[:, h, 0, :], BK[:, h, 0, :], reads=['BK0', 'AR0'], writes=['B4'],
               inc=(h == NH - 1))
        TT(s, 'dve', X1[:], bank(2, 128), K['MU1'][:], ALU.mult, ['B2', 'MU1'], ['X1'])
        TT(s, 'dve', X2[:], bank(3, 128), K['MU1'][:], ALU.mult, ['B3', 'MU1'], ['X2'])
        TT(s, 'dve', NTb[0][:], bank(4, 64), K['MLS'][:], ALU.mult, ['B4', 'MLS'], [('NT', 0)])
        CP(s, 'pool', Nb[0][:], X1[:, :, 0:64], ['X1'], [('N', 0)])
        TT(s, 'pool', Rb[0][:], X1[:, :, 0:64], K['I4'][:], ALU.add, ['X1', 'I4'], [('R', 0)])
        cur = 0
        for lv in range(5):
            nx = 1 - cur
            if lv < 4:
                for h in range(NH):
                    MM(s, PS[5][0:64, h * 64:(h + 1) * 64], NTb[cur][:, h, :], Nb[cur][:, h, :],
                       reads=[('NT', cur), ('N', cur)], writes=['B5'], inc=(h == NH - 1))
            for h in range(NH):
                MM(s, PS[6][0:64, h * 64:(h + 1) * 64], Nb[cur][:, h, :], NTb[cur][:, h, :],
                   reads=[('NT', cur), ('N', cur)], writes=['B6'], inc=(h == NH - 1))
            if lv < 4:
                CP(s, 'act', Nb[nx][:], bank(5, 64), ['B5'], [('N', nx)])
            CP(s, 'dve', NTb[nx][:], bank(6, 64), ['B6'], [('NT', nx)])
            for h in range(NH):
                MM(s, PS[7][0:64, h * 64:(h + 1) * 64], NTb[nx][:, h, :], Rb[cur][:, h, :],
                   reads=[('NT', nx), ('R', cur)], writes=['B7'], inc=(h == NH - 1))
            TT(s, 'dve', Rb[nx][:], Rb[cur][:], bank(7, 64), ALU.add, [('R', cur), 'B7'], [('R', nx)])
            cur = nx
        R = Rb[cur]
        Rk = ('R', cur)
        for h in range(NH):
            vh = nbuf['Vn'][b][:, cj, h * 64:(h + 1) * 64]
            MM(s, PS[2][0:64, h * 64:(h + 1) * 64], X2[:, h, 0:64], vh, start=True, stop=False,
               reads=['X2', nk('Vn')], writes=['B2'], inc=False)
            MM(s, PS[2][0:64, h * 64:(h + 1) * 64], AR[:, h, 0, :], S0[:, h, :], start=False, stop=True,
               reads=['AR0', 'S0'], writes=['B2'], inc=(h == NH - 1))
        CP(s, 'dve', Wsb[:], bank(2, 64), ['B2'], ['Wsb'])
        for h in range(NH):
            MM(s, PS[3][0:64, h * 64:(h + 1) * 64], R[:, h, :], Wsb[:, h, :], reads=[Rk, 'Wsb'], writes=['B3'],
               inc=(h == NH - 1))
        CP(s, 'dve', Usb[:], bank(3, 64), ['B3'], ['Usb'])
        for h in range(NH):
            vh = nbuf['Vn'][b][:, cj, h * 64:(h + 1) * 64]
            o = PS[4][0:64, h * 64:(h + 1) * 64]
            MM(s, o, AR[:, h, 1, :], S0[:, h, :], start=True, stop=False, reads=['AR1', 'S0'], writes=['B4'], inc=False)
            MM(s, o, X1[:, h, 64:128], Usb[:, h, :], start=False, stop=False, reads=['X1', 'Usb'], writes=['B4'], inc=False)
            MM(s, o, X2[:, h, 64:128], vh, start=False, stop=True, reads=['X2', nk('Vn')], writes=['B4'], inc=(h == NH - 1))
        for h in range(NH):
            vh = nbuf['Vn'][b][:, cj, h * 64:(h + 1) * 64]
            o = PS[7][0:64, h * 64:(h + 1) * 64]
            MM(s, o, BKn[:, 0, h * 64:(h + 1) * 64], Usb[:, h, :], start=True, stop=False, reads=['BKn0', 'Usb'], writes=['B7'], inc=False)
            MM(s, o, BKn[:, 1, h * 64:(h + 1) * 64], vh, start=False, stop=True, reads=['BKn1', nk('Vn')], writes=['B7'], inc=(h == NH - 1))
        CP(s, 'dve', Ysb[:], bank(4, 64), ['B4'], ['Ysb'])
        for h in range(NH):
            STT(s, S0[:, h, :], S0[:, h, :], PT[:, h, 63:64], PS[7][0:64, h * 64:(h + 1) * 64], ALU.mult, ALU.add,
                ['S0', 'PT', 'B7'], ['S0'])
        RED(s, st[:, 0, :], Ysb[:], ['Ysb'], ['st0'])
        TT(s, 'pool', Ysq[:], Ysb[:], Ysb[:], ALU.mult, ['Ysb'], ['Ysq'])
        RED(s, st[:, 1, :], Ysq[:], ['Ysq'], ['st1'])
        TS(s, 'dve', st[:, 2, :], st[:, 0, :], 1.0 / 64, None, ALU.mult, None, ['st0'], ['st2'])
        TT(s, 'dve', st[:, 3, :], st[:, 2, :], st[:, 2, :], ALU.mult, ['st2'], ['st3'])
        STT(s, st[:, 4, :], st[:, 1, :], 1.0 / 64, st[:, 3, :], ALU.mult, ALU.subtract, ['st1', 'st3'], ['st4'])
        ACTF(s, st[:, 5, :], st[:, 4, :], AF.Ln, ['st4', 'epst'], ['st5'], bias=epst[:], scale=1.0)
        ACTF(s, st[:, 5, :], st[:, 5, :], AF.Exp, ['st5'], ['st5'], scale=-0.5)
        for h in range(NH):
            TS(s, 'dve', Yn[:, h, :], Ysb[:, h, :], st[:, 2, h:h + 1], st[:, 5, h:h + 1], ALU.subtract, ALU.mult,
               ['Ysb', 'st2', 'st5'], ['Yn'])
        ynf = Yn[:].rearrange("p h x -> p (h x)")
        yo = Yo[ci % 2]
        yok = ('Yo', ci % 2)
        TT(s, 'pool', ynf, ynf, K['LGB'][:, 0, :], ALU.mult, ['Yn', 'LGB'], ['Yn'])
        TT(s, 'pool', ynf, ynf, K['LGB'][:, 1, :], ALU.add, ['Yn', 'LGB'], ['Yn'])
        TT(s, 'pool', ynf, ynf, nv('BONn'), ALU.add, ['Yn', nk('BONn')], ['Yn'])
        TT(s, 'pool', yo[:], ynf, nv('Gn'), ALU.mult, ['Yn', nk('Gn')], [yok])
        s.dma('sp', y[ci * C:(ci + 1) * C, :], yo[:], ('so', ci % 2), reads=[yok], writes=[('yd', ci % 2)])

    load_group(0)
    for gi in range(ngrp):
        if gi + 1 < ngrp:
            load_group(gi + 1)
        for cj in range(GC):
            chunk(gi * GC + cj)
    s.wait_all('sp', [('yd', 0), ('yd', 1)])


def host_consts_0b(lnx_g, lnx_b):
    j = np.arange(64)[:, None]
    x = np.arange(64)[None, :]
    su = (x > j).astype(np.float32)
    iu = (x >= j).astype(np.float32)
    sl = (x < j).astype(np.float32)
    MU1 = np.broadcast_to(np.concatenate([su, iu], 1)[:, None, :], (64, NH, 128))
    MLS = np.broadcast_to(sl[:, None, :], (64, NH, 64))
    I4 = np.broadcast_to(np.eye(64, dtype=np.float32)[:, None, :], (64, NH, 64))
    TRI = np.concatenate([iu, su], 1)
    UPS = sl
    LGB = np.broadcast_to(np.stack([lnx_g, lnx_b])[None], (64, 2, NH * 64))
    return {k: np.ascontiguousarray(v, dtype=np.float32) for k, v in
            dict(MU1=MU1, MLS=MLS, I4=I4, TRI=TRI, UPS=UPS, LGB=LGB).items()}


def host_inputs_0b(arrT, lnx_g, lnx_b):
    T = arrT['oR'].shape[1]
    tr = lambda a: np.ascontiguousarray(a.reshape(NH, 64, T).transpose(1, 0, 2))
    na = lambda a: np.ascontiguousarray(a.T)
    d = dict(RT=tr(arrT['oR']), KT=tr(arrT['oK']), AT=tr(arrT['oA']), BT=tr(arrT['oB']),
             Wn=na(arrT['oW']), Bn=na(arrT['oB']), Kn=na(arrT['oK']), Vn=na(arrT['oV']),
             BONn=na(arrT['oBon']), Gn=na(arrT['oG']))
    d.update(host_consts_0b(lnx_g, lnx_b))
    return d


MMDT = BF16


def build_0b(T=8192, GP=2):
    nc = bass.Bass('TRN2', target_bir_lowering=False)
    dt = nc.dram_tensor
    tin = {n: dt(n, [64, NH, T], F32, kind="ExternalInput").ap() for n in ('RT', 'KT', 'AT', 'BT')}
    nin = {n: dt(n, [T, NH * 64], F32, kind="ExternalInput").ap() for n in ('Wn', 'Bn', 'Kn', 'Vn', 'BONn', 'Gn')}
    cst = {n: dt(n, shp, F32, kind="ExternalInput").ap() for n, shp in
           (('MU1', [128, NH, 128]), ('MLS', [128, NH, 64]), ('I4', [128, NH, 64]), ('TRI', [128, 128]), ('UPS', [128, 64]),
            ('LGB', [128, 2, NH * 64]))}
    y = dt("y", [T, NH * 64], F32, kind="ExternalOutput").ap()
    s = Sched(nc)
    emit_0b(s, tin, nin, cst, y, T, GP)
    s.build()
    return nc


def emit_0b(s, tin, nin, cst, y, T, GP):
    K = {}
    for n, shp in (('MU1', [128, NH, 128]), ('MLS', [128, NH, 64]), ('I4', [128, NH, 64]), ('TRI', [128, 128]), ('UPS', [128, 64]),
                   ('LGB', [128, 2, NH * 64])):
        K[n] = s.sbuf('K_' + n, shp, F32)
        s.dma('sp', K[n][:], cst[n], ('k', n), writes=[n])
    epst = s.sbuf('epst', [128, 1], F32)
    s.op('dve', lambda e: e.memset(epst[:], RWKV_EPS), writes=['epst'])
    S0 = s.sbuf('S0', [128, NH, 64], MMDT)
    s.op('dve', lambda e: e.memset(S0[:], 0.0), writes=[('S0', 0), ('S0', 1)])
    PS = [s.psum('PS%d' % i, [128, 512]) for i in range(8)]
    HS = [slice(0, 64), slice(64, 128)]

    def bank(i, w):
        return PS[i][:, 0:NH * w].rearrange("p (h x) -> p h x", h=NH)
    B2 = lambda k: [(k, 0), (k, 1)]
    GT = GP * 2 * C
    tbuf = {n: [s.sbuf('t_%s%d' % (n, i), [128, NH, GP, C], F32) for i in range(2)] for n in tin}
    nbuf = {n: [s.sbuf('n_%s%d' % (n, i), [128, GP, NH * 64], MMDT if n == 'Vn' else F32) for i in range(2)] for n in nin}
    f4 = lambda name, w=64, dt_=F32: s.sbuf(name, [128, NH, w], dt_)
    PT, PINV, PPREV = f4('PT'), f4('PINV'), f4('PPREV')
    E1 = s.sbuf('E1', [128, NH * 64], F32)
    AR = s.sbuf('AR', [128, NH, 2, 64], MMDT)
    BK = s.sbuf('BK', [128, NH, 2, 64], MMDT)
    BKn = s.sbuf('BKn', [128, 2, NH * 64], MMDT)
    DG = f4('DG', 64, MMDT)
    X1, X2 = f4('X1', 128, MMDT), f4('X2', 128, MMDT)
    Nb = [f4('N0', 64, MMDT), f4('N1', 64, MMDT)]
    NTb = [f4('NT0', 64, MMDT), f4('NT1', 64, MMDT)]
    Rb = [f4('R0', 64, MMDT), f4('R1', 64, MMDT)]
    Wsb, Usb = f4('Wsb', 64, MMDT), f4('Usb', 64, MMDT)
    Ysb, Ysq, Yn = f4('Ysb'), f4('Ysq'), f4('Yn')
    st = s.sbuf('st', [128, 6, NH], F32)
    Yo = [s.sbuf('Yo%d' % i, [128, NH * 64], F32) for i in range(2)]
    npair = T // (2 * C)
    ngrp = npair // GP

    def load_group(gi):
        b = gi % 2
        t0 = gi * GT
        for n in tin:
            src = tin[n][:, :, t0:t0 + GT].rearrange("k h (g two t) -> k h g two t", two=2, t=C)
            for hf in range(2):
                s.dma('sp', tbuf[n][b][HS[hf], :, :, :], src[:, :, :, hf, :], ('lt', n, b, hf), writes=[('t', n, b, hf)])
        for n in nin:
            s.dma('pool', nbuf[n][b][:], nin[n][t0:t0 + GT, :].rearrange("(c p) n -> p c n", p=128), ('ln', n, b),
                  writes=[('n', n, b)])

    def pair(pi):
        gi, pj = divmod(pi, GP)
        b = gi % 2
        tv = lambda n: tbuf[n][b][:, :, pj, :]
        tk = lambda n: [('t', n, b, 0), ('t', n, b, 1)]
        nv = lambda n: nbuf[n][b][:, pj, :]
        nk = lambda n: ('n', n, b)
        for hf in range(2):
            P_ = HS[hf]
            for h in range(NH):
                MM(s, PS[0][P_, h * 128:(h + 1) * 128], nbuf['Wn'][b][P_, pj, h * 64:(h + 1) * 64], K['TRI'][P_, :],
                   reads=[nk('Wn'), 'TRI'], writes=['B0'], inc=(h == NH - 1 and hf == 1))
        for hf in range(2):
            P_ = HS[hf]
            MM(s, PS[1][P_, 0:NH * 64], K['UPS'][P_, :], nbuf['Wn'][b][P_, pj, :], reads=[nk('Wn'), 'UPS'], writes=['B1'],
               inc=(hf == 1))
        cumI = bank(0, 128)[:, :, 0:64]
        cumS = bank(0, 128)[:, :, 64:128]
        ACTF(s, PT[:], cumI, AF.Exp, ['B0'], ['PT'], scale=-C0)
        ACTF(s, PINV[:], cumI, AF.Exp, ['B0'], ['PINV'], scale=C0)
        ACTF(s, PPREV[:], cumS, AF.Exp, ['B0'], ['PPREV'], scale=-C0)
        ACTF(s, E1[:], PS[1][:, 0:NH * 64], AF.Exp, ['B1'], ['E1'], scale=-C0)
        TT(s, 'dve', AR[:, :, 0, :], tv('AT'), PPREV[:], ALU.mult, tk('AT') + ['PPREV'], ['AR0'])
        TT(s, 'pool', AR[:, :, 1, :], tv('RT'), PT[:], ALU.mult, tk('RT') + ['PT'], ['AR1'])
        TT(s, 'dve', BK[:, :, 0, :], tv('BT'), PINV[:], ALU.mult, tk('BT') + ['PINV'], ['BK0'])
        TT(s, 'pool', BK[:, :, 1, :], tv('KT'), PINV[:], ALU.mult, tk('KT') + ['PINV'], ['BK1'])
        TT(s, 'pool', BKn[:, 0, :], nv('Bn'), E1[:], ALU.mult, [nk('Bn'), 'E1'], ['BKn0'])
        TT(s, 'pool', BKn[:, 1, :], nv('Kn'), E1[:], ALU.mult, [nk('Kn'), 'E1'], ['BKn1'])
        for h in range(NH):
            TS(s, 'pool', DG[:, h, :], K['I4'][:, h, :], PT[:, h, 63:64], None, ALU.mult, None, ['I4', 'PT'], ['DG'])
        for hf in range(2):
            P_ = HS[hf]
            last = (hf == 1)
            for h in range(NH):
                arh = AR[P_, h, :, :].rearrange("p a x -> p (a x)")
                MM(s, PS[2][P_, h * 128:(h + 1) * 128], BK[P_, h, 0, :], arh, reads=['BK0', 'AR0', 'AR1'], writes=B2('B2'),
                   inc=(h == NH - 1 and last))
            for h in range(NH):
                arh = AR[P_, h, :, :].rearrange("p a x -> p (a x)")
                MM(s, PS[3][P_, h * 128:(h + 1) * 128], BK[P_, h, 1, :], arh, reads=['BK1', 'AR0', 'AR1'], writes=B2('B3'),
                   inc=(h == NH - 1 and last))
            for h in range(NH):
                MM(s, PS[4][P_, h * 64:(h + 1) * 64], AR[P_, h, 0, :], BK[P_, h, 0, :], reads=['BK0', 'AR0'], writes=B2('B4'),
                   inc=(h == NH - 1 and last))
        TT(s, 'dve', X1[:], bank(2, 128), K['MU1'][:], ALU.mult, B2('B2') + ['MU1'], ['X1'])
        TT(s, 'dve', X2[:], bank(3, 128), K['MU1'][:], ALU.mult, B2('B3') + ['MU1'], ['X2'])
        TT(s, 'dve', NTb[0][:], bank(4, 64), K['MLS'][:], ALU.mult, B2('B4') + ['MLS'], [('NT', 0)])
        CP(s, 'pool', Nb[0][:], X1[:, :, 0:64], ['X1'], [('N', 0)])
        TT(s, 'pool', Rb[0][:], X1[:, :, 0:64], K['I4'][:], ALU.add, ['X1', 'I4'], [('R', 0)])
        cur = 0
        for lv in range(5):
            nx = 1 - cur
            if lv < 4:
                for hf in range(2):
                    P_ = HS[hf]
                    for h in range(NH):
                        MM(s, PS[5][P_, h * 64:(h + 1) * 64], NTb[cur][P_, h, :], Nb[cur][P_, h, :],
                           reads=[('NT', cur), ('N', cur)], writes=['B5'], inc=(h == NH - 1 and hf == 1))
            for hf in range(2):
                P_ = HS[hf]
                for h in range(NH):
                    MM(s, PS[6][P_, h * 64:(h + 1) * 64], Nb[cur][P_, h, :], NTb[cur][P_, h, :],
                       reads=[('NT', cur), ('N', cur)], writes=['B6'], inc=(h == NH - 1 and hf == 1))
            if lv < 4:
                CP(s, 'act', Nb[nx][:], bank(5, 64), ['B5'], [('N', nx)])
            CP(s, 'dve', NTb[nx][:], bank(6, 64), ['B6'], [('NT', nx)])
            for hf in range(2):
                P_ = HS[hf]
                for h in range(NH):
                    MM(s, PS[7][P_, h * 64:(h + 1) * 64], NTb[nx][P_, h, :], Rb[cur][P_, h, :],
                       reads=[('NT', nx), ('R', cur)], writes=B2('B7'), inc=(h == NH - 1 and hf == 1))
            TT(s, 'dve', Rb[nx][:], Rb[cur][:], bank(7, 64), ALU.add, [('R', cur)] + B2('B7'), [('R', nx)])
            cur = nx
        R = Rb[cur]
        Rk = ('R', cur)
        for hf in range(2):
            P_ = HS[hf]
            Q_ = HS[1 - hf]
            vsl = lambda h: nbuf['Vn'][b][P_, pj, h * 64:(h + 1) * 64]
            for h in range(NH):
                o = PS[2][P_, h * 64:(h + 1) * 64]
                MM(s, o, X2[P_, h, 0:64], vsl(h), start=True, stop=False, reads=['X2', nk('Vn')], writes=[('B2', hf)], inc=False)
                MM(s, o, AR[P_, h, 0, :], S0[P_, h, :], start=False, stop=True, reads=['AR0', ('S0', hf)], writes=[('B2', hf)],
                   inc=(h == NH - 1))
            CP(s, 'dve', Wsb[P_, :, :], PS[2][P_, 0:NH * 64].rearrange("p (h x) -> p h x", h=NH), [('B2', hf)], [('Wsb', hf)])
            for h in range(NH):
                MM(s, PS[3][P_, h * 64:(h + 1) * 64], R[P_, h, :], Wsb[P_, h, :], reads=[Rk, ('Wsb', hf)], writes=[('B3', hf)],
                   inc=(h == NH - 1))
            CP(s, 'dve', Usb[P_, :, :], PS[3][P_, 0:NH * 64].rearrange("p (h x) -> p h x", h=NH), [('B3', hf)], [('Usb', hf)])
            for h in range(NH):
                o = PS[7][Q_, h * 64:(h + 1) * 64]
                MM(s, o, DG[P_, h, :], S0[P_, h, :], start=True, stop=False, reads=['DG', ('S0', hf)], writes=[('B7', 1 - hf)], inc=False)
                MM(s, o, BKn[P_, 0, h * 64:(h + 1) * 64], Usb[P_, h, :], start=False, stop=False, reads=['BKn0', ('Usb', hf)],
                   writes=[('B7', 1 - hf)], inc=False)
                MM(s, o, BKn[P_, 1, h * 64:(h + 1) * 64], vsl(h), start=False, stop=True, reads=['BKn1', nk('Vn')],
                   writes=[('B7', 1 - hf)], inc=(h == NH - 1))
            for h in range(NH):
                o = PS[4][P_, h * 64:(h + 1) * 64]
                MM(s, o, AR[P_, h, 1, :], S0[P_, h, :], start=True, stop=False, reads=['AR1', ('S0', hf)], writes=[('B4', hf)], inc=False)
                MM(s, o, X1[P_, h, 64:128], Usb[P_, h, :], start=False, stop=False, reads=['X1', ('Usb', hf)], writes=[('B4', hf)], inc=False)
                MM(s, o, X2[P_, h, 64:128], vsl(h), start=False, stop=True, reads=['X2', nk('Vn')], writes=[('B4', hf)],
                   inc=(h == NH - 1))
            CP(s, 'dve', S0[Q_, :, :], PS[7][Q_, 0:NH * 64].rearrange("p (h x) -> p h x", h=NH), [('B7', 1 - hf)], [('S0', 1 - hf)])
        CP(s, 'dve', Ysb[:], bank(4, 64), B2('B4'), ['Ysb'])
        RED(s, st[:, 0, :], Ysb[:], ['Ysb'], ['st0'])
        TT(s, 'pool', Ysq[:], Ysb[:], Ysb[:], ALU.mult, ['Ysb'], ['Ysq'])
        RED(s, st[:, 1, :], Ysq[:], ['Ysq'], ['st1'])
        TS(s, 'dve', st[:, 2, :], st[:, 0, :], 1.0 / 64, None, ALU.mult, None, ['st0'], ['st2'])
        TT(s, 'dve', st[:, 3, :], st[:, 2, :], st[:, 2, :], ALU.mult, ['st2'], ['st3'])
        STT(s, st[:, 4, :], st[:, 1, :], 1.0 / 64, st[:, 3, :], ALU.mult, ALU.subtract, ['st1', 'st3'], ['st4'])
        ACTF(s, st[:, 5, :], st[:, 4, :], AF.Ln, ['st4', 'epst'], ['st5'], bias=epst[:], scale=1.0)
        ACTF(s, st[:, 5, :], st[:, 5, :], AF.Exp, ['st5'], ['st5'], scale=-0.5)
        for h in range(NH):
            TS(s, 'dve', Yn[:, h, :], Ysb[:, h, :], st[:, 2, h:h + 1], st[:, 5, h:h + 1], ALU.subtract, ALU.mult,
               ['Ysb', 'st2', 'st5'], ['Yn'])
        ynf = Yn[:].rearrange("p h x -> p (h x)")
        yo = Yo[pi % 2]
        yok = ('Yo', pi % 2)
        TT(s, 'pool', ynf, ynf, K['LGB'][:, 0, :], ALU.mult, ['Yn', 'LGB'], ['Yn'])
        TT(s, 'pool', ynf, ynf, K['LGB'][:, 1, :], ALU.add, ['Yn', 'LGB'], ['Yn'])
        TT(s, 'pool', ynf, ynf, nv('BONn'), ALU.add, ['Yn', nk('BONn')], ['Yn'])
        TT(s, 'pool', yo[:], ynf, nv('Gn'), ALU.mult, ['Yn', nk('Gn')], [yok])
        s.dma('sp', y[pi * 2 * C:(pi + 1) * 2 * C, :], yo[:], ('so', pi % 2), reads=[yok], writes=[('yd', pi % 2)])

    load_group(0)
    for gi in range(ngrp):
        if gi + 1 < ngrp:
            load_group(gi + 1)
        for pj in range(GP):
            pair(gi * GP + pj)
    s.wait_all('sp', [('yd', 0), ('yd', 1)])


def host_consts_0b(lnx_g, lnx_b):
    j = np.arange(64)[:, None]
    x = np.arange(64)[None, :]
    su = (x > j).astype(np.float32)
    iu = (x >= j).astype(np.float32)
    sl = (x < j).astype(np.float32)
    two = lambda a: np.concatenate([a, a], axis=0)
    MU1 = two(np.broadcast_to(np.concatenate([su, iu], 1)[:, None, :], (64, NH, 128)))
    MLS = two(np.broadcast_to(sl[:, None, :], (64, NH, 64)))
    I4 = two(np.broadcast_to(np.eye(64, dtype=np.float32)[:, None, :], (64, NH, 64)))
    TRI = two(np.concatenate([iu, su], 1))
    UPS = two(sl)
    LGB = np.broadcast_to(np.stack([lnx_g, lnx_b])[None], (128, 2, NH * 64))
    return {k: np.ascontiguousarray(v, dtype=np.float32) for k, v in
            dict(MU1=MU1, MLS=MLS, I4=I4, TRI=TRI, UPS=UPS, LGB=LGB).items()}


def host_inputs_0b(arrT, lnx_g, lnx_b):
    T = arrT['oR'].shape[1]
    tr = lambda a: np.ascontiguousarray(a.reshape(NH, 64, T).transpose(1, 0, 2))
    na = lambda a: np.ascontiguousarray(a.T)
    d = dict(RT=tr(arrT['oR']), KT=tr(arrT['oK']), AT=tr(arrT['oA']), BT=tr(arrT['oB']),
             Wn=na(arrT['oW']), Bn=na(arrT['oB']), Kn=na(arrT['oK']), Vn=na(arrT['oV']),
             BONn=na(arrT['oBon']), Gn=na(arrT['oG']))
    d.update(host_consts_0b(lnx_g, lnx_b))
    return d


D = 1024
NE = 32
GE = 4
ALPHA = (2.0 * 2) ** 0.25
LN_EPS = 1e-5


def build_C(ntok=4096, st=1024, level=9):
    nc = bass.Bass('TRN2', target_bir_lowering=False)
    dt = nc.dram_tensor
    xin = dt("xin", [ntok, D], F32, kind="ExternalInput").ap()
    ymT = dt("ymT", [D, ntok], F32, kind="ExternalInput").ap()
    w_out = dt("w_out", [D, D], F32, kind="ExternalInput").ap()
    wr = dt("wr", [D, 36], F32, kind="ExternalInput").ap()
    br = dt("br", [1, 36], F32, kind="ExternalInput").ap()
    lnp = dt("lnp", [128, 4, D], F32, kind="ExternalInput").ap()
    ident_d = dt("ident", [128, 128], F32, kind="ExternalInput").ap()
    sel_d = dt("sel", [32, NE * 128], F32, kind="ExternalInput").ap()
    e_w1 = dt("e_w1", [NE, D, 128], F32, kind="ExternalInput").ap()
    e_w3 = dt("e_w3", [NE, D, 128], F32, kind="ExternalInput").ap()
    e_w2 = dt("e_w2", [NE, 128, D], F32, kind="ExternalInput").ap()
    xout = dt("xout", [ntok, D], F32, kind="ExternalOutput").ap()
    s = Sched(nc)
    emit_C(s, xin, ymT, w_out, wr, br, lnp, ident_d, sel_d, e_w1, e_w3, e_w2, xout, ntok, st, level)
    s.build()
    return nc


def emit_C(s, xin, ymT, w_out, wr, br, lnp, ident_d, sel_d, e_w1, e_w3, e_w2, xout, ntok, st, level=9):
    wout_bf = s.sbuf('wout_bf', [128, 8, D], BF16)
    wr_f = s.sbuf('wr_f', [128, 8, 36], F32)
    br_f = s.sbuf('br_f', [1, 36], F32)
    ones1 = s.sbuf('ones1', [1, 128], F32)
    LNP = s.sbuf('LNP', [128, 4, D], F32)
    ident = s.sbuf('ident', [128, 128], F32)
    sel = s.sbuf('sel', [32, NE * 128], BF16)
    for k in range(8):
        s.dma('pool', wout_bf[:, k, :], w_out[k * 128:(k + 1) * 128, :], 'c0', writes=[('wout_bf', k)])
    s.dma('sp', wr_f[:], wr.rearrange("(k p) n -> p k n", p=128), 'c1', writes=['wr_f'])
    s.dma('sp', br_f[:], br, 'c2', writes=['br_f'])
    for a in range(4):
        s.dma('sp', LNP[:, a, :], lnp[:, a, :], ('c3', a), writes=[('LNP', a)])
    s.dma('sp', ident[:], ident_d, 'c4', writes=['ident'])
    s.dma('pool', sel[:], sel_d, 'c5', writes=['sel'])
    s.op('dve', lambda e: e.memset(ones1[:], 1.0), writes=['ones1'])
    epst = s.sbuf('epst', [128, 1], F32)
    s.op('dve', lambda e: e.memset(epst[:], LN_EPS), writes=['epst'])
    WOK = [('wout_bf', k) for k in range(8)]

    nst = ntok // st
    nsub = st // 128
    ntt = st // 256
    F = s.sbuf('F', [128, nsub, D], F32)
    xaT = s.sbuf('xaT', [128, 8, st], BF16)
    gT = s.sbuf('gT', [32, st], BF16)
    P = [s.psum('P%d' % i, [128, 512]) for i in range(8)]
    ym_bf = [s.sbuf('ym_bf%d' % i, [128, 8, 128], BF16) for i in range(2)]
    xin_t = [s.sbuf('xin_t%d' % i, [128, D], F32) for i in range(2)]
    z_t = [s.sbuf('z_t%d' % i, [128, D], F32) for i in range(2)]
    xa_t = [s.sbuf('xa_t%d' % i, [128, D], F32) for i in range(2)]
    xaTf = [s.sbuf('xaTf%d' % i, [128, 8, 128], F32) for i in range(2)]
    st6 = [s.sbuf('st6_%d' % i, [128, 2, 6], F32) for i in range(2)]
    mv = [s.sbuf('mv%d' % i, [128, 2], F32) for i in range(2)]
    rstd = [s.sbuf('rstd%d' % i, [128, 1], F32) for i in range(2)]
    nmr = [s.sbuf('nmr%d' % i, [128, 1], F32) for i in range(2)]
    lg = [s.sbuf('lg%d' % i, [128, 36], F32) for i in range(2)]
    rt = [s.sbuf('rt%d' % i, [128, 16], F32) for i in range(2)]
    r8 = [s.sbuf('r8_%d' % i, [128, 5, 8], F32) for i in range(2)]
    gates = [s.sbuf('gates%d' % i, [128, 32], F32) for i in range(2)]
    W1g = [s.sbuf('W1g%d' % i, [128, GE, 8, 128], BF16) for i in range(2)]
    W3g = [s.sbuf('W3g%d' % i, [128, GE, 8, 128], BF16) for i in range(2)]
    W2g = [s.sbuf('W2g%d' % i, [128, GE, D], BF16) for i in range(2)]
    s1 = [s.sbuf('s1_%d' % i, [128, 256], F32) for i in range(2)]
    uu = [s.sbuf('uu_%d' % i, [128, 256], F32) for i in range(2)]
    hg = [s.sbuf('hg_%d' % i, [128, 256], BF16) for i in range(2)]
    o_t = [s.sbuf('o_t%d' % i, [128, D], F32) for i in range(2)]

    def layer_norm_tile(src, srck, dst, dstk, gi, pi, eng_gb='pool'):
        st_, mv_, rs_, nm_ = st6[pi], mv[pi], rstd[pi], nmr[pi]
        for hh in range(2):
            s.op('dve', lambda e, hh=hh: e.bn_stats(out=st_[:, hh, :], in_=src[:, hh * 512:(hh + 1) * 512]),
                 reads=[srck], writes=[('st6', pi, hh)])
        s.op('dve', lambda e: e.bn_aggr(out=mv_[:], in_=st_[:].rearrange("p a b -> p (a b)")),
             reads=[('st6', pi, 0), ('st6', pi, 1)], writes=[('mv', pi)])
        s.op('act', lambda e: e.activation(out=rs_[:], in_=mv_[:, 1:2], func=AF.Ln, bias=epst[:], scale=1.0),
             reads=[('mv', pi), 'epst'], writes=[('rstd', pi)])
        s.op('act', lambda e: e.activation(out=rs_[:], in_=rs_[:], func=AF.Exp, scale=-0.5),
             reads=[('rstd', pi)], writes=[('rstd', pi)])
        s.op('dve', lambda e: e.scalar_tensor_tensor(out=nm_[:], in0=mv_[:, 0:1], scalar=-1.0, in1=rs_[:],
                                                     op0=ALU.mult, op1=ALU.mult),
             reads=[('mv', pi), ('rstd', pi)], writes=[('nmr', pi)])
        s.op('act', lambda e: e.activation(out=dst[:], in_=src[:], func=AF.Identity, bias=nm_[:], scale=rs_[:]),
             reads=[srck, ('rstd', pi), ('nmr', pi)], writes=[dstk])
        s.op(eng_gb, lambda e: e.tensor_tensor(out=dst[:], in0=dst[:], in1=LNP[:, gi, :], op=ALU.mult),
             reads=[dstk, ('LNP', gi)], writes=[dstk])
        s.op(eng_gb, lambda e: e.tensor_tensor(out=dst[:], in0=dst[:], in1=LNP[:, gi + 1, :], op=ALU.add),
             reads=[dstk, ('LNP', gi + 1)], writes=[dstk])

    wl_cnt = [0]

    def load_group(gi):
        b = wl_cnt[0] % 2
        wl_cnt[0] += 1
        for el in range(GE):
            ex = gi * GE + el
            s.dma('pool', W1g[b][:, el, :, :], e_w1[ex].rearrange("(k p) j -> p k j", p=128), ('w1', b, el),
                  writes=[('W1g', b, el)])
            s.dma('pool', W3g[b][:, el, :, :], e_w3[ex].rearrange("(k p) j -> p k j", p=128), ('w3', b, el),
                  writes=[('W3g', b, el)])
            s.dma('pool', W2g[b][:, el, :], e_w2[ex], ('w2', b, el), writes=[('W2g', b, el)])
        return b

    ngrp = NE // GE
    for sti in range(nst):
        t0 = sti * st
        for sub in range(nsub):
            pi = sub % 2
            tok0 = t0 + sub * 128
            yb = ym_bf[pi]
            ybk = ('ym_bf', pi)
            xt, zt, xat = xin_t[pi], z_t[pi], xa_t[pi]
            s.dma('pool', yb[:], ymT[:, tok0:tok0 + 128].rearrange("(k p) t -> p k t", p=128), ('ym', pi), writes=[ybk])
            s.dma('sp', xt[:], xin[tok0:tok0 + 128, :], ('xin', pi), writes=[('xin_t', pi)])
            for hh in range(2):
                pb = P[2 * pi + hh]
                pk = ('P', 2 * pi + hh)
                for k in range(8):
                    s.op('pe', lambda e, pb=pb, k=k, hh=hh, yb=yb: e.matmul(
                        pb[:], lhsT=yb[:, k, :], rhs=wout_bf[:, k, hh * 512:(hh + 1) * 512],
                        start=(k == 0), stop=(k == 7)), reads=[ybk] + WOK, writes=[pk], inc=(k == 7))
                s.op('dve', lambda e, pb=pb, hh=hh, xt=xt, zt=zt: e.scalar_tensor_tensor(
                    out=zt[:, hh * 512:(hh + 1) * 512], in0=xt[:, hh * 512:(hh + 1) * 512], scalar=ALPHA,
                    in1=pb[:], op0=ALU.mult, op1=ALU.add), reads=[pk, ('xin_t', pi)], writes=[('z_t', pi)])
            if level < 0.5:
                s.op('act', lambda e, sub=sub, zt=zt: e.copy(out=F[:, sub, :], in_=zt[:]),
                     reads=[('z_t', pi)], writes=[('F', sub)])
                continue
            layer_norm_tile(zt, ('z_t', pi), xat, ('xa_t', pi), 0, pi)
            s.op('act', lambda e, sub=sub, xat=xat: e.mul(out=F[:, sub, :], in_=xat[:], mul=ALPHA),
                 reads=[('xa_t', pi)], writes=[('F', sub)])
            if level < 2:
                continue
            xf = xaTf[pi]
            for k in range(8):
                pb = P[4 + 2 * pi + k // 4]
                pk = ('P', 4 + 2 * pi + k // 4)
                s.op('pe', lambda e, pb=pb, k=k, xat=xat: e.transpose(
                    out=pb[:, (k % 4) * 128:(k % 4 + 1) * 128], in_=xat[:, k * 128:(k + 1) * 128], identity=ident[:]),
                    reads=[('xa_t', pi), 'ident'], writes=[pk])
            for hb in range(2):
                pb = P[4 + 2 * pi + hb]
                pk = ('P', 4 + 2 * pi + hb)
                s.op('act', lambda e, pb=pb, hb=hb, xf=xf: e.copy(
                    out=xf[:, hb * 4:(hb + 1) * 4, :], in_=pb[:].rearrange("p (k t) -> p k t", k=4)),
                    reads=[pk], writes=[('xaTf', pi, hb)])
                s.op('pool', lambda e, hb=hb, sub=sub, xf=xf: e.tensor_copy(
                    out=xaT[:, hb * 4:(hb + 1) * 4, sub * 128:(sub + 1) * 128], in_=xf[:, hb * 4:(hb + 1) * 4, :]),
                    reads=[('xaTf', pi, hb)], writes=[('xaT', sub)])
            if level < 3:
                continue
            pb = P[4 + 2 * pi]
            pk = ('P', 4 + 2 * pi)
            for k in range(8):
                s.op('pe', lambda e, pb=pb, k=k, xf=xf: e.matmul(
                    pb[:, 0:36], lhsT=xf[:, k, :], rhs=wr_f[:, k, :], start=(k == 0), stop=False),
                    reads=[('xaTf', pi, k // 4), 'wr_f'], writes=[pk], inc=False)
            s.op('pe', lambda e, pb=pb: e.matmul(pb[:, 0:36], lhsT=ones1[:], rhs=br_f[:], start=False, stop=True),
                 reads=['ones1', 'br_f'], writes=[pk])
            L = lg[pi]
            Lk = ('lg', pi)
            R = rt[pi]
            Rk = ('rt', pi)
            R8 = r8[pi]
            R8k = ('r8', pi)
            G = gates[pi]
            s.op('act', lambda e, pb=pb, L=L: e.copy(out=L[:], in_=pb[:, 0:36]), reads=[pk], writes=[Lk])
            if level < 4:
                continue
            V = lambda fn, rd, wr_: s.op('dve', fn, reads=rd, writes=wr_)
            V(lambda e, L=L, R=R: e.reduce_max(out=R[:, 0:1], in_=L[:, 0:4], axis=AX.X), [Lk], [Rk])
            V(lambda e, L=L, R=R: e.tensor_scalar(out=R[:, 4:8], in0=L[:, 0:4], scalar1=R[:, 0:1], scalar2=None,
                                                  op0=ALU.is_ge), [Lk, Rk], [Rk])
            V(lambda e, R=R: e.tensor_scalar(out=R[:, 1:2], in0=R[:, 0:1], scalar1=-1.0, scalar2=None, op0=ALU.mult),
              [Rk], [Rk])
            s.op('act', lambda e, L=L, R=R: e.activation(out=R[:, 8:12], in_=L[:, 0:4], func=AF.Exp,
                                                         bias=R[:, 1:2], scale=1.0), reads=[Lk, Rk], writes=[Rk])
            V(lambda e, R=R: e.reduce_sum(out=R[:, 2:3], in_=R[:, 8:12], axis=AX.X), [Rk], [Rk])
            V(lambda e, L=L, R=R, R8=R8: e.tensor_scalar(out=R8[:, 0, :], in0=L[:, 4:12], scalar1=R[:, 4:5],
                                                         scalar2=None, op0=ALU.mult), [Lk, Rk], [R8k])
            for g in range(1, 4):
                V(lambda e, L=L, R=R, R8=R8, g=g: e.scalar_tensor_tensor(
                    out=R8[:, 0, :], in0=L[:, 4 + 8 * g:12 + 8 * g], scalar=R[:, 4 + g:5 + g], in1=R8[:, 0, :],
                    op0=ALU.mult, op1=ALU.add), [Lk, Rk, R8k], [R8k])
            V(lambda e, R8=R8: e.max(out=R8[:, 1, :], in_=R8[:, 0, :]), [R8k], [R8k])
            V(lambda e, R8=R8: e.tensor_scalar(out=R8[:, 2, :], in0=R8[:, 0, :], scalar1=R8[:, 1, 1:2], scalar2=None,
                                               op0=ALU.is_ge), [R8k], [R8k])
            V(lambda e, R=R, R8=R8: e.tensor_scalar(out=R[:, 12:13], in0=R8[:, 1, 0:1], scalar1=-1.0, scalar2=None,
                                                    op0=ALU.mult), [R8k, Rk], [Rk])
            s.op('act', lambda e, R=R, R8=R8: e.activation(out=R8[:, 3, :], in_=R8[:, 0, :], func=AF.Exp,
                                                           bias=R[:, 12:13], scale=1.0), reads=[R8k, Rk], writes=[R8k])
            V(lambda e, R8=R8: e.tensor_tensor(out=R8[:, 4, :], in0=R8[:, 3, :], in1=R8[:, 2, :], op=ALU.mult),
              [R8k], [R8k])
            V(lambda e, R=R, R8=R8: e.reduce_sum(out=R[:, 13:14], in_=R8[:, 4, :], axis=AX.X), [R8k, Rk], [Rk])
            V(lambda e, R=R: e.tensor_tensor(out=R[:, 14:15], in0=R[:, 13:14], in1=R[:, 2:3], op=ALU.mult), [Rk], [Rk])
            V(lambda e, R=R: e.reciprocal(out=R[:, 15:16], in_=R[:, 14:15]), [Rk], [Rk])
            V(lambda e, R=R, R8=R8: e.tensor_scalar(out=R8[:, 3, :], in0=R8[:, 4, :], scalar1=R[:, 15:16],
                                                    scalar2=None, op0=ALU.mult), [R8k, Rk], [R8k])
            for g in range(4):
                V(lambda e, R=R, R8=R8, G=G, g=g: e.tensor_scalar(
                    out=G[:, 8 * g:8 * g + 8], in0=R8[:, 3, :], scalar1=R[:, 4 + g:5 + g], scalar2=None,
                    op0=ALU.mult), [R8k, Rk], [('gates', pi)])
            if level < 6:
                if level >= 5:
                    s.op('act', lambda e, sub=sub, G=G: e.copy(out=F[:, sub, 0:32], in_=G[:]),
                         reads=[('gates', pi), ('F', sub)], writes=[('F', sub)])
                continue
            pb2 = P[5 + 2 * pi]
            pk2 = ('P', 5 + 2 * pi)
            s.op('pe', lambda e, pb2=pb2, G=G: e.transpose(out=pb2[0:32, 0:128], in_=G[:], identity=ident[:]),
                 reads=[('gates', pi), 'ident'], writes=[pk2])
            s.op('act', lambda e, pb2=pb2, sub=sub: e.copy(out=gT[:, sub * 128:(sub + 1) * 128],
                                                          in_=pb2[0:32, 0:128]),
                 reads=[pk2], writes=[('gT', sub)])

        def moe_front(gi, b, tt, el, hb):
            c0 = tt * 256
            ex = gi * GE + el
            p1 = P[4 + 3 * hb]
            p1k = ('P', 4 + 3 * hb)
            p3 = P[5 + hb]
            p3k = ('P', 5 + hb)
            xk = [('xaT', 2 * tt), ('xaT', 2 * tt + 1)]
            for k in range(8):
                s.op('pe', lambda e, k=k: e.matmul(p1[:, 0:256], lhsT=W1g[b][:, el, k, :], rhs=xaT[:, k, c0:c0 + 256],
                                                   start=(k == 0), stop=(k == 7)),
                     reads=[('W1g', b, el)] + xk, writes=[p1k], inc=(k == 7))
            for k in range(8):
                s.op('pe', lambda e, k=k: e.matmul(p3[:, 0:256], lhsT=W3g[b][:, el, k, :], rhs=xaT[:, k, c0:c0 + 256],
                                                   start=(k == 0), stop=(k == 7)),
                     reads=[('W3g', b, el)] + xk, writes=[p3k], inc=False)
            s.op('pe', lambda e: e.matmul(p3[:, 256:512], lhsT=sel[:, ex * 128:(ex + 1) * 128], rhs=gT[:, c0:c0 + 256],
                                          start=True, stop=True), reads=['sel', ('gT', 2 * tt), ('gT', 2 * tt + 1)], writes=[p3k])
            s.op('act', lambda e: e.activation(out=s1[hb][:], in_=p1[:, 0:256], func=AF.Silu), reads=[p1k], writes=[('s1', hb)])
            s.op('dve', lambda e: e.tensor_tensor(out=uu[hb][:], in0=s1[hb][:], in1=p3[:, 256:512], op=ALU.mult),
                 reads=[('s1', hb), p3k], writes=[('uu', hb)])
            s.op('dve', lambda e: e.tensor_tensor(out=hg[hb][:], in0=uu[hb][:], in1=p3[:, 0:256], op=ALU.mult),
                 reads=[('uu', hb), p3k], writes=[('hg', hb)])

        def moe_back(gi, b, tt, el, hb):
            for sb in range(2):
                for hh in range(2):
                    pa = P[2 * sb + hh]
                    pak = ('P', 2 * sb + hh)
                    s.op('pe', lambda e, pa=pa, sb=sb, hh=hh: e.matmul(
                        pa[:], lhsT=hg[hb][:, sb * 128:(sb + 1) * 128], rhs=W2g[b][:, el, hh * 512:(hh + 1) * 512],
                        start=(el == 0), stop=(el == GE - 1)),
                        reads=[('hg', hb), ('W2g', b, el)], writes=[pak], inc=(sb == 1 and hh == 1))
            if el == GE - 1:
                for sb in range(2):
                    sub = 2 * tt + sb
                    for hh in range(2):
                        pa = P[2 * sb + hh]
                        pak = ('P', 2 * sb + hh)
                        s.op('dve', lambda e, pa=pa, sub=sub, hh=hh: e.tensor_tensor(
                            out=F[:, sub, hh * 512:(hh + 1) * 512], in0=F[:, sub, hh * 512:(hh + 1) * 512], in1=pa[:],
                            op=ALU.add), reads=[pak, ('F', sub)], writes=[('F', sub)])

        steps = []
        for gi in range(ngrp if level >= 7 else 0):
            for tt in range(ntt):
                for el in range(GE):
                    steps.append((gi, tt, el))
        pend = None
        gb = {}
        for n_, (gi, tt, el) in enumerate(steps):
            if gi not in gb:
                gb[gi] = load_group(gi)
            cur_ = (gi, gb[gi], tt, el, n_ % 2)
            moe_front(*cur_)
            if pend is not None:
                moe_back(*pend)
            pend = cur_
        if pend is not None:
            moe_back(*pend)
        for sub in range(nsub):
            pi = sub % 2
            ot = o_t[pi]
            if level < 0.3 or (5 <= level < 6):
                s.op('act', lambda e, sub=sub, ot=ot: e.copy(out=ot[:], in_=F[:, sub, :]),
                     reads=[('F', sub)], writes=[('o_t', pi)])
            else:
                layer_norm_tile(F[:, sub, :], ('F', sub), ot, ('o_t', pi), 2, pi)
            s.dma('sp', xout[t0 + sub * 128: t0 + (sub + 1) * 128, :], ot[:], ('st', pi), reads=[('o_t', pi)],
                  writes=[('xout', pi)])
    s.wait_all('sp', [('xout', 0), ('xout', 1)])


def host_inputs_C(xin, ymT, w_out, g1, b1, rg_w, rg_b, re_w, re_b, w1, w3, w2, g2, b2):
    wr = np.ascontiguousarray(np.concatenate([rg_w, re_w], axis=1))
    br = np.ascontiguousarray(np.concatenate([rg_b, re_b])[None, :])
    lnp = np.ascontiguousarray(np.broadcast_to(np.stack([g1, b1, g2, b2])[None], (128, 4, D)))
    ident = np.eye(128, dtype=np.float32)
    sel = np.zeros((32, NE, 128), np.float32)
    for e in range(NE):
        sel[e, e, :] = 1.0
    return dict(xin=np.ascontiguousarray(xin), ymT=np.ascontiguousarray(ymT), w_out=np.ascontiguousarray(w_out),
                wr=wr, br=br, lnp=lnp, ident=ident, sel=sel.reshape(32, NE * 128),
                e_w1=np.ascontiguousarray(w1), e_w3=np.ascontiguousarray(w3), e_w2=np.ascontiguousarray(w2))


HALO1 = 32
CONV_W = 31
LN_EPS1 = 1e-5


def build_1a(ntok=4096, tile=512):
    nc = bass.Bass('TRN2', target_bir_lowering=False)
    dt = nc.dram_tensor
    xT = dt("xT", [1024, HALO1 + ntok], F32, kind="ExternalInput").ap()
    w_ext = dt("w_ext", [1024, 4096], F32, kind="ExternalInput").ap()
    ccols = dt("ccols", [128, 4, 34], F32, kind="ExternalInput").ap()
    rot = dt("rot", [128, 2, ntok], F32, kind="ExternalInput").ap()
    ones_d = dt("ones", [128, 128], F32, kind="ExternalInput").ap()
    outs = {n: dt(n, [512, ntok], F32, kind="ExternalOutput").ap() for n in ('oYC', 'oQ', 'oKr', 'oV', 'oGr')}
    s = Sched(nc)
    emit_1a(s, xT, w_ext, ccols, rot, ones_d, outs, ntok, tile)
    s.build()
    return nc


def emit_1a(s, xT, w_ext, ccols_d, rot_d, ones_d, outs, ntok, T):
    W = s.sbuf('W', [128, 8, 4096], BF16)
    for k in range(8):
        for hf in range(2):
            s.dma('pool', W[:, k, hf * 2048:(hf + 1) * 2048], w_ext[k * 128:(k + 1) * 128, hf * 2048:(hf + 1) * 2048], 'w0',
                  writes=[('W', k, hf)])
    WK = [('W', k, hf) for k in range(8) for hf in range(2)]
    CC = s.sbuf('CC', [128, 4, 34], F32)
    s.dma('sp', CC[:], ccols_d, 'c0', writes=['CC'])
    ONES = s.sbuf('ONES', [128, 128], F32)
    s.dma('sp', ONES[:], ones_d, 'c1', writes=['ONES'])
    epst = s.sbuf('epst', [128, 1], F32)
    s.op('dve', lambda e: e.memset(epst[:], LN_EPS1), writes=['epst'])
    P = [s.psum('P%d' % i, [128, 512]) for i in range(8)]
    xb = [s.sbuf('xb%d' % i, [128, 8, T], BF16) for i in range(2)]
    xh = s.sbuf('xh', [128, 8, HALO1], BF16)
    CA = s.sbuf('CA', [128, T], F32)
    SG = s.sbuf('SG', [128, T], F32)
    CAH = s.sbuf('CAH', [128, 4, HALO1], F32)
    Ub = [s.sbuf('U%d' % i, [128, 4, HALO1 + T], F32) for i in range(2)]
    ACC = s.sbuf('ACC', [128, 4, T], F32)
    SQ = s.sbuf('SQ', [128, T], F32)
    XP = s.sbuf('XP', [128, 16, T], F32)
    ROT = [s.sbuf('ROT%d' % i, [128, 2, T], F32) for i in range(2)]
    MEAN = s.sbuf('MEAN', [128, T], F32)
    MSQ = s.sbuf('MSQ', [128, T], F32)
    RSTD = s.sbuf('RSTD', [128, T], F32)
    t1 = [s.sbuf('t1_%d' % i, [128, T], F32) for i in range(2)]
    t2 = [s.sbuf('t2_%d' % i, [128, T], F32) for i in range(2)]
    OB = {n: [s.sbuf('O_%s%d' % (n, i), [128, T], F32) for i in range(2)] for n in ('oYC', 'oQ', 'oKr', 'oV', 'oGr')}
    ocnt = {n: 0 for n in OB}

    def out_buf(n):
        i = ocnt[n] % 2
        ocnt[n] += 1
        return OB[n][i], (n, i)

    def proj(c, x_, XK, ncol, pb, pk):
        for k in range(8):
            MM(s, pb[:, 0:ncol], W[:, k, c * 128:(c + 1) * 128], x_[:, k, :], start=(k == 0), stop=(k == 7),
               reads=XK + WK, writes=[pk], inc=(k == 7))

    s.dma('pool', xh[:], xT[:, 0:HALO1].rearrange("(k p) t -> p k t", p=128), 'xh', writes=['xh'])
    for c in range(8):
        pb, pk = P[c % 2], ('P', c % 2)
        proj(c, xh, ['xh'], HALO1, pb, pk)
        if c < 4:
            CP(s, 'act', CAH[:, c, :], pb[:, 0:HALO1], [pk], [('CAH', c)])
        else:
            ACTF(s, SG[:, 0:HALO1], pb[:, 0:HALO1], AF.Sigmoid, [pk], ['SG'])
            TT(s, 'dve', Ub[0][:, c - 4, 0:HALO1], CAH[:, c - 4, :], SG[:, 0:HALO1], ALU.mult, [('CAH', c - 4), 'SG'], [('U', 0, c - 4)])

    def tile_body(ti):
        t0 = ti * T
        x_ = xb[ti % 2]
        XK = [('xb', ti % 2, k) for k in range(8)]
        for k in range(8):
            s.dma('pool', x_[:, k, :], xT[k * 128:(k + 1) * 128, HALO1 + t0:HALO1 + t0 + T], ('x', ti % 2), writes=[XK[k]])
        rt = ROT[ti % 2]
        rk = ('ROT', ti % 2)
        s.dma('sp', rt[:], rot_d[:, :, t0:t0 + T], ('rot', ti % 2), writes=[rk])
        tsl = slice(t0, t0 + T)
        ub = ti % 2
        U = Ub[ub]
        for cc in range(4):
            pb, pk = P[0], ('P', 0)
            proj(cc, x_, XK, T, pb, pk)
            CP(s, 'act', CA[:], pb[:], [pk], ['CA'])
            pb, pk = P[1], ('P', 1)
            proj(4 + cc, x_, XK, T, pb, pk)
            ACTF(s, SG[:], pb[:], AF.Sigmoid, [pk], ['SG'])
            TT(s, 'pool', U[:, cc, HALO1:HALO1 + T], CA[:], SG[:], ALU.mult, ['CA', 'SG'], [('U', ub, cc)])
        for cc in range(4):
            CP(s, 'pool', Ub[1 - ub][:, cc, 0:HALO1], U[:, cc, T:T + HALO1], [('U', ub, cc)], [('U', 1 - ub, cc)])
        for cc in range(4):
            TS(s, 'dve', ACC[:, cc, :], U[:, cc, 2:2 + T], CC[:, cc, 0:1], CC[:, cc, 31:32], ALU.mult, ALU.add,
               [('U', ub, cc), 'CC'], [('ACC', cc)])
            for j in range(1, CONV_W):
                STT(s, ACC[:, cc, :], U[:, cc, 2 + j:2 + j + T], CC[:, cc, j:j + 1], ACC[:, cc, :], ALU.mult, ALU.add,
                    [('U', ub, cc), 'CC', ('ACC', cc)], [('ACC', cc)])
        order = [(8 + i, i, 1.0) for i in range(4)] + [(24 + i, 8 + i, 1.0) for i in range(4)] + \
                [(12 + i, 4 + i, 0.125) for i in range(4)] + [(28 + i, 12 + i, 0.125) for i in range(4)]
        for n_, (c, slot, sc) in enumerate(order):
            pb, pk = P[n_ % 2], ('P', n_ % 2)
            proj(c, x_, XK, T, pb, pk)
            s.op('act', lambda e, pb=pb, slot=slot, sc=sc: e.mul(out=XP[:, slot, :], in_=pb[:], mul=sc), reads=[pk], writes=[('XP', slot)])
        for i in range(4):
            rows = slice(i * 128, (i + 1) * 128)
            pb, pk = P[0], ('P', 0)
            proj(16 + i, x_, XK, T, pb, pk)
            ob, obk = out_buf('oV')
            CP(s, 'act', ob[:], pb[:], [pk], [obk])
            s.dma('sp', outs['oV'][rows, tsl], ob[:], ('so',) + obk, reads=[obk], writes=[('d',) + obk])
            pb, pk = P[1], ('P', 1)
            proj(20 + i, x_, XK, T, pb, pk)
            ob, obk = out_buf('oGr')
            ACTF(s, ob[:], pb[:], AF.Silu, [pk], [obk])
            s.dma('sp', outs['oGr'][rows, tsl], ob[:], ('so',) + obk, reads=[obk], writes=[('d',) + obk])
        for i in range(8):
            name = 'oQ' if i < 4 else 'oKr'
            rows = slice((i % 4) * 128, (i % 4 + 1) * 128)
            a, b = t1[i % 2], t2[i % 2]
            TT(s, 'pool', a[:], XP[:, i, :], rt[:, 0, :], ALU.mult, [('XP', i), rk], [('t1', i % 2)])
            TT(s, 'pool', b[:], XP[:, 8 + i, :], rt[:, 1, :], ALU.mult, [('XP', 8 + i), rk], [('t2', i % 2)])
            ob, obk = out_buf(name)
            TT(s, 'pool', ob[:], a[:], b[:], ALU.add, [('t1', i % 2), ('t2', i % 2)], [obk])
            s.dma('sp', outs[name][rows, tsl], ob[:], ('so',) + obk, reads=[obk], writes=[('d',) + obk])
        for cc in range(4):
            MM(s, P[2][:], ONES[:], ACC[:, cc, :], start=(cc == 0), stop=(cc == 3), reads=['ONES', ('ACC', cc)], writes=[('P', 2)],
               inc=(cc == 3))
        for cc in range(4):
            s.op('act', lambda e, cc=cc: e.activation(out=SQ[:], in_=ACC[:, cc, :], func=AF.Square), reads=[('ACC', cc)], writes=['SQ'])
            MM(s, P[3][:], ONES[:], SQ[:], start=(cc == 0), stop=(cc == 3), reads=['ONES', 'SQ'], writes=[('P', 3)])
        s.op('act', lambda e: e.mul(out=MEAN[:], in_=P[2][:], mul=1.0 / 512), reads=[('P', 2)], writes=['MEAN'])
        TT(s, 'pool', MSQ[:], MEAN[:], MEAN[:], ALU.mult, ['MEAN'], ['MSQ'])
        STT(s, RSTD[:], P[3][:], 1.0 / 512, MSQ[:], ALU.mult, ALU.subtract, [('P', 3), 'MSQ'], ['RSTD'])
        ACTF(s, RSTD[:], RSTD[:], AF.Ln, ['RSTD', 'epst'], ['RSTD'], bias=epst[:], scale=1.0)
        ACTF(s, RSTD[:], RSTD[:], AF.Exp, ['RSTD'], ['RSTD'], scale=-0.5)
        for cc in range(4):
            rows = slice(cc * 128, (cc + 1) * 128)
            a = t1[cc % 2]
            TT(s, 'pool', a[:], ACC[:, cc, :], MEAN[:], ALU.subtract, [('ACC', cc), 'MEAN'], [('t1', cc % 2)])
            TT(s, 'pool', a[:], a[:], RSTD[:], ALU.mult, [('t1', cc % 2), 'RSTD'], [('t1', cc % 2)])
            ob, obk = out_buf('oYC')
            ACTF(s, ob[:], a[:], AF.Silu, [('t1', cc % 2), 'CC'], [obk], bias=CC[:, cc, 33:34], scale=CC[:, cc, 32:33])
            s.dma('sp', outs['oYC'][rows, tsl], ob[:], ('so',) + obk, reads=[obk], writes=[('d',) + obk])

    for ti in range(ntok // T):
        tile_body(ti)
    s.wait_all('sp', [k for k in s.last_w if isinstance(k, tuple) and k[0] == 'd'])


def rot_perm():
    idx = np.arange(512).reshape(8, 2, 32)[:, ::-1, :].reshape(-1)
    return idx


def host_inputs_1a(xT_halo, w_in, conv_w, conv_b, cln_g, cln_b, pos0, ntok=4096):
    perm = rot_perm()
    w_ext = np.concatenate([w_in, w_in[:, 1024:1536][:, perm], w_in[:, 1536:2048][:, perm]], axis=1)
    ccols = np.zeros((128, 4, 34), np.float32)
    ccols[:, :, 0:31] = conv_w.T.reshape(4, 128, 31).transpose(1, 0, 2)
    ccols[:, :, 31] = conv_b.reshape(4, 128).T
    ccols[:, :, 32] = cln_g.reshape(4, 128).T
    ccols[:, :, 33] = cln_b.reshape(4, 128).T
    half = 32
    inv = (np.float32(10000.0) ** (-np.arange(half, dtype=np.float32) / np.float32(half))).astype(np.float32)
    pos = np.arange(pos0, pos0 + ntok, dtype=np.float32)
    ang = (pos[:, None] * inv[None, :]).astype(np.float32)
    cos, sin = np.cos(ang).astype(np.float32), np.sin(ang).astype(np.float32)
    p = np.arange(128)
    i = p % 32
    second = (p % 64) >= 32
    rot = np.empty((128, 2, ntok), np.float32)
    rot[:, 0, :] = cos.T[i]
    rot[:, 1, :] = np.where(second[:, None], sin.T[i], -sin.T[i])
    return dict(xT=np.ascontiguousarray(xT_halo), w_ext=np.ascontiguousarray(w_ext), ccols=ccols, rot=rot,
                ones=np.ones((128, 128), np.float32))


RC = 128
RH = 4
RET_EPS = 1e-5


def build_1b(T=8192, GCH=2):
    nc = bass.Bass('TRN2', target_bir_lowering=False)
    dt = nc.dram_tensor
    tin = {n: dt(n, [64, RH, T], F32, kind="ExternalInput").ap() for n in ('QT', 'KT')}
    nin = {n: dt(n, [T, RH * 64], F32, kind="ExternalInput").ap() for n in ('Kn', 'Vn', 'Grn')}
    cst = {n: dt(n, shp, F32, kind="ExternalInput").ap() for n, shp in
           (('DM', [128, RH, 128]), ('XI', [64, RH, 128]), ('ZE', [128, RH * 64]), ('GCC', [64, RH]), ('GNB', [128, 2, RH * 64]))}
    y = dt("y", [T, RH * 64], F32, kind="ExternalOutput").ap()
    s = Sched(nc)
    emit_1b(s, tin, nin, cst, y, T, GCH)
    s.build()
    return nc


def emit_1b(s, tin, nin, cst, y, T, GCH):
    K = {}
    for n, shp in (('DM', [128, RH, 128]), ('XI', [64, RH, 128]), ('ZE', [128, RH * 64]), ('GCC', [64, RH]), ('GNB', [128, 2, RH * 64])):
        K[n] = s.sbuf('K_' + n, shp, F32)
        s.dma('sp', K[n][:], cst[n], ('k', n), writes=[n])
    epst = s.sbuf('epst', [128, 1], F32)
    s.op('dve', lambda e: e.memset(epst[:], RET_EPS), writes=['epst'])
    Rst = s.sbuf('Rst', [64, RH, 64], BF16)
    s.op('dve', lambda e: e.memset(Rst[:], 0.0), writes=['Rst'])
    PS = [s.psum('PS%d' % i, [128, 512]) for i in range(4)]
    GT = GCH * RC
    tbuf = {n: [s.sbuf('t_%s%d' % (n, i), [64, RH, GT], BF16) for i in range(2)] for n in tin}
    nbuf = {n: [s.sbuf('n_%s%d' % (n, i), [128, GCH, RH * 64], BF16 if n == 'Vn' else F32) for i in range(2)] for n in nin}
    QX = s.sbuf('QX', [64, RH, RC], BF16)
    KZ = s.sbuf('KZ', [128, RH * 64], BF16)
    SM = s.sbuf('SM', [128, RH, RC], BF16)
    Ysb = s.sbuf('Ysb', [128, RH, 64], F32)
    Ysq = s.sbuf('Ysq', [128, RH, 64], F32)
    Yn = s.sbuf('Yn', [128, RH, 64], F32)
    st = s.sbuf('st', [128, 6, RH], F32)
    Yo = [s.sbuf('Yo%d' % i, [128, RH * 64], F32) for i in range(2)]
    nchunk = T // RC
    ngrp = nchunk // GCH

    def load_group(gi):
        b = gi % 2
        t0 = gi * GT
        for n in tin:
            s.dma('pool', tbuf[n][b][:], tin[n][:, :, t0:t0 + GT], ('lt', n, b), writes=[('t', n, b)])
        for n in nin:
            s.dma('pool', nbuf[n][b][:], nin[n][t0:t0 + GT, :].rearrange("(c p) n -> p c n", p=128), ('ln', n, b),
                  writes=[('n', n, b)])

    def chunk(ci):
        gi, cj = divmod(ci, GCH)
        b = gi % 2
        tv = lambda n: tbuf[n][b][:, :, cj * RC:(cj + 1) * RC]
        tk = lambda n: ('t', n, b)
        nv = lambda n: nbuf[n][b][:, cj, :]
        nk = lambda n: ('n', n, b)
        TT(s, 'pool', QX[:], tv('QT'), K['XI'][:], ALU.mult, [tk('QT'), 'XI'], ['QX'])
        TT(s, 'pool', KZ[:], nv('Kn'), K['ZE'][:], ALU.mult, [nk('Kn'), 'ZE'], ['KZ'])
        for h in range(RH):
            MM(s, PS[0][:, h * RC:(h + 1) * RC], tbuf['KT'][b][:, h, cj * RC:(cj + 1) * RC], tbuf['QT'][b][:, h, cj * RC:(cj + 1) * RC],
               reads=[tk('KT'), tk('QT')], writes=['B0'], inc=(h == RH - 1))
        TT(s, 'dve', SM[:], PS[0][:, :].rearrange("p (h x) -> p h x", h=RH), K['DM'][:], ALU.mult, ['B0', 'DM'], ['SM'])
        for h in range(RH):
            vh = nbuf['Vn'][b][:, cj, h * 64:(h + 1) * 64]
            o = PS[1][:, h * 64:(h + 1) * 64]
            MM(s, o, SM[:, h, :], vh, start=True, stop=False, reads=['SM', nk('Vn')], writes=['B1'], inc=False)
            MM(s, o, QX[:, h, :], Rst[:, h, :], start=False, stop=True, reads=['QX', 'Rst'], writes=['B1'], inc=(h == RH - 1))
        for h in range(RH):
            vh = nbuf['Vn'][b][:, cj, h * 64:(h + 1) * 64]
            MM(s, PS[2][0:64, h * 64:(h + 1) * 64], KZ[:, h * 64:(h + 1) * 64], vh, reads=['KZ', nk('Vn')], writes=['B2'],
               inc=(h == RH - 1))
        CP(s, 'dve', Ysb[:], PS[1][:, 0:RH * 64].rearrange("p (h x) -> p h x", h=RH), ['B1'], ['Ysb'])
        for h in range(RH):
            STT(s, Rst[:, h, :], Rst[:, h, :], K['GCC'][:, h:h + 1], PS[2][0:64, h * 64:(h + 1) * 64], ALU.mult, ALU.add,
                ['Rst', 'GCC', 'B2'], ['Rst'])
        RED(s, st[:, 0, :], Ysb[:], ['Ysb'], ['st0'])
        TT(s, 'pool', Ysq[:], Ysb[:], Ysb[:], ALU.mult, ['Ysb'], ['Ysq'])
        RED(s, st[:, 1, :], Ysq[:], ['Ysq'], ['st1'])
        TS(s, 'dve', st[:, 2, :], st[:, 0, :], 1.0 / 64, None, ALU.mult, None, ['st0'], ['st2'])
        TT(s, 'dve', st[:, 3, :], st[:, 2, :], st[:, 2, :], ALU.mult, ['st2'], ['st3'])
        STT(s, st[:, 4, :], st[:, 1, :], 1.0 / 64, st[:, 3, :], ALU.mult, ALU.subtract, ['st1', 'st3'], ['st4'])
        ACTF(s, st[:, 5, :], st[:, 4, :], AF.Ln, ['st4', 'epst'], ['st5'], bias=epst[:], scale=1.0)
        ACTF(s, st[:, 5, :], st[:, 5, :], AF.Exp, ['st5'], ['st5'], scale=-0.5)
        for h in range(RH):
            TS(s, 'dve', Yn[:, h, :], Ysb[:, h, :], st[:, 2, h:h + 1], st[:, 5, h:h + 1], ALU.subtract, ALU.mult,
               ['Ysb', 'st2', 'st5'], ['Yn'])
        ynf = Yn[:].rearrange("p h x -> p (h x)")
        yo = Yo[ci % 2]
        yok = ('Yo', ci % 2)
        TT(s, 'pool', ynf, ynf, K['GNB'][:, 0, :], ALU.mult, ['Yn', 'GNB'], ['Yn'])
        TT(s, 'pool', ynf, ynf, K['GNB'][:, 1, :], ALU.add, ['Yn', 'GNB'], ['Yn'])
        TT(s, 'pool', yo[:], ynf, nv('Grn'), ALU.mult, ['Yn', nk('Grn')], [yok])
        s.dma('sp', y[ci * RC:(ci + 1) * RC, :], yo[:], ('so', ci % 2), reads=[yok], writes=[('yd', ci % 2)])

    load_group(0)
    for gi in range(ngrp):
        if gi + 1 < ngrp:
            load_group(gi + 1)
        for cj in range(GCH):
            chunk(gi * GCH + cj)
    s.wait_all('sp', [('yd', 0), ('yd', 1)])


def host_inputs_1b(arrT, gn_g, gn_b, head0):
    T = arrT['oQ'].shape[1]
    tr = lambda a: np.ascontiguousarray(a.reshape(RH, 64, T).transpose(1, 0, 2))
    na = lambda a: np.ascontiguousarray(a.T)
    hidx = np.arange(head0, head0 + RH, dtype=np.float32)
    log_gamma = np.log1p(-np.power(np.float32(2.0), -5.0 - hidx)).astype(np.float32)
    idx = np.arange(RC, dtype=np.float32)
    diff = idx[None, :] - idx[:, None]
    DM = np.where(diff[:, None, :] >= 0, np.exp(np.maximum(diff, 0.0)[:, None, :] * log_gamma[None, :, None]), 0.0)
    xi = np.exp((idx + 1.0)[None, :] * log_gamma[:, None])
    zeta = np.exp((RC - 1.0 - idx)[None, :] * log_gamma[:, None])
    gch = np.exp(RC * log_gamma)
    XI = np.broadcast_to(xi[None], (64, RH, RC))
    ZE = np.repeat(zeta.T, 64, axis=1)
    GCC = np.broadcast_to(gch[None], (64, RH))
    GNB = np.broadcast_to(np.stack([gn_g, gn_b])[None], (128, 2, RH * 64))
    f = lambda a: np.ascontiguousarray(a, dtype=np.float32)
    return dict(QT=tr(arrT['oQ']), KT=tr(arrT['oKr']), Kn=na(arrT['oKr']), Vn=na(arrT['oV']), Grn=na(arrT['oGr']),
                DM=f(DM), XI=f(XI), ZE=f(ZE), GCC=f(GCC), GNB=f(GNB))


NTOK = 4096
SHARDS = [(c // 2, (c % 2) * NTOK) for c in range(8)]


def _run(nc, in_maps):
    from concourse.bass_utils import run_bass_kernel_spmd
    return run_bass_kernel_spmd(nc, in_maps, core_ids=list(range(len(in_maps)))).results


def _post_mixer(inp, cur, ymTs, layer, w_out):
    ncC = build_C(ntok=NTOK, st=1024)
    maps = []
    for ci, (b, t0) in enumerate(SHARDS):
        maps.append(host_inputs_C(cur[b, t0:t0 + NTOK], ymTs[ci], w_out, inp['ln_mix_g'][layer], inp['ln_mix_b'][layer],
                                  inp['rg_w'][layer], inp['rg_b'][layer], inp['re_w'][layer], inp['re_b'][layer],
                                  inp['e_w1'][layer], inp['e_w3'][layer], inp['e_w2'][layer],
                                  inp['ln_ffn_g'][layer], inp['ln_ffn_b'][layer]))
    rc = _run(ncC, maps)
    nxt = np.empty_like(cur)
    for ci, (b, t0) in enumerate(SHARDS):
        nxt[b, t0:t0 + NTOK] = rc[ci]['xout']
    return nxt


def layer0(inp, x):
    nc0 = build_0a(ntok=NTOK)
    maps = []
    for (b, t0) in SHARDS:
        xT = np.zeros((D, HALO + NTOK), np.float32)
        xT[:, HALO:] = x[b, t0:t0 + NTOK].T
        if t0 > 0:
            xT[:, :HALO] = x[b, t0 - HALO:t0].T
        maps.append(host_inputs_0a(xT, inp['ev_w_in'][0], inp['ev_mu'][0], inp['ev_w0'][0], inp['ev_a0'][0],
                                   inp['ev_k_k'][0], inp['ev_k_a'][0], inp['ev_r_k'][0].reshape(-1),
                                   inp['ev_pool_scale'][0], inp['ev_w2'][0], inp['ev_a2'][0], inp['ev_g2'][0],
                                   inp['ev_pool_w'][0], t0 == 0))
    r0 = _run(nc0, maps)
    ncb = build_0b(T=2 * NTOK)
    maps = []
    for c in range(8):
        b, j = c // 2, c % 2
        rows = slice(256 * j, 256 * j + 256)
        arrT = {n: np.concatenate([r0[2 * b][n][rows], r0[2 * b + 1][n][rows]], axis=1)
                for n in ('oR', 'oK', 'oV', 'oA', 'oB', 'oW', 'oG', 'oBon')}
        maps.append(host_inputs_0b(arrT, inp['ev_lnx_g'][0][rows], inp['ev_lnx_b'][0][rows]))
    rb = _run(ncb, maps)
    ymTs = []
    for ci, (b, t0) in enumerate(SHARDS):
        ymT = np.empty((D, NTOK), np.float32)
        for j in range(2):
            ymT[256 * j:256 * j + 256] = rb[2 * b + j]['y'][t0:t0 + NTOK].T
        ymT[512:] = r0[ci]['oYP']
        ymTs.append(ymT)
    return _post_mixer(inp, x, ymTs, 0, inp['ev_w_out'][0])


def layer1(inp, x1):
    nc1 = build_1a(ntok=NTOK)
    maps = []
    for (b, t0) in SHARDS:
        xT = np.zeros((D, HALO1 + NTOK), np.float32)
        xT[:, HALO1:] = x1[b, t0:t0 + NTOK].T
        if t0 > 0:
            xT[:, :HALO1] = x1[b, t0 - HALO1:t0].T
        maps.append(host_inputs_1a(xT, inp['od_w_in'][0], inp['od_conv_w'][0], inp['od_conv_b'][0], inp['od_cln_g'][0],
                                   inp['od_cln_b'][0], t0, NTOK))
    r1 = _run(nc1, maps)
    ncb = build_1b(T=2 * NTOK)
    maps = []
    for c in range(8):
        b, j = c // 2, c % 2
        rows = slice(256 * j, 256 * j + 256)
        arrT = {n: np.concatenate([r1[2 * b][n][rows], r1[2 * b + 1][n][rows]], axis=1) for n in ('oQ', 'oKr', 'oV', 'oGr')}
        maps.append(host_inputs_1b(arrT, inp['od_gn_g'][0][rows], inp['od_gn_b'][0][rows], 4 * j))
    rb = _run(ncb, maps)
    ymTs = []
    for ci, (b, t0) in enumerate(SHARDS):
        ymT = np.empty((D, NTOK), np.float32)
        ymT[:512] = r1[ci]['oYC']
        for j in range(2):
            ymT[512 + 256 * j:512 + 256 * j + 256] = rb[2 * b + j]['y'][t0:t0 + NTOK].T
        ymTs.append(ymT)
    return _post_mixer(inp, x1, ymTs, 1, inp['od_w_out'][0])


def kernel(**inp):
    inp = {k: np.asarray(v) for k, v in inp.items()}
    x1 = layer0(inp, inp['x'])
    return layer1(inp, x1)
```

```python
import numpy as np


import concourse.bass as bass
import concourse.mybir as mybir

F32 = mybir.dt.float32
BF16 = mybir.dt.bfloat16
I32 = mybir.dt.int32
U32 = mybir.dt.uint32
AF = mybir.ActivationFunctionType
ALU = mybir.AluOpType
AX = mybir.AxisListType


class _Buf:
    def __init__(self, name, t):
        self.name = name
        self.t = t

    def __getitem__(self, idx):
        return self.t[idx]


class Sched:
    ENGS = ('pe', 'act', 'dve', 'pool', 'sp')

    def __init__(self, nc, self_wait=True):
        self.nc = nc
        self.self_wait = self_wait
        self.ops = {e: [] for e in self.ENGS}
        self.sems = {}
        self.cnt = {}
        self.waited = {e: {} for e in self.ENGS}
        self.last_w = {}
        self.readers = {}
        self.ctx = []
        self.pending_noinc = {e: False for e in self.ENGS}
        for e in ('pe', 'act', 'dve', 'pool'):
            self.sems[e] = nc.alloc_semaphore(name='sem_' + e)
            self.cnt[e] = 0
        self.ntile = 0

    def sbuf(self, name, shape, dtype):
        g = self.nc.sbuf_tensor('sb_' + name, list(shape), dtype)
        t = g.__enter__()
        self.ctx.append(g)
        return _Buf(name, t)

    def psum(self, name, shape, dtype=F32):
        g = self.nc.psum_tensor('ps_' + name, list(shape), dtype)
        t = g.__enter__()
        self.ctx.append(g)
        return _Buf(name, t)

    def dma_sem(self, key):
        if key not in self.sems:
            self.sems[key] = self.nc.alloc_semaphore(name='dsem_%d' % len(self.sems))
            self.cnt[key] = 0
        return key

    def _deps(self, reads, writes):
        deps = []
        for r in reads:
            if r in self.last_w:
                deps.append(self.last_w[r])
        for w in writes:
            if w in self.last_w:
                deps.append(self.last_w[w])
            deps.extend(self.readers.get(w, []))
        return deps

    def _emit_waits(self, eng, deps):
        waits = []
        best = {}
        for (sk, v) in deps:
            if sk == eng and (eng == 'pe' or not self.self_wait):
                continue
            if self.waited[eng].get(sk, 0) >= v:
                continue
            if best.get(sk, 0) < v:
                best[sk] = v
        for sk, v in best.items():
            self.waited[eng][sk] = v
            waits.append((self.sems[sk], v))
        return waits

    def _record(self, ev, reads, writes):
        for r in reads:
            self.readers.setdefault(r, []).append(ev)
        for w in writes:
            self.last_w[w] = ev
            self.readers[w] = []

    def op(self, eng, fn, reads=(), writes=(), inc=True):
        reads = [k for k in reads]
        writes = [k for k in writes]
        waits = self._emit_waits(eng, self._deps(reads, writes))
        sem = self.sems[eng]
        if inc:
            self.cnt[eng] += 1
            ev = (eng, self.cnt[eng])
            self.pending_noinc[eng] = False
        else:
            ev = (eng, self.cnt[eng] + 1)
            self.pending_noinc[eng] = True

        def emit(e, fn=fn, waits=waits, inc=inc, sem=sem):
            for (s, v) in waits:
                e.wait_ge(s, v)
            ins = fn(e)
            if inc:
                ins.then_inc(sem, 1)
        self.ops[eng].append(emit)
        self._record(ev, reads, writes)
        return ev

    def dma(self, eng, out, in_, semkey, reads=(), writes=(), **kw):
        self.dma_sem(semkey)
        reads = list(reads)
        writes = list(writes)
        waits = self._emit_waits(eng, self._deps(reads, writes))
        self.cnt[semkey] += 16
        ev = (semkey, self.cnt[semkey])
        sem = self.sems[semkey]

        def emit(e, waits=waits, sem=sem, out=out, in_=in_, kw=kw):
            for (s, v) in waits:
                e.wait_ge(s, v)
            e.dma_start(out=out, in_=in_, **kw).then_inc(sem, 16)
        self.ops[eng].append(emit)
        self._record(ev, reads, writes)
        return ev

    def wait_all(self, eng, keys):
        deps = []
        for k in keys:
            if k in self.last_w:
                deps.append(self.last_w[k])
        waits = self._emit_waits(eng, deps)

        def emit(e, waits=waits):
            for (s, v) in waits:
                e.wait_ge(s, v)
        self.ops[eng].append(emit)

    def build(self):
        nc = self.nc
        for e in self.ENGS:
            assert not self.pending_noinc[e], 'dangling noinc on ' + e
        with nc.Block() as block:
            @block.tensor
            def _(e):
                for f in self.ops['pe']:
                    f(e)

            @block.scalar
            def _(e):
                for f in self.ops['act']:
                    f(e)

            @block.vector
            def _(e):
                for f in self.ops['dve']:
                    f(e)

            @block.gpsimd
            def _(e):
                for f in self.ops['pool']:
                    f(e)

            @block.sync
            def _(e):
                for f in self.ops['sp']:
                    f(e)
        for g in reversed(self.ctx):
            g.__exit__(None, None, None)


D = 1024
HALO = 16
NCH = 18


def build_0a(ntok=4096, tile=512, dbg=None):
    nc = bass.Bass('TRN2', target_bir_lowering=False)
    dt = nc.dram_tensor
    xT = dt("xT", [D, HALO + ntok], F32, kind="ExternalInput").ap()
    w_in = dt("w_in", [D, 2304], F32, kind="ExternalInput").ap()
    cols = dt("cols", [128, 40], F32, kind="ExternalInput").ap()
    w2 = dt("w2", [64, 512], F32, kind="ExternalInput").ap()
    a2 = dt("a2", [64, 512], F32, kind="ExternalInput").ap()
    g2 = dt("g2", [128, 512], F32, kind="ExternalInput").ap()
    pool_w = dt("pool_w", [4, 128, 128], F32, kind="ExternalInput").ap()
    bones_d = dt("bones", [128, 128], F32, kind="ExternalInput").ap()
    invc_d = dt("invc", [128, 2, 4, tile], F32, kind="ExternalInput").ap()
    outs = {n: dt(n, [512, ntok], F32, kind="ExternalOutput").ap()
            for n in ('oR', 'oK', 'oV', 'oA', 'oB', 'oW', 'oG', 'oBon', 'oYP')}
    s = Sched(nc)
    emit_0a(s, xT, w_in, cols, w2, a2, g2, pool_w, bones_d, invc_d, outs, ntok, tile)
    s.build()
    return nc


OQ = 'sp'


def emit_0a(s, xT, w_in, cols_d, w2_d, a2_d, g2_d, pool_w_d, bones_d, invc_d, outs, ntok, tile):
    T = tile
    W = s.sbuf('W', [128, 8, 2304], BF16)
    for k in range(8):
        s.dma('pool', W[:, k, :], w_in[k * 128:(k + 1) * 128, :], 'w0', writes=[('W', k)])
    WK = [('W', k) for k in range(8)]
    COLS = s.sbuf('COLS', [128, 40], F32)
    s.dma('sp', COLS[:], cols_d, 'c0', writes=['COLS'])
    W2 = s.sbuf('W2', [64, 512], BF16)
    A2 = s.sbuf('A2', [128, 512], BF16)
    G2 = s.sbuf('G2', [128, 512], BF16)
    PW = s.sbuf('PW', [128, 4, 128], BF16)
    BONES = s.sbuf('BONES', [128, 128], BF16)
    INVC = s.sbuf('INVC', [128, 2, 4, T], F32)
    s.dma('pool', W2[:], w2_d, 'c1', writes=['W2'])
    s.dma('pool', A2[64:128, :], a2_d, 'c2', writes=['A2'])
    s.dma('pool', G2[:], g2_d, 'c3', writes=['G2'])
    for g in range(4):
        s.dma('pool', PW[:, g, :], pool_w_d[g], ('c4', g), writes=[('PW', g)])
    s.dma('pool', BONES[:], bones_d, 'c5', writes=['BONES'])
    for a in range(2):
        s.dma('sp', INVC[:, a, :, :], invc_d[:, a, :, :], ('c6', a), writes=[('INVC', a)])
    c24 = s.sbuf('c24', [128, 1], F32)
    s.op('dve', lambda e: e.memset(c24[:], 0.0), writes=['c24'])

    PR = s.sbuf('PR', [128, NCH, HALO + T], F32)
    Z = s.sbuf('Z', [128, 14, T], F32)
    xb = [s.sbuf('xb%d' % i, [128, 8, T], BF16) for i in range(2)]
    xh = s.sbuf('xh', [128, 8, HALO], BF16)
    P = [s.psum('P%d' % i, [128, 512]) for i in range(8)]
    tw = s.sbuf('tw', [64, T], BF16)
    al = s.sbuf('al', [128, T], BF16)
    sg = s.sbuf('sg', [128, T], BF16)
    tmp = [s.sbuf('tmp%d' % i, [128, T], F32) for i in range(4)]
    tb = [s.sbuf('tb%d' % i, [128, T], BF16) for i in range(2)]
    O = {n: [s.sbuf('O_%s%d' % (n, i), [128, T], F32) for i in range(2)] for n in ('oA', 'oB', 'oK', 'oW', 'oG', 'oBon', 'oYP')}
    SS = [s.sbuf('SS%d' % i, [128, HALO + T], F32) for i in range(2)]

    s.dma('pool', xh[:], xT[:, 0:HALO].rearrange("(k p) t -> p k t", p=128), 'xh', writes=['xh'])
    for c in range(NCH):
        pb = P[c % 2]
        pk = ('P', c % 2)
        for k in range(8):
            s.op('pe', lambda e, pb=pb, k=k, c=c: e.matmul(pb[:, 0:HALO], lhsT=W[:, k, c * 128:(c + 1) * 128],
                                                           rhs=xh[:, k, :], start=(k == 0), stop=(k == 7)),
                 reads=['xh'] + WK, writes=[pk], inc=(k == 7))
        s.op('act', lambda e, pb=pb, c=c: e.copy(out=PR[:, c, 0:HALO], in_=pb[:, 0:HALO]), reads=[pk], writes=[('PR', c)])

    ntile = ntok // T
    def tile_body(ti):
        t0 = ti * T
        x_ = xb[ti % 2]
        xk = ('xb', ti % 2)
        s.dma('pool', x_[:], xT[:, HALO + t0:HALO + t0 + T].rearrange("(k p) t -> p k t", p=128), ('x', ti % 2), writes=[xk])
        XK = [xk]
        for c in range(NCH):
            pb = P[c % 2]
            pk = ('P', c % 2)
            for k in range(8):
                s.op('pe', lambda e, pb=pb, k=k, c=c, x_=x_: e.matmul(pb[:], lhsT=W[:, k, c * 128:(c + 1) * 128],
                                                                      rhs=x_[:, k, :], start=(k == 0), stop=(k == 7)),
                     reads=XK + WK, writes=[pk], inc=(k == 7))
            s.op('act', lambda e, pb=pb, c=c: e.copy(out=PR[:, c, HALO:HALO + T], in_=pb[:]), reads=[pk], writes=[('PR', c)])
        for c in range(14):
            d_ = tmp[c % 2]
            dk = ('tmp', c % 2)
            s.op('dve', lambda e, c=c, d_=d_: e.tensor_tensor(out=d_[:], in0=PR[:, c, HALO - 1:HALO - 1 + T],
                                                              in1=PR[:, c, HALO:HALO + T], op=ALU.subtract),
                 reads=[('PR', c)], writes=[dk])
            s.op('dve', lambda e, c=c, d_=d_: e.scalar_tensor_tensor(out=Z[:, c, :], in0=d_[:], scalar=COLS[:, c:c + 1],
                                                                     in1=PR[:, c, HALO:HALO + T], op0=ALU.mult, op1=ALU.add),
                 reads=[dk, ('PR', c), 'COLS'], writes=[('Z', c)])
        s.op('act', lambda e: e.activation(out=tw[:], in_=Z[0:64, 12, :], func=AF.Tanh), reads=[('Z', 12)], writes=['tw'])
        s.op('act', lambda e: e.copy(out=al[64:128, :], in_=Z[64:128, 12, :]), reads=[('Z', 12)], writes=['al'])
        s.op('act', lambda e: e.activation(out=sg[:], in_=Z[:, 13, :], func=AF.Sigmoid), reads=[('Z', 13)], writes=['sg'])
        ob = ti % 2
        for cc in range(4):
            cs = slice(cc * 128, (cc + 1) * 128)
            rows = slice(cc * 128, (cc + 1) * 128)
            tsl = slice(t0, t0 + T)
            s.op('pe', lambda e, cs=cs: e.matmul(P[2][:], lhsT=W2[:, cs], rhs=tw[:], start=True, stop=True),
                 reads=['W2', 'tw'], writes=[('P', 2)])
            s.op('act', lambda e, cc=cc: e.activation(out=O['oW'][ob][:], in_=P[2][:], func=AF.Sigmoid,
                                                     bias=COLS[:, 14 + cc:15 + cc], scale=1.0),
                 reads=[('P', 2), 'COLS'], writes=[('oW', ob)])
            s.dma(OQ, outs['oW'][rows, tsl], O['oW'][ob][:], ('so', 'oW', ob), reads=[('oW', ob)], writes=[('d_oW', ob)])
            s.op('pe', lambda e, cs=cs: e.matmul(P[3][:], lhsT=A2[64:128, cs], rhs=al[64:128, :], start=True, stop=True),
                 reads=['A2', 'al'], writes=[('P', 3)])
            asig = tmp[2]
            s.op('act', lambda e, cc=cc: e.activation(out=asig[:], in_=P[3][:], func=AF.Sigmoid,
                                                     bias=COLS[:, 18 + cc:19 + cc], scale=1.0),
                 reads=[('P', 3), 'COLS'], writes=[('tmp', 2)])
            s.op('pe', lambda e, cs=cs: e.matmul(P[4][:], lhsT=G2[:, cs], rhs=sg[:], start=True, stop=True),
                 reads=['G2', 'sg'], writes=[('P', 4)])
            s.op('act', lambda e: e.copy(out=O['oG'][ob][:], in_=P[4][:]), reads=[('P', 4)], writes=[('oG', ob)])
            s.dma(OQ, outs['oG'][rows, tsl], O['oG'][ob][:], ('so', 'oG', ob), reads=[('oG', ob)], writes=[('d_oG', ob)])
            kk0 = tmp[3]
            s.op('dve', lambda e, cc=cc: e.tensor_scalar(out=kk0[:], in0=Z[:, 4 + cc, :], scalar1=COLS[:, 22 + cc:23 + cc],
                                                         scalar2=None, op0=ALU.mult),
                 reads=[('Z', 4 + cc), 'COLS'], writes=[('tmp', 3)])
            s.op('dve', lambda e: e.tensor_tensor(out=tb[0][:], in0=kk0[:], in1=kk0[:], op=ALU.mult),
                 reads=[('tmp', 3)], writes=[('tb', 0)])
            s.op('pe', lambda e: e.matmul(P[5][:], lhsT=BONES[:], rhs=tb[0][:], start=True, stop=True),
                 reads=['BONES', ('tb', 0)], writes=[('P', 5)])
            rn = tmp[0]
            s.op('dve', lambda e: e.tensor_scalar(out=rn[:], in0=P[5][:], scalar1=1e-24, scalar2=None, op0=ALU.max),
                 reads=[('P', 5)], writes=[('tmp', 0)])
            s.op('act', lambda e: e.activation(out=rn[:], in_=rn[:], func=AF.Ln), reads=[('tmp', 0)], writes=[('tmp', 0)])
            s.op('act', lambda e: e.activation(out=rn[:], in_=rn[:], func=AF.Exp, scale=-0.5), reads=[('tmp', 0)], writes=[('tmp', 0)])
            s.op('dve', lambda e: e.tensor_tensor(out=kk0[:], in0=kk0[:], in1=rn[:], op=ALU.mult),
                 reads=[('tmp', 3), ('tmp', 0)], writes=[('tmp', 3)])
            s.op('act', lambda e: e.mul(out=O['oA'][ob][:], in_=kk0[:], mul=-1.0), reads=[('tmp', 3)], writes=[('oA', ob)])
            s.dma(OQ, outs['oA'][rows, tsl], O['oA'][ob][:], ('so', 'oA', ob), reads=[('oA', ob)], writes=[('d_oA', ob)])
            s.op('dve', lambda e: e.tensor_tensor(out=O['oB'][ob][:], in0=kk0[:], in1=asig[:], op=ALU.mult),
                 reads=[('tmp', 3), ('tmp', 2)], writes=[('oB', ob)])
            s.dma(OQ, outs['oB'][rows, tsl], O['oB'][ob][:], ('so', 'oB', ob), reads=[('oB', ob)], writes=[('d_oB', ob)])
            t1 = tmp[1]
            s.op('dve', lambda e, cc=cc: e.tensor_scalar(out=t1[:], in0=asig[:], scalar1=-1.0, scalar2=COLS[:, 26 + cc:27 + cc],
                                                         op0=ALU.add, op1=ALU.mult),
                 reads=[('tmp', 2), 'COLS'], writes=[('tmp', 1)])
            s.op('dve', lambda e, cc=cc: e.scalar_tensor_tensor(out=O['oK'][ob][:], in0=t1[:], scalar=1.0, in1=Z[:, 4 + cc, :],
                                                                op0=ALU.add, op1=ALU.mult),
                 reads=[('tmp', 1), ('Z', 4 + cc)], writes=[('oK', ob)])
            s.dma(OQ, outs['oK'][rows, tsl], O['oK'][ob][:], ('so', 'oK', ob), reads=[('oK', ob)], writes=[('d_oK', ob)])
            s.dma(OQ, outs['oR'][rows, tsl], Z[:, cc, :], ('so', 'oR', cc), reads=[('Z', cc)], writes=[('d_oR', cc)])
            s.dma(OQ, outs['oV'][rows, tsl], Z[:, 8 + cc, :], ('so', 'oV', cc), reads=[('Z', 8 + cc)], writes=[('d_oV', cc)])
            s.op('dve', lambda e, cc=cc: e.scalar_tensor_tensor(out=tb[1][:], in0=Z[:, cc, :], scalar=COLS[:, 30 + cc:31 + cc],
                                                                in1=O['oK'][ob][:], op0=ALU.mult, op1=ALU.mult),
                 reads=[('Z', cc), 'COLS', ('oK', ob)], writes=[('tb', 1)])
            s.op('pe', lambda e: e.matmul(P[6][:], lhsT=BONES[:], rhs=tb[1][:], start=True, stop=True),
                 reads=['BONES', ('tb', 1)], writes=[('P', 6)])
            s.op('dve', lambda e, cc=cc: e.tensor_tensor(out=O['oBon'][ob][:], in0=Z[:, 8 + cc, :], in1=P[6][:], op=ALU.mult),
                 reads=[('Z', 8 + cc), ('P', 6)], writes=[('oBon', ob)])
            s.dma(OQ, outs['oBon'][rows, tsl], O['oBon'][ob][:], ('so', 'oBon', ob), reads=[('oBon', ob)], writes=[('d_oBon', ob)])
        for g in range(4):
            c = 14 + g
            src = PR
            cur = None
            n = HALO + T
            prev_ap = lambda lo, hi, c=c: PR[:, c, lo:hi]
            a_, b_ = SS[0], SS[1]
            sh = 1
            first = True
            for lv in range(g + 1):
                lo = 2 * sh - 1
                if first:
                    s.op('pool', lambda e, c=c, sh=sh, n=n, lo=lo: e.tensor_tensor(out=SS[0][:, lo:n], in0=PR[:, c, lo:n], in1=PR[:, c, sh - 1:n - sh], op=ALU.add),
                         reads=[('PR', c)], writes=[('SS', 0)])
                    first = False
                    cur = 0
                else:
                    src_, dst_ = SS[cur], SS[1 - cur]
                    s.op('pool', lambda e, sh=sh, n=n, lo=lo, src_=src_, dst_=dst_: e.tensor_tensor(out=dst_[:, lo:n], in0=src_[:, lo:n],
                                                                                                in1=src_[:, sh - 1:n - sh], op=ALU.add),
                         reads=[('SS', cur)], writes=[('SS', 1 - cur)])
                    cur = 1 - cur
                sh *= 2
            ia = 0 if ti == 0 else 1
            s.op('dve', lambda e, g=g, cur=cur, ia=ia: e.tensor_tensor(out=tmp[0][:], in0=SS[cur][:, HALO:HALO + T], in1=INVC[:, ia, g, :], op=ALU.mult),
                 reads=[('SS', cur), ('INVC', ia)], writes=[('tmp', 0)])
            s.op('dve', lambda e, c=c: e.tensor_tensor(out=tb[0][:], in0=tmp[0][:], in1=PR[:, c, HALO:HALO + T], op=ALU.subtract),
                 reads=[('tmp', 0), ('PR', c)], writes=[('tb', 0)])
            s.op('pe', lambda e, g=g: e.matmul(P[7][:], lhsT=PW[:, g, :], rhs=tb[0][:], start=True, stop=True),
                 reads=[('PW', g), ('tb', 0)], writes=[('P', 7)])
            s.op('act', lambda e, g=g: e.activation(out=O['oYP'][ob][:], in_=P[7][:], func=AF.Identity, scale=COLS[:, 34 + g:35 + g]),
                 reads=[('P', 7), 'COLS'], writes=[('oYP', ob)])
            s.dma(OQ, outs['oYP'][g * 128:(g + 1) * 128, t0:t0 + T], O['oYP'][ob][:], ('so', 'oYP', ob), reads=[('oYP', ob)], writes=[('d_oYP', ob)])
        for c in range(NCH):
            s.op('pool', lambda e, c=c: e.tensor_copy(out=PR[:, c, 0:HALO], in_=PR[:, c, T:T + HALO]),
                 reads=[('PR', c)], writes=[('PR', c)])
    for ti in range(ntile):
        tile_body(ti)
    s.wait_all(OQ, [k for k in s.last_w if isinstance(k, tuple) and isinstance(k[0], str) and k[0].startswith('d_')])


def host_inputs_0a(xT_halo, w_in, mu, w0, a0, k_k, k_a, r_k, pool_scale, w2, a2, g2, pool_w, first, tile=512):
    cols = np.zeros((128, 40), np.float32)
    cols[:, 0:14] = mu.reshape(14, 128).T
    cols[:, 14:18] = w0.reshape(4, 128).T
    cols[:, 18:22] = a0.reshape(4, 128).T
    cols[:, 22:26] = k_k.reshape(4, 128).T
    cols[:, 26:30] = k_a.reshape(4, 128).T
    cols[:, 30:34] = r_k.reshape(4, 128).T
    cols[:, 34:38] = pool_scale.reshape(4, 128).T
    bones = np.zeros((128, 128), np.float32)
    bones[:64, :64] = 1.0
    bones[64:, 64:] = 1.0
    invc = np.zeros((128, 2, 4, tile), np.float32)
    t = np.arange(tile)
    for g, win in enumerate((2, 4, 8, 16)):
        invc[:, 1, g, :] = 1.0 / win
        invc[:, 0, g, :] = (1.0 / np.minimum(t + 1, win)) if first else 1.0 / win
    return dict(xT=np.ascontiguousarray(xT_halo), w_in=np.ascontiguousarray(w_in), cols=cols,
                w2=np.ascontiguousarray(w2), a2=np.ascontiguousarray(a2), g2=np.ascontiguousarray(g2),
                pool_w=np.ascontiguousarray(pool_w), bones=bones, invc=invc)


C = 64
NH = 4
C0 = 0.6065306597126334
RWKV_EPS = 64e-5


def MM(s, out, lhsT, rhs, start=True, stop=True, reads=(), writes=(), inc=True):
    s.op('pe', lambda e: e.matmul(out, lhsT=lhsT, rhs=rhs, start=start, stop=stop), reads=reads, writes=writes, inc=inc)


def TT(s, eng, out, in0, in1, op, reads, writes):
    s.op(eng, lambda e: e.tensor_tensor(out=out, in0=in0, in1=in1, op=op), reads=reads, writes=writes)


def ACTF(s, out, in_, func, reads, writes, bias=None, scale=1.0):
    if bias is None:
        s.op('act', lambda e: e.activation(out=out, in_=in_, func=func, scale=scale), reads=reads, writes=writes)
    else:
        s.op('act', lambda e: e.activation(out=out, in_=in_, func=func, bias=bias, scale=scale), reads=reads, writes=writes)


def TS(s, eng, out, in0, s1, s2, op0, op1, reads, writes):
    if op1 is None:
        s.op(eng, lambda e: e.tensor_scalar(out=out, in0=in0, scalar1=s1, scalar2=None, op0=op0), reads=reads, writes=writes)
    else:
        s.op(eng, lambda e: e.tensor_scalar(out=out, in0=in0, scalar1=s1, scalar2=s2, op0=op0, op1=op1), reads=reads, writes=writes)


def STT(s, out, in0, scalar, in1, op0, op1, reads, writes):
    s.op('dve', lambda e: e.scalar_tensor_tensor(out=out, in0=in0, scalar=scalar, in1=in1, op0=op0, op1=op1),
         reads=reads, writes=writes)


def CP(s, eng, out, in_, reads, writes):
    if eng == 'act':
        s.op('act', lambda e: e.copy(out=out, in_=in_), reads=reads, writes=writes)
    else:
        s.op(eng, lambda e: e.tensor_copy(out=out, in_=in_), reads=reads, writes=writes)


def RED(s, out, in_, reads, writes):
    s.op('dve', lambda e: e.reduce_sum(out=out, in_=in_, axis=AX.X), reads=reads, writes=writes)


def build_0b(T=8192, GC=4):
    nc = bass.Bass('TRN2', target_bir_lowering=False)
    dt = nc.dram_tensor
    tin = {n: dt(n, [64, NH, T], F32, kind="ExternalInput").ap() for n in ('RT', 'KT', 'AT', 'BT')}
    nin = {n: dt(n, [T, NH * 64], F32, kind="ExternalInput").ap() for n in ('Wn', 'Bn', 'Kn', 'Vn', 'BONn', 'Gn')}
    cst = {n: dt(n, shp, F32, kind="ExternalInput").ap() for n, shp in
           (('MU1', [64, NH, 128]), ('MLS', [64, NH, 64]), ('I4', [64, NH, 64]), ('TRI', [64, 128]), ('UPS', [64, 64]),
            ('LGB', [64, 2, NH * 64]))}
    y = dt("y", [T, NH * 64], F32, kind="ExternalOutput").ap()
    s = Sched(nc)
    emit_0b(s, tin, nin, cst, y, T, GC)
    s.build()
    return nc


def emit_0b(s, tin, nin, cst, y, T, GC):
    K = {}
    for n, shp in (('MU1', [64, NH, 128]), ('MLS', [64, NH, 64]), ('I4', [64, NH, 64]), ('TRI', [64, 128]), ('UPS', [64, 64]),
                   ('LGB', [64, 2, NH * 64])):
        K[n] = s.sbuf('K_' + n, shp, F32)
        s.dma('sp', K[n][:], cst[n], ('k', n), writes=[n])
    epst = s.sbuf('epst', [64, 1], F32)
    s.op('dve', lambda e: e.memset(epst[:], RWKV_EPS), writes=['epst'])
    S0 = s.sbuf('S0', [64, NH, 64], F32)
    s.op('dve', lambda e: e.memset(S0[:], 0.0), writes=['S0'])
    PS = [s.psum('PS%d' % i, [128, 512]) for i in range(8)]

    def bank(i, w):
        return PS[i][0:64, 0:NH * w].rearrange("p (h x) -> p h x", h=NH)
    GT = GC * C
    tbuf = {n: [s.sbuf('t_%s%d' % (n, i), [64, NH, GT], F32) for i in range(2)] for n in tin}
    nbuf = {n: [s.sbuf('n_%s%d' % (n, i), [64, GC, NH * 64], F32) for i in range(2)] for n in nin}
    f4 = lambda name, w=64: s.sbuf(name, [64, NH, w], F32)
    PT, PINV, PPREV = f4('PT'), f4('PINV'), f4('PPREV')
    E1 = s.sbuf('E1', [64, NH * 64], F32)
    AR = s.sbuf('AR', [64, NH, 2, 64], F32)
    BK = s.sbuf('BK', [64, NH, 2, 64], F32)
    BKn = s.sbuf('BKn', [64, 2, NH * 64], F32)
    X1, X2 = f4('X1', 128), f4('X2', 128)
    Nb = [f4('N0'), f4('N1')]
    NTb = [f4('NT0'), f4('NT1')]
    Rb = [f4('R0'), f4('R1')]
    Wsb, Usb, Ysb, Ysq, Yn = f4('Wsb'), f4('Usb'), f4('Ysb'), f4('Ysq'), f4('Yn')
    st = s.sbuf('st', [64, 6, NH], F32)
    Yo = [s.sbuf('Yo%d' % i, [64, NH * 64], F32) for i in range(2)]
    nchunk = T // C
    ngrp = nchunk // GC

    def load_group(gi):
        b = gi % 2
        t0 = gi * GT
        for n in tin:
            s.dma('sp', tbuf[n][b][:], tin[n][:, :, t0:t0 + GT], ('lt', n, b), writes=[('t', n, b)])
        for n in nin:
            s.dma('pool', nbuf[n][b][:], nin[n][t0:t0 + GT, :].rearrange("(c p) n -> p c n", p=64), ('ln', n, b),
                  writes=[('n', n, b)])

    def chunk(ci):
        gi, cj = divmod(ci, GC)
        b = gi % 2
        tv = lambda n: tbuf[n][b][:, :, cj * C:(cj + 1) * C]
        tk = lambda n: ('t', n, b)
        nv = lambda n: nbuf[n][b][:, cj, :]
        nk = lambda n: ('n', n, b)
        for h in range(NH):
            MM(s, PS[0][0:64, h * 128:(h + 1) * 128], nbuf['Wn'][b][:, cj, h * 64:(h + 1) * 64], K['TRI'][:],
               reads=[nk('Wn'), 'TRI'], writes=['B0'], inc=(h == NH - 1))
        MM(s, PS[1][0:64, 0:NH * 64], K['UPS'][:], nv('Wn'), reads=[nk('Wn'), 'UPS'], writes=['B1'])
        cumI = bank(0, 128)[:, :, 0:64]
        cumS = bank(0, 128)[:, :, 64:128]
        ACTF(s, PT[:], cumI, AF.Exp, ['B0'], ['PT'], scale=-C0)
        ACTF(s, PINV[:], cumI, AF.Exp, ['B0'], ['PINV'], scale=C0)
        ACTF(s, PPREV[:], cumS, AF.Exp, ['B0'], ['PPREV'], scale=-C0)
        ACTF(s, E1[:], PS[1][0:64, 0:NH * 64], AF.Exp, ['B1'], ['E1'], scale=-C0)
        TT(s, 'dve', AR[:, :, 0, :], tv('AT'), PPREV[:], ALU.mult, [tk('AT'), 'PPREV'], ['AR0'])
        TT(s, 'pool', AR[:, :, 1, :], tv('RT'), PT[:], ALU.mult, [tk('RT'), 'PT'], ['AR1'])
        TT(s, 'dve', BK[:, :, 0, :], tv('BT'), PINV[:], ALU.mult, [tk('BT'), 'PINV'], ['BK0'])
        TT(s, 'pool', BK[:, :, 1, :], tv('KT'), PINV[:], ALU.mult, [tk('KT'), 'PINV'], ['BK1'])
        TT(s, 'pool', BKn[:, 0, :], nv('Bn'), E1[:], ALU.mult, [nk('Bn'), 'E1'], ['BKn0'])
        TT(s, 'pool', BKn[:, 1, :], nv('Kn'), E1[:], ALU.mult, [nk('Kn'), 'E1'], ['BKn1'])
        for h in range(NH):
            arh = AR[:, h, :, :].rearrange("p a x -> p (a x)")
            MM(s, PS[2][0:64, h * 128:(h + 1) * 128], BK[:, h, 0, :], arh, reads=['BK0', 'AR0', 'AR1'], writes=['B2'],
               inc=(h == NH - 1))
        for h in range(NH):
            arh = AR[:, h, :, :].rearrange("p a x -> p (a x)")
            MM(s, PS[3][0:64, h * 128:(h + 1) * 128], BK[:, h, 1, :], arh, reads=['BK1', 'AR0', 'AR1'], writes=['B3'],
               inc=(h == NH - 1))
        for h in range(NH):
            MM(s, PS[4][0:64, h * 64:(h + 1) * 64], AR[:, h, 0, :], BK[:, h, 0, :], reads=['BK0', 'AR0'], writes=['B4'],
               inc=(h == NH - 1))
        TT(s, 'dve', X1[:], bank(2, 128), K['MU1'][:], ALU.mult, ['B2', 'MU1'], ['X1'])
        TT(s, 'dve', X2[:], bank(3, 128), K['MU1'][:], ALU.mult, ['B3', 'MU1'], ['X2'])
        TT(s, 'dve', NTb[0][:], bank(4, 64), K['MLS'][:], ALU.mult, ['B4', 'MLS'], [('NT', 0)])
        CP(s, 'pool', Nb[0][:], X1[:, :, 0:64], ['X1'], [('N', 0)])
        TT(s, 'pool', Rb[0][:], X1[:, :, 0:64], K['I4'][:], ALU.add, ['X1', 'I4'], [('R', 0)])
        cur = 0
        for lv in range(5):
            nx = 1 - cur
            if lv < 4:
                for h in range(NH):
                    MM(s, PS[5][0:64, h * 64:(h + 1) * 64], NTb[cur][:, h, :], Nb[cur][:, h, :],
                       reads=[('NT', cur), ('N', cur)], writes=['B5'], inc=(h == NH - 1))
            for h in range(NH):
                MM(s, PS[6][0:64, h * 64:(h + 1) * 64], Nb[cur][:, h, :], NTb[cur][:, h, :],
                   reads=[('NT', cur), ('N', cur)], writes=['B6'], inc=(h == NH - 1))
            if lv < 4:
                CP(s, 'act', Nb[nx][:], bank(5, 64), ['B5'], [('N', nx)])
            CP(s, 'dve', NTb[nx][:], bank(6, 64), ['B6'], [('NT', nx)])
            for h in range(NH):
                MM(s, PS[7][0:64, h * 64:(h + 1) * 64], NTb[nx][:, h, :], Rb[cur][:, h, :],
                   reads=[('NT', nx), ('R', cur)], writes=['B7'], inc=(h == NH - 1))
            TT(s, 'dve', Rb[nx][:], Rb[cur][:], bank(7, 64), ALU.add, [('R', cur), 'B7'], [('R', nx)])
            cur = nx
        R = Rb[cur]
        Rk = ('R', cur)
        for h in range(NH):
            vh = nbuf['Vn'][b][:, cj, h * 64:(h + 1) * 64]
            MM(s, PS[2][0:64, h * 64:(h + 1) * 64], X2[:, h, 0:64], vh, start=True, stop=False,
               reads=['X2', nk('Vn')], writes=['B2'], inc=False)
            MM(s, PS[2][0:64, h * 64:(h + 1) * 64], AR[:, h, 0, :], S0[:, h, :], start=False, stop=True,
               reads=['AR0', 'S0'], writes=['B2'], inc=(h == NH - 1))
        CP(s, 'dve', Wsb[:], bank(2, 64), ['B2'], ['Wsb'])
        for h in range(NH):
            MM(s, PS[3][0:64, h * 64:(h + 1) * 64], R[:, h, :], Wsb[:, h, :], reads=[Rk, 'Wsb'], writes=['B3'],
               inc=(h == NH - 1))
        CP(s, 'dve', Usb[:], bank(3, 64), ['B3'], ['Usb'])
        for h in range(NH):
            vh = nbuf['Vn'][b][:, cj, h * 64:(h + 1) * 64]
            o = PS[4][0:64, h * 64:(h + 1) * 64]
            MM(s, o, AR[:, h, 1, :], S0[:, h, :], start=True, stop=False, reads=['AR1', 'S0'], writes=['B4'], inc=False)
            MM(s, o, X1[:, h, 64:128], Usb[:, h, :], start=False, stop=False, reads=['X1', 'Usb'], writes=['B4'], inc=False)
            MM(s, o, X2[:, h, 64:128], vh, start=False, stop=True, reads=['X2', nk('Vn')], writes=['B4'], inc=(h == NH - 1))
        for h in range(NH):
            vh = nbuf['Vn'][b][:, cj, h * 64:(h + 1) * 64]
            o = PS[7][0:64, h * 64:(h + 1) * 64]
            MM(s, o, BKn[:, 0, h * 64:(h + 1) * 64], Usb[:, h, :], start=True, stop=False, reads=['BKn0', 'Usb'], writes=['B7'], inc=False)
            MM(s, o, BKn[:, 1, h * 64:(h + 1) * 64], vh, start=False, stop=True, reads=['BKn1', nk('Vn')], writes=['B7'], inc=(h == NH - 1))
        CP(s, 'dve', Ysb[:], bank(4, 64), ['B4'], ['Ysb'])
        for h in range(NH):
            STT(s, S0[:, h, :], S0[:, h, :], PT[:, h, 63:64], PS[7][0:64, h * 64:(h + 1) * 64], ALU.mult, ALU.add,
                ['S0', 'PT', 'B7'], ['S0'])
        RED(s, st[:, 0, :], Ysb[:], ['Ysb'], ['st0'])
        TT(s, 'pool', Ysq[:], Ysb[:], Ysb[:], ALU.mult, ['Ysb'], ['Ysq'])
        RED(s, st[:, 1, :], Ysq[:], ['Ysq'], ['st1'])
        TS(s, 'dve', st[:, 2, :], st[:, 0, :], 1.0 / 64, None, ALU.mult, None, ['st0'], ['st2'])
        TT(s, 'dve', st[:, 3, :], st[:, 2, :], st[:, 2, :], ALU.mult, ['st2'], ['st3'])
        STT(s, st[:, 4, :], st[:, 1, :], 1.0 / 64, st[:, 3, :], ALU.mult, ALU.subtract, ['st1', 'st3'], ['st4'])
        ACTF(s, st[:, 5, :], st[:, 4, :], AF.Ln, ['st4', 'epst'], ['st5'], bias=epst[:], scale=1.0)
        ACTF(s, st[:, 5, :], st[:, 5, :], AF.Exp, ['st5'], ['st5'], scale=-0.5)
        for h in range(NH):
            TS(s, 'dve', Yn[:, h, :], Ysb[:, h, :], st[:, 2, h:h + 1], st[:, 5, h:h + 1], ALU.subtract, ALU.mult,
               ['Ysb', 'st2', 'st5'], ['Yn'])
        ynf = Yn[:].rearrange("p h x -> p (h x)")
        yo = Yo[ci % 2]
        yok = ('Yo', ci % 2)
        TT(s, 'pool', ynf, ynf, K['LGB'][:, 0, :], ALU.mult, ['Yn', 'LGB'], ['Yn'])
        TT(s, 'pool', ynf, ynf, K['LGB'][:, 1, :], ALU.add, ['Yn', 'LGB'], ['Yn'])
        TT(s, 'pool', ynf, ynf, nv('BONn'), ALU.add, ['Yn', nk('BONn')], ['Yn'])
        TT(s, 'pool', yo[:], ynf, nv('Gn'), ALU.mult, ['Yn', nk('Gn')], [yok])
        s.dma('sp', y[ci * C:(ci + 1) * C, :], yo[:], ('so', ci % 2), reads=[yok], writes=[('yd', ci % 2)])

    load_group(0)
    for gi in range(ngrp):
        if gi + 1 < ngrp:
            load_group(gi + 1)
        for cj in range(GC):
            chunk(gi * GC + cj)
    s.wait_all('sp', [('yd', 0), ('yd', 1)])


def host_consts_0b(lnx_g, lnx_b):
    j = np.arange(64)[:, None]
    x = np.arange(64)[None, :]
    su = (x > j).astype(np.float32)
    iu = (x >= j).astype(np.float32)
    sl = (x < j).astype(np.float32)
    MU1 = np.broadcast_to(np.concatenate([su, iu], 1)[:, None, :], (64, NH, 128))
    MLS = np.broadcast_to(sl[:, None, :], (64, NH, 64))
    I4 = np.broadcast_to(np.eye(64, dtype=np.float32)[:, None, :], (64, NH, 64))
    TRI = np.concatenate([iu, su], 1)
    UPS = sl
    LGB = np.broadcast_to(np.stack([lnx_g, lnx_b])[None], (64, 2, NH * 64))
    return {k: np.ascontiguousarray(v, dtype=np.float32) for k, v in
            dict(MU1=MU1, MLS=MLS, I4=I4, TRI=TRI, UPS=UPS, LGB=LGB).items()}


def host_inputs_0b(arrT, lnx_g, lnx_b):
    T = arrT['oR'].shape[1]
    tr = lambda a: np.ascontiguousarray(a.reshape(NH, 64, T).transpose(1, 0, 2))
    na = lambda a: np.ascontiguousarray(a.T)
    d = dict(RT=tr(arrT['oR']), KT=tr(arrT['oK']), AT=tr(arrT['oA']), BT=tr(arrT['oB']),
             Wn=na(arrT['oW']), Bn=na(arrT['oB']), Kn=na(arrT['oK']), Vn=na(arrT['oV']),
             BONn=na(arrT['oBon']), Gn=na(arrT['oG']))
    d.update(host_consts_0b(lnx_g, lnx_b))
    return d


MMDT = BF16


def build_0b(T=8192, GP=2):
    nc = bass.Bass('TRN2', target_bir_lowering=False)
    dt = nc.dram_tensor
    tin = {n: dt(n, [64, NH, T], F32, kind="ExternalInput").ap() for n in ('RT', 'KT', 'AT', 'BT')}
    nin = {n: dt(n, [T, NH * 64], F32, kind="ExternalInput").ap() for n in ('Wn', 'Bn', 'Kn', 'Vn', 'BONn', 'Gn')}
    cst = {n: dt(n, shp, F32, kind="ExternalInput").ap() for n, shp in
           (('MU1', [128, NH, 128]), ('MLS', [128, NH, 64]), ('I4', [128, NH, 64]), ('TRI', [128, 128]), ('UPS', [128, 64]),
            ('LGB', [128, 2, NH * 64]))}
    y = dt("y", [T, NH * 64], F32, kind="ExternalOutput").ap()
    s = Sched(nc)
    emit_0b(s, tin, nin, cst, y, T, GP)
    s.build()
    return nc


def emit_0b(s, tin, nin, cst, y, T, GP):
    K = {}
    for n, shp in (('MU1', [128, NH, 128]), ('MLS', [128, NH, 64]), ('I4', [128, NH, 64]), ('TRI', [128, 128]), ('UPS', [128, 64]),
                   ('LGB', [128, 2, NH * 64])):
        K[n] = s.sbuf('K_' + n, shp, F32)
        s.dma('sp', K[n][:], cst[n], ('k', n), writes=[n])
    epst = s.sbuf('epst', [128, 1], F32)
    s.op('dve', lambda e: e.memset(epst[:], RWKV_EPS), writes=['epst'])
    S0 = s.sbuf('S0', [128, NH, 64], MMDT)
    s.op('dve', lambda e: e.memset(S0[:], 0.0), writes=[('S0', 0), ('S0', 1)])
    PS = [s.psum('PS%d' % i, [128, 512]) for i in range(8)]
    HS = [slice(0, 64), slice(64, 128)]

    def bank(i, w):
        return PS[i][:, 0:NH * w].rearrange("p (h x) -> p h x", h=NH)
    B2 = lambda k: [(k, 0), (k, 1)]
    GT = GP * 2 * C
    tbuf = {n: [s.sbuf('t_%s%d' % (n, i), [128, NH, GP, C], F32) for i in range(2)] for n in tin}
    nbuf = {n: [s.sbuf('n_%s%d' % (n, i), [128, GP, NH * 64], MMDT if n == 'Vn' else F32) for i in range(2)] for n in nin}
    f4 = lambda name, w=64, dt_=F32: s.sbuf(name, [128, NH, w], dt_)
    PT, PINV, PPREV = f4('PT'), f4('PINV'), f4('PPREV')
    E1 = s.sbuf('E1', [128, NH * 64], F32)
    AR = s.sbuf('AR', [128, NH, 2, 64], MMDT)
    BK = s.sbuf('BK', [128, NH, 2, 64], MMDT)
    BKn = s.sbuf('BKn', [128, 2, NH * 64], MMDT)
    DG = f4('DG', 64, MMDT)
    X1, X2 = f4('X1', 128, MMDT), f4('X2', 128, MMDT)
    Nb = [f4('N0', 64, MMDT), f4('N1', 64, MMDT)]
    NTb = [f4('NT0', 64, MMDT), f4('NT1', 64, MMDT)]
    Rb = [f4('R0', 64, MMDT), f4('R1', 64, MMDT)]
    Wsb, Usb = f4('Wsb', 64, MMDT), f4('Usb', 64, MMDT)
    Ysb, Ysq, Yn = f4('Ysb'), f4('Ysq'), f4('Yn')
    st = s.sbuf('st', [128, 6, NH], F32)
    Yo = [s.sbuf('Yo%d' % i, [128, NH * 64], F32) for i in range(2)]
    npair = T // (2 * C)
    ngrp = npair // GP

    def load_group(gi):
        b = gi % 2
        t0 = gi * GT
        for n in tin:
            src = tin[n][:, :, t0:t0 + GT].rearrange("k h (g two t) -> k h g two t", two=2, t=C)
            for hf in range(2):
                s.dma('sp', tbuf[n][b][HS[hf], :, :, :], src[:, :, :, hf, :], ('lt', n, b, hf), writes=[('t', n, b, hf)])
        for n in nin:
            s.dma('pool', nbuf[n][b][:], nin[n][t0:t0 + GT, :].rearrange("(c p) n -> p c n", p=128), ('ln', n, b),
                  writes=[('n', n, b)])

    def pair(pi):
        gi, pj = divmod(pi, GP)
        b = gi % 2
        tv = lambda n: tbuf[n][b][:, :, pj, :]
        tk = lambda n: [('t', n, b, 0), ('t', n, b, 1)]
        nv = lambda n: nbuf[n][b][:, pj, :]
        nk = lambda n: ('n', n, b)
        for hf in range(2):
            P_ = HS[hf]
            for h in range(NH):
                MM(s, PS[0][P_, h * 128:(h + 1) * 128], nbuf['Wn'][b][P_, pj, h * 64:(h + 1) * 64], K['TRI'][P_, :],
                   reads=[nk('Wn'), 'TRI'], writes=['B0'], inc=(h == NH - 1 and hf == 1))
        for hf in range(2):
            P_ = HS[hf]
            MM(s, PS[1][P_, 0:NH * 64], K['UPS'][P_, :], nbuf['Wn'][b][P_, pj, :], reads=[nk('Wn'), 'UPS'], writes=['B1'],
               inc=(hf == 1))
        cumI = bank(0, 128)[:, :, 0:64]
        cumS = bank(0, 128)[:, :, 64:128]
        ACTF(s, PT[:], cumI, AF.Exp, ['B0'], ['PT'], scale=-C0)
        ACTF(s, PINV[:], cumI, AF.Exp, ['B0'], ['PINV'], scale=C0)
        ACTF(s, PPREV[:], cumS, AF.Exp, ['B0'], ['PPREV'], scale=-C0)
        ACTF(s, E1[:], PS[1][:, 0:NH * 64], AF.Exp, ['B1'], ['E1'], scale=-C0)
        TT(s, 'dve', AR[:, :, 0, :], tv('AT'), PPREV[:], ALU.mult, tk('AT') + ['PPREV'], ['AR0'])
        TT(s, 'pool', AR[:, :, 1, :], tv('RT'), PT[:], ALU.mult, tk('RT') + ['PT'], ['AR1'])
        TT(s, 'dve', BK[:, :, 0, :], tv('BT'), PINV[:], ALU.mult, tk('BT') + ['PINV'], ['BK0'])
        TT(s, 'pool', BK[:, :, 1, :], tv('KT'), PINV[:], ALU.mult, tk('KT') + ['PINV'], ['BK1'])
        TT(s, 'pool', BKn[:, 0, :], nv('Bn'), E1[:], ALU.mult, [nk('Bn'), 'E1'], ['BKn0'])
        TT(s, 'pool', BKn[:, 1, :], nv('Kn'), E1[:], ALU.mult, [nk('Kn'), 'E1'], ['BKn1'])
        for h in range(NH):
            TS(s, 'pool', DG[:, h, :], K['I4'][:, h, :], PT[:, h, 63:64], None, ALU.mult, None, ['I4', 'PT'], ['DG'])
        for hf in range(2):
            P_ = HS[hf]
            last = (hf == 1)
            for h in range(NH):
                arh = AR[P_, h, :, :].rearrange("p a x -> p (a x)")
                MM(s, PS[2][P_, h * 128:(h + 1) * 128], BK[P_, h, 0, :], arh, reads=['BK0', 'AR0', 'AR1'], writes=B2('B2'),
                   inc=(h == NH - 1 and last))
            for h in range(NH):
                arh = AR[P_, h, :, :].rearrange("p a x -> p (a x)")
                MM(s, PS[3][P_, h * 128:(h + 1) * 128], BK[P_, h, 1, :], arh, reads=['BK1', 'AR0', 'AR1'], writes=B2('B3'),
                   inc=(h == NH - 1 and last))
            for h in range(NH):
                MM(s, PS[4][P_, h * 64:(h + 1) * 64], AR[P_, h, 0, :], BK[P_, h, 0, :], reads=['BK0', 'AR0'], writes=B2('B4'),
                   inc=(h == NH - 1 and last))
        TT(s, 'dve', X1[:], bank(2, 128), K['MU1'][:], ALU.mult, B2('B2') + ['MU1'], ['X1'])
        TT(s, 'dve', X2[:], bank(3, 128), K['MU1'][:], ALU.mult, B2('B3') + ['MU1'], ['X2'])
        TT(s, 'dve', NTb[0][:], bank(4, 64), K['MLS'][:], ALU.mult, B2('B4') + ['MLS'], [('NT', 0)])
        CP(s, 'pool', Nb[0][:], X1[:, :, 0:64], ['X1'], [('N', 0)])
        TT(s, 'pool', Rb[0][:], X1[:, :, 0:64], K['I4'][:], ALU.add, ['X1', 'I4'], [('R', 0)])
        cur = 0
        for lv in range(5):
            nx = 1 - cur
            if lv < 4:
                for hf in range(2):
                    P_ = HS[hf]
                    for h in range(NH):
                        MM(s, PS[5][P_, h * 64:(h + 1) * 64], NTb[cur][P_, h, :], Nb[cur][P_, h, :],
                           reads=[('NT', cur), ('N', cur)], writes=['B5'], inc=(h == NH - 1 and hf == 1))
            for hf in range(2):
                P_ = HS[hf]
                for h in range(NH):
                    MM(s, PS[6][P_, h * 64:(h + 1) * 64], Nb[cur][P_, h, :], NTb[cur][P_, h, :],
                       reads=[('NT', cur), ('N', cur)], writes=['B6'], inc=(h == NH - 1 and hf == 1))
            if lv < 4:
                CP(s, 'act', Nb[nx][:], bank(5, 64), ['B5'], [('N', nx)])
            CP(s, 'dve', NTb[nx][:], bank(6, 64), ['B6'], [('NT', nx)])
            for hf in range(2):
                P_ = HS[hf]
                for h in range(NH):
                    MM(s, PS[7][P_, h * 64:(h + 1) * 64], NTb[nx][P_, h, :], Rb[cur][P_, h, :],
                       reads=[('NT', nx), ('R', cur)], writes=B2('B7'), inc=(h == NH - 1 and hf == 1))
            TT(s, 'dve', Rb[nx][:], Rb[cur][:], bank(7, 64), ALU.add, [('R', cur)] + B2('B7'), [('R', nx)])
            cur = nx
        R = Rb[cur]
        Rk = ('R', cur)
        for hf in range(2):
            P_ = HS[hf]
            Q_ = HS[1 - hf]
            vsl = lambda h: nbuf['Vn'][b][P_, pj, h * 64:(h + 1) * 64]
            for h in range(NH):
                o = PS[2][P_, h * 64:(h + 1) * 64]
                MM(s, o, X2[P_, h, 0:64], vsl(h), start=True, stop=False, reads=['X2', nk('Vn')], writes=[('B2', hf)], inc=False)
                MM(s, o, AR[P_, h, 0, :], S0[P_, h, :], start=False, stop=True, reads=['AR0', ('S0', hf)], writes=[('B2', hf)],
                   inc=(h == NH - 1))
            CP(s, 'dve', Wsb[P_, :, :], PS[2][P_, 0:NH * 64].rearrange("p (h x) -> p h x", h=NH), [('B2', hf)], [('Wsb', hf)])
            for h in range(NH):
                MM(s, PS[3][P_, h * 64:(h + 1) * 64], R[P_, h, :], Wsb[P_, h, :], reads=[Rk, ('Wsb', hf)], writes=[('B3', hf)],
                   inc=(h == NH - 1))
            CP(s, 'dve', Usb[P_, :, :], PS[3][P_, 0:NH * 64].rearrange("p (h x) -> p h x", h=NH), [('B3', hf)], [('Usb', hf)])
            for h in range(NH):
                o = PS[7][Q_, h * 64:(h + 1) * 64]
                MM(s, o, DG[P_, h, :], S0[P_, h, :], start=True, stop=False, reads=['DG', ('S0', hf)], writes=[('B7', 1 - hf)], inc=False)
                MM(s, o, BKn[P_, 0, h * 64:(h + 1) * 64], Usb[P_, h, :], start=False, stop=False, reads=['BKn0', ('Usb', hf)],
                   writes=[('B7', 1 - hf)], inc=False)
                MM(s, o, BKn[P_, 1, h * 64:(h + 1) * 64], vsl(h), start=False, stop=True, reads=['BKn1', nk('Vn')],
                   writes=[('B7', 1 - hf)], inc=(h == NH - 1))
            for h in range(NH):
                o = PS[4][P_, h * 64:(h + 1) * 64]
                MM(s, o, AR[P_, h, 1, :], S0[P_, h, :], start=True, stop=False, reads=['AR1', ('S0', hf)], writes=[('B4', hf)], inc=False)
                MM(s, o, X1[P_, h, 64:128], Usb[P_, h, :], start=False, stop=False, reads=['X1', ('Usb', hf)], writes=[('B4', hf)], inc=False)
                MM(s, o, X2[P_, h, 64:128], vsl(h), start=False, stop=True, reads=['X2', nk('Vn')], writes=[('B4', hf)],
                   inc=(h == NH - 1))
            CP(s, 'dve', S0[Q_, :, :], PS[7][Q_, 0:NH * 64].rearrange("p (h x) -> p h x", h=NH), [('B7', 1 - hf)], [('S0', 1 - hf)])
        CP(s, 'dve', Ysb[:], bank(4, 64), B2('B4'), ['Ysb'])
        RED(s, st[:, 0, :], Ysb[:], ['Ysb'], ['st0'])
        TT(s, 'pool', Ysq[:], Ysb[:], Ysb[:], ALU.mult, ['Ysb'], ['Ysq'])
        RED(s, st[:, 1, :], Ysq[:], ['Ysq'], ['st1'])
        TS(s, 'dve', st[:, 2, :], st[:, 0, :], 1.0 / 64, None, ALU.mult, None, ['st0'], ['st2'])
        TT(s, 'dve', st[:, 3, :], st[:, 2, :], st[:, 2, :], ALU.mult, ['st2'], ['st3'])
        STT(s, st[:, 4, :], st[:, 1, :], 1.0 / 64, st[:, 3, :], ALU.mult, ALU.subtract, ['st1', 'st3'], ['st4'])
        ACTF(s, st[:, 5, :], st[:, 4, :], AF.Ln, ['st4', 'epst'], ['st5'], bias=epst[:], scale=1.0)
        ACTF(s, st[:, 5, :], st[:, 5, :], AF.Exp, ['st5'], ['st5'], scale=-0.5)
        for h in range(NH):
            TS(s, 'dve', Yn[:, h, :], Ysb[:, h, :], st[:, 2, h:h + 1], st[:, 5, h:h + 1], ALU.subtract, ALU.mult,
               ['Ysb', 'st2', 'st5'], ['Yn'])
        ynf = Yn[:].rearrange("p h x -> p (h x)")
        yo = Yo[pi % 2]
        yok = ('Yo', pi % 2)
        TT(s, 'pool', ynf, ynf, K['LGB'][:, 0, :], ALU.mult, ['Yn', 'LGB'], ['Yn'])
        TT(s, 'pool', ynf, ynf, K['LGB'][:, 1, :], ALU.add, ['Yn', 'LGB'], ['Yn'])
        TT(s, 'pool', ynf, ynf, nv('BONn'), ALU.add, ['Yn', nk('BONn')], ['Yn'])
        TT(s, 'pool', yo[:], ynf, nv('Gn'), ALU.mult, ['Yn', nk('Gn')], [yok])
        s.dma('sp', y[pi * 2 * C:(pi + 1) * 2 * C, :], yo[:], ('so', pi % 2), reads=[yok], writes=[('yd', pi % 2)])

    load_group(0)
    for gi in range(ngrp):
        if gi + 1 < ngrp:
            load_group(gi + 1)
        for pj in range(GP):
            pair(gi * GP + pj)
    s.wait_all('sp', [('yd', 0), ('yd', 1)])


def host_consts_0b(lnx_g, lnx_b):
    j = np.arange(64)[:, None]
    x = np.arange(64)[None, :]
    su = (x > j).astype(np.float32)
    iu = (x >= j).astype(np.float32)
    sl = (x < j).astype(np.float32)
    two = lambda a: np.concatenate([a, a], axis=0)
    MU1 = two(np.broadcast_to(np.concatenate([su, iu], 1)[:, None, :], (64, NH, 128)))
    MLS = two(np.broadcast_to(sl[:, None, :], (64, NH, 64)))
    I4 = two(np.broadcast_to(np.eye(64, dtype=np.float32)[:, None, :], (64, NH, 64)))
    TRI = two(np.concatenate([iu, su], 1))
    UPS = two(sl)
    LGB = np.broadcast_to(np.stack([lnx_g, lnx_b])[None], (128, 2, NH * 64))
    return {k: np.ascontiguousarray(v, dtype=np.float32) for k, v in
            dict(MU1=MU1, MLS=MLS, I4=I4, TRI=TRI, UPS=UPS, LGB=LGB).items()}


def host_inputs_0b(arrT, lnx_g, lnx_b):
    T = arrT['oR'].shape[1]
    tr = lambda a: np.ascontiguousarray(a.reshape(NH, 64, T).transpose(1, 0, 2))
    na = lambda a: np.ascontiguousarray(a.T)
    d = dict(RT=tr(arrT['oR']), KT=tr(arrT['oK']), AT=tr(arrT['oA']), BT=tr(arrT['oB']),
             Wn=na(arrT['oW']), Bn=na(arrT['oB']), Kn=na(arrT['oK']), Vn=na(arrT['oV']),
             BONn=na(arrT['oBon']), Gn=na(arrT['oG']))
    d.update(host_consts_0b(lnx_g, lnx_b))
    return d


D = 1024
NE = 32
GE = 4
ALPHA = (2.0 * 2) ** 0.25
LN_EPS = 1e-5


def build_C(ntok=4096, st=1024, level=9):
    nc = bass.Bass('TRN2', target_bir_lowering=False)
    dt = nc.dram_tensor
    xin = dt("xin", [ntok, D], F32, kind="ExternalInput").ap()
    ymT = dt("ymT", [D, ntok], F32, kind="ExternalInput").ap()
    w_out = dt("w_out", [D, D], F32, kind="ExternalInput").ap()
    wr = dt("wr", [D, 36], F32, kind="ExternalInput").ap()
    br = dt("br", [1, 36], F32, kind="ExternalInput").ap()
    lnp = dt("lnp", [128, 4, D], F32, kind="ExternalInput").ap()
    ident_d = dt("ident", [128, 128], F32, kind="ExternalInput").ap()
    sel_d = dt("sel", [32, NE * 128], F32, kind="ExternalInput").ap()
    e_w1 = dt("e_w1", [NE, D, 128], F32, kind="ExternalInput").ap()
    e_w3 = dt("e_w3", [NE, D, 128], F32, kind="ExternalInput").ap()
    e_w2 = dt("e_w2", [NE, 128, D], F32, kind="ExternalInput").ap()
    xout = dt("xout", [ntok, D], F32, kind="ExternalOutput").ap()
    s = Sched(nc)
    emit_C(s, xin, ymT, w_out, wr, br, lnp, ident_d, sel_d, e_w1, e_w3, e_w2, xout, ntok, st, level)
    s.build()
    return nc


def emit_C(s, xin, ymT, w_out, wr, br, lnp, ident_d, sel_d, e_w1, e_w3, e_w2, xout, ntok, st, level=9):
    wout_bf = s.sbuf('wout_bf', [128, 8, D], BF16)
    wr_f = s.sbuf('wr_f', [128, 8, 36], F32)
    br_f = s.sbuf('br_f', [1, 36], F32)
    ones1 = s.sbuf('ones1', [1, 128], F32)
    LNP = s.sbuf('LNP', [128, 4, D], F32)
    ident = s.sbuf('ident', [128, 128], F32)
    sel = s.sbuf('sel', [32, NE * 128], BF16)
    for k in range(8):
        s.dma('pool', wout_bf[:, k, :], w_out[k * 128:(k + 1) * 128, :], 'c0', writes=[('wout_bf', k)])
    s.dma('sp', wr_f[:], wr.rearrange("(k p) n -> p k n", p=128), 'c1', writes=['wr_f'])
    s.dma('sp', br_f[:], br, 'c2', writes=['br_f'])
    for a in range(4):
        s.dma('sp', LNP[:, a, :], lnp[:, a, :], ('c3', a), writes=[('LNP', a)])
    s.dma('sp', ident[:], ident_d, 'c4', writes=['ident'])
    s.dma('pool', sel[:], sel_d, 'c5', writes=['sel'])
    s.op('dve', lambda e: e.memset(ones1[:], 1.0), writes=['ones1'])
    epst = s.sbuf('epst', [128, 1], F32)
    s.op('dve', lambda e: e.memset(epst[:], LN_EPS), writes=['epst'])
    WOK = [('wout_bf', k) for k in range(8)]

    nst = ntok // st
    nsub = st // 128
    ntt = st // 256
    F = s.sbuf('F', [128, nsub, D], F32)
    xaT = s.sbuf('xaT', [128, 8, st], BF16)
    gT = s.sbuf('gT', [32, st], BF16)
    P = [s.psum('P%d' % i, [128, 512]) for i in range(8)]
    ym_bf = [s.sbuf('ym_bf%d' % i, [128, 8, 128], BF16) for i in range(2)]
    xin_t = [s.sbuf('xin_t%d' % i, [128, D], F32) for i in range(2)]
    z_t = [s.sbuf('z_t%d' % i, [128, D], F32) for i in range(2)]
    xa_t = [s.sbuf('xa_t%d' % i, [128, D], F32) for i in range(2)]
    xaTf = [s.sbuf('xaTf%d' % i, [128, 8, 128], F32) for i in range(2)]
    st6 = [s.sbuf('st6_%d' % i, [128, 2, 6], F32) for i in range(2)]
    mv = [s.sbuf('mv%d' % i, [128, 2], F32) for i in range(2)]
    rstd = [s.sbuf('rstd%d' % i, [128, 1], F32) for i in range(2)]
    nmr = [s.sbuf('nmr%d' % i, [128, 1], F32) for i in range(2)]
    lg = [s.sbuf('lg%d' % i, [128, 36], F32) for i in range(2)]
    rt = [s.sbuf('rt%d' % i, [128, 16], F32) for i in range(2)]
    r8 = [s.sbuf('r8_%d' % i, [128, 5, 8], F32) for i in range(2)]
    gates = [s.sbuf('gates%d' % i, [128, 32], F32) for i in range(2)]
    W1g = [s.sbuf('W1g%d' % i, [128, GE, 8, 128], BF16) for i in range(2)]
    W3g = [s.sbuf('W3g%d' % i, [128, GE, 8, 128], BF16) for i in range(2)]
    W2g = [s.sbuf('W2g%d' % i, [128, GE, D], BF16) for i in range(2)]
    s1 = [s.sbuf('s1_%d' % i, [128, 256], F32) for i in range(2)]
    uu = [s.sbuf('uu_%d' % i, [128, 256], F32) for i in range(2)]
    hg = [s.sbuf('hg_%d' % i, [128, 256], BF16) for i in range(2)]
    o_t = [s.sbuf('o_t%d' % i, [128, D], F32) for i in range(2)]

    def layer_norm_tile(src, srck, dst, dstk, gi, pi, eng_gb='pool'):
        st_, mv_, rs_, nm_ = st6[pi], mv[pi], rstd[pi], nmr[pi]
        for hh in range(2):
            s.op('dve', lambda e, hh=hh: e.bn_stats(out=st_[:, hh, :], in_=src[:, hh * 512:(hh + 1) * 512]),
                 reads=[srck], writes=[('st6', pi, hh)])
        s.op('dve', lambda e: e.bn_aggr(out=mv_[:], in_=st_[:].rearrange("p a b -> p (a b)")),
             reads=[('st6', pi, 0), ('st6', pi, 1)], writes=[('mv', pi)])
        s.op('act', lambda e: e.activation(out=rs_[:], in_=mv_[:, 1:2], func=AF.Ln, bias=epst[:], scale=1.0),
             reads=[('mv', pi), 'epst'], writes=[('rstd', pi)])
        s.op('act', lambda e: e.activation(out=rs_[:], in_=rs_[:], func=AF.Exp, scale=-0.5),
             reads=[('rstd', pi)], writes=[('rstd', pi)])
        s.op('dve', lambda e: e.scalar_tensor_tensor(out=nm_[:], in0=mv_[:, 0:1], scalar=-1.0, in1=rs_[:],
                                                     op0=ALU.mult, op1=ALU.mult),
             reads=[('mv', pi), ('rstd', pi)], writes=[('nmr', pi)])
        s.op('act', lambda e: e.activation(out=dst[:], in_=src[:], func=AF.Identity, bias=nm_[:], scale=rs_[:]),
             reads=[srck, ('rstd', pi), ('nmr', pi)], writes=[dstk])
        s.op(eng_gb, lambda e: e.tensor_tensor(out=dst[:], in0=dst[:], in1=LNP[:, gi, :], op=ALU.mult),
             reads=[dstk, ('LNP', gi)], writes=[dstk])
        s.op(eng_gb, lambda e: e.tensor_tensor(out=dst[:], in0=dst[:], in1=LNP[:, gi + 1, :], op=ALU.add),
             reads=[dstk, ('LNP', gi + 1)], writes=[dstk])

    wl_cnt = [0]

    def load_group(gi):
        b = wl_cnt[0] % 2
        wl_cnt[0] += 1
        for el in range(GE):
            ex = gi * GE + el
            s.dma('pool', W1g[b][:, el, :, :], e_w1[ex].rearrange("(k p) j -> p k j", p=128), ('w1', b, el),
                  writes=[('W1g', b, el)])
            s.dma('pool', W3g[b][:, el, :, :], e_w3[ex].rearrange("(k p) j -> p k j", p=128), ('w3', b, el),
                  writes=[('W3g', b, el)])
            s.dma('pool', W2g[b][:, el, :], e_w2[ex], ('w2', b, el), writes=[('W2g', b, el)])
        return b

    ngrp = NE // GE
    for sti in range(nst):
        t0 = sti * st
        def stage_A(sub):
            pi = sub % 2
            tok0 = t0 + sub * 128
            yb = ym_bf[pi]
            ybk = ('ym_bf', pi)
            xt, zt, xat = xin_t[pi], z_t[pi], xa_t[pi]
            s.dma('pool', yb[:], ymT[:, tok0:tok0 + 128].rearrange("(k p) t -> p k t", p=128), ('ym', pi), writes=[ybk])
            s.dma('sp', xt[:], xin[tok0:tok0 + 128, :], ('xin', pi), writes=[('xin_t', pi)])
            for hh in range(2):
                pb = P[2 * pi + hh]
                pk = ('P', 2 * pi + hh)
                for k in range(8):
                    s.op('pe', lambda e, pb=pb, k=k, hh=hh: e.matmul(
                        pb[:], lhsT=yb[:, k, :], rhs=wout_bf[:, k, hh * 512:(hh + 1) * 512],
                        start=(k == 0), stop=(k == 7)), reads=[ybk] + WOK, writes=[pk], inc=(k == 7))
                s.op('dve', lambda e, pb=pb, hh=hh: e.scalar_tensor_tensor(
                    out=zt[:, hh * 512:(hh + 1) * 512], in0=xt[:, hh * 512:(hh + 1) * 512], scalar=ALPHA,
                    in1=pb[:], op0=ALU.mult, op1=ALU.add), reads=[pk, ('xin_t', pi)], writes=[('z_t', pi)])
            layer_norm_tile(zt, ('z_t', pi), xat, ('xa_t', pi), 0, pi)
            s.op('act', lambda e: e.mul(out=F[:, sub, :], in_=xat[:], mul=ALPHA), reads=[('xa_t', pi)], writes=[('F', sub)])

        def stage_B(sub):
            pi = sub % 2
            xat = xa_t[pi]
            xf = xaTf[pi]
            for k in range(8):
                pb = P[4 + 2 * pi + k // 4]
                pk = ('P', 4 + 2 * pi + k // 4)
                s.op('pe', lambda e, pb=pb, k=k: e.transpose(
                    out=pb[:, (k % 4) * 128:(k % 4 + 1) * 128], in_=xat[:, k * 128:(k + 1) * 128], identity=ident[:]),
                    reads=[('xa_t', pi), 'ident'], writes=[pk])
            for hb in range(2):
                pb = P[4 + 2 * pi + hb]
                pk = ('P', 4 + 2 * pi + hb)
                s.op('act', lambda e, pb=pb, hb=hb: e.copy(
                    out=xf[:, hb * 4:(hb + 1) * 4, :], in_=pb[:].rearrange("p (k t) -> p k t", k=4)),
                    reads=[pk], writes=[('xaTf', pi, hb)])
                s.op('pool', lambda e, hb=hb: e.tensor_copy(
                    out=xaT[:, hb * 4:(hb + 1) * 4, sub * 128:(sub + 1) * 128], in_=xf[:, hb * 4:(hb + 1) * 4, :]),
                    reads=[('xaTf', pi, hb)], writes=[('xaT', sub)])
            pb = P[4 + 2 * pi]
            pk = ('P', 4 + 2 * pi)
            for k in range(8):
                s.op('pe', lambda e, k=k: e.matmul(
                    pb[:, 0:36], lhsT=xf[:, k, :], rhs=wr_f[:, k, :], start=(k == 0), stop=False),
                    reads=[('xaTf', pi, k // 4), 'wr_f'], writes=[pk], inc=False)
            s.op('pe', lambda e: e.matmul(pb[:, 0:36], lhsT=ones1[:], rhs=br_f[:], start=False, stop=True),
                 reads=['ones1', 'br_f'], writes=[pk])
            L = lg[pi]
            s.op('act', lambda e: e.copy(out=L[:], in_=pb[:, 0:36]), reads=[pk], writes=[('lg', pi)])

        def stage_C(sub):
            pi = sub % 2
            L = lg[pi]
            Lk = ('lg', pi)
            R = rt[pi]
            Rk = ('rt', pi)
            R8 = r8[pi]
            R8k = ('r8', pi)
            G = gates[pi]
            V = lambda fn, rd, wr_: s.op('dve', fn, reads=rd, writes=wr_)
            V(lambda e: e.reduce_max(out=R[:, 0:1], in_=L[:, 0:4], axis=AX.X), [Lk], [Rk])
            V(lambda e: e.tensor_scalar(out=R[:, 4:8], in0=L[:, 0:4], scalar1=R[:, 0:1], scalar2=None, op0=ALU.is_ge), [Lk, Rk], [Rk])
            V(lambda e: e.tensor_scalar(out=R[:, 1:2], in0=R[:, 0:1], scalar1=-1.0, scalar2=None, op0=ALU.mult), [Rk], [Rk])
            s.op('act', lambda e: e.activation(out=R[:, 8:12], in_=L[:, 0:4], func=AF.Exp, bias=R[:, 1:2], scale=1.0),
                 reads=[Lk, Rk], writes=[Rk])
            V(lambda e: e.reduce_sum(out=R[:, 2:3], in_=R[:, 8:12], axis=AX.X), [Rk], [Rk])
            V(lambda e: e.tensor_scalar(out=R8[:, 0, :], in0=L[:, 4:12], scalar1=R[:, 4:5], scalar2=None, op0=ALU.mult), [Lk, Rk], [R8k])
            for g in range(1, 4):
                V(lambda e, g=g: e.scalar_tensor_tensor(
                    out=R8[:, 0, :], in0=L[:, 4 + 8 * g:12 + 8 * g], scalar=R[:, 4 + g:5 + g], in1=R8[:, 0, :],
                    op0=ALU.mult, op1=ALU.add), [Lk, Rk, R8k], [R8k])
            V(lambda e: e.max(out=R8[:, 1, :], in_=R8[:, 0, :]), [R8k], [R8k])
            V(lambda e: e.tensor_scalar(out=R8[:, 2, :], in0=R8[:, 0, :], scalar1=R8[:, 1, 1:2], scalar2=None, op0=ALU.is_ge), [R8k], [R8k])
            V(lambda e: e.tensor_scalar(out=R[:, 12:13], in0=R8[:, 1, 0:1], scalar1=-1.0, scalar2=None, op0=ALU.mult), [R8k, Rk], [Rk])
            s.op('act', lambda e: e.activation(out=R8[:, 3, :], in_=R8[:, 0, :], func=AF.Exp, bias=R[:, 12:13], scale=1.0),
                 reads=[R8k, Rk], writes=[R8k])
            V(lambda e: e.tensor_tensor(out=R8[:, 4, :], in0=R8[:, 3, :], in1=R8[:, 2, :], op=ALU.mult), [R8k], [R8k])
            V(lambda e: e.reduce_sum(out=R[:, 13:14], in_=R8[:, 4, :], axis=AX.X), [R8k, Rk], [Rk])
            V(lambda e: e.tensor_tensor(out=R[:, 14:15], in0=R[:, 13:14], in1=R[:, 2:3], op=ALU.mult), [Rk], [Rk])
            V(lambda e: e.reciprocal(out=R[:, 15:16], in_=R[:, 14:15]), [Rk], [Rk])
            V(lambda e: e.tensor_scalar(out=R8[:, 3, :], in0=R8[:, 4, :], scalar1=R[:, 15:16], scalar2=None, op0=ALU.mult), [R8k, Rk], [R8k])
            for g in range(4):
                V(lambda e, g=g: e.tensor_scalar(out=G[:, 8 * g:8 * g + 8], in0=R8[:, 3, :], scalar1=R[:, 4 + g:5 + g], scalar2=None,
                                                 op0=ALU.mult), [R8k, Rk], [('gates', pi)])

        def stage_D(sub):
            pi = sub % 2
            G = gates[pi]
            pb2 = P[5 + 2 * pi]
            pk2 = ('P', 5 + 2 * pi)
            s.op('pe', lambda e: e.transpose(out=pb2[0:32, 0:128], in_=G[:], identity=ident[:]),
                 reads=[('gates', pi), 'ident'], writes=[pk2])
            s.op('act', lambda e: e.copy(out=gT[:, sub * 128:(sub + 1) * 128], in_=pb2[0:32, 0:128]),
                 reads=[pk2], writes=[('gT', sub)])

        for step in range(nsub + 3):
            if 0 <= step - 3 < nsub:
                stage_D(step - 3)
            if step < nsub:
                stage_A(step)
            if 0 <= step - 1 < nsub:
                stage_B(step - 1)
            if 0 <= step - 2 < nsub:
                stage_C(step - 2)

        def moe_front(gi, b, tt, el, hb):
            c0 = tt * 256
            ex = gi * GE + el
            p1 = P[4 + 3 * hb]
            p1k = ('P', 4 + 3 * hb)
            p3 = P[5 + hb]
            p3k = ('P', 5 + hb)
            xk = [('xaT', 2 * tt), ('xaT', 2 * tt + 1)]
            for k in range(8):
                s.op('pe', lambda e, k=k: e.matmul(p1[:, 0:256], lhsT=W1g[b][:, el, k, :], rhs=xaT[:, k, c0:c0 + 256],
                                                   start=(k == 0), stop=(k == 7)),
                     reads=[('W1g', b, el)] + xk, writes=[p1k], inc=(k == 7))
            for k in range(8):
                s.op('pe', lambda e, k=k: e.matmul(p3[:, 0:256], lhsT=W3g[b][:, el, k, :], rhs=xaT[:, k, c0:c0 + 256],
                                                   start=(k == 0), stop=(k == 7)),
                     reads=[('W3g', b, el)] + xk, writes=[p3k], inc=False)
            s.op('pe', lambda e: e.matmul(p3[:, 256:512], lhsT=sel[:, ex * 128:(ex + 1) * 128], rhs=gT[:, c0:c0 + 256],
                                          start=True, stop=True), reads=['sel', ('gT', 2 * tt), ('gT', 2 * tt + 1)], writes=[p3k])
            s.op('act', lambda e: e.activation(out=s1[hb][:], in_=p1[:, 0:256], func=AF.Silu), reads=[p1k], writes=[('s1', hb)])
            s.op('dve', lambda e: e.tensor_tensor(out=uu[hb][:], in0=s1[hb][:], in1=p3[:, 256:512], op=ALU.mult),
                 reads=[('s1', hb), p3k], writes=[('uu', hb)])
            s.op('dve', lambda e: e.tensor_tensor(out=hg[hb][:], in0=uu[hb][:], in1=p3[:, 0:256], op=ALU.mult),
                 reads=[('uu', hb), p3k], writes=[('hg', hb)])

        def moe_back(gi, b, tt, el, hb):
            for sb in range(2):
                for hh in range(2):
                    pa = P[2 * sb + hh]
                    pak = ('P', 2 * sb + hh)
                    s.op('pe', lambda e, pa=pa, sb=sb, hh=hh: e.matmul(
                        pa[:], lhsT=hg[hb][:, sb * 128:(sb + 1) * 128], rhs=W2g[b][:, el, hh * 512:(hh + 1) * 512],
                        start=(el == 0), stop=(el == GE - 1)),
                        reads=[('hg', hb), ('W2g', b, el)], writes=[pak], inc=(sb == 1 and hh == 1))
            if el == GE - 1:
                for sb in range(2):
                    sub = 2 * tt + sb
                    for hh in range(2):
                        pa = P[2 * sb + hh]
                        pak = ('P', 2 * sb + hh)
                        s.op('dve', lambda e, pa=pa, sub=sub, hh=hh: e.tensor_tensor(
                            out=F[:, sub, hh * 512:(hh + 1) * 512], in0=F[:, sub, hh * 512:(hh + 1) * 512], in1=pa[:],
                            op=ALU.add), reads=[pak, ('F', sub)], writes=[('F', sub)])

        steps = []
        for gi in range(ngrp if level >= 7 else 0):
            for tt in range(ntt):
                for el in range(GE):
                    steps.append((gi, tt, el))
        pend = None
        gb = {}
        for n_, (gi, tt, el) in enumerate(steps):
            if gi not in gb:
                gb[gi] = load_group(gi)
            cur_ = (gi, gb[gi], tt, el, n_ % 2)
            moe_front(*cur_)
            if pend is not None:
                moe_back(*pend)
            pend = cur_
        if pend is not None:
            moe_back(*pend)
        for sub in range(nsub):
            pi = sub % 2
            ot = o_t[pi]
            if level < 0.3 or (5 <= level < 6):
                s.op('act', lambda e, sub=sub, ot=ot: e.copy(out=ot[:], in_=F[:, sub, :]),
                     reads=[('F', sub)], writes=[('o_t', pi)])
            else:
                layer_norm_tile(F[:, sub, :], ('F', sub), ot, ('o_t', pi), 2, pi)
            s.dma('sp', xout[t0 + sub * 128: t0 + (sub + 1) * 128, :], ot[:], ('st', pi), reads=[('o_t', pi)],
                  writes=[('xout', pi)])
    s.wait_all('sp', [('xout', 0), ('xout', 1)])


def host_inputs_C(xin, ymT, w_out, g1, b1, rg_w, rg_b, re_w, re_b, w1, w3, w2, g2, b2):
    wr = np.ascontiguousarray(np.concatenate([rg_w, re_w], axis=1))
    br = np.ascontiguousarray(np.concatenate([rg_b, re_b])[None, :])
    lnp = np.ascontiguousarray(np.broadcast_to(np.stack([g1, b1, g2, b2])[None], (128, 4, D)))
    ident = np.eye(128, dtype=np.float32)
    sel = np.zeros((32, NE, 128), np.float32)
    for e in range(NE):
        sel[e, e, :] = 1.0
    return dict(xin=np.ascontiguousarray(xin), ymT=np.ascontiguousarray(ymT), w_out=np.ascontiguousarray(w_out),
                wr=wr, br=br, lnp=lnp, ident=ident, sel=sel.reshape(32, NE * 128),
                e_w1=np.ascontiguousarray(w1), e_w3=np.ascontiguousarray(w3), e_w2=np.ascontiguousarray(w2))


HALO1 = 32
CONV_W = 31
LN_EPS1 = 1e-5


def build_1a(ntok=4096, tile=512):
    nc = bass.Bass('TRN2', target_bir_lowering=False)
    dt = nc.dram_tensor
    xT = dt("xT", [1024, HALO1 + ntok], F32, kind="ExternalInput").ap()
    w_ext = dt("w_ext", [1024, 4096], F32, kind="ExternalInput").ap()
    ccols = dt("ccols", [128, 4, 34], F32, kind="ExternalInput").ap()
    rot = dt("rot", [128, 2, ntok], F32, kind="ExternalInput").ap()
    ones_d = dt("ones", [128, 128], F32, kind="ExternalInput").ap()
    outs = {n: dt(n, [512, ntok], F32, kind="ExternalOutput").ap() for n in ('oYC', 'oQ', 'oKr', 'oV', 'oGr')}
    s = Sched(nc)
    emit_1a(s, xT, w_ext, ccols, rot, ones_d, outs, ntok, tile)
    s.build()
    return nc


def emit_1a(s, xT, w_ext, ccols_d, rot_d, ones_d, outs, ntok, T):
    W = s.sbuf('W', [128, 8, 4096], BF16)
    for k in range(8):
        for hf in range(2):
            s.dma('pool', W[:, k, hf * 2048:(hf + 1) * 2048], w_ext[k * 128:(k + 1) * 128, hf * 2048:(hf + 1) * 2048], 'w0',
                  writes=[('W', k, hf)])
    WK = [('W', k, hf) for k in range(8) for hf in range(2)]
    CC = s.sbuf('CC', [128, 4, 34], F32)
    s.dma('sp', CC[:], ccols_d, 'c0', writes=['CC'])
    ONES = s.sbuf('ONES', [128, 128], F32)
    s.dma('sp', ONES[:], ones_d, 'c1', writes=['ONES'])
    epst = s.sbuf('epst', [128, 1], F32)
    s.op('dve', lambda e: e.memset(epst[:], LN_EPS1), writes=['epst'])
    P = [s.psum('P%d' % i, [128, 512]) for i in range(8)]
    xb = [s.sbuf('xb%d' % i, [128, 8, T], BF16) for i in range(2)]
    xh = s.sbuf('xh', [128, 8, HALO1], BF16)
    CA = s.sbuf('CA', [128, T], F32)
    SG = s.sbuf('SG', [128, T], F32)
    CAH = s.sbuf('CAH', [128, 4, HALO1], F32)
    Ub = [s.sbuf('U%d' % i, [128, 4, HALO1 + T], F32) for i in range(2)]
    ACC = s.sbuf('ACC', [128, 4, T], F32)
    SQ = s.sbuf('SQ', [128, T], F32)
    XP = s.sbuf('XP', [128, 16, T], F32)
    ROT = [s.sbuf('ROT%d' % i, [128, 2, T], F32) for i in range(2)]
    MEAN = s.sbuf('MEAN', [128, T], F32)
    MSQ = s.sbuf('MSQ', [128, T], F32)
    RSTD = s.sbuf('RSTD', [128, T], F32)
    t1 = [s.sbuf('t1_%d' % i, [128, T], F32) for i in range(2)]
    t2 = [s.sbuf('t2_%d' % i, [128, T], F32) for i in range(2)]
    OB = {n: [s.sbuf('O_%s%d' % (n, i), [128, T], F32) for i in range(2)] for n in ('oYC', 'oQ', 'oKr', 'oV', 'oGr')}
    ocnt = {n: 0 for n in OB}

    def out_buf(n):
        i = ocnt[n] % 2
        ocnt[n] += 1
        return OB[n][i], (n, i)

    def proj(c, x_, XK, ncol, pb, pk):
        for k in range(8):
            MM(s, pb[:, 0:ncol], W[:, k, c * 128:(c + 1) * 128], x_[:, k, :], start=(k == 0), stop=(k == 7),
               reads=XK + WK, writes=[pk], inc=(k == 7))

    s.dma('pool', xh[:], xT[:, 0:HALO1].rearrange("(k p) t -> p k t", p=128), 'xh', writes=['xh'])
    for c in range(8):
        pb, pk = P[c % 2], ('P', c % 2)
        proj(c, xh, ['xh'], HALO1, pb, pk)
        if c < 4:
            CP(s, 'act', CAH[:, c, :], pb[:, 0:HALO1], [pk], [('CAH', c)])
        else:
            ACTF(s, SG[:, 0:HALO1], pb[:, 0:HALO1], AF.Sigmoid, [pk], ['SG'])
            TT(s, 'dve', Ub[0][:, c - 4, 0:HALO1], CAH[:, c - 4, :], SG[:, 0:HALO1], ALU.mult, [('CAH', c - 4), 'SG'], [('U', 0, c - 4)])

    def tile_body(ti):
        t0 = ti * T
        x_ = xb[ti % 2]
        XK = [('xb', ti % 2)]
        s.dma('pool', x_[:], xT[:, HALO1 + t0:HALO1 + t0 + T].rearrange("(k p) t -> p k t", p=128), ('x', ti % 2), writes=XK)
        rt = ROT[ti % 2]
        rk = ('ROT', ti % 2)
        s.dma('sp', rt[:], rot_d[:, :, t0:t0 + T], ('rot', ti % 2), writes=[rk])
        tsl = slice(t0, t0 + T)
        ub = ti % 2
        U = Ub[ub]
        for cc in range(4):
            pb, pk = P[0], ('P', 0)
            proj(cc, x_, XK, T, pb, pk)
            CP(s, 'act', CA[:], pb[:], [pk], ['CA'])
            pb, pk = P[1], ('P', 1)
            proj(4 + cc, x_, XK, T, pb, pk)
            ACTF(s, SG[:], pb[:], AF.Sigmoid, [pk], ['SG'])
            TT(s, 'pool', U[:, cc, HALO1:HALO1 + T], CA[:], SG[:], ALU.mult, ['CA', 'SG'], [('U', ub, cc)])
        for cc in range(4):
            CP(s, 'pool', Ub[1 - ub][:, cc, 0:HALO1], U[:, cc, T:T + HALO1], [('U', ub, cc)], [('U', 1 - ub, cc)])
        for cc in range(4):
            TS(s, 'dve', ACC[:, cc, :], U[:, cc, 2:2 + T], CC[:, cc, 0:1], CC[:, cc, 31:32], ALU.mult, ALU.add,
               [('U', ub, cc), 'CC'], [('ACC', cc)])
            for j in range(1, CONV_W):
                STT(s, ACC[:, cc, :], U[:, cc, 2 + j:2 + j + T], CC[:, cc, j:j + 1], ACC[:, cc, :], ALU.mult, ALU.add,
                    [('U', ub, cc), 'CC', ('ACC', cc)], [('ACC', cc)])
        order = [(8 + i, i, 1.0) for i in range(4)] + [(24 + i, 8 + i, 1.0) for i in range(4)] + \
                [(12 + i, 4 + i, 0.125) for i in range(4)] + [(28 + i, 12 + i, 0.125) for i in range(4)]
        for n_, (c, slot, sc) in enumerate(order):
            pb, pk = P[n_ % 2], ('P', n_ % 2)
            proj(c, x_, XK, T, pb, pk)
            s.op('act', lambda e, pb=pb, slot=slot, sc=sc: e.mul(out=XP[:, slot, :], in_=pb[:], mul=sc), reads=[pk], writes=[('XP', slot)])
        for i in range(4):
            rows = slice(i * 128, (i + 1) * 128)
            pb, pk = P[0], ('P', 0)
            proj(16 + i, x_, XK, T, pb, pk)
            ob, obk = out_buf('oV')
            CP(s, 'act', ob[:], pb[:], [pk], [obk])
            s.dma('sp', outs['oV'][rows, tsl], ob[:], ('so',) + obk, reads=[obk], writes=[('d',) + obk])
            pb, pk = P[1], ('P', 1)
            proj(20 + i, x_, XK, T, pb, pk)
            ob, obk = out_buf('oGr')
            ACTF(s, ob[:], pb[:], AF.Silu, [pk], [obk])
            s.dma('sp', outs['oGr'][rows, tsl], ob[:], ('so',) + obk, reads=[obk], writes=[('d',) + obk])
        for i in range(8):
            name = 'oQ' if i < 4 else 'oKr'
            rows = slice((i % 4) * 128, (i % 4 + 1) * 128)
            a, b = t1[i % 2], t2[i % 2]
            TT(s, 'pool', a[:], XP[:, i, :], rt[:, 0, :], ALU.mult, [('XP', i), rk], [('t1', i % 2)])
            TT(s, 'pool', b[:], XP[:, 8 + i, :], rt[:, 1, :], ALU.mult, [('XP', 8 + i), rk], [('t2', i % 2)])
            ob, obk = out_buf(name)
            TT(s, 'pool', ob[:], a[:], b[:], ALU.add, [('t1', i % 2), ('t2', i % 2)], [obk])
            s.dma('sp', outs[name][rows, tsl], ob[:], ('so',) + obk, reads=[obk], writes=[('d',) + obk])
        for cc in range(4):
            MM(s, P[2][:], ONES[:], ACC[:, cc, :], start=(cc == 0), stop=(cc == 3), reads=['ONES', ('ACC', cc)], writes=[('P', 2)],
               inc=(cc == 3))
        for cc in range(4):
            s.op('act', lambda e, cc=cc: e.activation(out=SQ[:], in_=ACC[:, cc, :], func=AF.Square), reads=[('ACC', cc)], writes=['SQ'])
            MM(s, P[3][:], ONES[:], SQ[:], start=(cc == 0), stop=(cc == 3), reads=['ONES', 'SQ'], writes=[('P', 3)])
        s.op('act', lambda e: e.mul(out=MEAN[:], in_=P[2][:], mul=1.0 / 512), reads=[('P', 2)], writes=['MEAN'])
        TT(s, 'pool', MSQ[:], MEAN[:], MEAN[:], ALU.mult, ['MEAN'], ['MSQ'])
        STT(s, RSTD[:], P[3][:], 1.0 / 512, MSQ[:], ALU.mult, ALU.subtract, [('P', 3), 'MSQ'], ['RSTD'])
        ACTF(s, RSTD[:], RSTD[:], AF.Ln, ['RSTD', 'epst'], ['RSTD'], bias=epst[:], scale=1.0)
        ACTF(s, RSTD[:], RSTD[:], AF.Exp, ['RSTD'], ['RSTD'], scale=-0.5)
        for cc in range(4):
            rows = slice(cc * 128, (cc + 1) * 128)
            a = t1[cc % 2]
            TT(s, 'pool', a[:], ACC[:, cc, :], MEAN[:], ALU.subtract, [('ACC', cc), 'MEAN'], [('t1', cc % 2)])
            TT(s, 'pool', a[:], a[:], RSTD[:], ALU.mult, [('t1', cc % 2), 'RSTD'], [('t1', cc % 2)])
            ob, obk = out_buf('oYC')
            ACTF(s, ob[:], a[:], AF.Silu, [('t1', cc % 2), 'CC'], [obk], bias=CC[:, cc, 33:34], scale=CC[:, cc, 32:33])
            s.dma('sp', outs['oYC'][rows, tsl], ob[:], ('so',) + obk, reads=[obk], writes=[('d',) + obk])

    for ti in range(ntok // T):
        tile_body(ti)
    s.wait_all('sp', [k for k in s.last_w if isinstance(k, tuple) and k[0] == 'd'])


def rot_perm():
    idx = np.arange(512).reshape(8, 2, 32)[:, ::-1, :].reshape(-1)
    return idx


def host_inputs_1a(xT_halo, w_in, conv_w, conv_b, cln_g, cln_b, pos0, ntok=4096):
    perm = rot_perm()
    w_ext = np.concatenate([w_in, w_in[:, 1024:1536][:, perm], w_in[:, 1536:2048][:, perm]], axis=1)
    ccols = np.zeros((128, 4, 34), np.float32)
    ccols[:, :, 0:31] = conv_w.T.reshape(4, 128, 31).transpose(1, 0, 2)
    ccols[:, :, 31] = conv_b.reshape(4, 128).T
    ccols[:, :, 32] = cln_g.reshape(4, 128).T
    ccols[:, :, 33] = cln_b.reshape(4, 128).T
    half = 32
    inv = (np.float32(10000.0) ** (-np.arange(half, dtype=np.float32) / np.float32(half))).astype(np.float32)
    pos = np.arange(pos0, pos0 + ntok, dtype=np.float32)
    ang = (pos[:, None] * inv[None, :]).astype(np.float32)
    cos, sin = np.cos(ang).astype(np.float32), np.sin(ang).astype(np.float32)
    p = np.arange(128)
    i = p % 32
    second = (p % 64) >= 32
    rot = np.empty((128, 2, ntok), np.float32)
    rot[:, 0, :] = cos.T[i]
    rot[:, 1, :] = np.where(second[:, None], sin.T[i], -sin.T[i])
    return dict(xT=np.ascontiguousarray(xT_halo), w_ext=np.ascontiguousarray(w_ext), ccols=ccols, rot=rot,
                ones=np.ones((128, 128), np.float32))


RC = 128
RH = 4
RET_EPS = 1e-5


def build_1b(T=8192, GCH=2):
    nc = bass.Bass('TRN2', target_bir_lowering=False)
    dt = nc.dram_tensor
    tin = {n: dt(n, [64, RH, T], F32, kind="ExternalInput").ap() for n in ('QT', 'KT')}
    nin = {n: dt(n, [T, RH * 64], F32, kind="ExternalInput").ap() for n in ('Kn', 'Vn', 'Grn')}
    cst = {n: dt(n, shp, F32, kind="ExternalInput").ap() for n, shp in
           (('DM', [128, RH, 128]), ('XI', [64, RH, 128]), ('ZE', [128, RH * 64]), ('GCC', [64, RH]), ('GNB', [128, 2, RH * 64]))}
    y = dt("y", [T, RH * 64], F32, kind="ExternalOutput").ap()
    s = Sched(nc)
    emit_1b(s, tin, nin, cst, y, T, GCH)
    s.build()
    return nc


def emit_1b(s, tin, nin, cst, y, T, GCH):
    K = {}
    for n, shp in (('DM', [128, RH, 128]), ('XI', [64, RH, 128]), ('ZE', [128, RH * 64]), ('GCC', [64, RH]), ('GNB', [128, 2, RH * 64])):
        K[n] = s.sbuf('K_' + n, shp, F32)
        s.dma('sp', K[n][:], cst[n], ('k', n), writes=[n])
    epst = s.sbuf('epst', [128, 1], F32)
    s.op('dve', lambda e: e.memset(epst[:], RET_EPS), writes=['epst'])
    Rst = s.sbuf('Rst', [64, RH, 64], BF16)
    s.op('dve', lambda e: e.memset(Rst[:], 0.0), writes=['Rst'])
    PS = [s.psum('PS%d' % i, [128, 512]) for i in range(4)]
    GT = GCH * RC
    tbuf = {n: [s.sbuf('t_%s%d' % (n, i), [64, RH, GT], BF16) for i in range(2)] for n in tin}
    nbuf = {n: [s.sbuf('n_%s%d' % (n, i), [128, GCH, RH * 64], BF16 if n == 'Vn' else F32) for i in range(2)] for n in nin}
    QX = s.sbuf('QX', [64, RH, RC], BF16)
    KZ = s.sbuf('KZ', [128, RH * 64], BF16)
    SM = s.sbuf('SM', [128, RH, RC], BF16)
    Ysb = s.sbuf('Ysb', [128, RH, 64], F32)
    Ysq = s.sbuf('Ysq', [128, RH, 64], F32)
    Yn = s.sbuf('Yn', [128, RH, 64], F32)
    st = s.sbuf('st', [128, 6, RH], F32)
    Yo = [s.sbuf('Yo%d' % i, [128, RH * 64], F32) for i in range(2)]
    nchunk = T // RC
    ngrp = nchunk // GCH

    def load_group(gi):
        b = gi % 2
        t0 = gi * GT
        for n in tin:
            s.dma('pool', tbuf[n][b][:], tin[n][:, :, t0:t0 + GT], ('lt', n, b), writes=[('t', n, b)])
        for n in nin:
            s.dma('pool', nbuf[n][b][:], nin[n][t0:t0 + GT, :].rearrange("(c p) n -> p c n", p=128), ('ln', n, b),
                  writes=[('n', n, b)])

    def chunk(ci):
        gi, cj = divmod(ci, GCH)
        b = gi % 2
        tv = lambda n: tbuf[n][b][:, :, cj * RC:(cj + 1) * RC]
        tk = lambda n: ('t', n, b)
        nv = lambda n: nbuf[n][b][:, cj, :]
        nk = lambda n: ('n', n, b)
        TT(s, 'pool', QX[:], tv('QT'), K['XI'][:], ALU.mult, [tk('QT'), 'XI'], ['QX'])
        TT(s, 'pool', KZ[:], nv('Kn'), K['ZE'][:], ALU.mult, [nk('Kn'), 'ZE'], ['KZ'])
        for h in range(RH):
            MM(s, PS[0][:, h * RC:(h + 1) * RC], tbuf['KT'][b][:, h, cj * RC:(cj + 1) * RC], tbuf['QT'][b][:, h, cj * RC:(cj + 1) * RC],
               reads=[tk('KT'), tk('QT')], writes=['B0'], inc=(h == RH - 1))
        TT(s, 'dve', SM[:], PS[0][:, :].rearrange("p (h x) -> p h x", h=RH), K['DM'][:], ALU.mult, ['B0', 'DM'], ['SM'])
        for h in range(RH):
            vh = nbuf['Vn'][b][:, cj, h * 64:(h + 1) * 64]
            o = PS[1][:, h * 64:(h + 1) * 64]
            MM(s, o, SM[:, h, :], vh, start=True, stop=False, reads=['SM', nk('Vn')], writes=['B1'], inc=False)
            MM(s, o, QX[:, h, :], Rst[:, h, :], start=False, stop=True, reads=['QX', 'Rst'], writes=['B1'], inc=(h == RH - 1))
        for h in range(RH):
            vh = nbuf['Vn'][b][:, cj, h * 64:(h + 1) * 64]
            MM(s, PS[2][0:64, h * 64:(h + 1) * 64], KZ[:, h * 64:(h + 1) * 64], vh, reads=['KZ', nk('Vn')], writes=['B2'],
               inc=(h == RH - 1))
        CP(s, 'dve', Ysb[:], PS[1][:, 0:RH * 64].rearrange("p (h x) -> p h x", h=RH), ['B1'], ['Ysb'])
        for h in range(RH):
            STT(s, Rst[:, h, :], Rst[:, h, :], K['GCC'][:, h:h + 1], PS[2][0:64, h * 64:(h + 1) * 64], ALU.mult, ALU.add,
                ['Rst', 'GCC', 'B2'], ['Rst'])
        RED(s, st[:, 0, :], Ysb[:], ['Ysb'], ['st0'])
        TT(s, 'pool', Ysq[:], Ysb[:], Ysb[:], ALU.mult, ['Ysb'], ['Ysq'])
        RED(s, st[:, 1, :], Ysq[:], ['Ysq'], ['st1'])
        TS(s, 'dve', st[:, 2, :], st[:, 0, :], 1.0 / 64, None, ALU.mult, None, ['st0'], ['st2'])
        TT(s, 'dve', st[:, 3, :], st[:, 2, :], st[:, 2, :], ALU.mult, ['st2'], ['st3'])
        STT(s, st[:, 4, :], st[:, 1, :], 1.0 / 64, st[:, 3, :], ALU.mult, ALU.subtract, ['st1', 'st3'], ['st4'])
        ACTF(s, st[:, 5, :], st[:, 4, :], AF.Ln, ['st4', 'epst'], ['st5'], bias=epst[:], scale=1.0)
        ACTF(s, st[:, 5, :], st[:, 5, :], AF.Exp, ['st5'], ['st5'], scale=-0.5)
        for h in range(RH):
            TS(s, 'dve', Yn[:, h, :], Ysb[:, h, :], st[:, 2, h:h + 1], st[:, 5, h:h + 1], ALU.subtract, ALU.mult,
               ['Ysb', 'st2', 'st5'], ['Yn'])
        ynf = Yn[:].rearrange("p h x -> p (h x)")
        yo = Yo[ci % 2]
        yok = ('Yo', ci % 2)
        TT(s, 'pool', ynf, ynf, K['GNB'][:, 0, :], ALU.mult, ['Yn', 'GNB'], ['Yn'])
        TT(s, 'pool', ynf, ynf, K['GNB'][:, 1, :], ALU.add, ['Yn', 'GNB'], ['Yn'])
        TT(s, 'pool', yo[:], ynf, nv('Grn'), ALU.mult, ['Yn', nk('Grn')], [yok])
        s.dma('sp', y[ci * RC:(ci + 1) * RC, :], yo[:], ('so', ci % 2), reads=[yok], writes=[('yd', ci % 2)])

    load_group(0)
    for gi in range(ngrp):
        if gi + 1 < ngrp:
            load_group(gi + 1)
        for cj in range(GCH):
            chunk(gi * GCH + cj)
    s.wait_all('sp', [('yd', 0), ('yd', 1)])


def host_inputs_1b(arrT, gn_g, gn_b, head0):
    T = arrT['oQ'].shape[1]
    tr = lambda a: np.ascontiguousarray(a.reshape(RH, 64, T).transpose(1, 0, 2))
    na = lambda a: np.ascontiguousarray(a.T)
    hidx = np.arange(head0, head0 + RH, dtype=np.float32)
    log_gamma = np.log1p(-np.power(np.float32(2.0), -5.0 - hidx)).astype(np.float32)
    idx = np.arange(RC, dtype=np.float32)
    diff = idx[None, :] - idx[:, None]
    DM = np.where(diff[:, None, :] >= 0, np.exp(np.maximum(diff, 0.0)[:, None, :] * log_gamma[None, :, None]), 0.0)
    xi = np.exp((idx + 1.0)[None, :] * log_gamma[:, None])
    zeta = np.exp((RC - 1.0 - idx)[None, :] * log_gamma[:, None])
    gch = np.exp(RC * log_gamma)
    XI = np.broadcast_to(xi[None], (64, RH, RC))
    ZE = np.repeat(zeta.T, 64, axis=1)
    GCC = np.broadcast_to(gch[None], (64, RH))
    GNB = np.broadcast_to(np.stack([gn_g, gn_b])[None], (128, 2, RH * 64))
    f = lambda a: np.ascontiguousarray(a, dtype=np.float32)
    return dict(QT=tr(arrT['oQ']), KT=tr(arrT['oKr']), Kn=na(arrT['oKr']), Vn=na(arrT['oV']), Grn=na(arrT['oGr']),
                DM=f(DM), XI=f(XI), ZE=f(ZE), GCC=f(GCC), GNB=f(GNB))


NTOK = 4096
SHARDS = [(c // 2, (c % 2) * NTOK) for c in range(8)]


def _run(nc, in_maps):
    from concourse.bass_utils import run_bass_kernel_spmd
    return run_bass_kernel_spmd(nc, in_maps, core_ids=list(range(len(in_maps)))).results


def _post_mixer(inp, cur, ymTs, layer, w_out):
    ncC = build_C(ntok=NTOK, st=1024)
    maps = []
    for ci, (b, t0) in enumerate(SHARDS):
        maps.append(host_inputs_C(cur[b, t0:t0 + NTOK], ymTs[ci], w_out, inp['ln_mix_g'][layer], inp['ln_mix_b'][layer],
                                  inp['rg_w'][layer], inp['rg_b'][layer], inp['re_w'][layer], inp['re_b'][layer],
                                  inp['e_w1'][layer], inp['e_w3'][layer], inp['e_w2'][layer],
                                  inp['ln_ffn_g'][layer], inp['ln_ffn_b'][layer]))
    rc = _run(ncC, maps)
    nxt = np.empty_like(cur)
    for ci, (b, t0) in enumerate(SHARDS):
        nxt[b, t0:t0 + NTOK] = rc[ci]['xout']
    return nxt


def layer0(inp, x):
    nc0 = build_0a(ntok=NTOK)
    maps = []
    for (b, t0) in SHARDS:
        xT = np.zeros((D, HALO + NTOK), np.float32)
        xT[:, HALO:] = x[b, t0:t0 + NTOK].T
        if t0 > 0:
            xT[:, :HALO] = x[b, t0 - HALO:t0].T
        maps.append(host_inputs_0a(xT, inp['ev_w_in'][0], inp['ev_mu'][0], inp['ev_w0'][0], inp['ev_a0'][0],
                                   inp['ev_k_k'][0], inp['ev_k_a'][0], inp['ev_r_k'][0].reshape(-1),
                                   inp['ev_pool_scale'][0], inp['ev_w2'][0], inp['ev_a2'][0], inp['ev_g2'][0],
                                   inp['ev_pool_w'][0], t0 == 0))
    r0 = _run(nc0, maps)
    ncb = build_0b(T=2 * NTOK)
    maps = []
    for c in range(8):
        b, j = c // 2, c % 2
        rows = slice(256 * j, 256 * j + 256)
        arrT = {n: np.concatenate([r0[2 * b][n][rows], r0[2 * b + 1][n][rows]], axis=1)
                for n in ('oR', 'oK', 'oV', 'oA', 'oB', 'oW', 'oG', 'oBon')}
        maps.append(host_inputs_0b(arrT, inp['ev_lnx_g'][0][rows], inp['ev_lnx_b'][0][rows]))
    rb = _run(ncb, maps)
    ymTs = []
    for ci, (b, t0) in enumerate(SHARDS):
        ymT = np.empty((D, NTOK), np.float32)
        for j in range(2):
            ymT[256 * j:256 * j + 256] = rb[2 * b + j]['y'][t0:t0 + NTOK].T
        ymT[512:] = r0[ci]['oYP']
        ymTs.append(ymT)
    return _post_mixer(inp, x, ymTs, 0, inp['ev_w_out'][0])


def layer1(inp, x1):
    nc1 = build_1a(ntok=NTOK)
    maps = []
    for (b, t0) in SHARDS:
        xT = np.zeros((D, HALO1 + NTOK), np.float32)
        xT[:, HALO1:] = x1[b, t0:t0 + NTOK].T
        if t0 > 0:
            xT[:, :HALO1] = x1[b, t0 - HALO1:t0].T
        maps.append(host_inputs_1a(xT, inp['od_w_in'][0], inp['od_conv_w'][0], inp['od_conv_b'][0], inp['od_cln_g'][0],
                                   inp['od_cln_b'][0], t0, NTOK))
    r1 = _run(nc1, maps)
    ncb = build_1b(T=2 * NTOK)
    maps = []
    for c in range(8):
        b, j = c // 2, c % 2
        rows = slice(256 * j, 256 * j + 256)
        arrT = {n: np.concatenate([r1[2 * b][n][rows], r1[2 * b + 1][n][rows]], axis=1) for n in ('oQ', 'oKr', 'oV', 'oGr')}
        maps.append(host_inputs_1b(arrT, inp['od_gn_g'][0][rows], inp['od_gn_b'][0][rows], 4 * j))
    rb = _run(ncb, maps)
    ymTs = []
    for ci, (b, t0) in enumerate(SHARDS):
        ymT = np.empty((D, NTOK), np.float32)
        ymT[:512] = r1[ci]['oYC']
        for j in range(2):
            ymT[512 + 256 * j:512 + 256 * j + 256] = rb[2 * b + j]['y'][t0:t0 + NTOK].T
        ymTs.append(ymT)
    return _post_mixer(inp, x1, ymTs, 1, inp['od_w_out'][0])


def kernel(**inp):
    inp = {k: np.asarray(v) for k, v in inp.items()}
    x1 = layer0(inp, inp['x'])
    return layer1(inp, x1)
```

```python
import numpy as np


import concourse.bass as bass
import concourse.mybir as mybir

F32 = mybir.dt.float32
BF16 = mybir.dt.bfloat16
I32 = mybir.dt.int32
U32 = mybir.dt.uint32
AF = mybir.ActivationFunctionType
ALU = mybir.AluOpType
AX = mybir.AxisListType


class _Buf:
    def __init__(self, name, t):
        self.name = name
        self.t = t

    def __getitem__(self, idx):
        return self.t[idx]


class Sched:
    ENGS = ('pe', 'act', 'dve', 'pool', 'sp')

    def __init__(self, nc, self_wait=True):
        self.nc = nc
        self.self_wait = self_wait
        self.ops = {e: [] for e in self.ENGS}
        self.sems = {}
        self.cnt = {}
        self.waited = {e: {} for e in self.ENGS}
        self.last_w = {}
        self.readers = {}
        self.ctx = []
        self.pending_noinc = {e: False for e in self.ENGS}
        for e in ('pe', 'act', 'dve', 'pool'):
            self.sems[e] = nc.alloc_semaphore(name='sem_' + e)
            self.cnt[e] = 0
        self.ntile = 0

    def sbuf(self, name, shape, dtype):
        g = self.nc.sbuf_tensor('sb_' + name, list(shape), dtype)
        t = g.__enter__()
        self.ctx.append(g)
        return _Buf(name, t)

    def psum(self, name, shape, dtype=F32):
        g = self.nc.psum_tensor('ps_' + name, list(shape), dtype)
        t = g.__enter__()
        self.ctx.append(g)
        return _Buf(name, t)

    def dma_sem(self, key):
        if key not in self.sems:
            self.sems[key] = self.nc.alloc_semaphore(name='dsem_%d' % len(self.sems))
            self.cnt[key] = 0
        return key

    def _deps(self, reads, writes):
        deps = []
        for r in reads:
            if r in self.last_w:
                deps.append(self.last_w[r])
        for w in writes:
            if w in self.last_w:
                deps.append(self.last_w[w])
            deps.extend(self.readers.get(w, []))
        return deps

    def _emit_waits(self, eng, deps):
        waits = []
        best = {}
        for (sk, v) in deps:
            if sk == eng and (eng == 'pe' or not self.self_wait):
                continue
            if self.waited[eng].get(sk, 0) >= v:
                continue
            if best.get(sk, 0) < v:
                best[sk] = v
        for sk, v in best.items():
            self.waited[eng][sk] = v
            waits.append((self.sems[sk], v))
        return waits

    def _record(self, ev, reads, writes):
        for r in reads:
            self.readers.setdefault(r, []).append(ev)
        for w in writes:
            self.last_w[w] = ev
            self.readers[w] = []

    def op(self, eng, fn, reads=(), writes=(), inc=True):
        reads = [k for k in reads]
        writes = [k for k in writes]
        waits = self._emit_waits(eng, self._deps(reads, writes))
        sem = self.sems[eng]
        if inc:
            self.cnt[eng] += 1
            ev = (eng, self.cnt[eng])
            self.pending_noinc[eng] = False
        else:
            ev = (eng, self.cnt[eng] + 1)
            self.pending_noinc[eng] = True

        def emit(e, fn=fn, waits=waits, inc=inc, sem=sem):
            for (s, v) in waits:
                e.wait_ge(s, v)
            ins = fn(e)
            if inc:
                ins.then_inc(sem, 1)
        self.ops[eng].append(emit)
        self._record(ev, reads, writes)
        return ev

    def dma(self, eng, out, in_, semkey, reads=(), writes=(), **kw):
        self.dma_sem(semkey)
        reads = list(reads)
        writes = list(writes)
        waits = self._emit_waits(eng, self._deps(reads, writes))
        self.cnt[semkey] += 16
        ev = (semkey, self.cnt[semkey])
        sem = self.sems[semkey]

        def emit(e, waits=waits, sem=sem, out=out, in_=in_, kw=kw):
            for (s, v) in waits:
                e.wait_ge(s, v)
            e.dma_start(out=out, in_=in_, **kw).then_inc(sem, 16)
        self.ops[eng].append(emit)
        self._record(ev, reads, writes)
        return ev

    def wait_all(self, eng, keys):
        deps = []
        for k in keys:
            if k in self.last_w:
                deps.append(self.last_w[k])
        waits = self._emit_waits(eng, deps)

        def emit(e, waits=waits):
            for (s, v) in waits:
                e.wait_ge(s, v)
        self.ops[eng].append(emit)

    def build(self):
        nc = self.nc
        for e in self.ENGS:
            assert not self.pending_noinc[e], 'dangling noinc on ' + e
        with nc.Block() as block:
            @block.tensor
            def _(e):
                for f in self.ops['pe']:
                    f(e)

            @block.scalar
            def _(e):
                for f in self.ops['act']:
                    f(e)

            @block.vector
            def _(e):
                for f in self.ops['dve']:
                    f(e)

            @block.gpsimd
            def _(e):
                for f in self.ops['pool']:
                    f(e)

            @block.sync
            def _(e):
                for f in self.ops['sp']:
                    f(e)
        for g in reversed(self.ctx):
            g.__exit__(None, None, None)


D = 1024
HALO = 16
NCH = 18


def build_0a(ntok=4096, tile=512, dbg=None):
    nc = bass.Bass('TRN2', target_bir_lowering=False)
    dt = nc.dram_tensor
    xT = dt("xT", [D, HALO + ntok], F32, kind="ExternalInput").ap()
    w_in = dt("w_in", [D, 2304], F32, kind="ExternalInput").ap()
    cols = dt("cols", [128, 40], F32, kind="ExternalInput").ap()
    w2 = dt("w2", [64, 512], F32, kind="ExternalInput").ap()
    a2 = dt("a2", [64, 512], F32, kind="ExternalInput").ap()
    g2 = dt("g2", [128, 512], F32, kind="ExternalInput").ap()
    pool_w = dt("pool_w", [4, 128, 128], F32, kind="ExternalInput").ap()
    bones_d = dt("bones", [128, 128], F32, kind="ExternalInput").ap()
    invc_d = dt("invc", [128, 2, 4, tile], F32, kind="ExternalInput").ap()
    outs = {n: dt(n, [512, ntok], F32, kind="ExternalOutput").ap()
            for n in ('oR', 'oK', 'oV', 'oA', 'oB', 'oW', 'oG', 'oBon', 'oYP')}
    s = Sched(nc)
    emit_0a(s, xT, w_in, cols, w2, a2, g2, pool_w, bones_d, invc_d, outs, ntok, tile)
    s.build()
    return nc


OQ = 'sp'


def emit_0a(s, xT, w_in, cols_d, w2_d, a2_d, g2_d, pool_w_d, bones_d, invc_d, outs, ntok, tile):
    T = tile
    W = s.sbuf('W', [128, 8, 2304], BF16)
    for k in range(8):
        s.dma('pool', W[:, k, :], w_in[k * 128:(k + 1) * 128, :], 'w0', writes=[('W', k)])
    WK = [('W', k) for k in range(8)]
    COLS = s.sbuf('COLS', [128, 40], F32)
    s.dma('sp', COLS[:], cols_d, 'c0', writes=['COLS'])
    W2 = s.sbuf('W2', [64, 512], BF16)
    A2 = s.sbuf('A2', [128, 512], BF16)
    G2 = s.sbuf('G2', [128, 512], BF16)
    PW = s.sbuf('PW', [128, 4, 128], BF16)
    BONES = s.sbuf('BONES', [128, 128], BF16)
    INVC = s.sbuf('INVC', [128, 2, 4, T], F32)
    s.dma('pool', W2[:], w2_d, 'c1', writes=['W2'])
    s.dma('pool', A2[64:128, :], a2_d, 'c2', writes=['A2'])
    s.dma('pool', G2[:], g2_d, 'c3', writes=['G2'])
    for g in range(4):
        s.dma('pool', PW[:, g, :], pool_w_d[g], ('c4', g), writes=[('PW', g)])
    s.dma('pool', BONES[:], bones_d, 'c5', writes=['BONES'])
    for a in range(2):
        s.dma('sp', INVC[:, a, :, :], invc_d[:, a, :, :], ('c6', a), writes=[('INVC', a)])
    c24 = s.sbuf('c24', [128, 1], F32)
    s.op('dve', lambda e: e.memset(c24[:], 0.0), writes=['c24'])

    PR = s.sbuf('PR', [128, NCH, HALO + T], F32)
    Z = s.sbuf('Z', [128, 14, T], F32)
    xb = [s.sbuf('xb%d' % i, [128, 8, T], BF16) for i in range(2)]
    xh = s.sbuf('xh', [128, 8, HALO], BF16)
    P = [s.psum('P%d' % i, [128, 512]) for i in range(8)]
    tw = s.sbuf('tw', [64, T], BF16)
    al = s.sbuf('al', [128, T], BF16)
    sg = s.sbuf('sg', [128, T], BF16)
    tmp = [s.sbuf('tmp%d' % i, [128, T], F32) for i in range(4)]
    tb = [s.sbuf('tb%d' % i, [128, T], BF16) for i in range(2)]
    O = {n: [s.sbuf('O_%s%d' % (n, i), [128, T], F32) for i in range(2)] for n in ('oA', 'oB', 'oK', 'oW', 'oG', 'oBon', 'oYP')}
    SS = [s.sbuf('SS%d' % i, [128, HALO + T], F32) for i in range(2)]

    s.dma('pool', xh[:], xT[:, 0:HALO].rearrange("(k p) t -> p k t", p=128), 'xh', writes=['xh'])
    for c in range(NCH):
        pb = P[c % 2]
        pk = ('P', c % 2)
        for k in range(8):
            s.op('pe', lambda e, pb=pb, k=k, c=c: e.matmul(pb[:, 0:HALO], lhsT=W[:, k, c * 128:(c + 1) * 128],
                                                           rhs=xh[:, k, :], start=(k == 0), stop=(k == 7)),
                 reads=['xh'] + WK, writes=[pk], inc=(k == 7))
        s.op('act', lambda e, pb=pb, c=c: e.copy(out=PR[:, c, 0:HALO], in_=pb[:, 0:HALO]), reads=[pk], writes=[('PR', c)])

    ntile = ntok // T
    def tile_body(ti):
        t0 = ti * T
        x_ = xb[ti % 2]
        xk = ('xb', ti % 2)
        s.dma('pool', x_[:], xT[:, HALO + t0:HALO + t0 + T].rearrange("(k p) t -> p k t", p=128), ('x', ti % 2), writes=[xk])
        XK = [xk]
        for c in range(NCH):
            pb = P[c % 2]
            pk = ('P', c % 2)
            for k in range(8):
                s.op('pe', lambda e, pb=pb, k=k, c=c, x_=x_: e.matmul(pb[:], lhsT=W[:, k, c * 128:(c + 1) * 128],
                                                                      rhs=x_[:, k, :], start=(k == 0), stop=(k == 7)),
                     reads=XK + WK, writes=[pk], inc=(k == 7))
            s.op('act', lambda e, pb=pb, c=c: e.copy(out=PR[:, c, HALO:HALO + T], in_=pb[:]), reads=[pk], writes=[('PR', c)])
        for c in range(14):
            d_ = tmp[c % 2]
            dk = ('tmp', c % 2)
            s.op('dve', lambda e, c=c, d_=d_: e.tensor_tensor(out=d_[:], in0=PR[:, c, HALO - 1:HALO - 1 + T],
                                                              in1=PR[:, c, HALO:HALO + T], op=ALU.subtract),
                 reads=[('PR', c)], writes=[dk])
            s.op('dve', lambda e, c=c, d_=d_: e.scalar_tensor_tensor(out=Z[:, c, :], in0=d_[:], scalar=COLS[:, c:c + 1],
                                                                     in1=PR[:, c, HALO:HALO + T], op0=ALU.mult, op1=ALU.add),
                 reads=[dk, ('PR', c), 'COLS'], writes=[('Z', c)])
        s.op('act', lambda e: e.activation(out=tw[:], in_=Z[0:64, 12, :], func=AF.Tanh), reads=[('Z', 12)], writes=['tw'])
        s.op('act', lambda e: e.copy(out=al[64:128, :], in_=Z[64:128, 12, :]), reads=[('Z', 12)], writes=['al'])
        s.op('act', lambda e: e.activation(out=sg[:], in_=Z[:, 13, :], func=AF.Sigmoid), reads=[('Z', 13)], writes=['sg'])
        ob = ti % 2
        for cc in range(4):
            cs = slice(cc * 128, (cc + 1) * 128)
            rows = slice(cc * 128, (cc + 1) * 128)
            tsl = slice(t0, t0 + T)
            s.op('pe', lambda e, cs=cs: e.matmul(P[2][:], lhsT=W2[:, cs], rhs=tw[:], start=True, stop=True),
                 reads=['W2', 'tw'], writes=[('P', 2)])
            s.op('act', lambda e, cc=cc: e.activation(out=O['oW'][ob][:], in_=P[2][:], func=AF.Sigmoid,
                                                     bias=COLS[:, 14 + cc:15 + cc], scale=1.0),
                 reads=[('P', 2), 'COLS'], writes=[('oW', ob)])
            s.dma(OQ, outs['oW'][rows, tsl], O['oW'][ob][:], ('so', 'oW', ob), reads=[('oW', ob)], writes=[('d_oW', ob)])
            s.op('pe', lambda e, cs=cs: e.matmul(P[3][:], lhsT=A2[64:128, cs], rhs=al[64:128, :], start=True, stop=True),
                 reads=['A2', 'al'], writes=[('P', 3)])
            asig = tmp[2]
            s.op('act', lambda e, cc=cc: e.activation(out=asig[:], in_=P[3][:], func=AF.Sigmoid,
                                                     bias=COLS[:, 18 + cc:19 + cc], scale=1.0),
                 reads=[('P', 3), 'COLS'], writes=[('tmp', 2)])
            s.op('pe', lambda e, cs=cs: e.matmul(P[4][:], lhsT=G2[:, cs], rhs=sg[:], start=True, stop=True),
                 reads=['G2', 'sg'], writes=[('P', 4)])
            s.op('act', lambda e: e.copy(out=O['oG'][ob][:], in_=P[4][:]), reads=[('P', 4)], writes=[('oG', ob)])
            s.dma(OQ, outs['oG'][rows, tsl], O['oG'][ob][:], ('so', 'oG', ob), reads=[('oG', ob)], writes=[('d_oG', ob)])
            kk0 = tmp[3]
            s.op('dve', lambda e, cc=cc: e.tensor_scalar(out=kk0[:], in0=Z[:, 4 + cc, :], scalar1=COLS[:, 22 + cc:23 + cc],
                                                         scalar2=None, op0=ALU.mult),
                 reads=[('Z', 4 + cc), 'COLS'], writes=[('tmp', 3)])
            s.op('dve', lambda e: e.tensor_tensor(out=tb[0][:], in0=kk0[:], in1=kk0[:], op=ALU.mult),
                 reads=[('tmp', 3)], writes=[('tb', 0)])
            s.op('pe', lambda e: e.matmul(P[5][:], lhsT=BONES[:], rhs=tb[0][:], start=True, stop=True),
                 reads=['BONES', ('tb', 0)], writes=[('P', 5)])
            rn = tmp[0]
            s.op('dve', lambda e: e.tensor_scalar(out=rn[:], in0=P[5][:], scalar1=1e-24, scalar2=None, op0=ALU.max),
                 reads=[('P', 5)], writes=[('tmp', 0)])
            s.op('act', lambda e: e.activation(out=rn[:], in_=rn[:], func=AF.Ln), reads=[('tmp', 0)], writes=[('tmp', 0)])
            s.op('act', lambda e: e.activation(out=rn[:], in_=rn[:], func=AF.Exp, scale=-0.5), reads=[('tmp', 0)], writes=[('tmp', 0)])
            s.op('dve', lambda e: e.tensor_tensor(out=kk0[:], in0=kk0[:], in1=rn[:], op=ALU.mult),
                 reads=[('tmp', 3), ('tmp', 0)], writes=[('tmp', 3)])
            s.op('act', lambda e: e.mul(out=O['oA'][ob][:], in_=kk0[:], mul=-1.0), reads=[('tmp', 3)], writes=[('oA', ob)])
            s.dma(OQ, outs['oA'][rows, tsl], O['oA'][ob][:], ('so', 'oA', ob), reads=[('oA', ob)], writes=[('d_oA', ob)])
            s.op('dve', lambda e: e.tensor_tensor(out=O['oB'][ob][:], in0=kk0[:], in1=asig[:], op=ALU.mult),
                 reads=[('tmp', 3), ('tmp', 2)], writes=[('oB', ob)])
            s.dma(OQ, outs['oB'][rows, tsl], O['oB'][ob][:], ('so', 'oB', ob), reads=[('oB', ob)], writes=[('d_oB', ob)])
            t1 = tmp[1]
            s.op('dve', lambda e, cc=cc: e.tensor_scalar(out=t1[:], in0=asig[:], scalar1=-1.0, scalar2=COLS[:, 26 + cc:27 + cc],
                                                         op0=ALU.add, op1=ALU.mult),
                 reads=[('tmp', 2), 'COLS'], writes=[('tmp', 1)])
            s.op('dve', lambda e, cc=cc: e.scalar_tensor_tensor(out=O['oK'][ob][:], in0=t1[:], scalar=1.0, in1=Z[:, 4 + cc, :],
                                                                op0=ALU.add, op1=ALU.mult),
                 reads=[('tmp', 1), ('Z', 4 + cc)], writes=[('oK', ob)])
            s.dma(OQ, outs['oK'][rows, tsl], O['oK'][ob][:], ('so', 'oK', ob), reads=[('oK', ob)], writes=[('d_oK', ob)])
            s.dma(OQ, outs['oR'][rows, tsl], Z[:, cc, :], ('so', 'oR', cc), reads=[('Z', cc)], writes=[('d_oR', cc)])
            s.dma(OQ, outs['oV'][rows, tsl], Z[:, 8 + cc, :], ('so', 'oV', cc), reads=[('Z', 8 + cc)], writes=[('d_oV', cc)])
            s.op('dve', lambda e, cc=cc: e.scalar_tensor_tensor(out=tb[1][:], in0=Z[:, cc, :], scalar=COLS[:, 30 + cc:31 + cc],
                                                                in1=O['oK'][ob][:], op0=ALU.mult, op1=ALU.mult),
                 reads=[('Z', cc), 'COLS', ('oK', ob)], writes=[('tb', 1)])
            s.op('pe', lambda e: e.matmul(P[6][:], lhsT=BONES[:], rhs=tb[1][:], start=True, stop=True),
                 reads=['BONES', ('tb', 1)], writes=[('P', 6)])
            s.op('dve', lambda e, cc=cc: e.tensor_tensor(out=O['oBon'][ob][:], in0=Z[:, 8 + cc, :], in1=P[6][:], op=ALU.mult),
                 reads=[('Z', 8 + cc), ('P', 6)], writes=[('oBon', ob)])
            s.dma(OQ, outs['oBon'][rows, tsl], O['oBon'][ob][:], ('so', 'oBon', ob), reads=[('oBon', ob)], writes=[('d_oBon', ob)])
        for g in range(4):
            c = 14 + g
            src = PR
            cur = None
            n = HALO + T
            prev_ap = lambda lo, hi, c=c: PR[:, c, lo:hi]
            a_, b_ = SS[0], SS[1]
            sh = 1
            first = True
            for lv in range(g + 1):
                lo = 2 * sh - 1
                if first:
                    s.op('pool', lambda e, c=c, sh=sh, n=n, lo=lo: e.tensor_tensor(out=SS[0][:, lo:n], in0=PR[:, c, lo:n], in1=PR[:, c, sh - 1:n - sh], op=ALU.add),
                         reads=[('PR', c)], writes=[('SS', 0)])
                    first = False
                    cur = 0
                else:
                    src_, dst_ = SS[cur], SS[1 - cur]
                    s.op('pool', lambda e, sh=sh, n=n, lo=lo, src_=src_, dst_=dst_: e.tensor_tensor(out=dst_[:, lo:n], in0=src_[:, lo:n],
                                                                                                in1=src_[:, sh - 1:n - sh], op=ALU.add),
                         reads=[('SS', cur)], writes=[('SS', 1 - cur)])
                    cur = 1 - cur
                sh *= 2
            ia = 0 if ti == 0 else 1
            s.op('dve', lambda e, g=g, cur=cur, ia=ia: e.tensor_tensor(out=tmp[0][:], in0=SS[cur][:, HALO:HALO + T], in1=INVC[:, ia, g, :], op=ALU.mult),
                 reads=[('SS', cur), ('INVC', ia)], writes=[('tmp', 0)])
            s.op('dve', lambda e, c=c: e.tensor_tensor(out=tb[0][:], in0=tmp[0][:], in1=PR[:, c, HALO:HALO + T], op=ALU.subtract),
                 reads=[('tmp', 0), ('PR', c)], writes=[('tb', 0)])
            s.op('pe', lambda e, g=g: e.matmul(P[7][:], lhsT=PW[:, g, :], rhs=tb[0][:], start=True, stop=True),
                 reads=[('PW', g), ('tb', 0)], writes=[('P', 7)])
            s.op('act', lambda e, g=g: e.activation(out=O['oYP'][ob][:], in_=P[7][:], func=AF.Identity, scale=COLS[:, 34 + g:35 + g]),
                 reads=[('P', 7), 'COLS'], writes=[('oYP', ob)])
            s.dma(OQ, outs['oYP'][g * 128:(g + 1) * 128, t0:t0 + T], O['oYP'][ob][:], ('so', 'oYP', ob), reads=[('oYP', ob)], writes=[('d_oYP', ob)])
        for c in range(NCH):
            s.op('pool', lambda e, c=c: e.tensor_copy(out=PR[:, c, 0:HALO], in_=PR[:, c, T:T + HALO]),
                 reads=[('PR', c)], writes=[('PR', c)])
    for ti in range(ntile):
        tile_body(ti)
    s.wait_all(OQ, [k for k in s.last_w if isinstance(k, tuple) and isinstance(k[0], str) and k[0].startswith('d_')])


def host_inputs_0a(xT_halo, w_in, mu, w0, a0, k_k, k_a, r_k, pool_scale, w2, a2, g2, pool_w, first, tile=512):
    cols = np.zeros((128, 40), np.float32)
    cols[:, 0:14] = mu.reshape(14, 128).T
    cols[:, 14:18] = w0.reshape(4, 128).T
    cols[:, 18:22] = a0.reshape(4, 128).T
    cols[:, 22:26] = k_k.reshape(4, 128).T
    cols[:, 26:30] = k_a.reshape(4, 128).T
    cols[:, 30:34] = r_k.reshape(4, 128).T
    cols[:, 34:38] = pool_scale.reshape(4, 128).T
    bones = np.zeros((128, 128), np.float32)
    bones[:64, :64] = 1.0
    bones[64:, 64:] = 1.0
    invc = np.zeros((128, 2, 4, tile), np.float32)
    t = np.arange(tile)
    for g, win in enumerate((2, 4, 8, 16)):
        invc[:, 1, g, :] = 1.0 / win
        invc[:, 0, g, :] = (1.0 / np.minimum(t + 1, win)) if first else 1.0 / win
    return dict(xT=np.ascontiguousarray(xT_halo), w_in=np.ascontiguousarray(w_in), cols=cols,
                w2=np.ascontiguousarray(w2), a2=np.ascontiguousarray(a2), g2=np.ascontiguousarray(g2),
                pool_w=np.ascontiguousarray(pool_w), bones=bones, invc=invc)


C = 64
NH = 4
C0 = 0.6065306597126334
RWKV_EPS = 64e-5


def MM(s, out, lhsT, rhs, start=True, stop=True, reads=(), writes=(), inc=True):
    s.op('pe', lambda e: e.matmul(out, lhsT=lhsT, rhs=rhs, start=start, stop=stop), reads=reads, writes=writes, inc=inc)


def TT(s, eng, out, in0, in1, op, reads, writes):
    s.op(eng, lambda e: e.tensor_tensor(out=out, in0=in0, in1=in1, op=op), reads=reads, writes=writes)


def ACTF(s, out, in_, func, reads, writes, bias=None, scale=1.0):
    if bias is None:
        s.op('act', lambda e: e.activation(out=out, in_=in_, func=func, scale=scale), reads=reads, writes=writes)
    else:
        s.op('act', lambda e: e.activation(out=out, in_=in_, func=func, bias=bias, scale=scale), reads=reads, writes=writes)


def TS(s, eng, out, in0, s1, s2, op0, op1, reads, writes):
    if op1 is None:
        s.op(eng, lambda e: e.tensor_scalar(out=out, in0=in0, scalar1=s1, scalar2=None, op0=op0), reads=reads, writes=writes)
    else:
        s.op(eng, lambda e: e.tensor_scalar(out=out, in0=in0, scalar1=s1, scalar2=s2, op0=op0, op1=op1), reads=reads, writes=writes)


def STT(s, out, in0, scalar, in1, op0, op1, reads, writes):
    s.op('dve', lambda e: e.scalar_tensor_tensor(out=out, in0=in0, scalar=scalar, in1=in1, op0=op0, op1=op1),
         reads=reads, writes=writes)


def CP(s, eng, out, in_, reads, writes):
    if eng == 'act':
        s.op('act', lambda e: e.copy(out=out, in_=in_), reads=reads, writes=writes)
    else:
        s.op(eng, lambda e: e.tensor_copy(out=out, in_=in_), reads=reads, writes=writes)


def RED(s, out, in_, reads, writes):
    s.op('dve', lambda e: e.reduce_sum(out=out, in_=in_, axis=AX.X), reads=reads, writes=writes)


def build_0b(T=8192, GC=4):
    nc = bass.Bass('TRN2', target_bir_lowering=False)
    dt = nc.dram_tensor
    tin = {n: dt(n, [64, NH, T], F32, kind="ExternalInput").ap() for n in ('RT', 'KT', 'AT', 'BT')}
    nin = {n: dt(n, [T, NH * 64], F32, kind="ExternalInput").ap() for n in ('Wn', 'Bn', 'Kn', 'Vn', 'BONn', 'Gn')}
    cst = {n: dt(n, shp, F32, kind="ExternalInput").ap() for n, shp in
           (('MU1', [64, NH, 128]), ('MLS', [64, NH, 64]), ('I4', [64, NH, 64]), ('TRI', [64, 128]), ('UPS', [64, 64]),
            ('LGB', [64, 2, NH * 64]))}
    y = dt("y", [T, NH * 64], F32, kind="ExternalOutput").ap()
    s = Sched(nc)
    emit_0b(s, tin, nin, cst, y, T, GC)
    s.build()
    return nc


def emit_0b(s, tin, nin, cst, y, T, GC):
    K = {}
    for n, shp in (('MU1', [64, NH, 128]), ('MLS', [64, NH, 64]), ('I4', [64, NH, 64]), ('TRI', [64, 128]), ('UPS', [64, 64]),
                   ('LGB', [64, 2, NH * 64])):
        K[n] = s.sbuf('K_' + n, shp, F32)
        s.dma('sp', K[n][:], cst[n], ('k', n), writes=[n])
    epst = s.sbuf('epst', [64, 1], F32)
    s.op('dve', lambda e: e.memset(epst[:], RWKV_EPS), writes=['epst'])
    S0 = s.sbuf('S0', [64, NH, 64], F32)
    s.op('dve', lambda e: e.memset(S0[:], 0.0), writes=['S0'])
    PS = [s.psum('PS%d' % i, [128, 512]) for i in range(8)]

    def bank(i, w):
        return PS[i][0:64, 0:NH * w].rearrange("p (h x) -> p h x", h=NH)
    GT = GC * C
    tbuf = {n: [s.sbuf('t_%s%d' % (n, i), [64, NH, GT], F32) for i in range(2)] for n in tin}
    nbuf = {n: [s.sbuf('n_%s%d' % (n, i), [64, GC, NH * 64], F32) for i in range(2)] for n in nin}
    f4 = lambda name, w=64: s.sbuf(name, [64, NH, w], F32)
    PT, PINV, PPREV = f4('PT'), f4('PINV'), f4('PPREV')
    E1 = s.sbuf('E1', [64, NH * 64], F32)
    AR = s.sbuf('AR', [64, NH, 2, 64], F32)
    BK = s.sbuf('BK', [64, NH, 2, 64], F32)
    BKn = s.sbuf('BKn', [64, 2, NH * 64], F32)
    X1, X2 = f4('X1', 128), f4('X2', 128)
    Nb = [f4('N0'), f4('N1')]
    NTb = [f4('NT0'), f4('NT1')]
    Rb = [f4('R0'), f4('R1')]
    Wsb, Usb, Ysb, Ysq, Yn = f4('Wsb'), f4('Usb'), f4('Ysb'), f4('Ysq'), f4('Yn')
    st = s.sbuf('st', [64, 6, NH], F32)
    Yo = [s.sbuf('Yo%d' % i, [64, NH * 64], F32) for i in range(2)]
    nchunk = T // C
    ngrp = nchunk // GC

    def load_group(gi):
        b = gi % 2
        t0 = gi * GT
        for n in tin:
            s.dma('sp', tbuf[n][b][:], tin[n][:, :, t0:t0 + GT], ('lt', n, b), writes=[('t', n, b)])
        for n in nin:
            s.dma('pool', nbuf[n][b][:], nin[n][t0:t0 + GT, :].rearrange("(c p) n -> p c n", p=64), ('ln', n, b),
                  writes=[('n', n, b)])

    def chunk(ci):
        gi, cj = divmod(ci, GC)
        b = gi % 2
        tv = lambda n: tbuf[n][b][:, :, cj * C:(cj + 1) * C]
        tk = lambda n: ('t', n, b)
        nv = lambda n: nbuf[n][b][:, cj, :]
        nk = lambda n: ('n', n, b)
        for h in range(NH):
            MM(s, PS[0][0:64, h * 128:(h + 1) * 128], nbuf['Wn'][b][:, cj, h * 64:(h + 1) * 64], K['TRI'][:],
               reads=[nk('Wn'), 'TRI'], writes=['B0'], inc=(h == NH - 1))
        MM(s, PS[1][0:64, 0:NH * 64], K['UPS'][:], nv('Wn'), reads=[nk('Wn'), 'UPS'], writes=['B1'])
        cumI = bank(0, 128)[:, :, 0:64]
        cumS = bank(0, 128)[:, :, 64:128]
        ACTF(s, PT[:], cumI, AF.Exp, ['B0'], ['PT'], scale=-C0)
        ACTF(s, PINV[:], cumI, AF.Exp, ['B0'], ['PINV'], scale=C0)
        ACTF(s, PPREV[:], cumS, AF.Exp, ['B0'], ['PPREV'], scale=-C0)
        ACTF(s, E1[:], PS[1][0:64, 0:NH * 64], AF.Exp, ['B1'], ['E1'], scale=-C0)
        TT(s, 'dve', AR[:, :, 0, :], tv('AT'), PPREV[:], ALU.mult, [tk('AT'), 'PPREV'], ['AR0'])
        TT(s, 'pool', AR[:, :, 1, :], tv('RT'), PT[:], ALU.mult, [tk('RT'), 'PT'], ['AR1'])
        TT(s, 'dve', BK[:, :, 0, :], tv('BT'), PINV[:], ALU.mult, [tk('BT'), 'PINV'], ['BK0'])
        TT(s, 'pool', BK[:, :, 1, :], tv('KT'), PINV[:], ALU.mult, [tk('KT'), 'PINV'], ['BK1'])
        TT(s, 'pool', BKn[:, 0, :], nv('Bn'), E1[:], ALU.mult, [nk('Bn'), 'E1'], ['BKn0'])
        TT(s, 'pool', BKn[:, 1, :], nv('Kn'), E1[:], ALU.mult, [nk('Kn'), 'E1'], ['BKn1'])
        for h in range(NH):
            arh = AR[:, h, :, :].rearrange("p a x -> p (a x)")
            MM(s, PS[2][0:64, h * 128:(h + 1) * 128], BK[:, h, 0, :], arh, reads=['BK0', 'AR0', 'AR1'], writes=['B2'],
               inc=(h == NH - 1))
        for h in range(NH):
            arh = AR[:, h, :, :].rearrange("p a x -> p (a x)")
            MM(s, PS[3][0:64, h * 128:(h + 1) * 128], BK[:, h, 1, :], arh, reads=['BK1', 'AR0', 'AR1'], writes=['B3'],
               inc=(h == NH - 1))
        for h in range(NH):
            MM(s, PS[4][0:64, h * 64:(h + 1) * 64], AR[:, h, 0, :], BK[:, h, 0, :], reads=['BK0', 'AR0'], writes=['B4'],
               inc=(h == NH - 1))
        TT(s, 'dve', X1[:], bank(2, 128), K['MU1'][:], ALU.mult, ['B2', 'MU1'], ['X1'])
        TT(s, 'dve', X2[:], bank(3, 128), K['MU1'][:], ALU.mult, ['B3', 'MU1'], ['X2'])
        TT(s, 'dve', NTb[0][:], bank(4, 64), K['MLS'][:], ALU.mult, ['B4', 'MLS'], [('NT', 0)])
        CP(s, 'pool', Nb[0][:], X1[:, :, 0:64], ['X1'], [('N', 0)])
        TT(s, 'pool', Rb[0][:], X1[:, :, 0:64], K['I4'][:], ALU.add, ['X1', 'I4'], [('R', 0)])
        cur = 0
        for lv in range(5):
            nx = 1 - cur
            if lv < 4:
                for h in range(NH):
                    MM(s, PS[5][0:64, h * 64:(h + 1) * 64], NTb[cur][:, h, :], Nb[cur][:, h, :],
                       reads=[('NT', cur), ('N', cur)], writes=['B5'], inc=(h == NH - 1))
            for h in range(NH):
                MM(s, PS[6][0:64, h * 64:(h + 1) * 64], Nb[cur][:, h, :], NTb[cur][:, h, :],
                   reads=[('NT', cur), ('N', cur)], writes=['B6'], inc=(h == NH - 1))
            if lv < 4:
                CP(s, 'act', Nb[nx][:], bank(5, 64), ['B5'], [('N', nx)])
            CP(s, 'dve', NTb[nx][:], bank(6, 64), ['B6'], [('NT', nx)])
            for h in range(NH):
                MM(s, PS[7][0:64, h * 64:(h + 1) * 64], NTb[nx][:, h, :], Rb[cur][:, h, :],
                   reads=[('NT', nx), ('R', cur)], writes=['B7'], inc=(h == NH - 1))
            TT(s, 'dve', Rb[nx][:], Rb[cur][:], bank(7, 64), ALU.add, [('R', cur), 'B7'], [('R', nx)])
            cur = nx
        R = Rb[cur]
        Rk = ('R', cur)
        for h in range(NH):
            vh = nbuf['Vn'][b][:, cj, h * 64:(h + 1) * 64]
            MM(s, PS[2][0:64, h * 64:(h + 1) * 64], X2[:, h, 0:64], vh, start=True, stop=False,
               reads=['X2', nk('Vn')], writes=['B2'], inc=False)
            MM(s, PS[2][0:64, h * 64:(h + 1) * 64], AR[:, h, 0, :], S0[:, h, :], start=False, stop=True,
               reads=['AR0', 'S0'], writes=['B2'], inc=(h == NH - 1))
        CP(s, 'dve', Wsb[:], bank(2, 64), ['B2'], ['Wsb'])
        for h in range(NH):
            MM(s, PS[3][0:64, h * 64:(h + 1) * 64], R[:, h, :], Wsb[:, h, :], reads=[Rk, 'Wsb'], writes=['B3'],
               inc=(h == NH - 1))
        CP(s, 'dve', Usb[:], bank(3, 64), ['B3'], ['Usb'])
        for h in range(NH):
            vh = nbuf['Vn'][b][:, cj, h * 64:(h + 1) * 64]
            o = PS[4][0:64, h * 64:(h + 1) * 64]
            MM(s, o, AR[:, h, 1, :], S0[:, h, :], start=True, stop=False, reads=['AR1', 'S0'], writes=['B4'], inc=False)
            MM(s, o, X1[:, h, 64:128], Usb[:, h, :], start=False, stop=False, reads=['X1', 'Usb'], writes=['B4'], inc=False)
            MM(s, o, X2[:, h, 64:128], vh, start=False, stop=True, reads=['X2', nk('Vn')], writes=['B4'], inc=(h == NH - 1))
        for h in range(NH):
            vh = nbuf['Vn'][b][:, cj, h * 64:(h + 1) * 64]
            o = PS[7][0:64, h * 64:(h + 1) * 64]
            MM(s, o, BKn[:, 0, h * 64:(h + 1) * 64], Usb[:, h, :], start=True, stop=False, reads=['BKn0', 'Usb'], writes=['B7'], inc=False)
            MM(s, o, BKn[:, 1, h * 64:(h + 1) * 64], vh, start=False, stop=True, reads=['BKn1', nk('Vn')], writes=['B7'], inc=(h == NH - 1))
        CP(s, 'dve', Ysb[:], bank(4, 64), ['B4'], ['Ysb'])
        for h in range(NH):
            STT(s, S0[:, h, :], S0[:, h, :], PT[:, h, 63:64], PS[7][0:64, h * 64:(h + 1) * 64], ALU.mult, ALU.add,
                ['S0', 'PT', 'B7'], ['S0'])
        RED(s, st[:, 0, :], Ysb[:], ['Ysb'], ['st0'])
        TT(s, 'pool', Ysq[:], Ysb[:], Ysb[:], ALU.mult, ['Ysb'], ['Ysq'])
        RED(s, st[:, 1, :], Ysq[:], ['Ysq'], ['st1'])
        TS(s, 'dve', st[:, 2, :], st[:, 0, :], 1.0 / 64, None, ALU.mult, None, ['st0'], ['st2'])
        TT(s, 'dve', st[:, 3, :], st[:, 2, :], st[:, 2, :], ALU.mult, ['st2'], ['st3'])
        STT(s, st[:, 4, :], st[:, 1, :], 1.0 / 64, st[:, 3, :], ALU.mult, ALU.subtract, ['st1', 'st3'], ['st4'])
        ACTF(s, st[:, 5, :], st[:, 4, :], AF.Ln, ['st4', 'epst'], ['st5'], bias=epst[:], scale=1.0)
        ACTF(s, st[:, 5, :], st[:, 5, :], AF.Exp, ['st5'], ['st5'], scale=-0.5)
        for h in range(NH):
            TS(s, 'dve', Yn[:, h, :], Ysb[:, h, :], st[:, 2, h:h + 1], st[:, 5, h:h + 1], ALU.subtract, ALU.mult,
               ['Ysb', 'st2', 'st5'], ['Yn'])
        ynf = Yn[:].rearrange("p h x -> p (h x)")
        yo = Yo[ci % 2]
        yok = ('Yo', ci % 2)
        TT(s, 'pool', ynf, ynf, K['LGB'][:, 0, :], ALU.mult, ['Yn', 'LGB'], ['Yn'])
        TT(s, 'pool', ynf, ynf, K['LGB'][:, 1, :], ALU.add, ['Yn', 'LGB'], ['Yn'])
        TT(s, 'pool', ynf, ynf, nv('BONn'), ALU.add, ['Yn', nk('BONn')], ['Yn'])
        TT(s, 'pool', yo[:], ynf, nv('Gn'), ALU.mult, ['Yn', nk('Gn')], [yok])
        s.dma('sp', y[ci * C:(ci + 1) * C, :], yo[:], ('so', ci % 2), reads=[yok], writes=[('yd', ci % 2)])

    load_group(0)
    for gi in range(ngrp):
        if gi + 1 < ngrp:
            load_group(gi + 1)
        for cj in range(GC):
            chunk(gi * GC + cj)
    s.wait_all('sp', [('yd', 0), ('yd', 1)])


def host_consts_0b(lnx_g, lnx_b):
    j = np.arange(64)[:, None]
    x = np.arange(64)[None, :]
    su = (x > j).astype(np.float32)
    iu = (x >= j).astype(np.float32)
    sl = (x < j).astype(np.float32)
    MU1 = np.broadcast_to(np.concatenate([su, iu], 1)[:, None, :], (64, NH, 128))
    MLS = np.broadcast_to(sl[:, None, :], (64, NH, 64))
    I4 = np.broadcast_to(np.eye(64, dtype=np.float32)[:, None, :], (64, NH, 64))
    TRI = np.concatenate([iu, su], 1)
    UPS = sl
    LGB = np.broadcast_to(np.stack([lnx_g, lnx_b])[None], (64, 2, NH * 64))
    return {k: np.ascontiguousarray(v, dtype=np.float32) for k, v in
            dict(MU1=MU1, MLS=MLS, I4=I4, TRI=TRI, UPS=UPS, LGB=LGB).items()}


def host_inputs_0b(arrT, lnx_g, lnx_b):
    T = arrT['oR'].shape[1]
    tr = lambda a: np.ascontiguousarray(a.reshape(NH, 64, T).transpose(1, 0, 2))
    na = lambda a: np.ascontiguousarray(a.T)
    d = dict(RT=tr(arrT['oR']), KT=tr(arrT['oK']), AT=tr(arrT['oA']), BT=tr(arrT['oB']),
             Wn=na(arrT['oW']), Bn=na(arrT['oB']), Kn=na(arrT['oK']), Vn=na(arrT['oV']),
             BONn=na(arrT['oBon']), Gn=na(arrT['oG']))
    d.update(host_consts_0b(lnx_g, lnx_b))
    return d


MMDT = BF16


def build_0b(T=8192, GP=2):
    nc = bass.Bass('TRN2', target_bir_lowering=False)
    dt = nc.dram_tensor
    tin = {n: dt(n, [64, NH, T], F32, kind="ExternalInput").ap() for n in ('RT', 'KT', 'AT', 'BT')}
    nin = {n: dt(n, [T, NH * 64], F32, kind="ExternalInput").ap() for n in ('Wn', 'Bn', 'Kn', 'Vn', 'BONn', 'Gn')}
    cst = {n: dt(n, shp, F32, kind="ExternalInput").ap() for n, shp in
           (('MU1', [128, NH, 128]), ('MLS', [128, NH, 64]), ('I4', [128, NH, 64]), ('TRI', [128, 128]), ('UPS', [128, 64]),
            ('LGB', [128, 2, NH * 64]))}
    y = dt("y", [T, NH * 64], F32, kind="ExternalOutput").ap()
    s = Sched(nc)
    emit_0b(s, tin, nin, cst, y, T, GP)
    s.build()
    return nc


def emit_0b(s, tin, nin, cst, y, T, GP):
    K = {}
    for n, shp in (('MU1', [128, NH, 128]), ('MLS', [128, NH, 64]), ('I4', [128, NH, 64]), ('TRI', [128, 128]), ('UPS', [128, 64]),
                   ('LGB', [128, 2, NH * 64])):
        K[n] = s.sbuf('K_' + n, shp, F32)
        s.dma('sp', K[n][:], cst[n], ('k', n), writes=[n])
    epst = s.sbuf('epst', [128, 1], F32)
    s.op('dve', lambda e: e.memset(epst[:], RWKV_EPS), writes=['epst'])
    S0 = s.sbuf('S0', [128, NH, 64], MMDT)
    s.op('dve', lambda e: e.memset(S0[:], 0.0), writes=[('S0', 0), ('S0', 1)])
    PS = [s.psum('PS%d' % i, [128, 512]) for i in range(8)]
    HS = [slice(0, 64), slice(64, 128)]

    def bank(i, w):
        return PS[i][:, 0:NH * w].rearrange("p (h x) -> p h x", h=NH)
    B2 = lambda k: [(k, 0), (k, 1)]
    GT = GP * 2 * C
    tbuf = {n: [s.sbuf('t_%s%d' % (n, i), [128, NH, GP, C], F32) for i in range(2)] for n in tin}
    nbuf = {n: [s.sbuf('n_%s%d' % (n, i), [128, GP, NH * 64], MMDT if n == 'Vn' else F32) for i in range(2)] for n in nin}
    f4 = lambda name, w=64, dt_=F32: s.sbuf(name, [128, NH, w], dt_)
    PT, PINV, PPREV = f4('PT'), f4('PINV'), f4('PPREV')
    E1 = s.sbuf('E1', [128, NH * 64], F32)
    AR = s.sbuf('AR', [128, NH, 2, 64], MMDT)
    BK = s.sbuf('BK', [128, NH, 2, 64], MMDT)
    BKn = s.sbuf('BKn', [128, 2, NH * 64], MMDT)
    DG = f4('DG', 64, MMDT)
    X1, X2 = f4('X1', 128, MMDT), f4('X2', 128, MMDT)
    Nb = [f4('N0', 64, MMDT), f4('N1', 64, MMDT)]
    NTb = [f4('NT0', 64, MMDT), f4('NT1', 64, MMDT)]
    Rb = [f4('R0', 64, MMDT), f4('R1', 64, MMDT)]
    Wsb, Usb = f4('Wsb', 64, MMDT), f4('Usb', 64, MMDT)
    Ysb, Ysq, Yn = f4('Ysb'), f4('Ysq'), f4('Yn')
    st = s.sbuf('st', [128, 6, NH], F32)
    Yo = [s.sbuf('Yo%d' % i, [128, NH * 64], F32) for i in range(2)]
    npair = T // (2 * C)
    ngrp = npair // GP

    def load_group(gi):
        b = gi % 2
        t0 = gi * GT
        for n in tin:
            src = tin[n][:, :, t0:t0 + GT].rearrange("k h (g two t) -> k h g two t", two=2, t=C)
            for hf in range(2):
                s.dma('sp', tbuf[n][b][HS[hf], :, :, :], src[:, :, :, hf, :], ('lt', n, b, hf), writes=[('t', n, b, hf)])
        for n in nin:
            s.dma('pool', nbuf[n][b][:], nin[n][t0:t0 + GT, :].rearrange("(c p) n -> p c n", p=128), ('ln', n, b),
                  writes=[('n', n, b)])

    def pair(pi):
        gi, pj = divmod(pi, GP)
        b = gi % 2
        tv = lambda n: tbuf[n][b][:, :, pj, :]
        tk = lambda n: [('t', n, b, 0), ('t', n, b, 1)]
        nv = lambda n: nbuf[n][b][:, pj, :]
        nk = lambda n: ('n', n, b)
        for hf in range(2):
            P_ = HS[hf]
            for h in range(NH):
                MM(s, PS[0][P_, h * 128:(h + 1) * 128], nbuf['Wn'][b][P_, pj, h * 64:(h + 1) * 64], K['TRI'][P_, :],
                   reads=[nk('Wn'), 'TRI'], writes=['B0'], inc=(h == NH - 1 and hf == 1))
        for hf in range(2):
            P_ = HS[hf]
            MM(s, PS[1][P_, 0:NH * 64], K['UPS'][P_, :], nbuf['Wn'][b][P_, pj, :], reads=[nk('Wn'), 'UPS'], writes=['B1'],
               inc=(hf == 1))
        cumI = bank(0, 128)[:, :, 0:64]
        cumS = bank(0, 128)[:, :, 64:128]
        ACTF(s, PT[:], cumI, AF.Exp, ['B0'], ['PT'], scale=-C0)
        ACTF(s, PINV[:], cumI, AF.Exp, ['B0'], ['PINV'], scale=C0)
        ACTF(s, PPREV[:], cumS, AF.Exp, ['B0'], ['PPREV'], scale=-C0)
        ACTF(s, E1[:], PS[1][:, 0:NH * 64], AF.Exp, ['B1'], ['E1'], scale=-C0)
        TT(s, 'dve', AR[:, :, 0, :], tv('AT'), PPREV[:], ALU.mult, tk('AT') + ['PPREV'], ['AR0'])
        TT(s, 'pool', AR[:, :, 1, :], tv('RT'), PT[:], ALU.mult, tk('RT') + ['PT'], ['AR1'])
        TT(s, 'dve', BK[:, :, 0, :], tv('BT'), PINV[:], ALU.mult, tk('BT') + ['PINV'], ['BK0'])
        TT(s, 'pool', BK[:, :, 1, :], tv('KT'), PINV[:], ALU.mult, tk('KT') + ['PINV'], ['BK1'])
        TT(s, 'pool', BKn[:, 0, :], nv('Bn'), E1[:], ALU.mult, [nk('Bn'), 'E1'], ['BKn0'])
        TT(s, 'pool', BKn[:, 1, :], nv('Kn'), E1[:], ALU.mult, [nk('Kn'), 'E1'], ['BKn1'])
        for h in range(NH):
            TS(s, 'pool', DG[:, h, :], K['I4'][:, h, :], PT[:, h, 63:64], None, ALU.mult, None, ['I4', 'PT'], ['DG'])
        for hf in range(2):
            P_ = HS[hf]
            last = (hf == 1)
            for h in range(NH):
                arh = AR[P_, h, :, :].rearrange("p a x -> p (a x)")
                MM(s, PS[2][P_, h * 128:(h + 1) * 128], BK[P_, h, 0, :], arh, reads=['BK0', 'AR0', 'AR1'], writes=B2('B2'),
                   inc=(h == NH - 1 and last))
            for h in range(NH):
                arh = AR[P_, h, :, :].rearrange("p a x -> p (a x)")
                MM(s, PS[3][P_, h * 128:(h + 1) * 128], BK[P_, h, 1, :], arh, reads=['BK1', 'AR0', 'AR1'], writes=B2('B3'),
                   inc=(h == NH - 1 and last))
            for h in range(NH):
                MM(s, PS[4][P_, h * 64:(h + 1) * 64], AR[P_, h, 0, :], BK[P_, h, 0, :], reads=['BK0', 'AR0'], writes=B2('B4'),
                   inc=(h == NH - 1 and last))
        TT(s, 'dve', X1[:], bank(2, 128), K['MU1'][:], ALU.mult, B2('B2') + ['MU1'], ['X1'])
        TT(s, 'dve', X2[:], bank(3, 128), K['MU1'][:], ALU.mult, B2('B3') + ['MU1'], ['X2'])
        TT(s, 'dve', NTb[0][:], bank(4, 64), K['MLS'][:], ALU.mult, B2('B4') + ['MLS'], [('NT', 0)])
        CP(s, 'pool', Nb[0][:], X1[:, :, 0:64], ['X1'], [('N', 0)])
        TT(s, 'pool', Rb[0][:], X1[:, :, 0:64], K['I4'][:], ALU.add, ['X1', 'I4'], [('R', 0)])
        cur = 0
        for lv in range(5):
            nx = 1 - cur
            if lv < 4:
                for hf in range(2):
                    P_ = HS[hf]
                    for h in range(NH):
                        MM(s, PS[5][P_, h * 64:(h + 1) * 64], NTb[cur][P_, h, :], Nb[cur][P_, h, :],
                           reads=[('NT', cur), ('N', cur)], writes=['B5'], inc=(h == NH - 1 and hf == 1))
            for hf in range(2):
                P_ = HS[hf]
                for h in range(NH):
                    MM(s, PS[6][P_, h * 64:(h + 1) * 64], Nb[cur][P_, h, :], NTb[cur][P_, h, :],
                       reads=[('NT', cur), ('N', cur)], writes=['B6'], inc=(h == NH - 1 and hf == 1))
            if lv < 4:
                CP(s, 'act', Nb[nx][:], bank(5, 64), ['B5'], [('N', nx)])
            CP(s, 'dve', NTb[nx][:], bank(6, 64), ['B6'], [('NT', nx)])
            for hf in range(2):
                P_ = HS[hf]
                for h in range(NH):
                    MM(s, PS[7][P_, h * 64:(h + 1) * 64], NTb[nx][P_, h, :], Rb[cur][P_, h, :],
                       reads=[('NT', nx), ('R', cur)], writes=B2('B7'), inc=(h == NH - 1 and hf == 1))
            TT(s, 'dve', Rb[nx][:], Rb[cur][:], bank(7, 64), ALU.add, [('R', cur)] + B2('B7'), [('R', nx)])
            cur = nx
        R = Rb[cur]
        Rk = ('R', cur)
        for hf in range(2):
            P_ = HS[hf]
            Q_ = HS[1 - hf]
            vsl = lambda h: nbuf['Vn'][b][P_, pj, h * 64:(h + 1) * 64]
            for h in range(NH):
                o = PS[2][P_, h * 64:(h + 1) * 64]
                MM(s, o, X2[P_, h, 0:64], vsl(h), start=True, stop=False, reads=['X2', nk('Vn')], writes=[('B2', hf)], inc=False)
                MM(s, o, AR[P_, h, 0, :], S0[P_, h, :], start=False, stop=True, reads=['AR0', ('S0', hf)], writes=[('B2', hf)],
                   inc=(h == NH - 1))
            CP(s, 'dve', Wsb[P_, :, :], PS[2][P_, 0:NH * 64].rearrange("p (h x) -> p h x", h=NH), [('B2', hf)], [('Wsb', hf)])
            for h in range(NH):
                MM(s, PS[3][P_, h * 64:(h + 1) * 64], R[P_, h, :], Wsb[P_, h, :], reads=[Rk, ('Wsb', hf)], writes=[('B3', hf)],
                   inc=(h == NH - 1))
            CP(s, 'dve', Usb[P_, :, :], PS[3][P_, 0:NH * 64].rearrange("p (h x) -> p h x", h=NH), [('B3', hf)], [('Usb', hf)])
            for h in range(NH):
                o = PS[7][Q_, h * 64:(h + 1) * 64]
                MM(s, o, DG[P_, h, :], S0[P_, h, :], start=True, stop=False, reads=['DG', ('S0', hf)], writes=[('B7', 1 - hf)], inc=False)
                MM(s, o, BKn[P_, 0, h * 64:(h + 1) * 64], Usb[P_, h, :], start=False, stop=False, reads=['BKn0', ('Usb', hf)],
                   writes=[('B7', 1 - hf)], inc=False)
                MM(s, o, BKn[P_, 1, h * 64:(h + 1) * 64], vsl(h), start=False, stop=True, reads=['BKn1', nk('Vn')],
                   writes=[('B7', 1 - hf)], inc=(h == NH - 1))
            for h in range(NH):
                o = PS[4][P_, h * 64:(h + 1) * 64]
                MM(s, o, AR[P_, h, 1, :], S0[P_, h, :], start=True, stop=False, reads=['AR1', ('S0', hf)], writes=[('B4', hf)], inc=False)
                MM(s, o, X1[P_, h, 64:128], Usb[P_, h, :], start=False, stop=False, reads=['X1', ('Usb', hf)], writes=[('B4', hf)], inc=False)
                MM(s, o, X2[P_, h, 64:128], vsl(h), start=False, stop=True, reads=['X2', nk('Vn')], writes=[('B4', hf)],
                   inc=(h == NH - 1))
            CP(s, 'dve', S0[Q_, :, :], PS[7][Q_, 0:NH * 64].rearrange("p (h x) -> p h x", h=NH), [('B7', 1 - hf)], [('S0', 1 - hf)])
        CP(s, 'dve', Ysb[:], bank(4, 64), B2('B4'), ['Ysb'])
        RED(s, st[:, 0, :], Ysb[:], ['Ysb'], ['st0'])
        TT(s, 'pool', Ysq[:], Ysb[:], Ysb[:], ALU.mult, ['Ysb'], ['Ysq'])
        RED(s, st[:, 1, :], Ysq[:], ['Ysq'], ['st1'])
        TS(s, 'dve', st[:, 2, :], st[:, 0, :], 1.0 / 64, None, ALU.mult, None, ['st0'], ['st2'])
        TT(s, 'dve', st[:, 3, :], st[:, 2, :], st[:, 2, :], ALU.mult, ['st2'], ['st3'])
        STT(s, st[:, 4, :], st[:, 1, :], 1.0 / 64, st[:, 3, :], ALU.mult, ALU.subtract, ['st1', 'st3'], ['st4'])
        ACTF(s, st[:, 5, :], st[:, 4, :], AF.Ln, ['st4', 'epst'], ['st5'], bias=epst[:], scale=1.0)
        ACTF(s, st[:, 5, :], st[:, 5, :], AF.Exp, ['st5'], ['st5'], scale=-0.5)
        for h in range(NH):
            TS(s, 'dve', Yn[:, h, :], Ysb[:, h, :], st[:, 2, h:h + 1], st[:, 5, h:h + 1], ALU.subtract, ALU.mult,
               ['Ysb', 'st2', 'st5'], ['Yn'])
        ynf = Yn[:].rearrange("p h x -> p (h x)")
        yo = Yo[pi % 2]
        yok = ('Yo', pi % 2)
        TT(s, 'pool', ynf, ynf, K['LGB'][:, 0, :], ALU.mult, ['Yn', 'LGB'], ['Yn'])
        TT(s, 'pool', ynf, ynf, K['LGB'][:, 1, :], ALU.add, ['Yn', 'LGB'], ['Yn'])
        TT(s, 'pool', ynf, ynf, nv('BONn'), ALU.add, ['Yn', nk('BONn')], ['Yn'])
        TT(s, 'pool', yo[:], ynf, nv('Gn'), ALU.mult, ['Yn', nk('Gn')], [yok])
        s.dma('sp', y[pi * 2 * C:(pi + 1) * 2 * C, :], yo[:], ('so', pi % 2), reads=[yok], writes=[('yd', pi % 2)])

    load_group(0)
    for gi in range(ngrp):
        if gi + 1 < ngrp:
            load_group(gi + 1)
        for pj in range(GP):
            pair(gi * GP + pj)
    s.wait_all('sp', [('yd', 0), ('yd', 1)])


def host_consts_0b(lnx_g, lnx_b):
    j = np.arange(64)[:, None]
    x = np.arange(64)[None, :]
    su = (x > j).astype(np.float32)
    iu = (x >= j).astype(np.float32)
    sl = (x < j).astype(np.float32)
    two = lambda a: np.concatenate([a, a], axis=0)
    MU1 = two(np.broadcast_to(np.concatenate([su, iu], 1)[:, None, :], (64, NH, 128)))
    MLS = two(np.broadcast_to(sl[:, None, :], (64, NH, 64)))
    I4 = two(np.broadcast_to(np.eye(64, dtype=np.float32)[:, None, :], (64, NH, 64)))
    TRI = two(np.concatenate([iu, su], 1))
    UPS = two(sl)
    LGB = np.broadcast_to(np.stack([lnx_g, lnx_b])[None], (128, 2, NH * 64))
    return {k: np.ascontiguousarray(v, dtype=np.float32) for k, v in
            dict(MU1=MU1, MLS=MLS, I4=I4, TRI=TRI, UPS=UPS, LGB=LGB).items()}


def host_inputs_0b(arrT, lnx_g, lnx_b):
    T = arrT['oR'].shape[1]
    tr = lambda a: np.ascontiguousarray(a.reshape(NH, 64, T).transpose(1, 0, 2))
    na = lambda a: np.ascontiguousarray(a.T)
    d = dict(RT=tr(arrT['oR']), KT=tr(arrT['oK']), AT=tr(arrT['oA']), BT=tr(arrT['oB']),
             Wn=na(arrT['oW']), Bn=na(arrT['oB']), Kn=na(arrT['oK']), Vn=na(arrT['oV']),
             BONn=na(arrT['oBon']), Gn=na(arrT['oG']))
    d.update(host_consts_0b(lnx_g, lnx_b))
    return d


D = 1024
NE = 32
GE = 2
ALPHA = (2.0 * 2) ** 0.25
LN_EPS = 1e-5


def build_C(ntok=4096, st=1024, level=9):
    nc = bass.Bass('TRN2', target_bir_lowering=False)
    dt = nc.dram_tensor
    xin = dt("xin", [ntok, D], F32, kind="ExternalInput").ap()
    ymT = dt("ymT", [D, ntok], F32, kind="ExternalInput").ap()
    w_out = dt("w_out", [D, D], F32, kind="ExternalInput").ap()
    wr = dt("wr", [D, 36], F32, kind="ExternalInput").ap()
    br = dt("br", [1, 36], F32, kind="ExternalInput").ap()
    lnp = dt("lnp", [128, 4, D], F32, kind="ExternalInput").ap()
    ident_d = dt("ident", [128, 128], F32, kind="ExternalInput").ap()
    sel_d = dt("sel", [32, NE * 128], F32, kind="ExternalInput").ap()
    e_w1 = dt("e_w1", [NE, D, 128], F32, kind="ExternalInput").ap()
    e_w3 = dt("e_w3", [NE, D, 128], F32, kind="ExternalInput").ap()
    e_w2 = dt("e_w2", [NE, 128, D], F32, kind="ExternalInput").ap()
    xout = dt("xout", [ntok, D], F32, kind="ExternalOutput").ap()
    s = Sched(nc)
    emit_C(s, xin, ymT, w_out, wr, br, lnp, ident_d, sel_d, e_w1, e_w3, e_w2, xout, ntok, st, level)
    s.build()
    return nc


def emit_C(s, xin, ymT, w_out, wr, br, lnp, ident_d, sel_d, e_w1, e_w3, e_w2, xout, ntok, st, level=9):
    wout_bf = s.sbuf('wout_bf', [128, 8, D], BF16)
    wr_f = s.sbuf('wr_f', [128, 8, 36], F32)
    br_f = s.sbuf('br_f', [1, 36], F32)
    ones1 = s.sbuf('ones1', [1, 128], F32)
    LNP = s.sbuf('LNP', [128, 4, D], F32)
    ident = s.sbuf('ident', [128, 128], F32)
    sel = s.sbuf('sel', [32, NE * 128], BF16)
    for k in range(8):
        s.dma('pool', wout_bf[:, k, :], w_out[k * 128:(k + 1) * 128, :], 'c0', writes=[('wout_bf', k)])
    s.dma('sp', wr_f[:], wr.rearrange("(k p) n -> p k n", p=128), 'c1', writes=['wr_f'])
    s.dma('sp', br_f[:], br, 'c2', writes=['br_f'])
    for a in range(4):
        s.dma('sp', LNP[:, a, :], lnp[:, a, :], ('c3', a), writes=[('LNP', a)])
    s.dma('sp', ident[:], ident_d, 'c4', writes=['ident'])
    s.dma('pool', sel[:], sel_d, 'c5', writes=['sel'])
    s.op('dve', lambda e: e.memset(ones1[:], 1.0), writes=['ones1'])
    epst = s.sbuf('epst', [128, 1], F32)
    s.op('dve', lambda e: e.memset(epst[:], LN_EPS), writes=['epst'])
    WOK = [('wout_bf', k) for k in range(8)]

    nst = ntok // st
    nsub = st // 128
    ntt = st // 256
    F = s.sbuf('F', [128, nsub, D], F32)
    xaT = s.sbuf('xaT', [128, 8, st], BF16)
    gT = s.sbuf('gT', [32, st], BF16)
    P = [s.psum('P%d' % i, [128, 512]) for i in range(8)]
    ym_bf = [s.sbuf('ym_bf%d' % i, [128, 8, 128], BF16) for i in range(2)]
    xin_t = [s.sbuf('xin_t%d' % i, [128, D], F32) for i in range(2)]
    z_t = [s.sbuf('z_t%d' % i, [128, D], F32) for i in range(2)]
    xa_t = [s.sbuf('xa_t%d' % i, [128, D], F32) for i in range(3)]
    xaTf = [s.sbuf('xaTf%d' % i, [128, 8, 128], F32) for i in range(2)]
    st6 = [s.sbuf('st6_%d' % i, [128, 2, 6], F32) for i in range(2)]
    mv = [s.sbuf('mv%d' % i, [128, 2], F32) for i in range(2)]
    rstd = [s.sbuf('rstd%d' % i, [128, 1], F32) for i in range(2)]
    nmr = [s.sbuf('nmr%d' % i, [128, 1], F32) for i in range(2)]
    lg = [s.sbuf('lg%d' % i, [128, 36], F32) for i in range(2)]
    rt = [s.sbuf('rt%d' % i, [128, 16], F32) for i in range(2)]
    r8 = [s.sbuf('r8_%d' % i, [128, 5, 8], F32) for i in range(2)]
    gates = [s.sbuf('gates%d' % i, [128, 32], F32) for i in range(2)]
    W1g = [s.sbuf('W1g%d' % i, [128, GE, 8, 128], BF16) for i in range(2)]
    W3g = [s.sbuf('W3g%d' % i, [128, GE, 8, 128], BF16) for i in range(2)]
    W2g = [s.sbuf('W2g%d' % i, [128, GE, D], BF16) for i in range(2)]
    s1 = [s.sbuf('s1_%d' % i, [128, 256], F32) for i in range(2)]
    uu = [s.sbuf('uu_%d' % i, [128, 256], F32) for i in range(2)]
    hg = [s.sbuf('hg_%d' % i, [128, 256], BF16) for i in range(2)]
    o_t = [s.sbuf('o_t%d' % i, [128, D], F32) for i in range(2)]

    def layer_norm_tile(src, srck, dst, dstk, gi, pi, eng_gb='pool'):
        st_, mv_, rs_, nm_ = st6[pi], mv[pi], rstd[pi], nmr[pi]
        for hh in range(2):
            s.op('dve', lambda e, hh=hh: e.bn_stats(out=st_[:, hh, :], in_=src[:, hh * 512:(hh + 1) * 512]),
                 reads=[srck], writes=[('st6', pi, hh)])
        s.op('dve', lambda e: e.bn_aggr(out=mv_[:], in_=st_[:].rearrange("p a b -> p (a b)")),
             reads=[('st6', pi, 0), ('st6', pi, 1)], writes=[('mv', pi)])
        s.op('act', lambda e: e.activation(out=rs_[:], in_=mv_[:, 1:2], func=AF.Ln, bias=epst[:], scale=1.0),
             reads=[('mv', pi), 'epst'], writes=[('rstd', pi)])
        s.op('act', lambda e: e.activation(out=rs_[:], in_=rs_[:], func=AF.Exp, scale=-0.5),
             reads=[('rstd', pi)], writes=[('rstd', pi)])
        s.op('dve', lambda e: e.scalar_tensor_tensor(out=nm_[:], in0=mv_[:, 0:1], scalar=-1.0, in1=rs_[:],
                                                     op0=ALU.mult, op1=ALU.mult),
             reads=[('mv', pi), ('rstd', pi)], writes=[('nmr', pi)])
        s.op('act', lambda e: e.activation(out=dst[:], in_=src[:], func=AF.Identity, bias=nm_[:], scale=rs_[:]),
             reads=[srck, ('rstd', pi), ('nmr', pi)], writes=[dstk])
        s.op(eng_gb, lambda e: e.tensor_tensor(out=dst[:], in0=dst[:], in1=LNP[:, gi, :], op=ALU.mult),
             reads=[dstk, ('LNP', gi)], writes=[dstk])
        s.op(eng_gb, lambda e: e.tensor_tensor(out=dst[:], in0=dst[:], in1=LNP[:, gi + 1, :], op=ALU.add),
             reads=[dstk, ('LNP', gi + 1)], writes=[dstk])

    wl_cnt = [0]

    def load_group(gi):
        b = wl_cnt[0] % 2
        wl_cnt[0] += 1
        for el in range(GE):
            ex = gi * GE + el
            s.dma('pool', W1g[b][:, el, :, :], e_w1[ex].rearrange("(k p) j -> p k j", p=128), ('w1', b, el),
                  writes=[('W1g', b, el)])
            s.dma('pool', W3g[b][:, el, :, :], e_w3[ex].rearrange("(k p) j -> p k j", p=128), ('w3', b, el),
                  writes=[('W3g', b, el)])
            s.dma('pool', W2g[b][:, el, :], e_w2[ex], ('w2', b, el), writes=[('W2g', b, el)])
        return b

    ngrp = NE // GE
    for sti in range(nst):
        t0 = sti * st
        def stage_L(sub):
            pi = sub % 2
            tok0 = t0 + sub * 128
            s.dma('pool', ym_bf[pi][:], ymT[:, tok0:tok0 + 128].rearrange("(k p) t -> p k t", p=128), ('ym', pi), writes=[('ym_bf', pi)])
            s.dma('sp', xin_t[pi][:], xin[tok0:tok0 + 128, :], ('xin', pi), writes=[('xin_t', pi)])

        def stage_A(sub):
            pi = sub % 2
            p3_ = sub % 3
            yb = ym_bf[pi]
            ybk = ('ym_bf', pi)
            xt, zt, xat = xin_t[pi], z_t[pi], xa_t[p3_]
            for hh in range(2):
                pb = P[2 * pi + hh]
                pk = ('P', 2 * pi + hh)
                for k in range(8):
                    s.op('pe', lambda e, pb=pb, k=k, hh=hh: e.matmul(
                        pb[:], lhsT=yb[:, k, :], rhs=wout_bf[:, k, hh * 512:(hh + 1) * 512],
                        start=(k == 0), stop=(k == 7)), reads=[ybk] + WOK, writes=[pk], inc=(k == 7))
                s.op('dve', lambda e, pb=pb, hh=hh: e.scalar_tensor_tensor(
                    out=zt[:, hh * 512:(hh + 1) * 512], in0=xt[:, hh * 512:(hh + 1) * 512], scalar=ALPHA,
                    in1=pb[:], op0=ALU.mult, op1=ALU.add), reads=[pk, ('xin_t', pi)], writes=[('z_t', pi)])
            layer_norm_tile(zt, ('z_t', pi), xat, ('xa_t', p3_), 0, pi)
            s.op('act', lambda e: e.mul(out=F[:, sub, :], in_=xat[:], mul=ALPHA), reads=[('xa_t', p3_)], writes=[('F', sub)])

        def stage_B(sub):
            pi = sub % 2
            p3_ = sub % 3
            xat = xa_t[p3_]
            xf = xaTf[pi]
            for k in range(8):
                pb = P[4 + 2 * pi + k // 4]
                pk = ('P', 4 + 2 * pi + k // 4)
                s.op('pe', lambda e, pb=pb, k=k: e.transpose(
                    out=pb[:, (k % 4) * 128:(k % 4 + 1) * 128], in_=xat[:, k * 128:(k + 1) * 128], identity=ident[:]),
                    reads=[('xa_t', p3_), 'ident'], writes=[pk])
            for hb in range(2):
                pb = P[4 + 2 * pi + hb]
                pk = ('P', 4 + 2 * pi + hb)
                s.op('act', lambda e, pb=pb, hb=hb: e.copy(
                    out=xf[:, hb * 4:(hb + 1) * 4, :], in_=pb[:].rearrange("p (k t) -> p k t", k=4)),
                    reads=[pk], writes=[('xaTf', pi, hb)])
                s.op('pool', lambda e, hb=hb: e.tensor_copy(
                    out=xaT[:, hb * 4:(hb + 1) * 4, sub * 128:(sub + 1) * 128], in_=xf[:, hb * 4:(hb + 1) * 4, :]),
                    reads=[('xaTf', pi, hb)], writes=[('xaT', sub)])
            pb = P[4 + 2 * pi]
            pk = ('P', 4 + 2 * pi)
            for k in range(8):
                s.op('pe', lambda e, k=k: e.matmul(
                    pb[:, 0:36], lhsT=xf[:, k, :], rhs=wr_f[:, k, :], start=(k == 0), stop=False),
                    reads=[('xaTf', pi, k // 4), 'wr_f'], writes=[pk], inc=False)
            s.op('pe', lambda e: e.matmul(pb[:, 0:36], lhsT=ones1[:], rhs=br_f[:], start=False, stop=True),
                 reads=['ones1', 'br_f'], writes=[pk])
            L = lg[pi]
            s.op('act', lambda e: e.copy(out=L[:], in_=pb[:, 0:36]), reads=[pk], writes=[('lg', pi)])

        def stage_C(sub):
            pi = sub % 2
            L = lg[pi]
            Lk = ('lg', pi)
            R = rt[pi]
            Rk = ('rt', pi)
            R8 = r8[pi]
            R8k = ('r8', pi)
            G = gates[pi]
            V = lambda fn, rd, wr_: s.op('dve', fn, reads=rd, writes=wr_)
            V(lambda e: e.reduce_max(out=R[:, 0:1], in_=L[:, 0:4], axis=AX.X), [Lk], [Rk])
            V(lambda e: e.tensor_scalar(out=R[:, 4:8], in0=L[:, 0:4], scalar1=R[:, 0:1], scalar2=None, op0=ALU.is_ge), [Lk, Rk], [Rk])
            V(lambda e: e.tensor_scalar(out=R[:, 1:2], in0=R[:, 0:1], scalar1=-1.0, scalar2=None, op0=ALU.mult), [Rk], [Rk])
            s.op('act', lambda e: e.activation(out=R[:, 8:12], in_=L[:, 0:4], func=AF.Exp, bias=R[:, 1:2], scale=1.0),
                 reads=[Lk, Rk], writes=[Rk])
            V(lambda e: e.reduce_sum(out=R[:, 2:3], in_=R[:, 8:12], axis=AX.X), [Rk], [Rk])
            V(lambda e: e.tensor_scalar(out=R8[:, 0, :], in0=L[:, 4:12], scalar1=R[:, 4:5], scalar2=None, op0=ALU.mult), [Lk, Rk], [R8k])
            for g in range(1, 4):
                V(lambda e, g=g: e.scalar_tensor_tensor(
                    out=R8[:, 0, :], in0=L[:, 4 + 8 * g:12 + 8 * g], scalar=R[:, 4 + g:5 + g], in1=R8[:, 0, :],
                    op0=ALU.mult, op1=ALU.add), [Lk, Rk, R8k], [R8k])
            V(lambda e: e.max(out=R8[:, 1, :], in_=R8[:, 0, :]), [R8k], [R8k])
            V(lambda e: e.tensor_scalar(out=R8[:, 2, :], in0=R8[:, 0, :], scalar1=R8[:, 1, 1:2], scalar2=None, op0=ALU.is_ge), [R8k], [R8k])
            V(lambda e: e.tensor_scalar(out=R[:, 12:13], in0=R8[:, 1, 0:1], scalar1=-1.0, scalar2=None, op0=ALU.mult), [R8k, Rk], [Rk])
            s.op('act', lambda e: e.activation(out=R8[:, 3, :], in_=R8[:, 0, :], func=AF.Exp, bias=R[:, 12:13], scale=1.0),
                 reads=[R8k, Rk], writes=[R8k])
            V(lambda e: e.tensor_tensor(out=R8[:, 4, :], in0=R8[:, 3, :], in1=R8[:, 2, :], op=ALU.mult), [R8k], [R8k])
            V(lambda e: e.reduce_sum(out=R[:, 13:14], in_=R8[:, 4, :], axis=AX.X), [R8k, Rk], [Rk])
            V(lambda e: e.tensor_tensor(out=R[:, 14:15], in0=R[:, 13:14], in1=R[:, 2:3], op=ALU.mult), [Rk], [Rk])
            V(lambda e: e.reciprocal(out=R[:, 15:16], in_=R[:, 14:15]), [Rk], [Rk])
            V(lambda e: e.tensor_scalar(out=R8[:, 3, :], in0=R8[:, 4, :], scalar1=R[:, 15:16], scalar2=None, op0=ALU.mult), [R8k, Rk], [R8k])
            for g in range(4):
                V(lambda e, g=g: e.tensor_scalar(out=G[:, 8 * g:8 * g + 8], in0=R8[:, 3, :], scalar1=R[:, 4 + g:5 + g], scalar2=None,
                                                 op0=ALU.mult), [R8k, Rk], [('gates', pi)])

        def stage_D(sub):
            pi = sub % 2
            G = gates[pi]
            pb2 = P[5 + 2 * pi]
            pk2 = ('P', 5 + 2 * pi)
            s.op('pe', lambda e: e.transpose(out=pb2[0:32, 0:128], in_=G[:], identity=ident[:]),
                 reads=[('gates', pi), 'ident'], writes=[pk2])
            s.op('act', lambda e: e.copy(out=gT[:, sub * 128:(sub + 1) * 128], in_=pb2[0:32, 0:128]),
                 reads=[pk2], writes=[('gT', sub)])

        stage_L(0)
        for step in range(nsub + 4):
            if step + 1 < nsub:
                stage_L(step + 1)
            if 0 <= step - 4 < nsub:
                stage_D(step - 4)
            if 0 <= step - 2 < nsub:
                stage_B(step - 2)
            if 0 <= step - 3 < nsub:
                stage_C(step - 3)
            if step < nsub:
                stage_A(step)

        def moe_front(gi, b, tt, el, hb):
            c0 = tt * 256
            ex = gi * GE + el
            p1 = P[4 + 3 * hb]
            p1k = ('P', 4 + 3 * hb)
            p3 = P[5 + hb]
            p3k = ('P', 5 + hb)
            xk = [('xaT', 2 * tt), ('xaT', 2 * tt + 1)]
            for k in range(8):
                s.op('pe', lambda e, k=k: e.matmul(p1[:, 0:256], lhsT=W1g[b][:, el, k, :], rhs=xaT[:, k, c0:c0 + 256],
                                                   start=(k == 0), stop=(k == 7)),
                     reads=[('W1g', b, el)] + xk, writes=[p1k], inc=(k == 7))
            for k in range(8):
                s.op('pe', lambda e, k=k: e.matmul(p3[:, 0:256], lhsT=W3g[b][:, el, k, :], rhs=xaT[:, k, c0:c0 + 256],
                                                   start=(k == 0), stop=(k == 7)),
                     reads=[('W3g', b, el)] + xk, writes=[p3k], inc=False)
            s.op('pe', lambda e: e.matmul(p3[:, 256:512], lhsT=sel[:, ex * 128:(ex + 1) * 128], rhs=gT[:, c0:c0 + 256],
                                          start=True, stop=True), reads=['sel', ('gT', 2 * tt), ('gT', 2 * tt + 1)], writes=[p3k])
            s.op('act', lambda e: e.activation(out=s1[hb][:], in_=p1[:, 0:256], func=AF.Silu), reads=[p1k], writes=[('s1', hb)])
            s.op('dve', lambda e: e.tensor_tensor(out=uu[hb][:], in0=s1[hb][:], in1=p3[:, 256:512], op=ALU.mult),
                 reads=[('s1', hb), p3k], writes=[('uu', hb)])
            s.op('dve', lambda e: e.tensor_tensor(out=hg[hb][:], in0=uu[hb][:], in1=p3[:, 0:256], op=ALU.mult),
                 reads=[('uu', hb), p3k], writes=[('hg', hb)])

        def moe_back(gi, b, tt, el, hb):
            for sb in range(2):
                for hh in range(2):
                    pa = P[2 * sb + hh]
                    pak = ('P', 2 * sb + hh)
                    s.op('pe', lambda e, pa=pa, sb=sb, hh=hh: e.matmul(
                        pa[:], lhsT=hg[hb][:, sb * 128:(sb + 1) * 128], rhs=W2g[b][:, el, hh * 512:(hh + 1) * 512],
                        start=(el == 0), stop=(el == GE - 1)),
                        reads=[('hg', hb), ('W2g', b, el)], writes=[pak], inc=(sb == 1 and hh == 1))
            if el == GE - 1:
                for sb in range(2):
                    sub = 2 * tt + sb
                    for hh in range(2):
                        pa = P[2 * sb + hh]
                        pak = ('P', 2 * sb + hh)
                        s.op('dve', lambda e, pa=pa, sub=sub, hh=hh: e.tensor_tensor(
                            out=F[:, sub, hh * 512:(hh + 1) * 512], in0=F[:, sub, hh * 512:(hh + 1) * 512], in1=pa[:],
                            op=ALU.add), reads=[pak, ('F', sub)], writes=[('F', sub)])

        steps = []
        for gi in range(ngrp if level >= 7 else 0):
            for tt in range(ntt):
                for el in range(GE):
                    steps.append((gi, tt, el))
        pend = None
        gb = {}
        for n_, (gi, tt, el) in enumerate(steps):
            if gi not in gb:
                gb[gi] = load_group(gi)
            cur_ = (gi, gb[gi], tt, el, n_ % 2)
            moe_front(*cur_)
            if pend is not None:
                moe_back(*pend)
            pend = cur_
        if pend is not None:
            moe_back(*pend)
        for sub in range(nsub):
            pi = sub % 2
            ot = o_t[pi]
            if level < 0.3 or (5 <= level < 6):
                s.op('act', lambda e, sub=sub, ot=ot: e.copy(out=ot[:], in_=F[:, sub, :]),
                     reads=[('F', sub)], writes=[('o_t', pi)])
            else:
                layer_norm_tile(F[:, sub, :], ('F', sub), ot, ('o_t', pi), 2, pi)
            s.dma('sp', xout[t0 + sub * 128: t0 + (sub + 1) * 128, :], ot[:], ('st', pi), reads=[('o_t', pi)],
                  writes=[('xout', pi)])
    s.wait_all('sp', [('xout', 0), ('xout', 1)])


def host_inputs_C(xin, ymT, w_out, g1, b1, rg_w, rg_b, re_w, re_b, w1, w3, w2, g2, b2):
    wr = np.ascontiguousarray(np.concatenate([rg_w, re_w], axis=1))
    br = np.ascontiguousarray(np.concatenate([rg_b, re_b])[None, :])
    lnp = np.ascontiguousarray(np.broadcast_to(np.stack([g1, b1, g2, b2])[None], (128, 4, D)))
    ident = np.eye(128, dtype=np.float32)
    sel = np.zeros((32, NE, 128), np.float32)
    for e in range(NE):
        sel[e, e, :] = 1.0
    return dict(xin=np.ascontiguousarray(xin), ymT=np.ascontiguousarray(ymT), w_out=np.ascontiguousarray(w_out),
                wr=wr, br=br, lnp=lnp, ident=ident, sel=sel.reshape(32, NE * 128),
                e_w1=np.ascontiguousarray(w1), e_w3=np.ascontiguousarray(w3), e_w2=np.ascontiguousarray(w2))


HALO1 = 32
CONV_W = 31
LN_EPS1 = 1e-5


def build_1a(ntok=4096, tile=512):
    nc = bass.Bass('TRN2', target_bir_lowering=False)
    dt = nc.dram_tensor
    xT = dt("xT", [1024, HALO1 + ntok], F32, kind="ExternalInput").ap()
    w_ext = dt("w_ext", [1024, 4096], F32, kind="ExternalInput").ap()
    ccols = dt("ccols", [128, 4, 34], F32, kind="ExternalInput").ap()
    rot = dt("rot", [128, 2, ntok], F32, kind="ExternalInput").ap()
    ones_d = dt("ones", [128, 128], F32, kind="ExternalInput").ap()
    outs = {n: dt(n, [512, ntok], F32, kind="ExternalOutput").ap() for n in ('oYC', 'oQ', 'oKr', 'oV', 'oGr')}
    s = Sched(nc)
    emit_1a(s, xT, w_ext, ccols, rot, ones_d, outs, ntok, tile)
    s.build()
    return nc


def emit_1a(s, xT, w_ext, ccols_d, rot_d, ones_d, outs, ntok, T):
    W = s.sbuf('W', [128, 8, 4096], BF16)
    for k in range(8):
        for hf in range(2):
            s.dma('pool', W[:, k, hf * 2048:(hf + 1) * 2048], w_ext[k * 128:(k + 1) * 128, hf * 2048:(hf + 1) * 2048], 'w0',
                  writes=[('W', k, hf)])
    WK = [('W', k, hf) for k in range(8) for hf in range(2)]
    CC = s.sbuf('CC', [128, 4, 34], F32)
    s.dma('sp', CC[:], ccols_d, 'c0', writes=['CC'])
    ONES = s.sbuf('ONES', [128, 128], F32)
    s.dma('sp', ONES[:], ones_d, 'c1', writes=['ONES'])
    epst = s.sbuf('epst', [128, 1], F32)
    s.op('dve', lambda e: e.memset(epst[:], LN_EPS1), writes=['epst'])
    P = [s.psum('P%d' % i, [128, 512]) for i in range(8)]
    xb = [s.sbuf('xb%d' % i, [128, 8, T], BF16) for i in range(2)]
    xh = s.sbuf('xh', [128, 8, HALO1], BF16)
    CA = s.sbuf('CA', [128, T], F32)
    SG = s.sbuf('SG', [128, T], F32)
    CAH = s.sbuf('CAH', [128, 4, HALO1], F32)
    Ub = [s.sbuf('U%d' % i, [128, 4, HALO1 + T], F32) for i in range(2)]
    ACC = s.sbuf('ACC', [128, 4, T], F32)
    SQ = s.sbuf('SQ', [128, T], F32)
    XP = s.sbuf('XP', [128, 16, T], F32)
    ROT = [s.sbuf('ROT%d' % i, [128, 2, T], F32) for i in range(2)]
    MEAN = s.sbuf('MEAN', [128, T], F32)
    MSQ = s.sbuf('MSQ', [128, T], F32)
    RSTD = s.sbuf('RSTD', [128, T], F32)
    t1 = [s.sbuf('t1_%d' % i, [128, T], F32) for i in range(2)]
    t2 = [s.sbuf('t2_%d' % i, [128, T], F32) for i in range(2)]
    OB = {n: [s.sbuf('O_%s%d' % (n, i), [128, T], F32) for i in range(2)] for n in ('oYC', 'oQ', 'oKr', 'oV', 'oGr')}
    ocnt = {n: 0 for n in OB}

    def out_buf(n):
        i = ocnt[n] % 2
        ocnt[n] += 1
        return OB[n][i], (n, i)

    def proj(c, x_, XK, ncol, pb, pk):
        for k in range(8):
            MM(s, pb[:, 0:ncol], W[:, k, c * 128:(c + 1) * 128], x_[:, k, :], start=(k == 0), stop=(k == 7),
               reads=XK + WK, writes=[pk], inc=(k == 7))

    s.dma('pool', xh[:], xT[:, 0:HALO1].rearrange("(k p) t -> p k t", p=128), 'xh', writes=['xh'])
    for c in range(8):
        pb, pk = P[c % 2], ('P', c % 2)
        proj(c, xh, ['xh'], HALO1, pb, pk)
        if c < 4:
            CP(s, 'act', CAH[:, c, :], pb[:, 0:HALO1], [pk], [('CAH', c)])
        else:
            ACTF(s, SG[:, 0:HALO1], pb[:, 0:HALO1], AF.Sigmoid, [pk], ['SG'])
            TT(s, 'dve', Ub[0][:, c - 4, 0:HALO1], CAH[:, c - 4, :], SG[:, 0:HALO1], ALU.mult, [('CAH', c - 4), 'SG'], [('U', 0, c - 4)])

    def tile_body(ti):
        t0 = ti * T
        x_ = xb[ti % 2]
        XK = [('xb', ti % 2)]
        s.dma('pool', x_[:], xT[:, HALO1 + t0:HALO1 + t0 + T].rearrange("(k p) t -> p k t", p=128), ('x', ti % 2), writes=XK)
        rt = ROT[ti % 2]
        rk = ('ROT', ti % 2)
        s.dma('sp', rt[:], rot_d[:, :, t0:t0 + T], ('rot', ti % 2), writes=[rk])
        tsl = slice(t0, t0 + T)
        ub = ti % 2
        U = Ub[ub]
        for cc in range(4):
            pb, pk = P[0], ('P', 0)
            proj(cc, x_, XK, T, pb, pk)
            CP(s, 'act', CA[:], pb[:], [pk], ['CA'])
            pb, pk = P[1], ('P', 1)
            proj(4 + cc, x_, XK, T, pb, pk)
            ACTF(s, SG[:], pb[:], AF.Sigmoid, [pk], ['SG'])
            TT(s, 'pool', U[:, cc, HALO1:HALO1 + T], CA[:], SG[:], ALU.mult, ['CA', 'SG'], [('U', ub, cc)])
        for cc in range(4):
            CP(s, 'pool', Ub[1 - ub][:, cc, 0:HALO1], U[:, cc, T:T + HALO1], [('U', ub, cc)], [('U', 1 - ub, cc)])
        for cc in range(4):
            TS(s, 'dve', ACC[:, cc, :], U[:, cc, 2:2 + T], CC[:, cc, 0:1], CC[:, cc, 31:32], ALU.mult, ALU.add,
               [('U', ub, cc), 'CC'], [('ACC', cc)])
            for j in range(1, CONV_W):
                STT(s, ACC[:, cc, :], U[:, cc, 2 + j:2 + j + T], CC[:, cc, j:j + 1], ACC[:, cc, :], ALU.mult, ALU.add,
                    [('U', ub, cc), 'CC', ('ACC', cc)], [('ACC', cc)])
        order = [(8 + i, i, 1.0) for i in range(4)] + [(24 + i, 8 + i, 1.0) for i in range(4)] + \
                [(12 + i, 4 + i, 0.125) for i in range(4)] + [(28 + i, 12 + i, 0.125) for i in range(4)]
        for n_, (c, slot, sc) in enumerate(order):
            pb, pk = P[n_ % 2], ('P', n_ % 2)
            proj(c, x_, XK, T, pb, pk)
            s.op('act', lambda e, pb=pb, slot=slot, sc=sc: e.mul(out=XP[:, slot, :], in_=pb[:], mul=sc), reads=[pk], writes=[('XP', slot)])
        for i in range(4):
            rows = slice(i * 128, (i + 1) * 128)
            pb, pk = P[0], ('P', 0)
            proj(16 + i, x_, XK, T, pb, pk)
            ob, obk = out_buf('oV')
            CP(s, 'act', ob[:], pb[:], [pk], [obk])
            s.dma('sp', outs['oV'][rows, tsl], ob[:], ('so',) + obk, reads=[obk], writes=[('d',) + obk])
            pb, pk = P[1], ('P', 1)
            proj(20 + i, x_, XK, T, pb, pk)
            ob, obk = out_buf('oGr')
            ACTF(s, ob[:], pb[:], AF.Silu, [pk], [obk])
            s.dma('sp', outs['oGr'][rows, tsl], ob[:], ('so',) + obk, reads=[obk], writes=[('d',) + obk])
        for i in range(8):
            name = 'oQ' if i < 4 else 'oKr'
            rows = slice((i % 4) * 128, (i % 4 + 1) * 128)
            a, b = t1[i % 2], t2[i % 2]
            TT(s, 'pool', a[:], XP[:, i, :], rt[:, 0, :], ALU.mult, [('XP', i), rk], [('t1', i % 2)])
            TT(s, 'pool', b[:], XP[:, 8 + i, :], rt[:, 1, :], ALU.mult, [('XP', 8 + i), rk], [('t2', i % 2)])
            ob, obk = out_buf(name)
            TT(s, 'pool', ob[:], a[:], b[:], ALU.add, [('t1', i % 2), ('t2', i % 2)], [obk])
            s.dma('sp', outs[name][rows, tsl], ob[:], ('so',) + obk, reads=[obk], writes=[('d',) + obk])
        for cc in range(4):
            MM(s, P[2][:], ONES[:], ACC[:, cc, :], start=(cc == 0), stop=(cc == 3), reads=['ONES', ('ACC', cc)], writes=[('P', 2)],
               inc=(cc == 3))
        for cc in range(4):
            s.op('act', lambda e, cc=cc: e.activation(out=SQ[:], in_=ACC[:, cc, :], func=AF.Square), reads=[('ACC', cc)], writes=['SQ'])
            MM(s, P[3][:], ONES[:], SQ[:], start=(cc == 0), stop=(cc == 3), reads=['ONES', 'SQ'], writes=[('P', 3)])
        s.op('act', lambda e: e.mul(out=MEAN[:], in_=P[2][:], mul=1.0 / 512), reads=[('P', 2)], writes=['MEAN'])
        TT(s, 'pool', MSQ[:], MEAN[:], MEAN[:], ALU.mult, ['MEAN'], ['MSQ'])
        STT(s, RSTD[:], P[3][:], 1.0 / 512, MSQ[:], ALU.mult, ALU.subtract, [('P', 3), 'MSQ'], ['RSTD'])
        ACTF(s, RSTD[:], RSTD[:], AF.Ln, ['RSTD', 'epst'], ['RSTD'], bias=epst[:], scale=1.0)
        ACTF(s, RSTD[:], RSTD[:], AF.Exp, ['RSTD'], ['RSTD'], scale=-0.5)
        for cc in range(4):
            rows = slice(cc * 128, (cc + 1) * 128)
            a = t1[cc % 2]
            TT(s, 'pool', a[:], ACC[:, cc, :], MEAN[:], ALU.subtract, [('ACC', cc), 'MEAN'], [('t1', cc % 2)])
            TT(s, 'pool', a[:], a[:], RSTD[:], ALU.mult, [('t1', cc % 2), 'RSTD'], [('t1', cc % 2)])
            ob, obk = out_buf('oYC')
            ACTF(s, ob[:], a[:], AF.Silu, [('t1', cc % 2), 'CC'], [obk], bias=CC[:, cc, 33:34], scale=CC[:, cc, 32:33])
            s.dma('sp', outs['oYC'][rows, tsl], ob[:], ('so',) + obk, reads=[obk], writes=[('d',) + obk])

    for ti in range(ntok // T):
        tile_body(ti)
    s.wait_all('sp', [k for k in s.last_w if isinstance(k, tuple) and k[0] == 'd'])


def rot_perm():
    idx = np.arange(512).reshape(8, 2, 32)[:, ::-1, :].reshape(-1)
    return idx


def host_inputs_1a(xT_halo, w_in, conv_w, conv_b, cln_g, cln_b, pos0, ntok=4096):
    perm = rot_perm()
    w_ext = np.concatenate([w_in, w_in[:, 1024:1536][:, perm], w_in[:, 1536:2048][:, perm]], axis=1)
    ccols = np.zeros((128, 4, 34), np.float32)
    ccols[:, :, 0:31] = conv_w.T.reshape(4, 128, 31).transpose(1, 0, 2)
    ccols[:, :, 31] = conv_b.reshape(4, 128).T
    ccols[:, :, 32] = cln_g.reshape(4, 128).T
    ccols[:, :, 33] = cln_b.reshape(4, 128).T
    half = 32
    inv = (np.float32(10000.0) ** (-np.arange(half, dtype=np.float32) / np.float32(half))).astype(np.float32)
    pos = np.arange(pos0, pos0 + ntok, dtype=np.float32)
    ang = (pos[:, None] * inv[None, :]).astype(np.float32)
    cos, sin = np.cos(ang).astype(np.float32), np.sin(ang).astype(np.float32)
    p = np.arange(128)
    i = p % 32
    second = (p % 64) >= 32
    rot = np.empty((128, 2, ntok), np.float32)
    rot[:, 0, :] = cos.T[i]
    rot[:, 1, :] = np.where(second[:, None], sin.T[i], -sin.T[i])
    return dict(xT=np.ascontiguousarray(xT_halo), w_ext=np.ascontiguousarray(w_ext), ccols=ccols, rot=rot,
                ones=np.ones((128, 128), np.float32))


RC = 128
RH = 4
RET_EPS = 1e-5


def build_1b(T=8192, GCH=2):
    nc = bass.Bass('TRN2', target_bir_lowering=False)
    dt = nc.dram_tensor
    tin = {n: dt(n, [64, RH, T], F32, kind="ExternalInput").ap() for n in ('QT', 'KT')}
    nin = {n: dt(n, [T, RH * 64], F32, kind="ExternalInput").ap() for n in ('Kn', 'Vn', 'Grn')}
    cst = {n: dt(n, shp, F32, kind="ExternalInput").ap() for n, shp in
           (('DM', [128, RH, 128]), ('XI', [64, RH, 128]), ('ZE', [128, RH * 64]), ('GCC', [64, RH]), ('GNB', [128, 2, RH * 64]))}
    y = dt("y", [T, RH * 64], F32, kind="ExternalOutput").ap()
    s = Sched(nc)
    emit_1b(s, tin, nin, cst, y, T, GCH)
    s.build()
    return nc


def emit_1b(s, tin, nin, cst, y, T, GCH):
    K = {}
    for n, shp in (('DM', [128, RH, 128]), ('XI', [64, RH, 128]), ('ZE', [128, RH * 64]), ('GCC', [64, RH]), ('GNB', [128, 2, RH * 64])):
        K[n] = s.sbuf('K_' + n, shp, F32)
        s.dma('sp', K[n][:], cst[n], ('k', n), writes=[n])
    epst = s.sbuf('epst', [128, 1], F32)
    s.op('dve', lambda e: e.memset(epst[:], RET_EPS), writes=['epst'])
    Rst = s.sbuf('Rst', [64, RH, 64], BF16)
    s.op('dve', lambda e: e.memset(Rst[:], 0.0), writes=['Rst'])
    PS = [s.psum('PS%d' % i, [128, 512]) for i in range(4)]
    GT = GCH * RC
    tbuf = {n: [s.sbuf('t_%s%d' % (n, i), [64, RH, GT], BF16) for i in range(2)] for n in tin}
    nbuf = {n: [s.sbuf('n_%s%d' % (n, i), [128, GCH, RH * 64], BF16 if n == 'Vn' else F32) for i in range(2)] for n in nin}
    QX = s.sbuf('QX', [64, RH, RC], BF16)
    KZ = s.sbuf('KZ', [128, RH * 64], BF16)
    SM = s.sbuf('SM', [128, RH, RC], BF16)
    Ysb = s.sbuf('Ysb', [128, RH, 64], F32)
    Ysq = s.sbuf('Ysq', [128, RH, 64], F32)
    Yn = s.sbuf('Yn', [128, RH, 64], F32)
    st = s.sbuf('st', [128, 6, RH], F32)
    Yo = [s.sbuf('Yo%d' % i, [128, RH * 64], F32) for i in range(2)]
    nchunk = T // RC
    ngrp = nchunk // GCH

    def load_group(gi):
        b = gi % 2
        t0 = gi * GT
        for n in tin:
            s.dma('pool', tbuf[n][b][:], tin[n][:, :, t0:t0 + GT], ('lt', n, b), writes=[('t', n, b)])
        for n in nin:
            s.dma('pool', nbuf[n][b][:], nin[n][t0:t0 + GT, :].rearrange("(c p) n -> p c n", p=128), ('ln', n, b),
                  writes=[('n', n, b)])

    def chunk(ci):
        gi, cj = divmod(ci, GCH)
        b = gi % 2
        tv = lambda n: tbuf[n][b][:, :, cj * RC:(cj + 1) * RC]
        tk = lambda n: ('t', n, b)
        nv = lambda n: nbuf[n][b][:, cj, :]
        nk = lambda n: ('n', n, b)
        TT(s, 'pool', QX[:], tv('QT'), K['XI'][:], ALU.mult, [tk('QT'), 'XI'], ['QX'])
        TT(s, 'pool', KZ[:], nv('Kn'), K['ZE'][:], ALU.mult, [nk('Kn'), 'ZE'], ['KZ'])
        for h in range(RH):
            MM(s, PS[0][:, h * RC:(h + 1) * RC], tbuf['KT'][b][:, h, cj * RC:(cj + 1) * RC], tbuf['QT'][b][:, h, cj * RC:(cj + 1) * RC],
               reads=[tk('KT'), tk('QT')], writes=['B0'], inc=(h == RH - 1))
        TT(s, 'dve', SM[:], PS[0][:, :].rearrange("p (h x) -> p h x", h=RH), K['DM'][:], ALU.mult, ['B0', 'DM'], ['SM'])
        for h in range(RH):
            vh = nbuf['Vn'][b][:, cj, h * 64:(h + 1) * 64]
            o = PS[1][:, h * 64:(h + 1) * 64]
            MM(s, o, SM[:, h, :], vh, start=True, stop=False, reads=['SM', nk('Vn')], writes=['B1'], inc=False)
            MM(s, o, QX[:, h, :], Rst[:, h, :], start=False, stop=True, reads=['QX', 'Rst'], writes=['B1'], inc=(h == RH - 1))
        for h in range(RH):
            vh = nbuf['Vn'][b][:, cj, h * 64:(h + 1) * 64]
            MM(s, PS[2][0:64, h * 64:(h + 1) * 64], KZ[:, h * 64:(h + 1) * 64], vh, reads=['KZ', nk('Vn')], writes=['B2'],
               inc=(h == RH - 1))
        CP(s, 'dve', Ysb[:], PS[1][:, 0:RH * 64].rearrange("p (h x) -> p h x", h=RH), ['B1'], ['Ysb'])
        for h in range(RH):
            STT(s, Rst[:, h, :], Rst[:, h, :], K['GCC'][:, h:h + 1], PS[2][0:64, h * 64:(h + 1) * 64], ALU.mult, ALU.add,
                ['Rst', 'GCC', 'B2'], ['Rst'])
        RED(s, st[:, 0, :], Ysb[:], ['Ysb'], ['st0'])
        TT(s, 'pool', Ysq[:], Ysb[:], Ysb[:], ALU.mult, ['Ysb'], ['Ysq'])
        RED(s, st[:, 1, :], Ysq[:], ['Ysq'], ['st1'])
        TS(s, 'dve', st[:, 2, :], st[:, 0, :], 1.0 / 64, None, ALU.mult, None, ['st0'], ['st2'])
        TT(s, 'dve', st[:, 3, :], st[:, 2, :], st[:, 2, :], ALU.mult, ['st2'], ['st3'])
        STT(s, st[:, 4, :], st[:, 1, :], 1.0 / 64, st[:, 3, :], ALU.mult, ALU.subtract, ['st1', 'st3'], ['st4'])
        ACTF(s, st[:, 5, :], st[:, 4, :], AF.Ln, ['st4', 'epst'], ['st5'], bias=epst[:], scale=1.0)
        ACTF(s, st[:, 5, :], st[:, 5, :], AF.Exp, ['st5'], ['st5'], scale=-0.5)
        for h in range(RH):
            TS(s, 'dve', Yn[:, h, :], Ysb[:, h, :], st[:, 2, h:h + 1], st[:, 5, h:h + 1], ALU.subtract, ALU.mult,
               ['Ysb', 'st2', 'st5'], ['Yn'])
        ynf = Yn[:].rearrange("p h x -> p (h x)")
        yo = Yo[ci % 2]
        yok = ('Yo', ci % 2)
        TT(s, 'pool', ynf, ynf, K['GNB'][:, 0, :], ALU.mult, ['Yn', 'GNB'], ['Yn'])
        TT(s, 'pool', ynf, ynf, K['GNB'][:, 1, :], ALU.add, ['Yn', 'GNB'], ['Yn'])
        TT(s, 'pool', yo[:], ynf, nv('Grn'), ALU.mult, ['Yn', nk('Grn')], [yok])
        s.dma('sp', y[ci * RC:(ci + 1) * RC, :], yo[:], ('so', ci % 2), reads=[yok], writes=[('yd', ci % 2)])

    load_group(0)
    for gi in range(ngrp):
        if gi + 1 < ngrp:
            load_group(gi + 1)
        for cj in range(GCH):
            chunk(gi * GCH + cj)
    s.wait_all('sp', [('yd', 0), ('yd', 1)])


def host_inputs_1b(arrT, gn_g, gn_b, head0):
    T = arrT['oQ'].shape[1]
    tr = lambda a: np.ascontiguousarray(a.reshape(RH, 64, T).transpose(1, 0, 2))
    na = lambda a: np.ascontiguousarray(a.T)
    hidx = np.arange(head0, head0 + RH, dtype=np.float32)
    log_gamma = np.log1p(-np.power(np.float32(2.0), -5.0 - hidx)).astype(np.float32)
    idx = np.arange(RC, dtype=np.float32)
    diff = idx[None, :] - idx[:, None]
    DM = np.where(diff[:, None, :] >= 0, np.exp(np.maximum(diff, 0.0)[:, None, :] * log_gamma[None, :, None]), 0.0)
    xi = np.exp((idx + 1.0)[None, :] * log_gamma[:, None])
    zeta = np.exp((RC - 1.0 - idx)[None, :] * log_gamma[:, None])
    gch = np.exp(RC * log_gamma)
    XI = np.broadcast_to(xi[None], (64, RH, RC))
    ZE = np.repeat(zeta.T, 64, axis=1)
    GCC = np.broadcast_to(gch[None], (64, RH))
    GNB = np.broadcast_to(np.stack([gn_g, gn_b])[None], (128, 2, RH * 64))
    f = lambda a: np.ascontiguousarray(a, dtype=np.float32)
    return dict(QT=tr(arrT['oQ']), KT=tr(arrT['oKr']), Kn=na(arrT['oKr']), Vn=na(arrT['oV']), Grn=na(arrT['oGr']),
                DM=f(DM), XI=f(XI), ZE=f(ZE), GCC=f(GCC), GNB=f(GNB))


NTOK = 4096
SHARDS = [(c // 2, (c % 2) * NTOK) for c in range(8)]


def _run(nc, in_maps):
    from concourse.bass_utils import run_bass_kernel_spmd
    return run_bass_kernel_spmd(nc, in_maps, core_ids=list(range(len(in_maps)))).results


def _post_mixer(inp, cur, ymTs, layer, w_out):
    ncC = build_C(ntok=NTOK, st=1024)
    maps = []
    for ci, (b, t0) in enumerate(SHARDS):
        maps.append(host_inputs_C(cur[b, t0:t0 + NTOK], ymTs[ci], w_out, inp['ln_mix_g'][layer], inp['ln_mix_b'][layer],
                                  inp['rg_w'][layer], inp['rg_b'][layer], inp['re_w'][layer], inp['re_b'][layer],
                                  inp['e_w1'][layer], inp['e_w3'][layer], inp['e_w2'][layer],
                                  inp['ln_ffn_g'][layer], inp['ln_ffn_b'][layer]))
    rc = _run(ncC, maps)
    nxt = np.empty_like(cur)
    for ci, (b, t0) in enumerate(SHARDS):
        nxt[b, t0:t0 + NTOK] = rc[ci]['xout']
    return nxt


def layer0(inp, x):
    nc0 = build_0a(ntok=NTOK)
    maps = []
    for (b, t0) in SHARDS:
        xT = np.zeros((D, HALO + NTOK), np.float32)
        xT[:, HALO:] = x[b, t0:t0 + NTOK].T
        if t0 > 0:
            xT[:, :HALO] = x[b, t0 - HALO:t0].T
        maps.append(host_inputs_0a(xT, inp['ev_w_in'][0], inp['ev_mu'][0], inp['ev_w0'][0], inp['ev_a0'][0],
                                   inp['ev_k_k'][0], inp['ev_k_a'][0], inp['ev_r_k'][0].reshape(-1),
                                   inp['ev_pool_scale'][0], inp['ev_w2'][0], inp['ev_a2'][0], inp['ev_g2'][0],
                                   inp['ev_pool_w'][0], t0 == 0))
    r0 = _run(nc0, maps)
    ncb = build_0b(T=2 * NTOK)
    maps = []
    for c in range(8):
        b, j = c // 2, c % 2
        rows = slice(256 * j, 256 * j + 256)
        arrT = {n: np.concatenate([r0[2 * b][n][rows], r0[2 * b + 1][n][rows]], axis=1)
                for n in ('oR', 'oK', 'oV', 'oA', 'oB', 'oW', 'oG', 'oBon')}
        maps.append(host_inputs_0b(arrT, inp['ev_lnx_g'][0][rows], inp['ev_lnx_b'][0][rows]))
    rb = _run(ncb, maps)
    ymTs = []
    for ci, (b, t0) in enumerate(SHARDS):
        ymT = np.empty((D, NTOK), np.float32)
        for j in range(2):
            ymT[256 * j:256 * j + 256] = rb[2 * b + j]['y'][t0:t0 + NTOK].T
        ymT[512:] = r0[ci]['oYP']
        ymTs.append(ymT)
    return _post_mixer(inp, x, ymTs, 0, inp['ev_w_out'][0])


def layer1(inp, x1):
    nc1 = build_1a(ntok=NTOK)
    maps = []
    for (b, t0) in SHARDS:
        xT = np.zeros((D, HALO1 + NTOK), np.float32)
        xT[:, HALO1:] = x1[b, t0:t0 + NTOK].T
        if t0 > 0:
            xT[:, :HALO1] = x1[b, t0 - HALO1:t0].T
        maps.append(host_inputs_1a(xT, inp['od_w_in'][0], inp['od_conv_w'][0], inp['od_conv_b'][0], inp['od_cln_g'][0],
                                   inp['od_cln_b'][0], t0, NTOK))
    r1 = _run(nc1, maps)
    ncb = build_1b(T=2 * NTOK)
    maps = []
    for c in range(8):
        b, j = c // 2, c % 2
        rows = slice(256 * j, 256 * j + 256)
        arrT = {n: np.concatenate([r1[2 * b][n][rows], r1[2 * b + 1][n][rows]], axis=1) for n in ('oQ', 'oKr', 'oV', 'oGr')}
        maps.append(host_inputs_1b(arrT, inp['od_gn_g'][0][rows], inp['od_gn_b'][0][rows], 4 * j))
    rb = _run(ncb, maps)
    ymTs = []
    for ci, (b, t0) in enumerate(SHARDS):
        ymT = np.empty((D, NTOK), np.float32)
        ymT[:512] = r1[ci]['oYC']
        for j in range(2):
            ymT[512 + 256 * j:512 + 256 * j + 256] = rb[2 * b + j]['y'][t0:t0 + NTOK].T
        ymTs.append(ymT)
    return _post_mixer(inp, x1, ymTs, 1, inp['od_w_out'][0])


def kernel(**inp):
    inp = {k: np.asarray(v) for k, v in inp.items()}
    x1 = layer0(inp, inp['x'])
    return layer1(inp, x1)
```
